# Optimizing a Trainium2 kernel written in Bass

```python
import math
import jax, jax.numpy as jnp
from jax import lax
import numpy as np

D_MODEL = 2048
BATCH = 4
SEQ = 4096
DEPTH = 2

CHUNK = 64
NORM_EPS = 1e-6
ROPE_THETA = 500000.0
N_BRANCH = 4
BRANCH_W = D_MODEL // 2

GLA_HEADS = 4
GLA_DK = BRANCH_W // 2 // GLA_HEADS
GLA_DV = BRANCH_W // GLA_HEADS
GLA_RANK = 16
GLA_TAU = 16.0

DSA_HEADS = 8
DSA_HD = BRANCH_W // DSA_HEADS
DSA_ROT = DSA_HD // 4
IDX_HEADS = 8
IDX_HD = 64
IDX_ROT = IDX_HD // 4
TOPK_MAX = 256
DSA_QBLOCK = 64

RWKV_HD = 64
RWKV_HEADS = BRANCH_W // RWKV_HD
RWKV_DECAY_RANK = 96
RWKV_A_RANK = 96
RWKV_DECAY_SCALE = 0.606531
RWKV_GN_EPS = 64e-5
RWKV_KK_EPS = 1e-12

SGU_CHUNK = 128
SGU_GROUPS = 8
SGU_GW = BRANCH_W // SGU_GROUPS
SGU_LN_EPS = 1e-5

A_COLS = (GLA_HEADS * GLA_DK, GLA_HEADS * GLA_DK, BRANCH_W, GLA_RANK, BRANCH_W)
B_COLS = (BRANCH_W, BRANCH_W, BRANCH_W, IDX_HEADS * IDX_HD, IDX_HD, IDX_HEADS, BRANCH_W)
C_COLS = (BRANCH_W, BRANCH_W, BRANCH_W, RWKV_DECAY_RANK, RWKV_A_RANK, BRANCH_W)
D_COLS = (2 * BRANCH_W, BRANCH_W)
GROUP_COLS = (sum(A_COLS), sum(B_COLS), sum(C_COLS), sum(D_COLS), N_BRANCH * D_MODEL)
N_IN_COLS = sum(GROUP_COLS)

kernel_name = "hybrid_gla_dsa_rwkv7_sgu_gated_merge"

F32 = jnp.float32


def _split(t, sizes):
    return jnp.split(t, np.cumsum(sizes)[:-1].tolist(), axis=-1)


def _rmsnorm(x, g):
    x32 = x.astype(F32)
    y = x32 * lax.rsqrt(jnp.mean(x32 * x32, axis=-1, keepdims=True) + NORM_EPS)
    return (y * g.astype(F32)).astype(x.dtype)


def _layernorm(x, g, b, eps):
    x32 = x.astype(F32)
    mu = jnp.mean(x32, axis=-1, keepdims=True)
    xc = x32 - mu
    var = jnp.mean(xc * xc, axis=-1, keepdims=True)
    return xc * lax.rsqrt(var + eps) * g.astype(F32) + b.astype(F32)


def _rope_tables(positions, rot_dim):
    inv = ROPE_THETA ** (-jnp.arange(0, rot_dim, 2, dtype=F32) / rot_dim)
    ang = positions.astype(F32)[..., None] * inv
    return jnp.cos(ang), jnp.sin(ang)


def _partial_rope(t, cos, sin):
    half = cos.shape[-1]
    t32 = t.astype(F32)
    x1, x2, rest = t32[..., :half], t32[..., half:2 * half], t32[..., 2 * half:]
    c, s = cos[:, :, None, :], sin[:, :, None, :]
    return jnp.concatenate([x1 * c - x2 * s, x2 * c + x1 * s, rest], axis=-1).astype(t.dtype)


def _token_shift(t):
    return jnp.pad(t[:, :-1], ((0, 0), (1, 0), (0, 0)))


def _gla_branch(q, k, v, g_lr, gate, w_g2, b_g, head_g):
    bsz, seq, _ = q.shape
    nc = seq // CHUNK
    log_a = jax.nn.log_sigmoid((g_lr @ w_g2 + b_g).astype(F32)) / GLA_TAU

    def chunks(t, hd):
        return t.astype(F32).reshape(bsz, nc, CHUNK, GLA_HEADS, hd).swapaxes(0, 1)

    qc = chunks(q, GLA_DK) * GLA_DK ** -0.5
    kc, lc = chunks(k, GLA_DK), chunks(log_a, GLA_DK)
    vc = chunks(v, GLA_DV)

    def step(state, inp):
        q_c, k_c, v_c, l_c = inp
        cum = jnp.cumsum(l_c, axis=1)
        total = cum[:, -1]
        k_dec = k_c * jnp.exp(total[:, None] - cum)
        state = state * jnp.exp(total)[..., None] + jnp.einsum('bchk,bchv->bhkv', k_dec, v_c)
        out = jnp.einsum('bchk,bhkv->bchv', q_c, state)
        return state, out

    state0 = jnp.zeros((bsz, GLA_HEADS, GLA_DK, GLA_DV), F32)
    _, o = lax.scan(step, state0, (qc, kc, vc, lc))
    o = o.swapaxes(0, 1).reshape(bsz, seq, GLA_HEADS, GLA_DV)
    o = _rmsnorm(o, head_g)
    return o.reshape(bsz, seq, BRANCH_W) * jax.nn.silu(gate.astype(F32))


def _dsa_branch(q, k, v, iq, ik, iw, gate, cos_a, sin_a, cos_i, sin_i):
    bsz, seq, _ = q.shape
    topk = min(TOPK_MAX, seq // 4)
    q = _partial_rope(q.reshape(bsz, seq, DSA_HEADS, DSA_HD), cos_a, sin_a)
    k = _partial_rope(k.reshape(bsz, seq, DSA_HEADS, DSA_HD), cos_a, sin_a)
    v = v.reshape(bsz, seq, DSA_HEADS, DSA_HD)
    iq = _partial_rope(iq.reshape(bsz, seq, IDX_HEADS, IDX_HD), cos_i, sin_i)
    ik = _partial_rope(ik[:, :, None, :], cos_i, sin_i)[:, :, 0]
    iw = iw.astype(F32) * (IDX_HEADS * IDX_HD) ** -0.5
    nqb = seq // DSA_QBLOCK
    key_chunk = jnp.arange(seq) // CHUNK

    def blocks(t):
        return t.reshape(bsz, nqb, DSA_QBLOCK, *t.shape[2:]).swapaxes(0, 1)

    def attend(args):
        qb, iqb, iwb, q0 = args
        rel = jax.nn.relu(jnp.einsum('bqhd,bsd->bqhs', iqb, ik).astype(F32))
        score = jnp.einsum('bqh,bqhs->bqs', iwb, rel)
        q_chunk = (q0 + jnp.arange(DSA_QBLOCK)) // CHUNK
        admissible = key_chunk[None, :] <= q_chunk[:, None]
        score = jnp.where(admissible[None], score, -jnp.inf)
        top_val, top_idx = lax.top_k(score, topk)
        valid = jnp.isfinite(top_val)
        k_sel = jax.vmap(lambda t, i: t[i])(k, top_idx)
        v_sel = jax.vmap(lambda t, i: t[i])(v, top_idx)
        logits = jnp.einsum('bqhd,bqkhd->bqhk', qb, k_sel).astype(F32) * DSA_HD ** -0.5
        logits = jnp.where(valid[:, :, None, :], logits, -jnp.inf)
        probs = jax.nn.softmax(logits, axis=-1)
        return jnp.einsum('bqhk,bqkhd->bqhd', probs, v_sel.astype(F32))

    starts = jnp.arange(nqb, dtype=jnp.int32) * DSA_QBLOCK
    o = lax.map(attend, (blocks(q), blocks(iq), blocks(iw), starts))
    o = o.swapaxes(0, 1).reshape(bsz, seq, BRANCH_W)
    return o * jax.nn.silu(gate.astype(F32))


def _rwkv7_branch(c, mu, w0, w_w2, a0, w_a2, k_k, k_a, r_k, lnx_g, lnx_b):
    bsz, seq, _ = c.shape
    c = c.astype(F32)
    c = c + (_token_shift(c) - c) * mu.astype(F32)
    r, k, v, w_lr, a_lr, gate = _split(c, C_COLS)
    w_log = -RWKV_DECAY_SCALE * jax.nn.sigmoid(w0 + jnp.tanh(w_lr) @ w_w2.astype(F32))
    a = jax.nn.sigmoid(a0 + a_lr @ w_a2.astype(F32))
    kk = k * k_k
    k = k * (1.0 + (a - 1.0) * k_a)

    def heads(t):
        return t.reshape(bsz, seq, RWKV_HEADS, RWKV_HD)

    r, k, v, w_log, a, kk = (heads(t) for t in (r, k, v, w_log, a, kk))
    kk = kk * lax.rsqrt(jnp.sum(kk * kk, axis=-1, keepdims=True) + RWKV_KK_EPS)

    def step(state, inp):
        r_t, w_t, k_t, v_t, kk_t, a_t = inp
        sa = jnp.einsum('bhvk,bhk->bhv', state, -kk_t)
        state = (state * w_t[:, :, None, :]
                 + sa[..., None] * (kk_t * a_t)[:, :, None, :]
                 + v_t[..., None] * k_t[:, :, None, :])
        return state, jnp.einsum('bhvk,bhk->bhv', state, r_t)

    xs = tuple(jnp.moveaxis(t, 1, 0) for t in (r, jnp.exp(w_log), k, v, kk, a))
    state0 = jnp.zeros((bsz, RWKV_HEADS, RWKV_HD, RWKV_HD), F32)
    _, y = lax.scan(step, state0, xs)
    y = jnp.moveaxis(y, 0, 1)
    y = _layernorm(y, lnx_g.reshape(RWKV_HEADS, RWKV_HD), lnx_b.reshape(RWKV_HEADS, RWKV_HD), RWKV_GN_EPS)
    y = y + jnp.sum(r * k * r_k.astype(F32), axis=-1, keepdims=True) * v
    return y.reshape(bsz, seq, BRANCH_W) * jax.nn.silu(gate)


def _sgu_branch(z, gate, ln_g, ln_b, w_s, b_s):
    bsz, seq, _ = z.shape
    z = jax.nn.gelu(z.astype(F32), approximate=False)
    u, v = jnp.split(z, 2, axis=-1)
    v = _layernorm(v, ln_g, ln_b, SGU_LN_EPS)
    nch = seq // SGU_CHUNK
    v = v.reshape(bsz, nch, SGU_CHUNK, SGU_GROUPS, SGU_GW)
    pos_chunk = jnp.arange(SGU_CHUNK) // CHUNK
    mask = pos_chunk[None, :] <= pos_chunk[:, None]
    ws = jnp.where(mask[None], w_s.astype(F32), 0.0)
    sv = jnp.einsum('gij,bcjgd->bcigd', ws, v) + b_s.astype(F32).T[None, None, :, :, None]
    return u * sv.reshape(bsz, seq, BRANCH_W) * jax.nn.silu(gate.astype(F32))


def setup_inputs(seed: int = 0) -> dict:
    key = jax.random.key(seed)
    ks = jax.random.split(key, 32)
    L, W = DEPTH, BRANCH_W

    def nrm(k, shape, scale):
        return jax.random.normal(k, shape, F32) * scale

    def gain(k, shape):
        return 1.0 + nrm(k, shape, 0.1)

    positions = (jax.random.randint(ks[1], (BATCH, 1), 0, 4096) * CHUNK
                 + jnp.arange(SEQ)[None, :]).astype(jnp.int32)
    return {
        "x": nrm(ks[0], (BATCH, SEQ, D_MODEL), 1.0),
        "positions": positions,
        "norm_pre": gain(ks[2], (L, D_MODEL)),
        "norm_post": gain(ks[3], (L, D_MODEL)),
        "w_in": nrm(ks[4], (L, D_MODEL, N_IN_COLS), D_MODEL ** -0.5),
        "gla_w_g2": nrm(ks[5], (L, GLA_RANK, GLA_HEADS * GLA_DK), GLA_RANK ** -0.5),
        "gla_b_g": 0.5 + nrm(ks[6], (L, GLA_HEADS * GLA_DK), 0.1),
        "gla_head_g": gain(ks[7], (L, GLA_DV)),
        "rwkv_mu": jax.random.uniform(ks[8], (L, sum(C_COLS)), F32),
        "rwkv_w0": nrm(ks[9], (L, W), 1.0),
        "rwkv_w_w2": nrm(ks[10], (L, RWKV_DECAY_RANK, W), 0.5 * RWKV_DECAY_RANK ** -0.5),
        "rwkv_a0": nrm(ks[11], (L, W), 0.5),
        "rwkv_w_a2": nrm(ks[12], (L, RWKV_A_RANK, W), 0.5 * RWKV_A_RANK ** -0.5),
        "rwkv_k_k": 0.85 + nrm(ks[13], (L, W), 0.05),
        "rwkv_k_a": gain(ks[14], (L, W)),
        "rwkv_r_k": nrm(ks[15], (L, RWKV_HEADS, RWKV_HD), 0.1),
        "rwkv_lnx_g": gain(ks[16], (L, W)),
        "rwkv_lnx_b": nrm(ks[17], (L, W), 0.02),
        "sgu_ln_g": gain(ks[18], (L, W)),
        "sgu_ln_b": nrm(ks[19], (L, W), 0.02),
        "sgu_w_s": nrm(ks[20], (L, SGU_GROUPS, SGU_CHUNK, SGU_CHUNK), SGU_CHUNK ** -0.5),
        "sgu_b_s": gain(ks[21], (L, SGU_GROUPS, SGU_CHUNK)),
        "w_proj": nrm(ks[22], (L, N_BRANCH, W, D_MODEL), W ** -0.5),
        "w_out": nrm(ks[23], (L, D_MODEL, D_MODEL), D_MODEL ** -0.5),
    }


def reference(x, positions, norm_pre, norm_post, w_in, gla_w_g2, gla_b_g, gla_head_g,
              rwkv_mu, rwkv_w0, rwkv_w_w2, rwkv_a0, rwkv_w_a2, rwkv_k_k, rwkv_k_a, rwkv_r_k,
              rwkv_lnx_g, rwkv_lnx_b, sgu_ln_g, sgu_ln_b, sgu_w_s, sgu_b_s, w_proj, w_out):
    bsz, seq, _ = x.shape
    cos_a, sin_a = _rope_tables(positions, DSA_ROT)
    cos_i, sin_i = _rope_tables(positions, IDX_ROT)
    for l in range(DEPTH):
        h = _rmsnorm(x, norm_pre[l])
        p = h @ w_in[l]
        pa, pb, pc, pd, pm = _split(p, GROUP_COLS)
        ya = _gla_branch(*_split(pa, A_COLS), gla_w_g2[l], gla_b_g[l], gla_head_g[l])
        yb = _dsa_branch(*_split(pb, B_COLS), cos_a, sin_a, cos_i, sin_i)
        yc = _rwkv7_branch(pc, rwkv_mu[l], rwkv_w0[l], rwkv_w_w2[l], rwkv_a0[l], rwkv_w_a2[l],
                           rwkv_k_k[l], rwkv_k_a[l], rwkv_r_k[l], rwkv_lnx_g[l], rwkv_lnx_b[l])
        yd = _sgu_branch(*_split(pd, D_COLS), sgu_ln_g[l], sgu_ln_b[l], sgu_w_s[l], sgu_b_s[l])
        ys = jnp.stack([ya, yb, yc, yd], axis=2).astype(x.dtype)
        proj = jnp.einsum('bsgw,gwd->bsgd', ys, w_proj[l])
        gates = jax.nn.sigmoid(pm.reshape(bsz, seq, N_BRANCH, D_MODEL).astype(F32))
        merged = jnp.einsum('bsgd,bsgd->bsd', gates, proj.astype(F32)).astype(x.dtype)
        x = x + _rmsnorm(merged @ w_out[l], norm_post[l])
    return x
```

```python
import numpy as np
from contextlib import ExitStack, contextmanager
import concourse.bass as bass
import concourse.mybir as mybir
from concourse.bass_utils import run_bass_kernel_spmd

F32 = mybir.dt.float32
BF16 = mybir.dt.bfloat16
I32 = mybir.dt.int32
ALU = mybir.AluOpType
AF = mybir.ActivationFunctionType
AX = mybir.AxisListType

D = 2048
S = 4096
L = 2
NIN = 23320
PAD = 64
TP = S + PAD
NEG = -1.0e30


class Buf:
    __slots__ = ("t", "w", "r", "sem", "semkey", "semv", "name", "acc", "wl")

    def __init__(self, t, name, acc=False):
        self.t = t
        self.name = name
        self.acc = acc
        self.wl = []
        self.w = None
        self.r = []
        self.sem = None
        self.semkey = None
        self.semv = 0

    def __getitem__(self, k):
        return self.t[k]


class Prog:
    def __init__(self, nc, es):
        self.nc = nc
        self.es = es
        self.eng = {"pe": nc.tensor, "act": nc.scalar, "dve": nc.vector, "pool": nc.gpsimd, "sp": nc.sync}
        self.esem = {}
        self.ecnt = {}
        self.ekey = {}
        self.eepoch = {}
        for e in ("pe", "act", "dve", "pool"):
            self.esem[e] = es.enter_context(nc.semaphore("e_" + e))
            self.ecnt[e] = 0
            self.eepoch[e] = 0
            self.ekey[e] = e + "#0"
        self.nwait = 0
        self.seen = {e: {} for e in self.eng}
        self.nbuf = 0
        self.scopes = [es]
        self.scope_bufs = [[]]
        self.sempool = []
        self.allsems = {}
        self.nsem = 0
        self.ninst = 0

    @contextmanager
    def scope(self):
        es = ExitStack()
        self.scopes.append(es)
        self.scope_bufs.append([])
        yield
        self.barrier()
        for b in self.scope_bufs.pop():
            if b.sem is not None:
                self.sempool.append((b.sem, b.semkey, b.semv))
        self.scopes.pop()
        es.close()

    def sb(self, shape, dt, name=None):
        self.nbuf += 1
        name = (name or "sb") + "_%d" % self.nbuf
        t = self.scopes[-1].enter_context(self.nc.sbuf_tensor(name, list(shape), dt))
        b = Buf(t, name)
        self.scope_bufs[-1].append(b)
        return b

    def ps(self, shape, dt, name=None):
        self.nbuf += 1
        name = (name or "ps") + "_%d" % self.nbuf
        t = self.scopes[-1].enter_context(self.nc.psum_tensor(name, list(shape), dt))
        b = Buf(t, name)
        self.scope_bufs[-1].append(b)
        return b

    def dram(self, name, shape, dt, kind="Internal"):
        t = self.nc.dram_tensor(name, list(shape), dt, kind=kind).ap()
        return Buf(t, name, acc=True)

    def _wait(self, e, ev, raw=True):
        if ev is None:
            return
        key, sem, val = ev
        if e == "pe" and key.startswith("pe#"):
            return
        if self.seen[e].get(key, 0) >= val:
            return
        self.eng[e].wait_ge(sem, val)
        self.nwait += 1
        self.seen[e][key] = val

    def _deps(self, e, reads, writes):
        for b in reads:
            self._wait(e, b.w, True)
            for ev in b.wl:
                self._wait(e, ev, True)
        for b in writes:
            if not b.acc:
                self._wait(e, b.w, False)
            for ev in b.r:
                self._wait(e, ev, False)

    @staticmethod
    def _compact(evs):
        best = {}
        for ev in evs:
            k = ev[0]
            if k not in best or best[k][2] < ev[2]:
                best[k] = ev
        return list(best.values())

    def _record(self, ev, reads, writes):
        for b in writes:
            if b.acc:
                b.wl.append(ev)
                if len(b.wl) > 16:
                    b.wl = self._compact(b.wl)
            else:
                b.w = ev
            b.r = []
        for b in reads:
            if b not in writes:
                b.r.append(ev)
                if len(b.r) > 16:
                    b.r = self._compact(b.r)

    def op(self, e, fn, reads=(), writes=()):
        self._deps(e, reads, writes)
        ins = fn(self.eng[e])
        self.ecnt[e] += 1
        self.ninst += 1
        ins.then_inc(self.esem[e], 1)
        self._record((self.ekey[e], self.esem[e], self.ecnt[e]), reads, writes)
        return ins

    def _getsem(self, b):
        if b.sem is None:
            if self.sempool:
                b.sem, b.semkey, b.semv = self.sempool.pop()
            else:
                self.nsem += 1
                b.semkey = "d%d" % self.nsem
                b.sem = self.es.enter_context(self.nc.semaphore(b.semkey))
                b.semv = 0
            self.allsems[b.semkey] = b

    def dma(self, q, pairs, reads=(), writes=(), sembuf=None, **kw):
        self._deps(q, reads, writes)
        sb = sembuf
        if sb is None:
            cands = [b for b in list(writes) + list(reads) if not b.acc]
            sb = cands[0] if cands else (writes[0] if writes else reads[0])
        self._getsem(sb)
        self._wait(q, (sb.semkey, sb.sem, sb.semv))
        for (o, i) in pairs:
            self.eng[q].dma_start(out=o, in_=i, **kw).then_inc(sb.sem, 16)
            sb.semv += 16
            self.ninst += 1
        self._record((sb.semkey, sb.sem, sb.semv), reads, writes)

    def barrier(self):
        evs = [(self.ekey[c], self.esem[c], self.ecnt[c], c) for c in self.esem]
        for k, b in self.allsems.items():
            evs.append((k, b.sem, b.semv, None))
        for (sem, key, v) in self.sempool:
            evs.append((key, sem, v, None))
        for e in self.eng:
            for ev in evs:
                if ev[3] != e and ev[2] > 0:
                    if self.seen[e].get(ev[0], 0) < ev[2]:
                        self.eng[e].wait_ge(ev[1], ev[2])
                        self.nwait += 1
                        self.seen[e][ev[0]] = ev[2]
        for c in list(self.esem):
            if self.ecnt[c] > 12000:
                self.eepoch[c] += 1
                self.ekey[c] = "%s#%d" % (c, self.eepoch[c])
                self.esem[c] = self.es.enter_context(self.nc.semaphore("e_%s_%d" % (c, self.eepoch[c])))
                self.ecnt[c] = 0


SEGS = [
    (0, 512, "F", "gla_q"), (512, 512, "T", "gla_k"), (1024, 1024, "T", "gla_v"),
    (2048, 16, "F", "gla_g"), (2064, 1024, "F", "gla_gate"),
    (3088, 1024, "F", "dsa_q"), (4112, 1024, "F", "dsa_k"), (5136, 1024, "T", "dsa_v"),
    (6160, 512, "F", "dsa_iq"), (6672, 64, "F", "dsa_ik"), (6736, 8, "T", "dsa_iw"),
    (6744, 1024, "F", "dsa_gate"),
    (7768, 1024, "T", "rw_r"), (8792, 1024, "T", "rw_k"), (9816, 1024, "F", "rw_v"),
    (10840, 96, "T", "rw_wlr"), (10936, 96, "T", "rw_alr"), (11032, 1024, "F", "rw_gate"),
    (12056, 1024, "F", "sgu_u"), (13080, 1024, "T", "sgu_zv"), (14104, 1024, "F", "sgu_gate"),
    (15128, 8192, "H", "pm"),
]
FM_ROWS = {}
TM_COLS = {}
_r = 0
_c = 0
for (_c0, _n, _m, _nm) in SEGS:
    if _m == "F":
        FM_ROWS[_nm] = _r
        _r += _n
    elif _m == "T":
        TM_COLS[_nm] = _c
        _c += _n
NFM = _r
NTM = _c


def host_consts():
    c = {}
    c["c_ident"] = np.eye(128, dtype=np.float32)
    s = np.arange(64)
    c["c_ustrict"] = (s[:, None] > s[None, :]).astype(np.float32)
    i = np.arange(128)
    c["c_sgumask"] = ((i[None, :] // 64) <= (i[:, None] // 64)).astype(np.float32)
    inv_a = (500000.0 ** (-(np.arange(0, 32, 2, dtype=np.float32) / np.float32(32)))).astype(np.float32)
    inv_i = (500000.0 ** (-(np.arange(0, 16, 2, dtype=np.float32) / np.float32(16)))).astype(np.float32)
    inv = np.zeros((128, 2), np.float32)
    inv[:, 0] = inv_a[i % 16]
    inv[:, 1] = inv_i[i % 8]
    c["c_inv"] = inv
    sel = np.zeros((128, 64, 128), np.float32)
    for tl in range(64):
        sel[tl, tl, 0:64] = 1.0
        sel[64 + tl, tl, 64:128] = 1.0
    c["c_sel"] = sel
    c["c_blk"] = ((i[:, None] // 64) == (i[None, :] // 64)).astype(np.float32)
    c["c_i2"] = ((i[:, None] % 64) == s[None, :]).astype(np.float32)
    return c


def build_nc(S=4096, dbg=None):
    dbg = dbg or {}
    TP = S + PAD
    NT = S // 128
    NB = S // 512
    NCH = S // 64
    KTOP = min(256, S // 4)
    nc = bass.Bass("TRN2", target_bir_lowering=False)
    es = ExitStack()
    P = Prog(nc, es)
    I = {}

    def inp(name, shape, dt=F32):
        I[name] = P.dram(name, shape, dt, kind="ExternalInput")
        return I[name]

    x_in = inp("x", [S, D])
    pos_in = inp("pos", [1, S], I32)
    inp("norm_pre", [L, D]); inp("norm_post", [L, D]); inp("w_in", [L, D, NIN])
    inp("gla_w_g2", [L, 16, 512]); inp("gla_b_g", [L, 512]); inp("gla_head_g", [L, 256])
    inp("rwkv_mu", [L, 4288]); inp("rwkv_w0", [L, 1024]); inp("rwkv_w_w2", [L, 96, 1024])
    inp("rwkv_a0", [L, 1024]); inp("rwkv_w_a2", [L, 96, 1024]); inp("rwkv_k_k", [L, 1024])
    inp("rwkv_k_a", [L, 1024]); inp("rwkv_r_k", [L, 16, 64]); inp("rwkv_lnx_g", [L, 1024])
    inp("rwkv_lnx_b", [L, 1024]); inp("sgu_ln_g", [L, 1024]); inp("sgu_ln_b", [L, 1024])
    inp("sgu_w_s", [L, 8, 128, 128]); inp("sgu_b_s", [L, 8, 128]); inp("w_proj", [L, 4, 1024, D])
    inp("w_out", [L, D, D])
    inp("c_ident", [128, 128]); inp("c_ustrict", [64, 64]); inp("c_sgumask", [128, 128])
    inp("c_inv", [128, 2]); inp("c_sel", [128, 64, 128]); inp("c_blk", [128, 128]); inp("c_i2", [128, 64])
    y_out = P.dram("y", [S, D], F32, kind="ExternalOutput")

    ext_in = dbg.get("ext_in", ())
    ext_out = dbg.get("ext_out", ())

    def scr(name, shape, dt):
        kind = "ExternalInput" if name in ext_in else ("ExternalOutput" if name in ext_out else "Internal")
        return P.dram(name, shape, dt, kind=kind)
    fm = scr("fm", [NFM, TP], F32)
    tm = scr("tm", [S, NTM], F32)
    pmT = scr("pmT", [8192, S], BF16)
    ysT = scr("ysT", [4096, S], BF16)
    mT = scr("mT", [D, S], BF16)
    tabs = scr("tabs", [4, 128, S], F32)
    maskT = scr("maskT", [NT, S, 128], BF16)
    rwtm = scr("rwtm", [S, 5, 1024], F32)
    rwbon = scr("rwbon", [S, 16], F32)
    xcur = [x_in, scr("x1", [S, D], F32), y_out]

    if "fm_in" in dbg:
        pass

    ident_f = P.sb([128, 128], F32, "identf")
    ident_b = P.sb([128, 128], BF16, "identb")
    ones_b = P.sb([128, 128], BF16, "onesb")
    zeros_f = P.sb([128, PAD], F32, "zerosf")
    P.dma("sp", [(ident_f[:], I["c_ident"][:, :])], reads=[I["c_ident"]], writes=[ident_f])
    P.op("dve", lambda e: e.tensor_copy(ident_b[:], ident_f[:]), [ident_f], [ident_b])
    P.op("pool", lambda e: e.memset(ones_b[:], 1.0), [], [ones_b])
    P.op("pool", lambda e: e.memset(zeros_f[:], 0.0), [], [zeros_f])
    for nm in ("rw_v", "rw_gate"):
        for r8 in range(8):
            r0 = FM_ROWS[nm] + r8 * 128
            P.dma("sp", [(fm[r0:r0 + 128, 0:PAD], zeros_f[:])], reads=[zeros_f], writes=[fm])

    def evac(i, out_ap, in_ap, reads, writes):
        if i % 2 == 0:
            P.op("act", lambda e: e.copy(out_ap, in_ap), reads, writes)
        else:
            P.op("dve", lambda e: e.tensor_copy(out_ap, in_ap), reads, writes)

    def V(e, fn, reads, writes):
        P.op(e, fn, reads, writes)

    def phase_tables():
        with P.scope():
            posi = P.sb([128, S], I32, "posi")
            posf = P.sb([128, S], F32, "posf")
            inv = P.sb([128, 2], F32, "inv")
            ang = P.sb([128, S], F32, "ang")
            kf = P.sb([128, S], F32, "kf")
            ki = P.sb([128, S], I32, "ki")
            r1 = P.sb([128, S], F32, "r1")
            y = P.sb([128, S], F32, "y")
            m = P.sb([128, S], F32, "m")
            P.dma("sp", [(posi[:], pos_in[0:1, :].to_broadcast([128, S]))], reads=[pos_in], writes=[posi])
            P.dma("sp", [(inv[:], I["c_inv"][:, :])], reads=[I["c_inv"]], writes=[inv])
            V("dve", lambda e: e.tensor_copy(posf[:], posi[:]), [posi], [posf])
            for which in range(2):
                V("dve", lambda e: e.tensor_scalar(ang[:], posf[:], inv[:, which:which + 1], None, op0=ALU.mult), [posf, inv], [ang])
                V("dve", lambda e: e.tensor_scalar(kf[:], ang[:], float(1.0 / (2 * np.pi)), None, op0=ALU.mult), [ang], [kf])
                V("dve", lambda e: e.tensor_copy(ki[:], kf[:]), [kf], [ki])
                V("dve", lambda e: e.tensor_copy(kf[:], ki[:]), [ki], [kf])
                V("dve", lambda e: e.scalar_tensor_tensor(r1[:], kf[:], -6.28125, ang[:], op0=ALU.mult, op1=ALU.add), [kf, ang], [r1])
                V("dve", lambda e: e.scalar_tensor_tensor(ang[:], kf[:], -0.0019353071795864769, r1[:], op0=ALU.mult, op1=ALU.add), [kf, r1], [ang])
                for cs in range(2):
                    sh = float(np.pi / 2) if cs == 0 else 0.0
                    V("dve", lambda e: e.tensor_scalar(y[:], ang[:], sh, None, op0=ALU.add), [ang], [y])
                    V("dve", lambda e: e.tensor_scalar(m[:], y[:], float(np.pi), None, op0=ALU.is_gt), [y], [m])
                    V("dve", lambda e: e.scalar_tensor_tensor(y[:], m[:], float(-2 * np.pi), y[:], op0=ALU.mult, op1=ALU.add), [m, y], [y])
                    V("dve", lambda e: e.tensor_scalar(m[:], y[:], float(-np.pi), None, op0=ALU.is_lt), [y], [m])
                    V("dve", lambda e: e.scalar_tensor_tensor(y[:], m[:], float(2 * np.pi), y[:], op0=ALU.mult, op1=ALU.add), [m, y], [y])
                    V("dve", lambda e: e.tensor_scalar(y[:], y[:], float(np.pi), float(-np.pi), op0=ALU.min, op1=ALU.max), [y], [y])
                    V("act", lambda e: e.activation(r1[:], y[:], AF.Sin), [y], [r1])
                    P.dma("sp", [(tabs.t[which * 2 + cs, :, :], r1[:])], reads=[r1], writes=[tabs])

    def phase_proj(l, xin):
        with P.scope():
            hT = P.sb([128, 16, S], BF16, "hT")
            with P.scope():
                gbc = P.sb([128, D], F32, "gbc")
                P.dma("sp", [(gbc[:], I["norm_pre"][l:l + 1, :].to_broadcast([128, D]))], reads=[I["norm_pre"]], writes=[gbc])
                xt = [P.sb([128, D], F32, "xt") for _ in range(2)]
                junk = P.sb([128, D], BF16, "junk")
                hb = [P.sb([128, D], BF16, "hb") for _ in range(2)]
                ss = [P.sb([128, 1], F32, "ss") for _ in range(2)]
                rs = [P.sb([128, 1], F32, "rs") for _ in range(2)]
                pst = [P.ps([128, 4, 128], BF16, "pst") for _ in range(4)]
                P.dma("sp", [(xt[0][:], xin[0:128, :])], reads=[xin], writes=[xt[0]])
                for i in range(NT):
                    if i + 1 < NT:
                        P.dma("sp", [(xt[(i + 1) % 2][:], xin[(i + 1) * 128:(i + 2) * 128, :])], reads=[xin], writes=[xt[(i + 1) % 2]])
                    xi, si, ri, hi = xt[i % 2], ss[i % 2], rs[i % 2], hb[i % 2]
                    V("act", lambda e: e.activation(junk[:], xi[:], AF.Square, accum_out=si[:]), [xi], [junk, si])
                    V("act", lambda e: e.activation(ri[:], si[:], AF.Sqrt, bias=1e-6, scale=1.0 / D), [si], [ri])
                    V("dve", lambda e: e.reciprocal(ri[:], ri[:]), [ri], [ri])
                    V("dve", lambda e: e.scalar_tensor_tensor(hi[:], xi[:], ri[:, 0:1], gbc[:], op0=ALU.mult, op1=ALU.mult), [xi, ri, gbc], [hi])
                    for g4 in range(4):
                        pt = pst[g4]
                        for j in range(4):
                            kc = g4 * 4 + j
                            V("pe", lambda e: e.transpose(pt[:, j, :], hi[:, kc * 128:(kc + 1) * 128], ident_b[:]), [hi, ident_b], [pt])
                        evac(g4, hT[:, g4 * 4:(g4 + 1) * 4, i * 128:(i + 1) * 128], pt[:], [pt], [hT])
            with P.scope():
                wf = [P.sb([128, 16, 128], F32, "wf") for _ in range(2)]
                wb = [P.sb([128, 16, 128], BF16, "wb") for _ in range(2)]
                stg = [P.sb([128, S], F32, "stg") for _ in range(2)]
                stgh = [P.sb([128, S], BF16, "stgh") for _ in range(2)]
                pp = [P.ps([128, 512], F32, "pp") for _ in range(6)]
                slabs = []
                for (c0, n, mode, nm) in SEGS:
                    off = 0
                    while off < n:
                        m = min(128, n - off)
                        slabs.append((c0 + off, m, mode, nm, off))
                        off += m
                w_l = I["w_in"]

                def load(s):
                    c0, m, mode, nm, off = slabs[s]
                    src = w_l.t[l, :, c0:c0 + m].rearrange("(kc p) m -> p kc m", p=128)
                    P.dma("sp", [(wf[s % 2][:, k4 * 4:(k4 + 1) * 4, 0:m], src[:, k4 * 4:(k4 + 1) * 4, :]) for k4 in range(4)],
                          reads=[w_l], writes=[wf[s % 2]])

                def cast(s):
                    c0, m, mode, nm, off = slabs[s]
                    V("pool", lambda e: e.tensor_copy(wb[s % 2][:, :, 0:m], wf[s % 2][:, :, 0:m]), [wf[s % 2]], [wb[s % 2]])

                load(0)
                cast(0)
                if len(slabs) > 1:
                    load(1)
                pi = 0
                for s in range(len(slabs)):
                    c0, m, mode, nm, off = slabs[s]
                    if s + 1 < len(slabs):
                        cast(s + 1)
                    if s + 2 < len(slabs):
                        load(s + 2)
                    w_s = wb[s % 2]
                    if mode in ("F", "H"):
                        st = stg[s % 2] if mode == "F" else stgh[s % 2]
                        for tb in range(NB):
                            ps = pp[pi % 6]
                            for kc in range(16):
                                V("pe", lambda e: e.matmul(ps[0:m, :], w_s[:, kc, 0:m], hT[:, kc, tb * 512:(tb + 1) * 512],
                                                           start=(kc == 0), stop=(kc == 15)), [w_s, hT], [ps])
                            evac(pi, st[0:m, tb * 512:(tb + 1) * 512], ps[0:m, :], [ps], [st])
                            pi += 1
                        if mode == "F":
                            r0 = FM_ROWS[nm] + off
                            P.dma("sp", [(fm[r0:r0 + m, PAD:TP], st[0:m, :])], reads=[st], writes=[fm])
                        else:
                            P.dma("sp", [(pmT[off:off + m, :], st[0:m, :])], reads=[st], writes=[pmT])
                    else:
                        st = stg[s % 2]
                        st3 = st[:].rearrange("p (t m) -> p t m", m=128)
                        for t4 in range(NT // 4):
                            ps = pp[pi % 6]
                            ps3 = ps[:].rearrange("p (t m) -> p t m", m=128)
                            for j in range(4):
                                tt = t4 * 4 + j
                                for kc in range(16):
                                    V("pe", lambda e: e.matmul(ps3[:, j, 0:m], hT[:, kc, tt * 128:(tt + 1) * 128], w_s[:, kc, 0:m],
                                                               start=(kc == 0), stop=(kc == 15)), [w_s, hT], [ps])
                            evac(pi, st3[:, t4 * 4:(t4 + 1) * 4, 0:m], ps3[:, :, 0:m], [ps], [st])
                            pi += 1
                        cc = TM_COLS[nm] + off
                        P.dma("sp", [(tm.t[:, cc:cc + m].rearrange("(t p) m -> p t m", p=128), st3[:, 0:NT, 0:m])], reads=[st], writes=[tm])

    def phase_rope(l):
        with P.scope():
            A = P.sb([128, S], F32, "ropeA")
            B = P.sb([128, S], F32, "ropeB")
            C = P.sb([128, S], F32, "ropeC")
            Sn = P.sb([128, S], F32, "ropeS")
            t1 = P.sb([128, S], F32, "ropet1")
            t2 = P.sb([128, S], F32, "ropet2")
            A2 = P.sb([128, S], F32, "ropeA2")
            B2 = P.sb([128, S], F32, "ropeB2")
            for (nm, nh, hd, half, tab) in (("dsa_q", 8, 128, 16, 0), ("dsa_k", 8, 128, 16, 0), ("dsa_iq", 8, 64, 8, 2), ("dsa_ik", 1, 64, 8, 2)):
                np_ = nh * half
                r0 = FM_ROWS[nm]
                rows = fm.t[r0:r0 + nh * hd, PAD:TP].rearrange("(h r) t -> h r t", r=hd)
                P.dma("sp", [(C[0:np_, :], tabs.t[tab, 0:np_, :]), (Sn[0:np_, :], tabs.t[tab + 1, 0:np_, :])], reads=[tabs], writes=[C, Sn], sembuf=C)
                P.dma("sp", [(A[0:np_, :], rows[:, 0:half, :]), (B[0:np_, :], rows[:, half:2 * half, :])], reads=[fm], writes=[A, B], sembuf=A)
                V("dve", lambda e: e.tensor_tensor(t1[0:np_, :], A[0:np_, :], C[0:np_, :], ALU.mult), [A, C], [t1])
                V("pool", lambda e: e.tensor_tensor(t2[0:np_, :], B[0:np_, :], Sn[0:np_, :], ALU.mult), [B, Sn], [t2])
                V("dve", lambda e: e.tensor_tensor(A2[0:np_, :], t1[0:np_, :], t2[0:np_, :], ALU.subtract), [t1, t2], [A2])
                V("dve", lambda e: e.tensor_tensor(t1[0:np_, :], B[0:np_, :], C[0:np_, :], ALU.mult), [B, C], [t1])
                V("pool", lambda e: e.tensor_tensor(t2[0:np_, :], A[0:np_, :], Sn[0:np_, :], ALU.mult), [A, Sn], [t2])
                V("dve", lambda e: e.tensor_tensor(B2[0:np_, :], t1[0:np_, :], t2[0:np_, :], ALU.add), [t1, t2], [B2])
                P.dma("sp", [(rows[:, 0:half, :], A2[0:np_, :]), (rows[:, half:2 * half, :], B2[0:np_, :])], reads=[A2, B2], writes=[fm], sembuf=A2)

    def phase_gla(l):
        with P.scope():
            qT = P.sb([128, 4, S], BF16, "gqT")
            oT = P.sb([128, 8, S], BF16, "goT")
            stg = [P.sb([128, S], F32, "gstg") for _ in range(2)]
            glr = P.sb([17, S], F32, "glr")
            wg = P.sb([17, 512], F32, "gwg")
            ust = P.sb([64, 64], F32, "gust")
            ones64 = P.sb([64, 1], F32, "gones")
            hg = P.sb([128, 2], F32, "ghg")
            state = P.sb([128, 4, 256], F32, "gstate")
            state_b = P.sb([128, 4, 256], BF16, "gstateb")
            kc_ = [P.sb([64, 512], F32, "gk") for _ in range(2)]
            vc_ = [P.sb([64, 1024], F32, "gv") for _ in range(2)]
            vb = P.sb([64, 1024], BF16, "gvb")
            ee = P.sb([64, 512], F32, "gee")
            lsp = P.sb([64, 512], F32, "glsp")
            dkk = P.sb([64, 512], F32, "gdk")
            kdec = P.sb([64, 512], BF16, "gkdec")
            dec = P.sb([128, 4], F32, "gdec")
            pz = P.ps([64, 512], F32, "gpz")
            pG = P.ps([64, 512], F32, "gpG")
            ptot = P.ps([128, 4], F32, "gptot")
            pkv = [P.ps([128, 2, 256], F32, "gpkv") for _ in range(2)]
            po = P.ps([128, 8, 64], F32, "gpo")
            V("pool", lambda e: e.memset(glr[:], 1.0), [], [glr])
            V("pool", lambda e: e.memset(ones64[:], 1.0), [], [ones64])
            V("pool", lambda e: e.memset(state[:], 0.0), [], [state])
            r0 = FM_ROWS["gla_g"]
            P.dma("sp", [(glr[0:16, :], fm[r0:r0 + 16, PAD:TP])], reads=[fm], writes=[glr])
            P.dma("sp", [(wg[0:16, :], I["gla_w_g2"].t[l, :, :]), (wg[16:17, :], I["gla_b_g"][l:l + 1, :])], reads=[I["gla_w_g2"], I["gla_b_g"]], writes=[wg])
            P.dma("sp", [(ust[:], I["c_ustrict"][:, :])], reads=[I["c_ustrict"]], writes=[ust])
            P.dma("sp", [(hg[:], I["gla_head_g"].t[l, :].rearrange("(a p) -> p a", p=128))], reads=[I["gla_head_g"]], writes=[hg], allow_slow_non_contiguous=True)
            r0 = FM_ROWS["gla_q"]
            for h in range(4):
                st = stg[h % 2]
                P.dma("sp", [(st[:], fm[r0 + h * 128:r0 + (h + 1) * 128, PAD:TP])], reads=[fm], writes=[st])
                V("pool", lambda e: e.tensor_copy(qT[:, h, :], st[:]), [st], [qT])
            ck = TM_COLS["gla_k"]
            cv = TM_COLS["gla_v"]

            def loadkv(c):
                P.dma("sp", [(kc_[c % 2][:], tm[c * 64:(c + 1) * 64, ck:ck + 512])], reads=[tm], writes=[kc_[c % 2]])
                P.dma("sp", [(vc_[c % 2][:], tm[c * 64:(c + 1) * 64, cv:cv + 1024])], reads=[tm], writes=[vc_[c % 2]])
            loadkv(0)
            for c in range(NCH):
                if c + 1 < NCH:
                    loadkv(c + 1)
                t0 = c * 64
                kk_, vv_ = kc_[c % 2], vc_[c % 2]
                V("pe", lambda e: e.matmul(pz[:], glr[:, t0:t0 + 64], wg[:], start=True, stop=True), [glr, wg], [pz])
                V("act", lambda e: e.activation(ee[:], pz[:], AF.Exp, scale=-1.0), [pz], [ee])
                V("act", lambda e: e.activation(lsp[:], ee[:], AF.Ln, bias=1.0), [ee], [lsp])
                V("pe", lambda e: e.matmul(pG[:], ust[:], lsp[:], start=True, stop=True), [ust, lsp], [pG])
                for h in range(4):
                    V("pe", lambda e: e.matmul(ptot[:, h:h + 1], lsp[:, h * 128:(h + 1) * 128], ones64[:], start=True, stop=True), [lsp, ones64], [ptot])
                V("act", lambda e: e.activation(dkk[:], pG[:], AF.Exp, scale=-1.0 / 16.0), [pG], [dkk])
                V("act", lambda e: e.activation(dec[:], ptot[:], AF.Exp, scale=-1.0 / 16.0), [ptot], [dec])
                V("dve", lambda e: e.tensor_tensor(kdec[:], kk_[:], dkk[:], ALU.mult), [kk_, dkk], [kdec])
                V("pool", lambda e: e.tensor_copy(vb[:], vv_[:]), [vv_], [vb])
                for h in range(4):
                    pk = pkv[h // 2]
                    V("pe", lambda e: e.matmul(pk[:, h % 2, :], kdec[:, h * 128:(h + 1) * 128], vb[:, h * 256:(h + 1) * 256], start=True, stop=True), [kdec, vb], [pk])
                for h in range(4):
                    pk = pkv[h // 2]
                    V("dve", lambda e: e.scalar_tensor_tensor(state[:, h, :], state[:, h, :], dec[:, h:h + 1], pk[:, h % 2, :], op0=ALU.mult, op1=ALU.add), [state, dec, pk], [state])
                V("act", lambda e: e.copy(state_b[:], state[:]), [state], [state_b])
                for h in range(4):
                    for vh in range(2):
                        V("pe", lambda e: e.matmul(po[:, h * 2 + vh, :], state_b[:, h, vh * 128:(vh + 1) * 128], qT[:, h, t0:t0 + 64], start=True, stop=True), [state_b, qT], [po])
                evac(c, oT[:, :, t0:t0 + 64], po[:], [po], [oT])
            sq = P.sb([128, 2, 512], BF16, "gsq")
            pms = [P.ps([128, 512], F32, "gpms") for _ in range(2)]
            rstd = P.sb([128, 512], F32, "grstd")
            yy = P.sb([128, 512], F32, "gyy")
            sg = P.sb([128, 512], F32, "gsg")
            yo = [P.sb([128, S], BF16, "gyo") for _ in range(2)]
            sc = float(128.0 ** -0.5)
            hgs = P.sb([128, 2], F32, "ghgs")
            V("dve", lambda e: e.tensor_scalar(hgs[:], hg[:], sc, None, op0=ALU.mult), [hg], [hgs])
            rg = FM_ROWS["gla_gate"]
            it = 0
            for h in range(4):
                for vh in range(2):
                    gt = stg[it % 2]
                    yob = yo[it % 2]
                    P.dma("sp", [(gt[:], fm[rg + h * 256 + vh * 128:rg + h * 256 + (vh + 1) * 128, PAD:TP])], reads=[fm], writes=[gt])
                    for tb in range(NB):
                        ts_ = slice(tb * 512, (tb + 1) * 512)
                        pm_ = pms[(it * NB + tb) % 2]
                        if vh == 0:
                            for v2 in range(2):
                                V("act", lambda e: e.activation(sq[:, v2, :], oT[:, h * 2 + v2, ts_], AF.Square), [oT], [sq])
                        else:
                            for v2 in range(2):
                                V("act", lambda e: e.activation(sq[:, v2, :], oT[:, h * 2 + v2, ts_], AF.Square), [oT], [sq])
                        for v2 in range(2):
                            V("pe", lambda e: e.matmul(pm_[:], ones_b[:], sq[:, v2, :], start=(v2 == 0), stop=(v2 == 1)), [ones_b, sq], [pm_])
                        V("act", lambda e: e.activation(rstd[:], pm_[:], AF.Sqrt, bias=1e-6, scale=sc * sc / 256.0), [pm_], [rstd])
                        V("dve", lambda e: e.reciprocal(rstd[:], rstd[:]), [rstd], [rstd])
                        V("act", lambda e: e.activation(sg[:], gt[:, ts_], AF.Silu), [gt], [sg])
                        V("dve", lambda e: e.tensor_tensor(yy[:], oT[:, h * 2 + vh, ts_], rstd[:], ALU.mult), [oT, rstd], [yy])
                        V("dve", lambda e: e.scalar_tensor_tensor(yob[:, ts_], yy[:], hgs[:, vh:vh + 1], sg[:], op0=ALU.mult, op1=ALU.mult), [yy, hgs, sg], [yob])
                    ro = h * 256 + vh * 128
                    P.dma("sp", [(ysT[ro:ro + 128, :], yob[:])], reads=[yob], writes=[ysT])
                    it += 1

    def phase_sgu(l):
        with P.scope():
            lng = P.sb([128, 1024], F32, "slng")
            lnb = P.sb([128, 1024], F32, "slnb")
            bsb = P.sb([128, 8, 128], F32, "sbsb")
            msk = P.sb([128, 128], F32, "smsk")
            wsT = P.sb([128, 8, 128], BF16, "swsT")
            wtmp = P.sb([128, 128], F32, "swtmp")
            pT = P.ps([128, 128], F32, "spT")
            P.dma("sp", [(lng[:], I["sgu_ln_g"][l:l + 1, :].to_broadcast([128, 1024]))], reads=[I["sgu_ln_g"]], writes=[lng])
            P.dma("sp", [(lnb[:], I["sgu_ln_b"][l:l + 1, :].to_broadcast([128, 1024]))], reads=[I["sgu_ln_b"]], writes=[lnb])
            P.dma("sp", [(bsb[:].rearrange("p g i -> p (g i)"), I["sgu_b_s"].t[l:l + 1, :, :].rearrange("a g i -> a (g i)").to_broadcast([128, 1024]))], reads=[I["sgu_b_s"]], writes=[bsb])
            P.dma("sp", [(msk[:], I["c_sgumask"][:, :])], reads=[I["c_sgumask"]], writes=[msk])
            for g in range(8):
                P.dma("sp", [(wtmp[:], I["sgu_w_s"].t[l, g, :, :])], reads=[I["sgu_w_s"]], writes=[wtmp])
                V("dve", lambda e: e.tensor_tensor(wtmp[:], wtmp[:], msk[:], ALU.mult), [wtmp, msk], [wtmp])
                V("pe", lambda e: e.transpose(pT[:], wtmp[:], ident_f[:]), [wtmp, ident_f], [pT])
                V("act", lambda e: e.copy(wsT[:, g, :], pT[:]), [pT], [wsT])
            zv = [P.sb([128, 1024], F32, "szv") for _ in range(2)]
            uu = [P.sb([128, 8, 128], F32, "suu") for _ in range(2)]
            gg = [P.sb([128, 8, 128], F32, "sgg") for _ in range(2)]
            g1 = P.sb([128, 1024], F32, "sg1")
            junk = P.sb([128, 1024], BF16, "sjunk")
            vn = P.sb([128, 1024], F32, "svn")
            vln = P.sb([128, 1024], BF16, "svln")
            s1 = P.sb([128, 1], F32, "ss1"); s2 = P.sb([128, 1], F32, "ss2"); mm = P.sb([128, 1], F32, "smm")
            msq = P.sb([128, 1], F32, "smsq"); var = P.sb([128, 1], F32, "svar"); rstd = P.sb([128, 1], F32, "srstd")
            nmr = P.sb([128, 1], F32, "snmr")
            psv = [P.ps([128, 4, 128], F32, "spsv") for _ in range(2)]
            t1 = P.sb([128, 8, 128], F32, "st1")
            ug = P.sb([128, 8, 128], F32, "sug")
            sg = P.sb([128, 8, 128], F32, "ssg")
            yb = [P.sb([128, 8, 128], BF16, "syb") for _ in range(2)]
            cz = TM_COLS["sgu_zv"]
            ru = FM_ROWS["sgu_u"]
            rgt = FM_ROWS["sgu_gate"]

            def loadc(c):
                P.dma("sp", [(zv[c % 2][:], tm[c * 128:(c + 1) * 128, cz:cz + 1024])], reads=[tm], writes=[zv[c % 2]])
                P.dma("sp", [(uu[c % 2][:], fm.t[ru:ru + 1024, PAD + c * 128:PAD + (c + 1) * 128].rearrange("(g d) t -> d g t", d=128))], reads=[fm], writes=[uu[c % 2]])
                P.dma("sp", [(gg[c % 2][:], fm.t[rgt:rgt + 1024, PAD + c * 128:PAD + (c + 1) * 128].rearrange("(g d) t -> d g t", d=128))], reads=[fm], writes=[gg[c % 2]])
            loadc(0)
            for c in range(NT):
                if c + 1 < NT:
                    loadc(c + 1)
                z, u_, g_ = zv[c % 2], uu[c % 2], gg[c % 2]
                ybc = yb[c % 2]
                V("act", lambda e: e.activation(g1[:], z[:], AF.Gelu), [z], [g1])
                V("dve", lambda e: e.tensor_reduce(s1[:], g1[:], AX.X, ALU.add), [g1], [s1])
                V("act", lambda e: e.activation(junk[:], g1[:], AF.Square, accum_out=s2[:]), [g1], [junk, s2])
                V("dve", lambda e: e.tensor_scalar(mm[:], s1[:], 1.0 / 1024.0, None, op0=ALU.mult), [s1], [mm])
                V("dve", lambda e: e.tensor_tensor(msq[:], mm[:], mm[:], ALU.mult), [mm], [msq])
                V("dve", lambda e: e.scalar_tensor_tensor(var[:], s2[:], 1.0 / 1024.0, msq[:], op0=ALU.mult, op1=ALU.subtract), [s2, msq], [var])
                V("act", lambda e: e.activation(rstd[:], var[:], AF.Sqrt, bias=1e-5, scale=1.0), [var], [rstd])
                V("dve", lambda e: e.reciprocal(rstd[:], rstd[:]), [rstd], [rstd])
                V("dve", lambda e: e.scalar_tensor_tensor(nmr[:], mm[:], -1.0, rstd[:], op0=ALU.mult, op1=ALU.mult), [mm, rstd], [nmr])
                V("act", lambda e: e.activation(vn[:], g1[:], AF.Identity, bias=nmr[:, 0:1], scale=rstd[:, 0:1]), [g1, nmr, rstd], [vn])
                V("dve", lambda e: e.tensor_tensor(vn[:], vn[:], lng[:], ALU.mult), [vn, lng], [vn])
                V("dve", lambda e: e.tensor_tensor(vln[:], vn[:], lnb[:], ALU.add), [vn, lnb], [vln])
                for g in range(8):
                    pv = psv[g // 4]
                    V("pe", lambda e: e.matmul(pv[:, g % 4, :], vln[:, g * 128:(g + 1) * 128], wsT[:, g, :], start=True, stop=True), [vln, wsT], [pv])
                for g2 in range(2):
                    V("dve", lambda e: e.tensor_tensor(t1[:, g2 * 4:(g2 + 1) * 4, :], psv[g2][:], bsb[:, g2 * 4:(g2 + 1) * 4, :], ALU.add), [psv[g2], bsb], [t1])
                V("act", lambda e: e.activation(ug[:], u_[:], AF.Gelu), [u_], [ug])
                V("act", lambda e: e.activation(sg[:], g_[:], AF.Silu), [g_], [sg])
                V("pool", lambda e: e.tensor_tensor(ug[:], ug[:], sg[:], ALU.mult), [ug, sg], [ug])
                V("dve", lambda e: e.tensor_tensor(ybc[:], t1[:], ug[:], ALU.mult), [t1, ug], [ybc])
                P.dma("sp", [(ysT.t[3072:4096, c * 128:(c + 1) * 128].rearrange("(g d) t -> d g t", d=128), ybc[:])], reads=[ybc], writes=[ysT])

    def phase_m1(l):
        with P.scope():
            wp = P.sb([128, 32, D], BF16, "mwp")
            wst = [P.sb([128, 2048], F32, "mwst") for _ in range(2)]
            wpj = I["w_proj"]
            k = 0
            for g in range(4):
                for wc in range(8):
                    st = wst[k % 2]
                    P.dma("sp", [(st[:], wpj.t[l, g, wc * 128:(wc + 1) * 128, :])], reads=[wpj], writes=[st])
                    if k % 2 == 0:
                        V("pool", lambda e: e.tensor_copy(wp[:, g * 8 + wc, :], st[:]), [st], [wp])
                    else:
                        V("dve", lambda e: e.tensor_copy(wp[:, g * 8 + wc, :], st[:]), [st], [wp])
                    k += 1
            TB = 256
            yt = [P.sb([128, 32, TB], BF16, "myt") for _ in range(2)]
            pmt = [P.sb([128, 4, TB], BF16, "mpm") for _ in range(3)]
            sig = P.sb([128, 4, TB], F32, "msig")
            acc = P.sb([128, TB], F32, "macc")
            tmp = P.sb([128, TB], F32, "mtmp")
            mo = [P.sb([128, 16, TB], BF16, "mmo") for _ in range(2)]
            pps = [P.ps([128, TB], F32, "mpp") for _ in range(6)]
            nblk = S // TB

            def loady(b):
                P.dma("sp", [(yt[b % 2][:, q * 8:(q + 1) * 8, :], ysT.t[q * 1024:(q + 1) * 1024, b * TB:(b + 1) * TB].rearrange("(c p) t -> p c t", p=128)) for q in range(4)],
                      reads=[ysT], writes=[yt[b % 2]])
            loady(0)
            pi = 0
            it = 0
            for b in range(nblk):
                if b + 1 < nblk:
                    loady(b + 1)
                ytb = yt[b % 2]
                mob = mo[b % 2]
                for dc in range(16):
                    pmb = pmt[it % 3]
                    it += 1
                    P.dma("sp", [(pmb[:], pmT.t[:, b * TB:(b + 1) * TB].rearrange("(g r) t -> r g t", g=4)[dc * 128:(dc + 1) * 128, :, :])], reads=[pmT], writes=[pmb])
                    V("act", lambda e: e.activation(sig[:], pmb[:], AF.Sigmoid), [pmb], [sig])
                    for g in range(4):
                        ps = pps[pi % 6]
                        pi += 1
                        for wc in range(8):
                            V("pe", lambda e: e.matmul(ps[:], wp[:, g * 8 + wc, dc * 128:(dc + 1) * 128], ytb[:, g * 8 + wc, :], start=(wc == 0), stop=(wc == 7)), [wp, ytb], [ps])
                        if g == 0:
                            V("dve", lambda e: e.tensor_tensor(acc[:], ps[:], sig[:, g, :], ALU.mult), [ps, sig], [acc])
                        elif g < 3:
                            V("dve", lambda e: e.tensor_tensor(tmp[:], ps[:], sig[:, g, :], ALU.mult), [ps, sig], [tmp])
                            V("pool", lambda e: e.tensor_tensor(acc[:], acc[:], tmp[:], ALU.add), [acc, tmp], [acc])
                        else:
                            V("dve", lambda e: e.tensor_tensor(tmp[:], ps[:], sig[:, g, :], ALU.mult), [ps, sig], [tmp])
                            V("pool", lambda e: e.tensor_tensor(mob[:, dc, :], acc[:], tmp[:], ALU.add), [acc, tmp], [mob])
                P.dma("sp", [(mT.t[:, b * TB:(b + 1) * TB].rearrange("(c p) t -> p c t", p=128), mob[:])], reads=[mob], writes=[mT])

    def phase_m2(l, xin, xout):
        with P.scope():
            wo = P.sb([128, 16, D], BF16, "owo")
            wst = [P.sb([128, 2048], F32, "owst") for _ in range(2)]
            for dc in range(16):
                st = wst[dc % 2]
                P.dma("sp", [(st[:], I["w_out"].t[l, dc * 128:(dc + 1) * 128, :])], reads=[I["w_out"]], writes=[st])
                if dc % 2 == 0:
                    V("pool", lambda e: e.tensor_copy(wo[:, dc, :], st[:]), [st], [wo])
                else:
                    V("dve", lambda e: e.tensor_copy(wo[:, dc, :], st[:]), [st], [wo])
            gbc = P.sb([128, D], F32, "ogbc")
            P.dma("sp", [(gbc[:], I["norm_post"][l:l + 1, :].to_broadcast([128, D]))], reads=[I["norm_post"]], writes=[gbc])
            mt = [P.sb([128, 16, 128], BF16, "omt") for _ in range(2)]
            xt = [P.sb([128, D], F32, "oxt") for _ in range(2)]
            ot = P.sb([128, D], F32, "oot")
            junk = P.sb([128, D], BF16, "ojunk")
            res = [P.sb([128, D], F32, "ores") for _ in range(2)]
            ssq = P.sb([128, 1], F32, "ossq")
            rs = P.sb([128, 1], F32, "ors")
            pps = [P.ps([128, 512], F32, "opp") for _ in range(4)]

            def loadt(i):
                P.dma("sp", [(mt[i % 2][:], mT.t[:, i * 128:(i + 1) * 128].rearrange("(c p) t -> p c t", p=128))], reads=[mT], writes=[mt[i % 2]])
                P.dma("sp", [(xt[i % 2][:], xin[i * 128:(i + 1) * 128, :])], reads=[xin], writes=[xt[i % 2]])
            loadt(0)
            for i in range(NT):
                if i + 1 < NT:
                    loadt(i + 1)
                mti, xti, rsi = mt[i % 2], xt[i % 2], res[i % 2]
                for nb in range(4):
                    ps = pps[nb]
                    for dc in range(16):
                        V("pe", lambda e: e.matmul(ps[:], mti[:, dc, :], wo[:, dc, nb * 512:(nb + 1) * 512], start=(dc == 0), stop=(dc == 15)), [mti, wo], [ps])
                    evac(nb, ot[:, nb * 512:(nb + 1) * 512], ps[:], [ps], [ot])
                V("act", lambda e: e.activation(junk[:], ot[:], AF.Square, accum_out=ssq[:]), [ot], [junk, ssq])
                V("act", lambda e: e.activation(rs[:], ssq[:], AF.Sqrt, bias=1e-6, scale=1.0 / D), [ssq], [rs])
                V("dve", lambda e: e.reciprocal(rs[:], rs[:]), [rs], [rs])
                V("dve", lambda e: e.scalar_tensor_tensor(ot[:], ot[:], rs[:, 0:1], gbc[:], op0=ALU.mult, op1=ALU.mult), [ot, rs, gbc], [ot])
                V("pool", lambda e: e.tensor_tensor(rsi[:], ot[:], xti[:], ALU.add), [ot, xti], [rsi])
                P.dma("sp", [(xout[i * 128:(i + 1) * 128, :], rsi[:])], reads=[rsi], writes=[xout])


    def phase_dsa1(l):
        with P.scope():
            iqT = P.sb([64, 8, S], BF16, "diqT")
            ikT = P.sb([64, S], BF16, "dikT")
            iw = P.sb([128, NT, 8], F32, "diw")
            stg = [P.sb([64, S], F32, "dstg") for _ in range(2)]
            score = P.sb([128, S], F32, "dscore")
            work = P.sb([128, S], F32, "dwork")
            maskb = P.sb([128, S], BF16, "dmaskb")
            mts = [P.sb([128, NT, 128], BF16, "dmts") for _ in range(2)]
            rl = [P.sb([128, 512], F32, "drl") for _ in range(2)]
            m8 = P.sb([128, 8], F32, "dm8")
            thr = P.sb([128, 1], F32, "dthr")
            pps = [P.ps([128, 512], F32, "dpp") for _ in range(4)]
            ptr = [P.ps([128, 4, 128], BF16, "dptr") for _ in range(2)]
            r0 = FM_ROWS["dsa_iq"]
            for h in range(8):
                st = stg[h % 2]
                P.dma("sp", [(st[:], fm[r0 + h * 64:r0 + (h + 1) * 64, PAD:TP])], reads=[fm], writes=[st])
                V("pool", lambda e: e.tensor_copy(iqT[:, h, :], st[:]), [st], [iqT])
            r0 = FM_ROWS["dsa_ik"]
            P.dma("sp", [(stg[0][:], fm[r0:r0 + 64, PAD:TP])], reads=[fm], writes=[stg[0]])
            V("pool", lambda e: e.tensor_copy(ikT[:], stg[0][:]), [stg[0]], [ikT])
            cw = TM_COLS["dsa_iw"]
            P.dma("sp", [(iw[:], tm.t[:, cw:cw + 8].rearrange("(t p) c -> p t c", p=128))], reads=[tm], writes=[iw])
            V("dve", lambda e: e.tensor_scalar(iw[:], iw[:], float(512.0 ** -0.5), None, op0=ALU.mult), [iw], [iw])
            k = 0
            kt = 0
            for qt in range(NT):
                nk = (qt + 1) * 128
                for sb in range((nk + 511) // 512):
                    w = min(512, nk - sb * 512)
                    cs = slice(sb * 512, sb * 512 + w)
                    for h in range(8):
                        ps = pps[k % 4]
                        rt = rl[k % 2]
                        k += 1
                        V("pe", lambda e: e.matmul(ps[:, 0:w], iqT[:, h, qt * 128:(qt + 1) * 128], ikT[:, cs], start=True, stop=True), [iqT, ikT], [ps])
                        V("act", lambda e: e.activation(rt[:, 0:w], ps[:, 0:w], AF.Relu), [ps], [rt])
                        if h == 0:
                            V("dve", lambda e: e.tensor_scalar(score[:, cs], rt[:, 0:w], iw[:, qt, 0:1], None, op0=ALU.mult), [rt, iw], [score])
                        else:
                            V("dve", lambda e: e.scalar_tensor_tensor(score[:, cs], rt[:, 0:w], iw[:, qt, h:h + 1], score[:, cs], op0=ALU.mult, op1=ALU.add), [rt, iw, score], [score])
                V("pool", lambda e: e.memset(score[0:64, nk - 64:nk], NEG), [], [score])
                if nk <= KTOP:
                    V("pool", lambda e: e.memset(thr[:], NEG / 2), [], [thr])
                else:
                    src = score
                    for r in range(KTOP // 8):
                        V("dve", lambda e: e.max(out=m8[:], in_=src[:, 0:nk]), [src], [m8])
                        if r < KTOP // 8 - 1:
                            V("dve", lambda e: e.match_replace(out=work[:, 0:nk], in_to_replace=m8[:], in_values=src[:, 0:nk], imm_value=NEG), [m8, src], [work])
                            src = work
                        else:
                            V("dve", lambda e: e.tensor_copy(thr[:], m8[:, 7:8]), [m8], [thr])
                V("dve", lambda e: e.tensor_scalar(maskb[:, 0:nk], score[:, 0:nk], thr[:, 0:1], None, op0=ALU.is_ge), [score, thr], [maskb])
                mtb = mts[qt % 2]
                for j4 in range((qt + 4) // 4):
                    n = min(4, qt + 1 - j4 * 4)
                    pt = ptr[kt % 2]
                    kt += 1
                    for jj in range(n):
                        j = j4 * 4 + jj
                        V("pe", lambda e: e.transpose(pt[:, jj, :], maskb[:, j * 128:(j + 1) * 128], ident_b[:]), [maskb, ident_b], [pt])
                    evac(kt, mtb[:, j4 * 4:j4 * 4 + n, :], pt[:, 0:n, :], [pt], [mtb])
                P.dma("sp", [(maskT.t[qt, 0:nk, :].rearrange("(j p) t -> p j t", p=128), mtb[:, 0:qt + 1, :])], reads=[mtb], writes=[maskT])

    def phase_dsa2(l):
        with P.scope():
            stg = [P.sb([128, S], F32, "astg") for _ in range(2)]
            kT = P.sb([128, S], BF16, "akT")
            qT = P.sb([128, S], BF16, "aqT")
            Vh = P.sb([128, NT, 128], BF16, "aVh")
            sg = P.sb([128, S], F32, "asg")
            mk = [P.sb([128, 4, 128], BF16, "amk") for _ in range(3)]
            ex = [P.sb([128, 4, 128], BF16, "aex") for _ in range(2)]
            ptt = [P.sb([128, 4, 128], BF16, "aptt") for _ in range(2)]
            pl = [P.ps([128, 4, 128], F32, "apl") for _ in range(2)]
            po = [P.ps([128, 512], F32, "apo") for _ in range(2)]
            pd = [P.ps([128, 512], F32, "apd") for _ in range(2)]
            rden = P.sb([128, 128], F32, "arden")
            o1 = P.sb([128, 128], F32, "ao1")
            yo = [P.sb([128, S], BF16, "ayo") for _ in range(2)]
            sc = float(128.0 ** -0.5)
            rk, rq, rg = FM_ROWS["dsa_k"], FM_ROWS["dsa_q"], FM_ROWS["dsa_gate"]
            cv = TM_COLS["dsa_v"]
            kb = 0
            for h in range(8):
                P.dma("sp", [(stg[0][:], fm[rk + h * 128:rk + (h + 1) * 128, PAD:TP])], reads=[fm], writes=[stg[0]])
                V("pool", lambda e: e.tensor_copy(kT[:], stg[0][:]), [stg[0]], [kT])
                P.dma("sp", [(stg[1][:], fm[rq + h * 128:rq + (h + 1) * 128, PAD:TP])], reads=[fm], writes=[stg[1]])
                V("pool", lambda e: e.tensor_copy(qT[:], stg[1][:]), [stg[1]], [qT])
                P.dma("sp", [(stg[0][:].rearrange("p (j v) -> p j v", v=128), tm.t[:, cv + h * 128:cv + (h + 1) * 128].rearrange("(j p) v -> p j v", p=128))], reads=[tm], writes=[stg[0]])
                V("pool", lambda e: e.tensor_copy(Vh[:], stg[0][:].rearrange("p (j v) -> p j v", v=128)), [stg[0]], [Vh])
                P.dma("sp", [(stg[1][:], fm[rg + h * 128:rg + (h + 1) * 128, PAD:TP])], reads=[fm], writes=[stg[1]])
                V("act", lambda e: e.activation(sg[:], stg[1][:], AF.Silu), [stg[1]], [sg])
                yob = yo[h % 2]
                for qt in range(NT):
                    nj = qt + 1
                    po_, pd_ = po[qt % 2], pd[qt % 2]
                    qs = slice(qt * 128, (qt + 1) * 128)
                    for j4 in range((nj + 3) // 4):
                        n = min(4, nj - j4 * 4)
                        mkb, plb, exb, ptb = mk[kb % 3], pl[kb % 2], ex[kb % 2], ptt[kb % 2]
                        kb += 1
                        P.dma("sp", [(mkb[:, 0:n, :], maskT.t[qt, j4 * 512:j4 * 512 + n * 128, :].rearrange("(j p) t -> p j t", p=128))], reads=[maskT], writes=[mkb])
                        for jj in range(n):
                            j = j4 * 4 + jj
                            V("pe", lambda e: e.matmul(plb[:, jj, :], kT[:, j * 128:(j + 1) * 128], qT[:, qs], start=True, stop=True), [kT, qT], [plb])
                        V("act", lambda e: e.activation(exb[:, 0:n, :], plb[:, 0:n, :], AF.Exp, scale=sc), [plb], [exb])
                        V("dve", lambda e: e.tensor_tensor(ptb[:, 0:n, :], exb[:, 0:n, :], mkb[:, 0:n, :], ALU.mult), [exb, mkb], [ptb])
                        for jj in range(n):
                            j = j4 * 4 + jj
                            V("pe", lambda e: e.matmul(po_[:, 0:128], Vh[:, j, :], ptb[:, jj, :], start=(j == 0), stop=(j == nj - 1)), [Vh, ptb], [po_])
                            V("pe", lambda e: e.matmul(pd_[:, 0:128], ones_b[:], ptb[:, jj, :], start=(j == 0), stop=(j == nj - 1)), [ones_b, ptb], [pd_])
                    V("dve", lambda e: e.reciprocal(rden[:], pd_[:, 0:128]), [pd_], [rden])
                    V("dve", lambda e: e.tensor_tensor(o1[:], po_[:, 0:128], rden[:], ALU.mult), [po_, rden], [o1])
                    V("pool", lambda e: e.tensor_tensor(yob[:, qs], o1[:], sg[:, qs], ALU.mult), [o1, sg], [yob])
                P.dma("sp", [(ysT[1024 + h * 128:1024 + (h + 1) * 128, :], yob[:])], reads=[yob], writes=[ysT])

    def phase_rwp(l):
        with P.scope():
            c0 = TM_COLS["rw_r"]
            mub = P.sb([128, 2240], F32, "rmub")
            mu = I["rwkv_mu"]
            P.dma("sp", [(mub[:, 0:2048], mu[l:l + 1, 0:2048].to_broadcast([128, 2048])), (mub[:, 2048:2240], mu[l:l + 1, 3072:3264].to_broadcast([128, 192]))], reads=[mu], writes=[mub])

            def bc(name, n=1024):
                t = P.sb([128, n], F32, "rb" + name)
                src = I[name]
                if name == "rwkv_r_k":
                    ap = src.t[l:l + 1, :, :].rearrange("a h k -> a (h k)").to_broadcast([128, n])
                else:
                    ap = src[l:l + 1, :].to_broadcast([128, n])
                P.dma("sp", [(t[:], ap)], reads=[src], writes=[t])
                return t
            kkb, kab, w0b, a0b, rkb = bc("rwkv_k_k"), bc("rwkv_k_a"), bc("rwkv_w0"), bc("rwkv_a0"), bc("rwkv_r_k")
            omka = P.sb([128, 1024], F32, "romka")
            V("dve", lambda e: e.tensor_scalar(omka[:], kab[:], -1.0, 1.0, op0=ALU.mult, op1=ALU.add), [kab], [omka])
            ww2 = P.sb([96, 1024], F32, "rww2")
            wa2 = P.sb([96, 1024], F32, "rwa2")
            P.dma("sp", [(ww2[:], I["rwkv_w_w2"].t[l, :, :])], reads=[I["rwkv_w_w2"]], writes=[ww2])
            P.dma("sp", [(wa2[:], I["rwkv_w_a2"].t[l, :, :])], reads=[I["rwkv_w_a2"]], writes=[wa2])
            cur = [P.sb([128, 2240], F32, "rcur") for _ in range(2)]
            prv = [P.sb([128, 2240], F32, "rprv") for _ in range(2)]
            dd = P.sb([128, 2240], F32, "rdd")
            th = P.sb([128, 96], F32, "rth")
            thT = P.sb([96, 128], F32, "rthT")
            alT = P.sb([96, 128], F32, "ralT")
            ptT = [P.ps([96, 128], F32, "rptT") for _ in range(2)]
            pz = [P.ps([128, 2, 512], F32, "rpz") for _ in range(2)]
            zs = P.sb([128, 1024], F32, "rzs")
            aa = P.sb([128, 1024], F32, "raa")
            kk = P.sb([128, 1024], F32, "rkk")
            sq = P.sb([128, 1024], F32, "rsq")
            ssq = P.sb([128, 16], F32, "rssq")
            tt = P.sb([128, 1024], F32, "rtt")
            out5 = [P.sb([128, 5, 1024], F32, "rout5") for _ in range(2)]
            bon = [P.sb([128, 16], F32, "rbon") for _ in range(2)]

            def loadt(i):
                P.dma("sp", [(cur[i % 2][:], tm[i * 128:(i + 1) * 128, c0:c0 + 2240])], reads=[tm], writes=[cur[i % 2]])
                if i == 0:
                    V("pool", lambda e: e.memset(prv[0][0:1, :], 0.0), [], [prv[0]])
                    P.dma("sp", [(prv[0][1:128, :], tm[0:127, c0:c0 + 2240])], reads=[tm], writes=[prv[0]])
                else:
                    P.dma("sp", [(prv[i % 2][:], tm[i * 128 - 1:i * 128 + 127, c0:c0 + 2240])], reads=[tm], writes=[prv[i % 2]])
            loadt(0)
            for i in range(NT):
                if i + 1 < NT:
                    loadt(i + 1)
                cu, pr, o5, bn = cur[i % 2], prv[i % 2], out5[i % 2], bon[i % 2]
                V("dve", lambda e: e.tensor_tensor(dd[:], pr[:], cu[:], ALU.subtract), [pr, cu], [dd])
                V("pool", lambda e: e.tensor_tensor(dd[:], dd[:], mub[:], ALU.mult), [dd, mub], [dd])
                V("dve", lambda e: e.tensor_tensor(cu[:], cu[:], dd[:], ALU.add), [cu, dd], [cu])
                rr_, k_ = cu[:, 0:1024], cu[:, 1024:2048]
                V("act", lambda e: e.activation(th[:], cu[:, 2048:2144], AF.Tanh), [cu], [th])
                V("pe", lambda e: e.transpose(ptT[0][:], th[:], ident_f[:]), [th, ident_f], [ptT[0]])
                V("act", lambda e: e.copy(thT[:], ptT[0][:]), [ptT[0]], [thT])
                V("pe", lambda e: e.transpose(ptT[1][:], cu[:, 2144:2240], ident_f[:]), [cu, ident_f], [ptT[1]])
                V("dve", lambda e: e.tensor_copy(alT[:], ptT[1][:]), [ptT[1]], [alT])
                for n2 in range(2):
                    V("pe", lambda e: e.matmul(pz[0][:, n2, :], thT[:], ww2[:, n2 * 512:(n2 + 1) * 512], start=True, stop=True), [thT, ww2], [pz[0]])
                    V("pe", lambda e: e.matmul(pz[1][:, n2, :], alT[:], wa2[:, n2 * 512:(n2 + 1) * 512], start=True, stop=True), [alT, wa2], [pz[1]])
                V("dve", lambda e: e.tensor_tensor(zs[:], pz[0][:].rearrange("p a b -> p (a b)"), w0b[:], ALU.add), [pz[0], w0b], [zs])
                V("act", lambda e: e.activation(zs[:], zs[:], AF.Sigmoid), [zs], [zs])
                V("act", lambda e: e.activation(o5[:, 0, :], zs[:], AF.Exp, scale=-0.606531), [zs], [o5])
                V("dve", lambda e: e.tensor_tensor(aa[:], pz[1][:].rearrange("p a b -> p (a b)"), a0b[:], ALU.add), [pz[1], a0b], [aa])
                V("act", lambda e: e.activation(aa[:], aa[:], AF.Sigmoid), [aa], [aa])
                V("dve", lambda e: e.tensor_tensor(kk[:], k_, kkb[:], ALU.mult), [cu, kkb], [kk])
                V("act", lambda e: e.activation(sq[:], kk[:], AF.Square), [kk], [sq])
                V("dve", lambda e: e.tensor_reduce(ssq[:], sq[:].rearrange("p (h k) -> p h k", k=64), AX.X, ALU.add), [sq], [ssq])
                V("act", lambda e: e.activation(ssq[:], ssq[:], AF.Sqrt, bias=1e-12, scale=1.0), [ssq], [ssq])
                V("dve", lambda e: e.reciprocal(ssq[:], ssq[:]), [ssq], [ssq])
                V("dve", lambda e: e.tensor_tensor(o5[:, 1, :].rearrange("p (h k) -> p h k", k=64), kk[:].rearrange("p (h k) -> p h k", k=64),
                                                   ssq[:, :].unsqueeze(2).to_broadcast([128, 16, 64]), ALU.mult), [kk, ssq], [o5])
                V("dve", lambda e: e.scalar_tensor_tensor(o5[:, 2, :], o5[:, 1, :], -1.0, aa[:], op0=ALU.mult, op1=ALU.mult), [o5, aa], [o5])
                V("pool", lambda e: e.tensor_tensor(tt[:], aa[:], kab[:], ALU.mult), [aa, kab], [tt])
                V("pool", lambda e: e.tensor_tensor(tt[:], tt[:], omka[:], ALU.add), [tt, omka], [tt])
                V("dve", lambda e: e.tensor_tensor(o5[:, 3, :], k_, tt[:], ALU.mult), [cu, tt], [o5])
                V("pool", lambda e: e.tensor_copy(o5[:, 4, :], rr_), [cu], [o5])
                V("pool", lambda e: e.tensor_tensor(tt[:], rr_, rkb[:], ALU.mult), [cu, rkb], [tt])
                V("dve", lambda e: e.tensor_tensor(tt[:], tt[:], o5[:, 3, :], ALU.mult), [tt, o5], [tt])
                V("dve", lambda e: e.tensor_reduce(bn[:], tt[:].rearrange("p (h k) -> p h k", k=64), AX.X, ALU.add), [tt], [bn])
                P.dma("sp", [(rwtm[i * 128:(i + 1) * 128, :, :], o5[:])], reads=[o5], writes=[rwtm])
                P.dma("sp", [(rwbon[i * 128:(i + 1) * 128, :], bn[:])], reads=[bn], writes=[rwbon])

    def phase_rws(l):
        with P.scope():
            sel = P.sb([128, 64, 128], F32, "wsel")
            blk = P.sb([128, 128], F32, "wblk")
            blkm = P.sb([128, 128], F32, "wblkm")
            i2 = P.sb([128, 64], F32, "wi2")
            P.dma("sp", [(sel[:], I["c_sel"][:, :, :])], reads=[I["c_sel"]], writes=[sel])
            P.dma("sp", [(blk[:], I["c_blk"][:, :])], reads=[I["c_blk"]], writes=[blk])
            P.dma("sp", [(i2[:], I["c_i2"][:, :])], reads=[I["c_i2"]], writes=[i2])
            V("dve", lambda e: e.tensor_scalar(blkm[:], blk[:], 1.0 / 64.0, None, op0=ALU.mult), [blk], [blkm])

            def hv(name, off=0):
                t = P.sb([128, 8], F32, "w" + name)
                src = I[name]
                P.dma("sp", [(t[hf * 64:(hf + 1) * 64, :], src.t[l, off + hf * 512:off + (hf + 1) * 512].rearrange("(h v) -> v h", v=64)) for hf in range(2)],
                      reads=[src], writes=[t], allow_slow_non_contiguous=True)
                return t
            lng, lnb = hv("rwkv_lnx_g"), hv("rwkv_lnx_b")
            muv, mug = hv("rwkv_mu", 2048), hv("rwkv_mu", 3264)
            St = P.sb([128, 8, 64], F32, "wS")
            V("pool", lambda e: e.memset(St[:], 0.0), [], [St])
            X = [P.sb([128, 5, 512], F32, "wX") for _ in range(2)]
            vch = [P.sb([128, 8, 65], F32, "wvch") for _ in range(2)]
            gch = [P.sb([128, 8, 65], F32, "wgch") for _ in range(2)]
            s2 = [P.sb([128, 8], F32, "ws2") for _ in range(2)]
            vl = P.sb([128, 8, 64], F32, "wvl")
            gl = P.sb([128, 8, 64], F32, "wgl")
            bcs = [[P.sb([128, 512], F32, "wbc") for _ in range(5)] for _ in range(2)]
            pb = [P.ps([128, 512], F32, "wpb") for _ in range(5)]
            pq = [P.ps([128, 512], F32, "wpq") for _ in range(2)]
            Y = P.sb([128, 8, 64], F32, "wY")
            tA = P.sb([128, 8, 64], F32, "wtA")
            tB = [P.sb([128, 8, 64], F32, "wtB") for _ in range(2)]
            sa = P.sb([128, 8], F32, "wsa")
            mean = P.sb([128, 512], F32, "wmean")
            yc = P.sb([128, 8, 64], F32, "wyc")
            sq = P.sb([128, 512], F32, "wsq")
            rstd = P.sb([128, 512], F32, "wrstd")
            rh = [P.sb([128, 64], F32, "wrh") for _ in range(2)]
            yb = [P.sb([128, 8, 64], BF16, "wyb") for _ in range(2)]
            rv, rg = FM_ROWS["rw_v"], FM_ROWS["rw_gate"]

            def b3(t2):
                return t2[:, :].unsqueeze(2).to_broadcast([128, 8, 64])

            def loadc(c):
                t0 = c * 64
                P.dma("sp", [(X[c % 2][hf * 64:(hf + 1) * 64, :, :], rwtm.t[t0:t0 + 64, :, hf * 512:(hf + 1) * 512]) for hf in range(2)], reads=[rwtm], writes=[X[c % 2]])
                P.dma("sp", [(vch[c % 2][hf * 64:(hf + 1) * 64, :, :], fm.t[rv + hf * 512:rv + (hf + 1) * 512, PAD + t0 - 1:PAD + t0 + 64].rearrange("(h v) t -> v h t", v=64)) for hf in range(2)],
                      reads=[fm], writes=[vch[c % 2]])
                P.dma("sp", [(gch[c % 2][hf * 64:(hf + 1) * 64, :, :], fm.t[rg + hf * 512:rg + (hf + 1) * 512, PAD + t0 - 1:PAD + t0 + 64].rearrange("(h v) t -> v h t", v=64)) for hf in range(2)],
                      reads=[fm], writes=[gch[c % 2]])
                P.dma("sp", [(s2[c % 2][hf * 64:(hf + 1) * 64, :], rwbon[t0:t0 + 64, hf * 8:(hf + 1) * 8]) for hf in range(2)], reads=[rwbon], writes=[s2[c % 2]])
            loadc(0)
            step = 0
            for c in range(NCH):
                if c + 1 < NCH:
                    loadc(c + 1)
                t0 = c * 64
                Xc, vc, gc, s2c = X[c % 2], vch[c % 2], gch[c % 2], s2[c % 2]
                for (src, dst, mu_) in ((vc, vl, muv), (gc, gl, mug)):
                    V("pool", lambda e: e.tensor_tensor(dst[:], src[:, :, 0:64], src[:, :, 1:65], ALU.subtract), [src], [dst])
                    V("pool", lambda e: e.tensor_tensor(dst[:], dst[:], b3(mu_), ALU.mult), [dst, mu_], [dst])
                    V("pool", lambda e: e.tensor_tensor(dst[:], dst[:], src[:, :, 1:65], ALU.add), [dst, src], [dst])
                for tl in range(64):
                    bset = bcs[step % 2]
                    tBs = tB[step % 2]
                    step += 1
                    for p in range(5):
                        V("pe", lambda e: e.matmul(pb[p][:], sel[:, tl, :], Xc[:, p, :], start=True, stop=True), [sel, Xc], [pb[p]])
                        V("act", lambda e: e.copy(bset[p][:], pb[p][:]), [pb[p]], [bset[p]])
                    wB, kkB, kkanB, k2B, rB = [b[:].rearrange("p (h k) -> p h k", k=64) for b in bset]
                    V("pool", lambda e: e.tensor_tensor(tBs[:], k2B, vl[:, :, tl:tl + 1].to_broadcast([128, 8, 64]), ALU.mult), [bset[3], vl], [tBs])
                    V("dve", lambda e: e.tensor_tensor(tA[:], St[:], kkB, ALU.mult), [St, bset[1]], [tA])
                    V("dve", lambda e: e.tensor_reduce(sa[:], tA[:], AX.X, ALU.add), [tA], [sa])
                    V("dve", lambda e: e.tensor_tensor(St[:], St[:], wB, ALU.mult), [St, bset[0]], [St])
                    V("dve", lambda e: e.tensor_tensor(tA[:], kkanB, b3(sa), ALU.mult), [bset[2], sa], [tA])
                    V("dve", lambda e: e.tensor_tensor(St[:], St[:], tA[:], ALU.add), [St, tA], [St])
                    V("dve", lambda e: e.tensor_tensor(St[:], St[:], tBs[:], ALU.add), [St, tBs], [St])
                    V("dve", lambda e: e.tensor_tensor(tA[:], St[:], rB, ALU.mult), [St, bset[4]], [tA])
                    V("dve", lambda e: e.tensor_reduce(Y[:, :, tl], tA[:], AX.X, ALU.add), [tA], [Y])
                Yf = Y[:].rearrange("p h t -> p (h t)")
                V("pe", lambda e: e.matmul(pq[0][:], blkm[:], Yf, start=True, stop=True), [blkm, Y], [pq[0]])
                V("dve", lambda e: e.tensor_tensor(yc[:].rearrange("p h t -> p (h t)"), Yf, pq[0][:], ALU.subtract), [Y, pq[0]], [yc])
                V("act", lambda e: e.activation(sq[:], yc[:].rearrange("p h t -> p (h t)"), AF.Square), [yc], [sq])
                V("pe", lambda e: e.matmul(pq[1][:], blkm[:], sq[:], start=True, stop=True), [blkm, sq], [pq[1]])
                V("act", lambda e: e.activation(rstd[:], pq[1][:], AF.Sqrt, bias=64e-5, scale=1.0), [pq[1]], [rstd])
                V("dve", lambda e: e.reciprocal(rstd[:], rstd[:]), [rstd], [rstd])
                V("dve", lambda e: e.tensor_tensor(yc[:].rearrange("p h t -> p (h t)"), yc[:].rearrange("p h t -> p (h t)"), rstd[:], ALU.mult), [yc, rstd], [yc])
                V("dve", lambda e: e.tensor_tensor(yc[:], yc[:], b3(lng), ALU.mult), [yc, lng], [yc])
                V("dve", lambda e: e.tensor_tensor(yc[:], yc[:], b3(lnb), ALU.add), [yc, lnb], [yc])
                for h8 in range(8):
                    rhh = rh[h8 % 2]
                    V("pool", lambda e: e.tensor_scalar(rhh[:], i2[:], s2c[:, h8:h8 + 1], None, op0=ALU.mult), [i2, s2c], [rhh])
                    V("pe", lambda e: e.matmul(pq[0][:, h8 * 64:(h8 + 1) * 64], blk[:], rhh[:], start=True, stop=True), [blk, rhh], [pq[0]])
                V("dve", lambda e: e.tensor_tensor(tA[:].rearrange("p h t -> p (h t)"), pq[0][:], vl[:].rearrange("p h t -> p (h t)"), ALU.mult), [pq[0], vl], [tA])
                V("dve", lambda e: e.tensor_tensor(yc[:], yc[:], tA[:], ALU.add), [yc, tA], [yc])
                V("act", lambda e: e.activation(gl[:], gl[:], AF.Silu), [gl], [gl])
                ybc = yb[c % 2]
                V("dve", lambda e: e.tensor_tensor(ybc[:], yc[:], gl[:], ALU.mult), [yc, gl], [ybc])
                P.dma("sp", [(ysT.t[2048 + hf * 512:2048 + (hf + 1) * 512, t0:t0 + 64].rearrange("(h v) t -> v h t", v=64), ybc[hf * 64:(hf + 1) * 64, :, :]) for hf in range(2)],
                      reads=[ybc], writes=[ysT])

    PH = {"tables": phase_tables, "proj": phase_proj, "rope": phase_rope, "gla": phase_gla, "sgu": phase_sgu,
          "m1": phase_m1, "m2": phase_m2, "dsa1": phase_dsa1, "dsa2": phase_dsa2, "rwp": phase_rwp, "rws": phase_rws}
    plan = dbg.get("plan")
    if plan is None:
        plan = [("tables",)]
        for l in range(L):
            plan += [("proj", l, l), ("rope", l), ("gla", l), ("dsa1", l), ("dsa2", l), ("rwp", l), ("rws", l), ("sgu", l), ("m1", l), ("m2", l, l, l + 1)]
    for st in plan:
        nm = st[0]
        if nm == "tables":
            phase_tables()
        elif nm == "proj":
            phase_proj(st[1], xcur[st[2]])
        elif nm == "m2":
            phase_m2(st[1], xcur[st[2]], xcur[st[3]])
        else:
            PH[nm](st[1])
    P.barrier()
    es.close()
    global LAST_P
    LAST_P = P
    return nc


def make_inputs(inputs, b):
    m = {"x": np.ascontiguousarray(inputs["x"][b]), "pos": np.ascontiguousarray(inputs["positions"][b:b + 1]).astype(np.int32)}
    for k, v in inputs.items():
        if k in ("x", "positions"):
            continue
        m[k] = np.ascontiguousarray(v)
    m.update(host_consts())
    return m


def kernel(**inputs):
    nc = build_nc()
    in_maps = [make_inputs(inputs, c % 4) for c in range(8)]
    res = run_bass_kernel_spmd(nc, in_maps, core_ids=list(range(8)))
    return np.stack([res.results[c]["y"] for c in range(4)], axis=0).astype(np.float32)
```

```python
import numpy as np
from contextlib import ExitStack, contextmanager
import concourse.bass as bass
import concourse.mybir as mybir
from concourse.bass_utils import run_bass_kernel_spmd

F32 = mybir.dt.float32
BF16 = mybir.dt.bfloat16
I32 = mybir.dt.int32
ALU = mybir.AluOpType
AF = mybir.ActivationFunctionType
AX = mybir.AxisListType

D = 2048
S = 4096
L = 2
NIN = 23320
PAD = 64
TP = S + PAD
NEG = -1.0e30


class Buf:
    __slots__ = ("t", "w", "r", "sem", "semkey", "semv", "name", "acc", "wl", "psum")

    def __init__(self, t, name, acc=False):
        self.t = t
        self.name = name
        self.acc = acc
        self.psum = False
        self.wl = []
        self.w = None
        self.r = []
        self.sem = None
        self.semkey = None
        self.semv = 0

    def __getitem__(self, k):
        return self.t[k]


class Prog:
    def __init__(self, nc, es):
        self.nc = nc
        self.es = es
        self.eng = {"pe": nc.tensor, "act": nc.scalar, "dve": nc.vector, "pool": nc.gpsimd, "sp": nc.sync}
        self.esem = {}
        self.ecnt = {}
        self.ekey = {}
        self.eepoch = {}
        for e in ("pe", "act", "dve", "pool"):
            self.esem[e] = es.enter_context(nc.semaphore("e_" + e))
            self.ecnt[e] = 0
            self.eepoch[e] = 0
            self.ekey[e] = e + "#0"
        self.nwait = 0
        self.seen = {e: {} for e in self.eng}
        self.nbuf = 0
        self.scopes = [es]
        self.scope_bufs = [[]]
        self.sempool = []
        self.allsems = {}
        self.nsem = 0
        self.ninst = 0

    @contextmanager
    def scope(self):
        es = ExitStack()
        self.scopes.append(es)
        self.scope_bufs.append([])
        yield
        self.barrier()
        for b in self.scope_bufs.pop():
            if b.sem is not None:
                self.sempool.append((b.sem, b.semkey, b.semv))
        self.scopes.pop()
        es.close()

    def sb(self, shape, dt, name=None):
        self.nbuf += 1
        name = (name or "sb") + "_%d" % self.nbuf
        t = self.scopes[-1].enter_context(self.nc.sbuf_tensor(name, list(shape), dt))
        b = Buf(t, name)
        self.scope_bufs[-1].append(b)
        return b

    def ps(self, shape, dt, name=None):
        self.nbuf += 1
        name = (name or "ps") + "_%d" % self.nbuf
        t = self.scopes[-1].enter_context(self.nc.psum_tensor(name, list(shape), dt))
        b = Buf(t, name)
        b.psum = True
        self.scope_bufs[-1].append(b)
        return b

    def dram(self, name, shape, dt, kind="Internal"):
        t = self.nc.dram_tensor(name, list(shape), dt, kind=kind).ap()
        return Buf(t, name, acc=True)

    def _wait(self, e, ev, raw=True):
        if ev is None:
            return
        key, sem, val = ev
        if e == "pe" and key.startswith("pe#"):
            return
        if self.seen[e].get(key, 0) >= val:
            return
        self.eng[e].wait_ge(sem, val)
        self.nwait += 1
        self.seen[e][key] = val

    def _deps(self, e, reads, writes):
        for b in reads:
            self._wait(e, b.w, True)
            for ev in b.wl:
                self._wait(e, ev, True)
            if b.psum:
                for ev in b.r:
                    if not ev[0].startswith(e + "#"):
                        self._wait(e, ev, False)
        for b in writes:
            if not b.acc:
                self._wait(e, b.w, False)
            for ev in b.r:
                self._wait(e, ev, False)

    @staticmethod
    def _compact(evs):
        best = {}
        for ev in evs:
            k = ev[0]
            if k not in best or best[k][2] < ev[2]:
                best[k] = ev
        return list(best.values())

    def _record(self, ev, reads, writes):
        for b in writes:
            if b.acc:
                b.wl.append(ev)
                if len(b.wl) > 16:
                    b.wl = self._compact(b.wl)
            else:
                b.w = ev
            b.r = []
        for b in reads:
            if b not in writes:
                b.r.append(ev)
                if len(b.r) > 16:
                    b.r = self._compact(b.r)

    def op(self, e, fn, reads=(), writes=()):
        self._deps(e, reads, writes)
        ins = fn(self.eng[e])
        self.ecnt[e] += 1
        self.ninst += 1
        ins.then_inc(self.esem[e], 1)
        self._record((self.ekey[e], self.esem[e], self.ecnt[e]), reads, writes)
        return ins

    def _getsem(self, b):
        if b.sem is None:
            if self.sempool:
                b.sem, b.semkey, b.semv = self.sempool.pop()
            else:
                self.nsem += 1
                b.semkey = "d%d" % self.nsem
                b.sem = self.es.enter_context(self.nc.semaphore(b.semkey))
                b.semv = 0
            self.allsems[b.semkey] = b

    def dma(self, q, pairs, reads=(), writes=(), sembuf=None, **kw):
        self._deps(q, reads, writes)
        sb = sembuf
        if sb is None:
            cands = [b for b in list(writes) + list(reads) if not b.acc]
            sb = cands[0] if cands else (writes[0] if writes else reads[0])
        self._getsem(sb)
        self._wait(q, (sb.semkey, sb.sem, sb.semv))
        for (o, i) in pairs:
            self.eng[q].dma_start(out=o, in_=i, **kw).then_inc(sb.sem, 16)
            sb.semv += 16
            self.ninst += 1
        self._record((sb.semkey, sb.sem, sb.semv), reads, writes)

    def barrier(self):
        evs = [(self.ekey[c], self.esem[c], self.ecnt[c], c) for c in self.esem]
        for k, b in self.allsems.items():
            evs.append((k, b.sem, b.semv, None))
        for (sem, key, v) in self.sempool:
            evs.append((key, sem, v, None))
        for e in self.eng:
            for ev in evs:
                if ev[3] != e and ev[2] > 0:
                    if self.seen[e].get(ev[0], 0) < ev[2]:
                        self.eng[e].wait_ge(ev[1], ev[2])
                        self.nwait += 1
                        self.seen[e][ev[0]] = ev[2]
        for c in list(self.esem):
            if self.ecnt[c] > 12000:
                self.eepoch[c] += 1
                self.ekey[c] = "%s#%d" % (c, self.eepoch[c])
                self.esem[c] = self.es.enter_context(self.nc.semaphore("e_%s_%d" % (c, self.eepoch[c])))
                self.ecnt[c] = 0


SEGS = [
    (0, 512, "F", "gla_q"), (512, 512, "T", "gla_k"), (1024, 1024, "T", "gla_v"),
    (2048, 16, "F", "gla_g"), (2064, 1024, "F", "gla_gate"),
    (3088, 1024, "F", "dsa_q"), (4112, 1024, "F", "dsa_k"), (5136, 1024, "T", "dsa_v"),
    (6160, 512, "F", "dsa_iq"), (6672, 64, "F", "dsa_ik"), (6736, 8, "T", "dsa_iw"),
    (6744, 1024, "F", "dsa_gate"),
    (7768, 1024, "T", "rw_r"), (8792, 1024, "T", "rw_k"), (9816, 1024, "T", "rw_vT"), (9816, 1024, "F", "rw_v"),
    (10840, 96, "T", "rw_wlr"), (10936, 96, "T", "rw_alr"), (11032, 1024, "F", "rw_gate"),
    (12056, 1024, "F", "sgu_u"), (13080, 1024, "T", "sgu_zv"), (14104, 1024, "F", "sgu_gate"),
    (15128, 8192, "H", "pm"),
]
FM_ROWS = {}
TM_COLS = {}
_r = 0
_c = 0
for (_c0, _n, _m, _nm) in SEGS:
    if _m == "F":
        FM_ROWS[_nm] = _r
        _r += _n
    elif _m == "T":
        TM_COLS[_nm] = _c
        _c += _n
NFM = _r
NTM = _c


def host_consts():
    c = {}
    c["c_ident"] = np.eye(128, dtype=np.float32)
    s = np.arange(64)
    c["c_ustrict"] = (s[:, None] > s[None, :]).astype(np.float32)
    i = np.arange(128)
    c["c_sgumask"] = ((i[None, :] // 64) <= (i[:, None] // 64)).astype(np.float32)
    inv_a = (500000.0 ** (-(np.arange(0, 32, 2, dtype=np.float32) / np.float32(32)))).astype(np.float32)
    inv_i = (500000.0 ** (-(np.arange(0, 16, 2, dtype=np.float32) / np.float32(16)))).astype(np.float32)
    inv = np.zeros((128, 2), np.float32)
    inv[:, 0] = inv_a[i % 16]
    inv[:, 1] = inv_i[i % 8]
    c["c_inv"] = inv
    sel = np.zeros((128, 64, 128), np.float32)
    for tl in range(64):
        sel[tl, tl, 0:64] = 1.0
        sel[64 + tl, tl, 64:128] = 1.0
    c["c_sel"] = sel
    c["c_blk"] = ((i[:, None] // 64) == (i[None, :] // 64)).astype(np.float32)
    c["c_i2"] = ((i[:, None] % 64) == s[None, :]).astype(np.float32)
    j = np.arange(64)
    sidx = j % 32
    m = np.zeros((64, 64), np.float32)
    m[:, 0:32] = (sidx[:, None] < np.arange(32)[None, :])
    m[:, 32:64] = (sidx[:, None] <= np.arange(32)[None, :])
    c["c_m32tp"] = m
    t32 = np.arange(32)
    c["c_m32n"] = (t32[None, :] < t32[:, None]).astype(np.float32)
    blk32 = (i[:, None] // 32) == (i[None, :] // 32)
    c["c_lmat32"] = (blk32 & (i[:, None] <= i[None, :])).astype(np.float32)
    c["c_umat32"] = (blk32 & (i[:, None] > i[None, :])).astype(np.float32)
    c["c_ind32"] = ((i[:, None] // 32) == np.arange(4)[None, :]).astype(np.float32)
    return c


def build_nc(S=4096, dbg=None):
    dbg = dbg or {}
    TP = S + PAD
    NT = S // 128
    NB = S // 512
    NCH = S // 64
    KTOP = min(256, S // 4)
    nc = bass.Bass("TRN2", target_bir_lowering=False)
    es = ExitStack()
    P = Prog(nc, es)
    I = {}

    def inp(name, shape, dt=F32):
        I[name] = P.dram(name, shape, dt, kind="ExternalInput")
        return I[name]

    x_in = inp("x", [S, D])
    pos_in = inp("pos", [1, S], I32)
    inp("norm_pre", [L, D]); inp("norm_post", [L, D]); inp("w_in", [L, D, NIN])
    inp("gla_w_g2", [L, 16, 512]); inp("gla_b_g", [L, 512]); inp("gla_head_g", [L, 256])
    inp("rwkv_mu", [L, 4288]); inp("rwkv_w0", [L, 1024]); inp("rwkv_w_w2", [L, 96, 1024])
    inp("rwkv_a0", [L, 1024]); inp("rwkv_w_a2", [L, 96, 1024]); inp("rwkv_k_k", [L, 1024])
    inp("rwkv_k_a", [L, 1024]); inp("rwkv_r_k", [L, 16, 64]); inp("rwkv_lnx_g", [L, 1024])
    inp("rwkv_lnx_b", [L, 1024]); inp("sgu_ln_g", [L, 1024]); inp("sgu_ln_b", [L, 1024])
    inp("sgu_w_s", [L, 8, 128, 128]); inp("sgu_b_s", [L, 8, 128]); inp("w_proj", [L, 4, 1024, D])
    inp("w_out", [L, D, D])
    inp("c_ident", [128, 128]); inp("c_ustrict", [64, 64]); inp("c_sgumask", [128, 128])
    inp("c_inv", [128, 2]); inp("c_sel", [128, 64, 128]); inp("c_blk", [128, 128]); inp("c_i2", [128, 64])
    inp("c_m32tp", [64, 64]); inp("c_m32n", [32, 32]); inp("c_lmat32", [128, 128]); inp("c_umat32", [128, 128]); inp("c_ind32", [128, 4])
    y_out = P.dram("y", [S, D], F32, kind="ExternalOutput")

    ext_in = dbg.get("ext_in", ())
    ext_out = dbg.get("ext_out", ())

    def scr(name, shape, dt):
        kind = "ExternalInput" if name in ext_in else ("ExternalOutput" if name in ext_out else "Internal")
        return P.dram(name, shape, dt, kind=kind)
    fm = scr("fm", [NFM, TP], F32)
    tm = scr("tm", [S, NTM], F32)
    pmT = scr("pmT", [8192, S], BF16)
    ysT = scr("ysT", [4096, S], BF16)
    mT = scr("mT", [D, S], BF16)
    tabs = scr("tabs", [4, 128, S], F32)
    maskT = scr("maskT", [NT, S, 128], BF16)
    rwtm = scr("rwtm", [S, 5, 1024], F32)
    rwbon = scr("rwbon", [S, 16], F32)
    NC32 = S // 32
    rwtm2 = scr("rwtm2", [S, 3, 1024], F32)
    rwxt = scr("rwxt", [64, 16, NT, 512], F32)
    rwdec = scr("rwdec", [64, 16, NC32], F32)
    xcur = [x_in, scr("x1", [S, D], F32), y_out]

    if "fm_in" in dbg:
        pass

    ident_f = P.sb([128, 128], F32, "identf")
    ident_b = P.sb([128, 128], BF16, "identb")
    ones_b = P.sb([128, 128], BF16, "onesb")
    zeros_f = P.sb([128, PAD], F32, "zerosf")
    P.dma("sp", [(ident_f[:], I["c_ident"][:, :])], reads=[I["c_ident"]], writes=[ident_f])
    P.op("dve", lambda e: e.tensor_copy(ident_b[:], ident_f[:]), [ident_f], [ident_b])
    P.op("pool", lambda e: e.memset(ones_b[:], 1.0), [], [ones_b])
    P.op("pool", lambda e: e.memset(zeros_f[:], 0.0), [], [zeros_f])
    for nm in ("rw_v", "rw_gate"):
        for r8 in range(8):
            r0 = FM_ROWS[nm] + r8 * 128
            P.dma("sp", [(fm[r0:r0 + 128, 0:PAD], zeros_f[:])], reads=[zeros_f], writes=[fm])

    def evac(i, out_ap, in_ap, reads, writes):
        if i % 2 == 0:
            P.op("act", lambda e: e.copy(out_ap, in_ap), reads, writes)
        else:
            P.op("dve", lambda e: e.tensor_copy(out_ap, in_ap), reads, writes)

    def V(e, fn, reads, writes):
        P.op(e, fn, reads, writes)

    def phase_tables():
        with P.scope():
            posi = P.sb([128, S], I32, "posi")
            posf = P.sb([128, S], F32, "posf")
            inv = P.sb([128, 2], F32, "inv")
            ang = P.sb([128, S], F32, "ang")
            kf = P.sb([128, S], F32, "kf")
            ki = P.sb([128, S], I32, "ki")
            r1 = P.sb([128, S], F32, "r1")
            y = P.sb([128, S], F32, "y")
            m = P.sb([128, S], F32, "m")
            P.dma("sp", [(posi[:], pos_in[0:1, :].to_broadcast([128, S]))], reads=[pos_in], writes=[posi])
            P.dma("sp", [(inv[:], I["c_inv"][:, :])], reads=[I["c_inv"]], writes=[inv])
            V("dve", lambda e: e.tensor_copy(posf[:], posi[:]), [posi], [posf])
            for which in range(2):
                V("dve", lambda e: e.tensor_scalar(ang[:], posf[:], inv[:, which:which + 1], None, op0=ALU.mult), [posf, inv], [ang])
                V("dve", lambda e: e.tensor_scalar(kf[:], ang[:], float(1.0 / (2 * np.pi)), None, op0=ALU.mult), [ang], [kf])
                V("dve", lambda e: e.tensor_copy(ki[:], kf[:]), [kf], [ki])
                V("dve", lambda e: e.tensor_copy(kf[:], ki[:]), [ki], [kf])
                V("dve", lambda e: e.scalar_tensor_tensor(r1[:], kf[:], -6.28125, ang[:], op0=ALU.mult, op1=ALU.add), [kf, ang], [r1])
                V("dve", lambda e: e.scalar_tensor_tensor(ang[:], kf[:], -0.0019353071795864769, r1[:], op0=ALU.mult, op1=ALU.add), [kf, r1], [ang])
                for cs in range(2):
                    sh = float(np.pi / 2) if cs == 0 else 0.0
                    V("dve", lambda e: e.tensor_scalar(y[:], ang[:], sh, None, op0=ALU.add), [ang], [y])
                    V("dve", lambda e: e.tensor_scalar(m[:], y[:], float(np.pi), None, op0=ALU.is_gt), [y], [m])
                    V("dve", lambda e: e.scalar_tensor_tensor(y[:], m[:], float(-2 * np.pi), y[:], op0=ALU.mult, op1=ALU.add), [m, y], [y])
                    V("dve", lambda e: e.tensor_scalar(m[:], y[:], float(-np.pi), None, op0=ALU.is_lt), [y], [m])
                    V("dve", lambda e: e.scalar_tensor_tensor(y[:], m[:], float(2 * np.pi), y[:], op0=ALU.mult, op1=ALU.add), [m, y], [y])
                    V("dve", lambda e: e.tensor_scalar(y[:], y[:], float(np.pi), float(-np.pi), op0=ALU.min, op1=ALU.max), [y], [y])
                    V("act", lambda e: e.activation(r1[:], y[:], AF.Sin), [y], [r1])
                    P.dma("sp", [(tabs.t[which * 2 + cs, :, :], r1[:])], reads=[r1], writes=[tabs])

    def phase_proj(l, xin):
        with P.scope():
            hT = P.sb([128, 16, S], BF16, "hT")
            with P.scope():
                gbc = P.sb([128, D], F32, "gbc")
                P.dma("sp", [(gbc[:], I["norm_pre"][l:l + 1, :].to_broadcast([128, D]))], reads=[I["norm_pre"]], writes=[gbc])
                xt = [P.sb([128, D], F32, "xt") for _ in range(2)]
                junk = P.sb([128, D], BF16, "junk")
                hb = [P.sb([128, D], BF16, "hb") for _ in range(2)]
                ss = [P.sb([128, 1], F32, "ss") for _ in range(2)]
                rs = [P.sb([128, 1], F32, "rs") for _ in range(2)]
                pst = [P.ps([128, 4, 128], BF16, "pst") for _ in range(4)]
                P.dma("sp", [(xt[0][:], xin[0:128, :])], reads=[xin], writes=[xt[0]])
                for i in range(NT):
                    if i + 1 < NT:
                        P.dma("sp", [(xt[(i + 1) % 2][:], xin[(i + 1) * 128:(i + 2) * 128, :])], reads=[xin], writes=[xt[(i + 1) % 2]])
                    xi, si, ri, hi = xt[i % 2], ss[i % 2], rs[i % 2], hb[i % 2]
                    V("act", lambda e: e.activation(junk[:], xi[:], AF.Square, accum_out=si[:]), [xi], [junk, si])
                    V("act", lambda e: e.activation(ri[:], si[:], AF.Sqrt, bias=1e-6, scale=1.0 / D), [si], [ri])
                    V("dve", lambda e: e.reciprocal(ri[:], ri[:]), [ri], [ri])
                    V("dve", lambda e: e.scalar_tensor_tensor(hi[:], xi[:], ri[:, 0:1], gbc[:], op0=ALU.mult, op1=ALU.mult), [xi, ri, gbc], [hi])
                    for g4 in range(4):
                        pt = pst[g4]
                        for j in range(4):
                            kc = g4 * 4 + j
                            V("pe", lambda e: e.transpose(pt[:, j, :], hi[:, kc * 128:(kc + 1) * 128], ident_b[:]), [hi, ident_b], [pt])
                        evac(g4, hT[:, g4 * 4:(g4 + 1) * 4, i * 128:(i + 1) * 128], pt[:], [pt], [hT])
            with P.scope():
                wf = [P.sb([128, 16, 128], F32, "wf") for _ in range(2)]
                wb = [P.sb([128, 16, 128], BF16, "wb") for _ in range(2)]
                stg = [P.sb([128, S], F32, "stg") for _ in range(2)]
                stgh = [P.sb([128, S], BF16, "stgh") for _ in range(2)]
                pp = [P.ps([128, 512], F32, "pp") for _ in range(6)]
                slabs = []
                for (c0, n, mode, nm) in SEGS:
                    off = 0
                    while off < n:
                        m = min(128, n - off)
                        slabs.append((c0 + off, m, mode, nm, off))
                        off += m
                w_l = I["w_in"]

                def load(s):
                    c0, m, mode, nm, off = slabs[s]
                    src = w_l.t[l, :, c0:c0 + m].rearrange("(kc p) m -> p kc m", p=128)
                    P.dma("sp", [(wf[s % 2][:, k4 * 4:(k4 + 1) * 4, 0:m], src[:, k4 * 4:(k4 + 1) * 4, :]) for k4 in range(4)],
                          reads=[w_l], writes=[wf[s % 2]])

                def cast(s):
                    c0, m, mode, nm, off = slabs[s]
                    V("pool", lambda e: e.tensor_copy(wb[s % 2][:, :, 0:m], wf[s % 2][:, :, 0:m]), [wf[s % 2]], [wb[s % 2]])

                load(0)
                cast(0)
                if len(slabs) > 1:
                    load(1)
                pi = 0
                for s in range(len(slabs)):
                    c0, m, mode, nm, off = slabs[s]
                    if s + 1 < len(slabs):
                        cast(s + 1)
                    if s + 2 < len(slabs):
                        load(s + 2)
                    w_s = wb[s % 2]
                    if mode in ("F", "H"):
                        st = stg[s % 2] if mode == "F" else stgh[s % 2]
                        for tb in range(NB):
                            ps = pp[pi % 6]
                            for kc in range(16):
                                V("pe", lambda e: e.matmul(ps[0:m, :], w_s[:, kc, 0:m], hT[:, kc, tb * 512:(tb + 1) * 512],
                                                           start=(kc == 0), stop=(kc == 15)), [w_s, hT], [ps])
                            evac(pi, st[0:m, tb * 512:(tb + 1) * 512], ps[0:m, :], [ps], [st])
                            pi += 1
                        if mode == "F":
                            r0 = FM_ROWS[nm] + off
                            P.dma("sp", [(fm[r0:r0 + m, PAD:TP], st[0:m, :])], reads=[st], writes=[fm])
                        else:
                            P.dma("sp", [(pmT[off:off + m, :], st[0:m, :])], reads=[st], writes=[pmT])
                    else:
                        st = stg[s % 2]
                        st3 = st[:].rearrange("p (t m) -> p t m", m=128)
                        for t4 in range(NT // 4):
                            ps = pp[pi % 6]
                            ps3 = ps[:].rearrange("p (t m) -> p t m", m=128)
                            for j in range(4):
                                tt = t4 * 4 + j
                                for kc in range(16):
                                    V("pe", lambda e: e.matmul(ps3[:, j, 0:m], hT[:, kc, tt * 128:(tt + 1) * 128], w_s[:, kc, 0:m],
                                                               start=(kc == 0), stop=(kc == 15)), [w_s, hT], [ps])
                            evac(pi, st3[:, t4 * 4:(t4 + 1) * 4, 0:m], ps3[:, :, 0:m], [ps], [st])
                            pi += 1
                        cc = TM_COLS[nm] + off
                        P.dma("sp", [(tm.t[:, cc:cc + m].rearrange("(t p) m -> p t m", p=128), st3[:, 0:NT, 0:m])], reads=[st], writes=[tm])

    def phase_rope(l):
        with P.scope():
            A = P.sb([128, S], F32, "ropeA")
            B = P.sb([128, S], F32, "ropeB")
            C = P.sb([128, S], F32, "ropeC")
            Sn = P.sb([128, S], F32, "ropeS")
            t1 = P.sb([128, S], F32, "ropet1")
            t2 = P.sb([128, S], F32, "ropet2")
            A2 = P.sb([128, S], F32, "ropeA2")
            B2 = P.sb([128, S], F32, "ropeB2")
            for (nm, nh, hd, half, tab) in (("dsa_q", 8, 128, 16, 0), ("dsa_k", 8, 128, 16, 0), ("dsa_iq", 8, 64, 8, 2), ("dsa_ik", 1, 64, 8, 2)):
                np_ = nh * half
                r0 = FM_ROWS[nm]
                rows = fm.t[r0:r0 + nh * hd, PAD:TP].rearrange("(h r) t -> h r t", r=hd)
                P.dma("sp", [(C[0:np_, :], tabs.t[tab, 0:np_, :]), (Sn[0:np_, :], tabs.t[tab + 1, 0:np_, :])], reads=[tabs], writes=[C, Sn], sembuf=C)
                P.dma("sp", [(A[0:np_, :], rows[:, 0:half, :]), (B[0:np_, :], rows[:, half:2 * half, :])], reads=[fm], writes=[A, B], sembuf=A)
                V("dve", lambda e: e.tensor_tensor(t1[0:np_, :], A[0:np_, :], C[0:np_, :], ALU.mult), [A, C], [t1])
                V("pool", lambda e: e.tensor_tensor(t2[0:np_, :], B[0:np_, :], Sn[0:np_, :], ALU.mult), [B, Sn], [t2])
                V("dve", lambda e: e.tensor_tensor(A2[0:np_, :], t1[0:np_, :], t2[0:np_, :], ALU.subtract), [t1, t2], [A2])
                V("dve", lambda e: e.tensor_tensor(t1[0:np_, :], B[0:np_, :], C[0:np_, :], ALU.mult), [B, C], [t1])
                V("pool", lambda e: e.tensor_tensor(t2[0:np_, :], A[0:np_, :], Sn[0:np_, :], ALU.mult), [A, Sn], [t2])
                V("dve", lambda e: e.tensor_tensor(B2[0:np_, :], t1[0:np_, :], t2[0:np_, :], ALU.add), [t1, t2], [B2])
                P.dma("sp", [(rows[:, 0:half, :], A2[0:np_, :]), (rows[:, half:2 * half, :], B2[0:np_, :])], reads=[A2, B2], writes=[fm], sembuf=A2)

    def phase_gla(l):
        with P.scope():
            qT = P.sb([128, 4, S], BF16, "gqT")
            oT = P.sb([128, 8, S], BF16, "goT")
            stg = [P.sb([128, S], F32, "gstg") for _ in range(2)]
            glr = P.sb([17, S], F32, "glr")
            wg = P.sb([17, 512], F32, "gwg")
            ust = P.sb([64, 64], F32, "gust")
            ones64 = P.sb([64, 1], F32, "gones")
            hg = P.sb([128, 2], F32, "ghg")
            state = P.sb([128, 4, 256], F32, "gstate")
            state_b = P.sb([128, 4, 256], BF16, "gstateb")
            kc_ = [P.sb([64, 512], F32, "gk") for _ in range(2)]
            vc_ = [P.sb([64, 1024], F32, "gv") for _ in range(2)]
            vb = P.sb([64, 1024], BF16, "gvb")
            ee = P.sb([64, 512], F32, "gee")
            lsp = P.sb([64, 512], F32, "glsp")
            dkk = P.sb([64, 512], F32, "gdk")
            kdec = P.sb([64, 512], BF16, "gkdec")
            dec = P.sb([128, 4], F32, "gdec")
            pz = P.ps([64, 512], F32, "gpz")
            pG = P.ps([64, 512], F32, "gpG")
            ptot = P.ps([128, 4], F32, "gptot")
            pkv = [P.ps([128, 2, 256], F32, "gpkv") for _ in range(2)]
            po = P.ps([128, 8, 64], F32, "gpo")
            V("pool", lambda e: e.memset(glr[:], 1.0), [], [glr])
            V("pool", lambda e: e.memset(ones64[:], 1.0), [], [ones64])
            V("pool", lambda e: e.memset(state[:], 0.0), [], [state])
            r0 = FM_ROWS["gla_g"]
            P.dma("sp", [(glr[0:16, :], fm[r0:r0 + 16, PAD:TP])], reads=[fm], writes=[glr])
            P.dma("sp", [(wg[0:16, :], I["gla_w_g2"].t[l, :, :]), (wg[16:17, :], I["gla_b_g"][l:l + 1, :])], reads=[I["gla_w_g2"], I["gla_b_g"]], writes=[wg])
            P.dma("sp", [(ust[:], I["c_ustrict"][:, :])], reads=[I["c_ustrict"]], writes=[ust])
            P.dma("sp", [(hg[:], I["gla_head_g"].t[l, :].rearrange("(a p) -> p a", p=128))], reads=[I["gla_head_g"]], writes=[hg], allow_slow_non_contiguous=True)
            r0 = FM_ROWS["gla_q"]
            for h in range(4):
                st = stg[h % 2]
                P.dma("sp", [(st[:], fm[r0 + h * 128:r0 + (h + 1) * 128, PAD:TP])], reads=[fm], writes=[st])
                V("pool", lambda e: e.tensor_copy(qT[:, h, :], st[:]), [st], [qT])
            ck = TM_COLS["gla_k"]
            cv = TM_COLS["gla_v"]

            def loadkv(c):
                P.dma("sp", [(kc_[c % 2][:], tm[c * 64:(c + 1) * 64, ck:ck + 512])], reads=[tm], writes=[kc_[c % 2]])
                P.dma("sp", [(vc_[c % 2][:], tm[c * 64:(c + 1) * 64, cv:cv + 1024])], reads=[tm], writes=[vc_[c % 2]])
            loadkv(0)
            for c in range(NCH):
                if c + 1 < NCH:
                    loadkv(c + 1)
                t0 = c * 64
                kk_, vv_ = kc_[c % 2], vc_[c % 2]
                V("pe", lambda e: e.matmul(pz[:], glr[:, t0:t0 + 64], wg[:], start=True, stop=True), [glr, wg], [pz])
                V("act", lambda e: e.activation(ee[:], pz[:], AF.Exp, scale=-1.0), [pz], [ee])
                V("act", lambda e: e.activation(lsp[:], ee[:], AF.Ln, bias=1.0), [ee], [lsp])
                V("pe", lambda e: e.matmul(pG[:], ust[:], lsp[:], start=True, stop=True), [ust, lsp], [pG])
                for h in range(4):
                    V("pe", lambda e: e.matmul(ptot[:, h:h + 1], lsp[:, h * 128:(h + 1) * 128], ones64[:], start=True, stop=True), [lsp, ones64], [ptot])
                V("act", lambda e: e.activation(dkk[:], pG[:], AF.Exp, scale=-1.0 / 16.0), [pG], [dkk])
                V("act", lambda e: e.activation(dec[:], ptot[:], AF.Exp, scale=-1.0 / 16.0), [ptot], [dec])
                V("dve", lambda e: e.tensor_tensor(kdec[:], kk_[:], dkk[:], ALU.mult), [kk_, dkk], [kdec])
                V("pool", lambda e: e.tensor_copy(vb[:], vv_[:]), [vv_], [vb])
                for h in range(4):
                    pk = pkv[h // 2]
                    V("pe", lambda e: e.matmul(pk[:, h % 2, :], kdec[:, h * 128:(h + 1) * 128], vb[:, h * 256:(h + 1) * 256], start=True, stop=True), [kdec, vb], [pk])
                for h in range(4):
                    pk = pkv[h // 2]
                    V("dve", lambda e: e.scalar_tensor_tensor(state[:, h, :], state[:, h, :], dec[:, h:h + 1], pk[:, h % 2, :], op0=ALU.mult, op1=ALU.add), [state, dec, pk], [state])
                V("act", lambda e: e.copy(state_b[:], state[:]), [state], [state_b])
                for h in range(4):
                    for vh in range(2):
                        V("pe", lambda e: e.matmul(po[:, h * 2 + vh, :], state_b[:, h, vh * 128:(vh + 1) * 128], qT[:, h, t0:t0 + 64], start=True, stop=True), [state_b, qT], [po])
                evac(c, oT[:, :, t0:t0 + 64], po[:], [po], [oT])
            sq = P.sb([128, 2, 512], BF16, "gsq")
            pms = [P.ps([128, 512], F32, "gpms") for _ in range(2)]
            rstd = P.sb([128, 512], F32, "grstd")
            yy = P.sb([128, 512], F32, "gyy")
            sg = P.sb([128, 512], F32, "gsg")
            yo = [P.sb([128, S], BF16, "gyo") for _ in range(2)]
            sc = float(128.0 ** -0.5)
            hgs = P.sb([128, 2], F32, "ghgs")
            V("dve", lambda e: e.tensor_scalar(hgs[:], hg[:], sc, None, op0=ALU.mult), [hg], [hgs])
            rg = FM_ROWS["gla_gate"]
            it = 0
            for h in range(4):
                for vh in range(2):
                    gt = stg[it % 2]
                    yob = yo[it % 2]
                    P.dma("sp", [(gt[:], fm[rg + h * 256 + vh * 128:rg + h * 256 + (vh + 1) * 128, PAD:TP])], reads=[fm], writes=[gt])
                    for tb in range(NB):
                        ts_ = slice(tb * 512, (tb + 1) * 512)
                        pm_ = pms[(it * NB + tb) % 2]
                        if vh == 0:
                            for v2 in range(2):
                                V("act", lambda e: e.activation(sq[:, v2, :], oT[:, h * 2 + v2, ts_], AF.Square), [oT], [sq])
                        else:
                            for v2 in range(2):
                                V("act", lambda e: e.activation(sq[:, v2, :], oT[:, h * 2 + v2, ts_], AF.Square), [oT], [sq])
                        for v2 in range(2):
                            V("pe", lambda e: e.matmul(pm_[:], ones_b[:], sq[:, v2, :], start=(v2 == 0), stop=(v2 == 1)), [ones_b, sq], [pm_])
                        V("act", lambda e: e.activation(rstd[:], pm_[:], AF.Sqrt, bias=1e-6, scale=sc * sc / 256.0), [pm_], [rstd])
                        V("dve", lambda e: e.reciprocal(rstd[:], rstd[:]), [rstd], [rstd])
                        V("act", lambda e: e.activation(sg[:], gt[:, ts_], AF.Silu), [gt], [sg])
                        V("dve", lambda e: e.tensor_tensor(yy[:], oT[:, h * 2 + vh, ts_], rstd[:], ALU.mult), [oT, rstd], [yy])
                        V("dve", lambda e: e.scalar_tensor_tensor(yob[:, ts_], yy[:], hgs[:, vh:vh + 1], sg[:], op0=ALU.mult, op1=ALU.mult), [yy, hgs, sg], [yob])
                    ro = h * 256 + vh * 128
                    P.dma("sp", [(ysT[ro:ro + 128, :], yob[:])], reads=[yob], writes=[ysT])
                    it += 1

    def phase_sgu(l):
        with P.scope():
            lng = P.sb([128, 1024], F32, "slng")
            lnb = P.sb([128, 1024], F32, "slnb")
            bsb = P.sb([128, 8, 128], F32, "sbsb")
            msk = P.sb([128, 128], F32, "smsk")
            wsT = P.sb([128, 8, 128], BF16, "swsT")
            wtmp = P.sb([128, 128], F32, "swtmp")
            pT = P.ps([128, 128], F32, "spT")
            P.dma("sp", [(lng[:], I["sgu_ln_g"][l:l + 1, :].to_broadcast([128, 1024]))], reads=[I["sgu_ln_g"]], writes=[lng])
            P.dma("sp", [(lnb[:], I["sgu_ln_b"][l:l + 1, :].to_broadcast([128, 1024]))], reads=[I["sgu_ln_b"]], writes=[lnb])
            P.dma("sp", [(bsb[:].rearrange("p g i -> p (g i)"), I["sgu_b_s"].t[l:l + 1, :, :].rearrange("a g i -> a (g i)").to_broadcast([128, 1024]))], reads=[I["sgu_b_s"]], writes=[bsb])
            P.dma("sp", [(msk[:], I["c_sgumask"][:, :])], reads=[I["c_sgumask"]], writes=[msk])
            for g in range(8):
                P.dma("sp", [(wtmp[:], I["sgu_w_s"].t[l, g, :, :])], reads=[I["sgu_w_s"]], writes=[wtmp])
                V("dve", lambda e: e.tensor_tensor(wtmp[:], wtmp[:], msk[:], ALU.mult), [wtmp, msk], [wtmp])
                V("pe", lambda e: e.transpose(pT[:], wtmp[:], ident_f[:]), [wtmp, ident_f], [pT])
                V("act", lambda e: e.copy(wsT[:, g, :], pT[:]), [pT], [wsT])
            zv = [P.sb([128, 1024], F32, "szv") for _ in range(2)]
            uu = [P.sb([128, 8, 128], F32, "suu") for _ in range(2)]
            gg = [P.sb([128, 8, 128], F32, "sgg") for _ in range(2)]
            g1 = P.sb([128, 1024], F32, "sg1")
            junk = P.sb([128, 1024], BF16, "sjunk")
            vn = P.sb([128, 1024], F32, "svn")
            vln = P.sb([128, 1024], BF16, "svln")
            s1 = P.sb([128, 1], F32, "ss1"); s2 = P.sb([128, 1], F32, "ss2"); mm = P.sb([128, 1], F32, "smm")
            msq = P.sb([128, 1], F32, "smsq"); var = P.sb([128, 1], F32, "svar"); rstd = P.sb([128, 1], F32, "srstd")
            nmr = P.sb([128, 1], F32, "snmr")
            psv = [P.ps([128, 4, 128], F32, "spsv") for _ in range(2)]
            t1 = P.sb([128, 8, 128], F32, "st1")
            ug = P.sb([128, 8, 128], F32, "sug")
            sg = P.sb([128, 8, 128], F32, "ssg")
            yb = [P.sb([128, 8, 128], BF16, "syb") for _ in range(2)]
            cz = TM_COLS["sgu_zv"]
            ru = FM_ROWS["sgu_u"]
            rgt = FM_ROWS["sgu_gate"]

            def loadc(c):
                P.dma("sp", [(zv[c % 2][:], tm[c * 128:(c + 1) * 128, cz:cz + 1024])], reads=[tm], writes=[zv[c % 2]])
                P.dma("sp", [(uu[c % 2][:], fm.t[ru:ru + 1024, PAD + c * 128:PAD + (c + 1) * 128].rearrange("(g d) t -> d g t", d=128))], reads=[fm], writes=[uu[c % 2]])
                P.dma("sp", [(gg[c % 2][:], fm.t[rgt:rgt + 1024, PAD + c * 128:PAD + (c + 1) * 128].rearrange("(g d) t -> d g t", d=128))], reads=[fm], writes=[gg[c % 2]])
            loadc(0)
            for c in range(NT):
                if c + 1 < NT:
                    loadc(c + 1)
                z, u_, g_ = zv[c % 2], uu[c % 2], gg[c % 2]
                ybc = yb[c % 2]
                V("act", lambda e: e.activation(g1[:], z[:], AF.Gelu), [z], [g1])
                V("dve", lambda e: e.tensor_reduce(s1[:], g1[:], AX.X, ALU.add), [g1], [s1])
                V("act", lambda e: e.activation(junk[:], g1[:], AF.Square, accum_out=s2[:]), [g1], [junk, s2])
                V("dve", lambda e: e.tensor_scalar(mm[:], s1[:], 1.0 / 1024.0, None, op0=ALU.mult), [s1], [mm])
                V("dve", lambda e: e.tensor_tensor(msq[:], mm[:], mm[:], ALU.mult), [mm], [msq])
                V("dve", lambda e: e.scalar_tensor_tensor(var[:], s2[:], 1.0 / 1024.0, msq[:], op0=ALU.mult, op1=ALU.subtract), [s2, msq], [var])
                V("act", lambda e: e.activation(rstd[:], var[:], AF.Sqrt, bias=1e-5, scale=1.0), [var], [rstd])
                V("dve", lambda e: e.reciprocal(rstd[:], rstd[:]), [rstd], [rstd])
                V("dve", lambda e: e.scalar_tensor_tensor(nmr[:], mm[:], -1.0, rstd[:], op0=ALU.mult, op1=ALU.mult), [mm, rstd], [nmr])
                V("act", lambda e: e.activation(vn[:], g1[:], AF.Identity, bias=nmr[:, 0:1], scale=rstd[:, 0:1]), [g1, nmr, rstd], [vn])
                V("dve", lambda e: e.tensor_tensor(vn[:], vn[:], lng[:], ALU.mult), [vn, lng], [vn])
                V("dve", lambda e: e.tensor_tensor(vln[:], vn[:], lnb[:], ALU.add), [vn, lnb], [vln])
                for g in range(8):
                    pv = psv[g // 4]
                    V("pe", lambda e: e.matmul(pv[:, g % 4, :], vln[:, g * 128:(g + 1) * 128], wsT[:, g, :], start=True, stop=True), [vln, wsT], [pv])
                for g2 in range(2):
                    V("dve", lambda e: e.tensor_tensor(t1[:, g2 * 4:(g2 + 1) * 4, :], psv[g2][:], bsb[:, g2 * 4:(g2 + 1) * 4, :], ALU.add), [psv[g2], bsb], [t1])
                V("act", lambda e: e.activation(ug[:], u_[:], AF.Gelu), [u_], [ug])
                V("act", lambda e: e.activation(sg[:], g_[:], AF.Silu), [g_], [sg])
                V("pool", lambda e: e.tensor_tensor(ug[:], ug[:], sg[:], ALU.mult), [ug, sg], [ug])
                V("dve", lambda e: e.tensor_tensor(ybc[:], t1[:], ug[:], ALU.mult), [t1, ug], [ybc])
                P.dma("sp", [(ysT.t[3072:4096, c * 128:(c + 1) * 128].rearrange("(g d) t -> d g t", d=128), ybc[:])], reads=[ybc], writes=[ysT])

    def phase_m1(l):
        with P.scope():
            wp = P.sb([128, 32, D], BF16, "mwp")
            wst = [P.sb([128, 2048], F32, "mwst") for _ in range(2)]
            wpj = I["w_proj"]
            k = 0
            for g in range(4):
                for wc in range(8):
                    st = wst[k % 2]
                    P.dma("sp", [(st[:], wpj.t[l, g, wc * 128:(wc + 1) * 128, :])], reads=[wpj], writes=[st])
                    if k % 2 == 0:
                        V("pool", lambda e: e.tensor_copy(wp[:, g * 8 + wc, :], st[:]), [st], [wp])
                    else:
                        V("dve", lambda e: e.tensor_copy(wp[:, g * 8 + wc, :], st[:]), [st], [wp])
                    k += 1
            TB = 256
            yt = [P.sb([128, 32, TB], BF16, "myt") for _ in range(2)]
            pmt = [P.sb([128, 4, TB], BF16, "mpm") for _ in range(3)]
            sig = P.sb([128, 4, TB], F32, "msig")
            acc = P.sb([128, TB], F32, "macc")
            tmp = P.sb([128, TB], F32, "mtmp")
            mo = [P.sb([128, 16, TB], BF16, "mmo") for _ in range(2)]
            pps = [P.ps([128, TB], F32, "mpp") for _ in range(6)]
            nblk = S // TB

            def loady(b):
                P.dma("sp", [(yt[b % 2][:, q * 8:(q + 1) * 8, :], ysT.t[q * 1024:(q + 1) * 1024, b * TB:(b + 1) * TB].rearrange("(c p) t -> p c t", p=128)) for q in range(4)],
                      reads=[ysT], writes=[yt[b % 2]])
            loady(0)
            pi = 0
            it = 0
            for b in range(nblk):
                if b + 1 < nblk:
                    loady(b + 1)
                ytb = yt[b % 2]
                mob = mo[b % 2]
                for dc in range(16):
                    pmb = pmt[it % 3]
                    it += 1
                    P.dma("sp", [(pmb[:], pmT.t[:, b * TB:(b + 1) * TB].rearrange("(g r) t -> r g t", g=4)[dc * 128:(dc + 1) * 128, :, :])], reads=[pmT], writes=[pmb])
                    V("act", lambda e: e.activation(sig[:], pmb[:], AF.Sigmoid), [pmb], [sig])
                    for g in range(4):
                        ps = pps[pi % 6]
                        pi += 1
                        for wc in range(8):
                            V("pe", lambda e: e.matmul(ps[:], wp[:, g * 8 + wc, dc * 128:(dc + 1) * 128], ytb[:, g * 8 + wc, :], start=(wc == 0), stop=(wc == 7)), [wp, ytb], [ps])
                        if g == 0:
                            V("dve", lambda e: e.tensor_tensor(acc[:], ps[:], sig[:, g, :], ALU.mult), [ps, sig], [acc])
                        elif g < 3:
                            V("dve", lambda e: e.tensor_tensor(tmp[:], ps[:], sig[:, g, :], ALU.mult), [ps, sig], [tmp])
                            V("pool", lambda e: e.tensor_tensor(acc[:], acc[:], tmp[:], ALU.add), [acc, tmp], [acc])
                        else:
                            V("dve", lambda e: e.tensor_tensor(tmp[:], ps[:], sig[:, g, :], ALU.mult), [ps, sig], [tmp])
                            V("pool", lambda e: e.tensor_tensor(mob[:, dc, :], acc[:], tmp[:], ALU.add), [acc, tmp], [mob])
                P.dma("sp", [(mT.t[:, b * TB:(b + 1) * TB].rearrange("(c p) t -> p c t", p=128), mob[:])], reads=[mob], writes=[mT])

    def phase_m2(l, xin, xout):
        with P.scope():
            wo = P.sb([128, 16, D], BF16, "owo")
            wst = [P.sb([128, 2048], F32, "owst") for _ in range(2)]
            for dc in range(16):
                st = wst[dc % 2]
                P.dma("sp", [(st[:], I["w_out"].t[l, dc * 128:(dc + 1) * 128, :])], reads=[I["w_out"]], writes=[st])
                if dc % 2 == 0:
                    V("pool", lambda e: e.tensor_copy(wo[:, dc, :], st[:]), [st], [wo])
                else:
                    V("dve", lambda e: e.tensor_copy(wo[:, dc, :], st[:]), [st], [wo])
            gbc = P.sb([128, D], F32, "ogbc")
            P.dma("sp", [(gbc[:], I["norm_post"][l:l + 1, :].to_broadcast([128, D]))], reads=[I["norm_post"]], writes=[gbc])
            mt = [P.sb([128, 16, 128], BF16, "omt") for _ in range(2)]
            xt = [P.sb([128, D], F32, "oxt") for _ in range(2)]
            ot = P.sb([128, D], F32, "oot")
            junk = P.sb([128, D], BF16, "ojunk")
            res = [P.sb([128, D], F32, "ores") for _ in range(2)]
            ssq = P.sb([128, 1], F32, "ossq")
            rs = P.sb([128, 1], F32, "ors")
            pps = [P.ps([128, 512], F32, "opp") for _ in range(4)]

            def loadt(i):
                P.dma("sp", [(mt[i % 2][:], mT.t[:, i * 128:(i + 1) * 128].rearrange("(c p) t -> p c t", p=128))], reads=[mT], writes=[mt[i % 2]])
                P.dma("sp", [(xt[i % 2][:], xin[i * 128:(i + 1) * 128, :])], reads=[xin], writes=[xt[i % 2]])
            loadt(0)
            for i in range(NT):
                if i + 1 < NT:
                    loadt(i + 1)
                mti, xti, rsi = mt[i % 2], xt[i % 2], res[i % 2]
                for nb in range(4):
                    ps = pps[nb]
                    for dc in range(16):
                        V("pe", lambda e: e.matmul(ps[:], mti[:, dc, :], wo[:, dc, nb * 512:(nb + 1) * 512], start=(dc == 0), stop=(dc == 15)), [mti, wo], [ps])
                    evac(nb, ot[:, nb * 512:(nb + 1) * 512], ps[:], [ps], [ot])
                V("act", lambda e: e.activation(junk[:], ot[:], AF.Square, accum_out=ssq[:]), [ot], [junk, ssq])
                V("act", lambda e: e.activation(rs[:], ssq[:], AF.Sqrt, bias=1e-6, scale=1.0 / D), [ssq], [rs])
                V("dve", lambda e: e.reciprocal(rs[:], rs[:]), [rs], [rs])
                V("dve", lambda e: e.scalar_tensor_tensor(ot[:], ot[:], rs[:, 0:1], gbc[:], op0=ALU.mult, op1=ALU.mult), [ot, rs, gbc], [ot])
                V("pool", lambda e: e.tensor_tensor(rsi[:], ot[:], xti[:], ALU.add), [ot, xti], [rsi])
                P.dma("sp", [(xout[i * 128:(i + 1) * 128, :], rsi[:])], reads=[rsi], writes=[xout])


    def phase_dsa1(l):
        with P.scope():
            iqT = P.sb([64, 8, S], BF16, "diqT")
            ikT = P.sb([64, S], BF16, "dikT")
            iw = P.sb([128, NT, 8], F32, "diw")
            stg = [P.sb([64, S], F32, "dstg") for _ in range(2)]
            score = P.sb([128, S], F32, "dscore")
            work = P.sb([128, S], F32, "dwork")
            maskb = P.sb([128, S], BF16, "dmaskb")
            mts = [P.sb([128, NT, 128], BF16, "dmts") for _ in range(2)]
            rl = [P.sb([128, 512], F32, "drl") for _ in range(2)]
            m8 = P.sb([128, 8], F32, "dm8")
            thr = P.sb([128, 1], F32, "dthr")
            pps = [P.ps([128, 512], F32, "dpp") for _ in range(4)]
            ptr = [P.ps([128, 4, 128], BF16, "dptr") for _ in range(2)]
            r0 = FM_ROWS["dsa_iq"]
            for h in range(8):
                st = stg[h % 2]
                P.dma("sp", [(st[:], fm[r0 + h * 64:r0 + (h + 1) * 64, PAD:TP])], reads=[fm], writes=[st])
                V("pool", lambda e: e.tensor_copy(iqT[:, h, :], st[:]), [st], [iqT])
            r0 = FM_ROWS["dsa_ik"]
            P.dma("sp", [(stg[0][:], fm[r0:r0 + 64, PAD:TP])], reads=[fm], writes=[stg[0]])
            V("pool", lambda e: e.tensor_copy(ikT[:], stg[0][:]), [stg[0]], [ikT])
            cw = TM_COLS["dsa_iw"]
            P.dma("sp", [(iw[:], tm.t[:, cw:cw + 8].rearrange("(t p) c -> p t c", p=128))], reads=[tm], writes=[iw])
            V("dve", lambda e: e.tensor_scalar(iw[:], iw[:], float(512.0 ** -0.5), None, op0=ALU.mult), [iw], [iw])
            k = 0
            kt = 0
            for qt in range(NT):
                nk = (qt + 1) * 128
                for sb in range((nk + 511) // 512):
                    w = min(512, nk - sb * 512)
                    cs = slice(sb * 512, sb * 512 + w)
                    for h in range(8):
                        ps = pps[k % 4]
                        rt = rl[k % 2]
                        k += 1
                        V("pe", lambda e: e.matmul(ps[:, 0:w], iqT[:, h, qt * 128:(qt + 1) * 128], ikT[:, cs], start=True, stop=True), [iqT, ikT], [ps])
                        V("act", lambda e: e.activation(rt[:, 0:w], ps[:, 0:w], AF.Relu), [ps], [rt])
                        if h == 0:
                            V("dve", lambda e: e.tensor_scalar(score[:, cs], rt[:, 0:w], iw[:, qt, 0:1], None, op0=ALU.mult), [rt, iw], [score])
                        else:
                            V("dve", lambda e: e.scalar_tensor_tensor(score[:, cs], rt[:, 0:w], iw[:, qt, h:h + 1], score[:, cs], op0=ALU.mult, op1=ALU.add), [rt, iw, score], [score])
                V("pool", lambda e: e.memset(score[0:64, nk - 64:nk], NEG), [], [score])
                if nk <= KTOP:
                    V("pool", lambda e: e.memset(thr[:], NEG / 2), [], [thr])
                else:
                    src = score
                    for r in range(KTOP // 8):
                        V("dve", lambda e: e.max(out=m8[:], in_=src[:, 0:nk]), [src], [m8])
                        if r < KTOP // 8 - 1:
                            V("dve", lambda e: e.match_replace(out=work[:, 0:nk], in_to_replace=m8[:], in_values=src[:, 0:nk], imm_value=NEG), [m8, src], [work])
                            src = work
                        else:
                            V("dve", lambda e: e.tensor_copy(thr[:], m8[:, 7:8]), [m8], [thr])
                V("dve", lambda e: e.tensor_scalar(maskb[:, 0:nk], score[:, 0:nk], thr[:, 0:1], None, op0=ALU.is_ge), [score, thr], [maskb])
                mtb = mts[qt % 2]
                for j4 in range((qt + 4) // 4):
                    n = min(4, qt + 1 - j4 * 4)
                    pt = ptr[kt % 2]
                    kt += 1
                    for jj in range(n):
                        j = j4 * 4 + jj
                        V("pe", lambda e: e.transpose(pt[:, jj, :], maskb[:, j * 128:(j + 1) * 128], ident_b[:]), [maskb, ident_b], [pt])
                    evac(kt, mtb[:, j4 * 4:j4 * 4 + n, :], pt[:, 0:n, :], [pt], [mtb])
                P.dma("sp", [(maskT.t[qt, 0:nk, :].rearrange("(j p) t -> p j t", p=128), mtb[:, 0:qt + 1, :])], reads=[mtb], writes=[maskT])

    def phase_dsa2(l):
        with P.scope():
            stg = [P.sb([128, S], F32, "astg") for _ in range(2)]
            kT = P.sb([128, S], BF16, "akT")
            qT = P.sb([128, S], BF16, "aqT")
            Vh = P.sb([128, NT, 128], BF16, "aVh")
            sg = P.sb([128, S], F32, "asg")
            mk = [P.sb([128, 4, 128], BF16, "amk") for _ in range(3)]
            ex = [P.sb([128, 4, 128], BF16, "aex") for _ in range(2)]
            ptt = [P.sb([128, 4, 128], BF16, "aptt") for _ in range(2)]
            pl = [P.ps([128, 4, 128], F32, "apl") for _ in range(2)]
            po = [P.ps([128, 512], F32, "apo") for _ in range(2)]
            pd = [P.ps([128, 512], F32, "apd") for _ in range(2)]
            rden = P.sb([128, 128], F32, "arden")
            o1 = P.sb([128, 128], F32, "ao1")
            yo = [P.sb([128, S], BF16, "ayo") for _ in range(2)]
            sc = float(128.0 ** -0.5)
            rk, rq, rg = FM_ROWS["dsa_k"], FM_ROWS["dsa_q"], FM_ROWS["dsa_gate"]
            cv = TM_COLS["dsa_v"]
            kb = 0
            for h in range(8):
                P.dma("sp", [(stg[0][:], fm[rk + h * 128:rk + (h + 1) * 128, PAD:TP])], reads=[fm], writes=[stg[0]])
                V("pool", lambda e: e.tensor_copy(kT[:], stg[0][:]), [stg[0]], [kT])
                P.dma("sp", [(stg[1][:], fm[rq + h * 128:rq + (h + 1) * 128, PAD:TP])], reads=[fm], writes=[stg[1]])
                V("pool", lambda e: e.tensor_copy(qT[:], stg[1][:]), [stg[1]], [qT])
                P.dma("sp", [(stg[0][:].rearrange("p (j v) -> p j v", v=128), tm.t[:, cv + h * 128:cv + (h + 1) * 128].rearrange("(j p) v -> p j v", p=128))], reads=[tm], writes=[stg[0]])
                V("pool", lambda e: e.tensor_copy(Vh[:], stg[0][:].rearrange("p (j v) -> p j v", v=128)), [stg[0]], [Vh])
                P.dma("sp", [(stg[1][:], fm[rg + h * 128:rg + (h + 1) * 128, PAD:TP])], reads=[fm], writes=[stg[1]])
                V("act", lambda e: e.activation(sg[:], stg[1][:], AF.Silu), [stg[1]], [sg])
                yob = yo[h % 2]
                for qt in range(NT):
                    nj = qt + 1
                    po_, pd_ = po[qt % 2], pd[qt % 2]
                    qs = slice(qt * 128, (qt + 1) * 128)
                    for j4 in range((nj + 3) // 4):
                        n = min(4, nj - j4 * 4)
                        mkb, plb, exb, ptb = mk[kb % 3], pl[kb % 2], ex[kb % 2], ptt[kb % 2]
                        kb += 1
                        P.dma("sp", [(mkb[:, 0:n, :], maskT.t[qt, j4 * 512:j4 * 512 + n * 128, :].rearrange("(j p) t -> p j t", p=128))], reads=[maskT], writes=[mkb])
                        for jj in range(n):
                            j = j4 * 4 + jj
                            V("pe", lambda e: e.matmul(plb[:, jj, :], kT[:, j * 128:(j + 1) * 128], qT[:, qs], start=True, stop=True), [kT, qT], [plb])
                        V("act", lambda e: e.activation(exb[:, 0:n, :], plb[:, 0:n, :], AF.Exp, scale=sc), [plb], [exb])
                        V("dve", lambda e: e.tensor_tensor(ptb[:, 0:n, :], exb[:, 0:n, :], mkb[:, 0:n, :], ALU.mult), [exb, mkb], [ptb])
                        for jj in range(n):
                            j = j4 * 4 + jj
                            V("pe", lambda e: e.matmul(po_[:, 0:128], Vh[:, j, :], ptb[:, jj, :], start=(j == 0), stop=(j == nj - 1)), [Vh, ptb], [po_])
                            V("pe", lambda e: e.matmul(pd_[:, 0:128], ones_b[:], ptb[:, jj, :], start=(j == 0), stop=(j == nj - 1)), [ones_b, ptb], [pd_])
                    V("dve", lambda e: e.reciprocal(rden[:], pd_[:, 0:128]), [pd_], [rden])
                    V("dve", lambda e: e.tensor_tensor(o1[:], po_[:, 0:128], rden[:], ALU.mult), [po_, rden], [o1])
                    V("pool", lambda e: e.tensor_tensor(yob[:, qs], o1[:], sg[:, qs], ALU.mult), [o1, sg], [yob])
                P.dma("sp", [(ysT[1024 + h * 128:1024 + (h + 1) * 128, :], yob[:])], reads=[yob], writes=[ysT])

    def phase_rwp(l):
        with P.scope():
            c0 = TM_COLS["rw_r"]
            mub = P.sb([128, 2240], F32, "rmub")
            mu = I["rwkv_mu"]
            P.dma("sp", [(mub[:, 0:2048], mu[l:l + 1, 0:2048].to_broadcast([128, 2048])), (mub[:, 2048:2240], mu[l:l + 1, 3072:3264].to_broadcast([128, 192]))], reads=[mu], writes=[mub])

            def bc(name, n=1024):
                t = P.sb([128, n], F32, "rb" + name)
                src = I[name]
                if name == "rwkv_r_k":
                    ap = src.t[l:l + 1, :, :].rearrange("a h k -> a (h k)").to_broadcast([128, n])
                else:
                    ap = src[l:l + 1, :].to_broadcast([128, n])
                P.dma("sp", [(t[:], ap)], reads=[src], writes=[t])
                return t
            kkb, kab, w0b, a0b, rkb = bc("rwkv_k_k"), bc("rwkv_k_a"), bc("rwkv_w0"), bc("rwkv_a0"), bc("rwkv_r_k")
            omka = P.sb([128, 1024], F32, "romka")
            V("dve", lambda e: e.tensor_scalar(omka[:], kab[:], -1.0, 1.0, op0=ALU.mult, op1=ALU.add), [kab], [omka])
            ww2 = P.sb([96, 1024], F32, "rww2")
            wa2 = P.sb([96, 1024], F32, "rwa2")
            P.dma("sp", [(ww2[:], I["rwkv_w_w2"].t[l, :, :])], reads=[I["rwkv_w_w2"]], writes=[ww2])
            P.dma("sp", [(wa2[:], I["rwkv_w_a2"].t[l, :, :])], reads=[I["rwkv_w_a2"]], writes=[wa2])
            cur = [P.sb([128, 2240], F32, "rcur") for _ in range(2)]
            prv = [P.sb([128, 2240], F32, "rprv") for _ in range(2)]
            dd = P.sb([128, 2240], F32, "rdd")
            th = P.sb([128, 96], F32, "rth")
            thT = P.sb([96, 128], F32, "rthT")
            alT = P.sb([96, 128], F32, "ralT")
            ptT = [P.ps([96, 128], F32, "rptT") for _ in range(2)]
            pz = [P.ps([128, 2, 512], F32, "rpz") for _ in range(2)]
            zs = P.sb([128, 1024], F32, "rzs")
            aa = P.sb([128, 1024], F32, "raa")
            kk = P.sb([128, 1024], F32, "rkk")
            sq = P.sb([128, 1024], F32, "rsq")
            ssq = P.sb([128, 16], F32, "rssq")
            tt = P.sb([128, 1024], F32, "rtt")
            out5 = [P.sb([128, 5, 1024], F32, "rout5") for _ in range(2)]
            bon = [P.sb([128, 16], F32, "rbon") for _ in range(2)]

            def loadt(i):
                P.dma("sp", [(cur[i % 2][:], tm[i * 128:(i + 1) * 128, c0:c0 + 2240])], reads=[tm], writes=[cur[i % 2]])
                if i == 0:
                    V("pool", lambda e: e.memset(prv[0][0:1, :], 0.0), [], [prv[0]])
                    P.dma("sp", [(prv[0][1:128, :], tm[0:127, c0:c0 + 2240])], reads=[tm], writes=[prv[0]])
                else:
                    P.dma("sp", [(prv[i % 2][:], tm[i * 128 - 1:i * 128 + 127, c0:c0 + 2240])], reads=[tm], writes=[prv[i % 2]])
            loadt(0)
            for i in range(NT):
                if i + 1 < NT:
                    loadt(i + 1)
                cu, pr, o5, bn = cur[i % 2], prv[i % 2], out5[i % 2], bon[i % 2]
                V("dve", lambda e: e.tensor_tensor(dd[:], pr[:], cu[:], ALU.subtract), [pr, cu], [dd])
                V("pool", lambda e: e.tensor_tensor(dd[:], dd[:], mub[:], ALU.mult), [dd, mub], [dd])
                V("dve", lambda e: e.tensor_tensor(cu[:], cu[:], dd[:], ALU.add), [cu, dd], [cu])
                rr_, k_ = cu[:, 0:1024], cu[:, 1024:2048]
                V("act", lambda e: e.activation(th[:], cu[:, 2048:2144], AF.Tanh), [cu], [th])
                V("pe", lambda e: e.transpose(ptT[0][:], th[:], ident_f[:]), [th, ident_f], [ptT[0]])
                V("act", lambda e: e.copy(thT[:], ptT[0][:]), [ptT[0]], [thT])
                V("pe", lambda e: e.transpose(ptT[1][:], cu[:, 2144:2240], ident_f[:]), [cu, ident_f], [ptT[1]])
                V("dve", lambda e: e.tensor_copy(alT[:], ptT[1][:]), [ptT[1]], [alT])
                for n2 in range(2):
                    V("pe", lambda e: e.matmul(pz[0][:, n2, :], thT[:], ww2[:, n2 * 512:(n2 + 1) * 512], start=True, stop=True), [thT, ww2], [pz[0]])
                    V("pe", lambda e: e.matmul(pz[1][:, n2, :], alT[:], wa2[:, n2 * 512:(n2 + 1) * 512], start=True, stop=True), [alT, wa2], [pz[1]])
                V("dve", lambda e: e.tensor_tensor(zs[:], pz[0][:].rearrange("p a b -> p (a b)"), w0b[:], ALU.add), [pz[0], w0b], [zs])
                V("act", lambda e: e.activation(zs[:], zs[:], AF.Sigmoid), [zs], [zs])
                V("act", lambda e: e.activation(o5[:, 0, :], zs[:], AF.Exp, scale=-0.606531), [zs], [o5])
                V("dve", lambda e: e.tensor_tensor(aa[:], pz[1][:].rearrange("p a b -> p (a b)"), a0b[:], ALU.add), [pz[1], a0b], [aa])
                V("act", lambda e: e.activation(aa[:], aa[:], AF.Sigmoid), [aa], [aa])
                V("dve", lambda e: e.tensor_tensor(kk[:], k_, kkb[:], ALU.mult), [cu, kkb], [kk])
                V("act", lambda e: e.activation(sq[:], kk[:], AF.Square), [kk], [sq])
                V("dve", lambda e: e.tensor_reduce(ssq[:], sq[:].rearrange("p (h k) -> p h k", k=64), AX.X, ALU.add), [sq], [ssq])
                V("act", lambda e: e.activation(ssq[:], ssq[:], AF.Sqrt, bias=1e-12, scale=1.0), [ssq], [ssq])
                V("dve", lambda e: e.reciprocal(ssq[:], ssq[:]), [ssq], [ssq])
                V("dve", lambda e: e.tensor_tensor(o5[:, 1, :].rearrange("p (h k) -> p h k", k=64), kk[:].rearrange("p (h k) -> p h k", k=64),
                                                   ssq[:, :].unsqueeze(2).to_broadcast([128, 16, 64]), ALU.mult), [kk, ssq], [o5])
                V("dve", lambda e: e.scalar_tensor_tensor(o5[:, 2, :], o5[:, 1, :], -1.0, aa[:], op0=ALU.mult, op1=ALU.mult), [o5, aa], [o5])
                V("pool", lambda e: e.tensor_tensor(tt[:], aa[:], kab[:], ALU.mult), [aa, kab], [tt])
                V("pool", lambda e: e.tensor_tensor(tt[:], tt[:], omka[:], ALU.add), [tt, omka], [tt])
                V("dve", lambda e: e.tensor_tensor(o5[:, 3, :], k_, tt[:], ALU.mult), [cu, tt], [o5])
                V("pool", lambda e: e.tensor_copy(o5[:, 4, :], rr_), [cu], [o5])
                V("pool", lambda e: e.tensor_tensor(tt[:], rr_, rkb[:], ALU.mult), [cu, rkb], [tt])
                V("dve", lambda e: e.tensor_tensor(tt[:], tt[:], o5[:, 3, :], ALU.mult), [tt, o5], [tt])
                V("dve", lambda e: e.tensor_reduce(bn[:], tt[:].rearrange("p (h k) -> p h k", k=64), AX.X, ALU.add), [tt], [bn])
                P.dma("sp", [(rwtm[i * 128:(i + 1) * 128, :, :], o5[:])], reads=[o5], writes=[rwtm])
                P.dma("sp", [(rwbon[i * 128:(i + 1) * 128, :], bn[:])], reads=[bn], writes=[rwbon])

    def phase_rws(l):
        with P.scope():
            sel = P.sb([128, 64, 128], F32, "wsel")
            blk = P.sb([128, 128], F32, "wblk")
            blkm = P.sb([128, 128], F32, "wblkm")
            i2 = P.sb([128, 64], F32, "wi2")
            P.dma("sp", [(sel[:], I["c_sel"][:, :, :])], reads=[I["c_sel"]], writes=[sel])
            P.dma("sp", [(blk[:], I["c_blk"][:, :])], reads=[I["c_blk"]], writes=[blk])
            P.dma("sp", [(i2[:], I["c_i2"][:, :])], reads=[I["c_i2"]], writes=[i2])
            V("dve", lambda e: e.tensor_scalar(blkm[:], blk[:], 1.0 / 64.0, None, op0=ALU.mult), [blk], [blkm])

            def hv(name, off=0):
                t = P.sb([128, 8], F32, "w" + name)
                src = I[name]
                P.dma("sp", [(t[hf * 64:(hf + 1) * 64, :], src.t[l, off + hf * 512:off + (hf + 1) * 512].rearrange("(h v) -> v h", v=64)) for hf in range(2)],
                      reads=[src], writes=[t], allow_slow_non_contiguous=True)
                return t
            lng, lnb = hv("rwkv_lnx_g"), hv("rwkv_lnx_b")
            muv, mug = hv("rwkv_mu", 2048), hv("rwkv_mu", 3264)
            St = P.sb([128, 8, 64], F32, "wS")
            V("pool", lambda e: e.memset(St[:], 0.0), [], [St])
            X = [P.sb([128, 5, 512], F32, "wX") for _ in range(2)]
            vch = [P.sb([128, 8, 65], F32, "wvch") for _ in range(2)]
            gch = [P.sb([128, 8, 65], F32, "wgch") for _ in range(2)]
            s2 = [P.sb([128, 8], F32, "ws2") for _ in range(2)]
            vl = P.sb([128, 8, 64], F32, "wvl")
            gl = P.sb([128, 8, 64], F32, "wgl")
            bcs = [[P.sb([128, 512], F32, "wbc") for _ in range(5)] for _ in range(2)]
            pb = [P.ps([128, 512], F32, "wpb") for _ in range(5)]
            pq = [P.ps([128, 512], F32, "wpq") for _ in range(2)]
            Y = P.sb([128, 8, 64], F32, "wY")
            tA = P.sb([128, 8, 64], F32, "wtA")
            tB = [P.sb([128, 8, 64], F32, "wtB") for _ in range(2)]
            sa = P.sb([128, 8], F32, "wsa")
            mean = P.sb([128, 512], F32, "wmean")
            yc = P.sb([128, 8, 64], F32, "wyc")
            sq = P.sb([128, 512], F32, "wsq")
            rstd = P.sb([128, 512], F32, "wrstd")
            rh = [P.sb([128, 64], F32, "wrh") for _ in range(2)]
            yb = [P.sb([128, 8, 64], BF16, "wyb") for _ in range(2)]
            rv, rg = FM_ROWS["rw_v"], FM_ROWS["rw_gate"]

            def b3(t2):
                return t2[:, :].unsqueeze(2).to_broadcast([128, 8, 64])

            def loadc(c):
                t0 = c * 64
                P.dma("sp", [(X[c % 2][hf * 64:(hf + 1) * 64, :, :], rwtm.t[t0:t0 + 64, :, hf * 512:(hf + 1) * 512]) for hf in range(2)], reads=[rwtm], writes=[X[c % 2]])
                P.dma("sp", [(vch[c % 2][hf * 64:(hf + 1) * 64, :, :], fm.t[rv + hf * 512:rv + (hf + 1) * 512, PAD + t0 - 1:PAD + t0 + 64].rearrange("(h v) t -> v h t", v=64)) for hf in range(2)],
                      reads=[fm], writes=[vch[c % 2]])
                P.dma("sp", [(gch[c % 2][hf * 64:(hf + 1) * 64, :, :], fm.t[rg + hf * 512:rg + (hf + 1) * 512, PAD + t0 - 1:PAD + t0 + 64].rearrange("(h v) t -> v h t", v=64)) for hf in range(2)],
                      reads=[fm], writes=[gch[c % 2]])
                P.dma("sp", [(s2[c % 2][hf * 64:(hf + 1) * 64, :], rwbon[t0:t0 + 64, hf * 8:(hf + 1) * 8]) for hf in range(2)], reads=[rwbon], writes=[s2[c % 2]])
            loadc(0)
            step = 0
            for c in range(NCH):
                if c + 1 < NCH:
                    loadc(c + 1)
                t0 = c * 64
                Xc, vc, gc, s2c = X[c % 2], vch[c % 2], gch[c % 2], s2[c % 2]
                for (src, dst, mu_) in ((vc, vl, muv), (gc, gl, mug)):
                    V("pool", lambda e: e.tensor_tensor(dst[:], src[:, :, 0:64], src[:, :, 1:65], ALU.subtract), [src], [dst])
                    V("pool", lambda e: e.tensor_tensor(dst[:], dst[:], b3(mu_), ALU.mult), [dst, mu_], [dst])
                    V("pool", lambda e: e.tensor_tensor(dst[:], dst[:], src[:, :, 1:65], ALU.add), [dst, src], [dst])
                for tl in range(64):
                    bset = bcs[step % 2]
                    tBs = tB[step % 2]
                    step += 1
                    for p in range(5):
                        V("pe", lambda e: e.matmul(pb[p][:], sel[:, tl, :], Xc[:, p, :], start=True, stop=True), [sel, Xc], [pb[p]])
                        V("act", lambda e: e.copy(bset[p][:], pb[p][:]), [pb[p]], [bset[p]])
                    wB, kkB, kkanB, k2B, rB = [b[:].rearrange("p (h k) -> p h k", k=64) for b in bset]
                    V("pool", lambda e: e.tensor_tensor(tBs[:], k2B, vl[:, :, tl:tl + 1].to_broadcast([128, 8, 64]), ALU.mult), [bset[3], vl], [tBs])
                    V("dve", lambda e: e.tensor_tensor(tA[:], St[:], kkB, ALU.mult), [St, bset[1]], [tA])
                    V("dve", lambda e: e.tensor_reduce(sa[:], tA[:], AX.X, ALU.add), [tA], [sa])
                    V("dve", lambda e: e.tensor_tensor(St[:], St[:], wB, ALU.mult), [St, bset[0]], [St])
                    V("dve", lambda e: e.tensor_tensor(tA[:], kkanB, b3(sa), ALU.mult), [bset[2], sa], [tA])
                    V("dve", lambda e: e.tensor_tensor(St[:], St[:], tA[:], ALU.add), [St, tA], [St])
                    V("dve", lambda e: e.tensor_tensor(St[:], St[:], tBs[:], ALU.add), [St, tBs], [St])
                    V("dve", lambda e: e.tensor_tensor(tA[:], St[:], rB, ALU.mult), [St, bset[4]], [tA])
                    V("dve", lambda e: e.tensor_reduce(Y[:, :, tl], tA[:], AX.X, ALU.add), [tA], [Y])
                Yf = Y[:].rearrange("p h t -> p (h t)")
                V("pe", lambda e: e.matmul(pq[0][:], blkm[:], Yf, start=True, stop=True), [blkm, Y], [pq[0]])
                V("dve", lambda e: e.tensor_tensor(yc[:].rearrange("p h t -> p (h t)"), Yf, pq[0][:], ALU.subtract), [Y, pq[0]], [yc])
                V("act", lambda e: e.activation(sq[:], yc[:].rearrange("p h t -> p (h t)"), AF.Square), [yc], [sq])
                V("pe", lambda e: e.matmul(pq[1][:], blkm[:], sq[:], start=True, stop=True), [blkm, sq], [pq[1]])
                V("act", lambda e: e.activation(rstd[:], pq[1][:], AF.Sqrt, bias=64e-5, scale=1.0), [pq[1]], [rstd])
                V("dve", lambda e: e.reciprocal(rstd[:], rstd[:]), [rstd], [rstd])
                V("dve", lambda e: e.tensor_tensor(yc[:].rearrange("p h t -> p (h t)"), yc[:].rearrange("p h t -> p (h t)"), rstd[:], ALU.mult), [yc, rstd], [yc])
                V("dve", lambda e: e.tensor_tensor(yc[:], yc[:], b3(lng), ALU.mult), [yc, lng], [yc])
                V("dve", lambda e: e.tensor_tensor(yc[:], yc[:], b3(lnb), ALU.add), [yc, lnb], [yc])
                for h8 in range(8):
                    rhh = rh[h8 % 2]
                    V("pool", lambda e: e.tensor_scalar(rhh[:], i2[:], s2c[:, h8:h8 + 1], None, op0=ALU.mult), [i2, s2c], [rhh])
                    V("pe", lambda e: e.matmul(pq[0][:, h8 * 64:(h8 + 1) * 64], blk[:], rhh[:], start=True, stop=True), [blk, rhh], [pq[0]])
                V("dve", lambda e: e.tensor_tensor(tA[:].rearrange("p h t -> p (h t)"), pq[0][:], vl[:].rearrange("p h t -> p (h t)"), ALU.mult), [pq[0], vl], [tA])
                V("dve", lambda e: e.tensor_tensor(yc[:], yc[:], tA[:], ALU.add), [yc, tA], [yc])
                V("act", lambda e: e.activation(gl[:], gl[:], AF.Silu), [gl], [gl])
                ybc = yb[c % 2]
                V("dve", lambda e: e.tensor_tensor(ybc[:], yc[:], gl[:], ALU.mult), [yc, gl], [ybc])
                P.dma("sp", [(ysT.t[2048 + hf * 512:2048 + (hf + 1) * 512, t0:t0 + 64].rearrange("(h v) t -> v h t", v=64), ybc[hf * 64:(hf + 1) * 64, :, :]) for hf in range(2)],
                      reads=[ybc], writes=[ysT])


    def phase_rwp2(l):
        with P.scope():
            c0 = TM_COLS["rw_r"]
            NCc = 3264
            mub = P.sb([128, NCc], F32, "rmub")
            mu = I["rwkv_mu"]
            P.dma("sp", [(mub[:], mu[l:l + 1, 0:NCc].to_broadcast([128, NCc]))], reads=[mu], writes=[mub])

            def bc(name, n=1024):
                t = P.sb([128, n], F32, "rb" + name)
                src = I[name]
                if name == "rwkv_r_k":
                    ap = src.t[l:l + 1, :, :].rearrange("a h k -> a (h k)").to_broadcast([128, n])
                else:
                    ap = src[l:l + 1, :].to_broadcast([128, n])
                P.dma("sp", [(t[:], ap)], reads=[src], writes=[t])
                return t
            kkb, kab, w0b, a0b, rkb = bc("rwkv_k_k"), bc("rwkv_k_a"), bc("rwkv_w0"), bc("rwkv_a0"), bc("rwkv_r_k")
            omka = P.sb([128, 1024], F32, "romka")
            V("dve", lambda e: e.tensor_scalar(omka[:], kab[:], -1.0, 1.0, op0=ALU.mult, op1=ALU.add), [kab], [omka])
            ww2 = P.sb([96, 1024], F32, "rww2")
            wa2 = P.sb([96, 1024], F32, "rwa2")
            lmat = P.sb([128, 128], F32, "rlmat")
            umat = P.sb([128, 128], F32, "rumat")
            ind = P.sb([128, 4], F32, "rind")
            P.dma("sp", [(ww2[:], I["rwkv_w_w2"].t[l, :, :])], reads=[I["rwkv_w_w2"]], writes=[ww2])
            P.dma("sp", [(wa2[:], I["rwkv_w_a2"].t[l, :, :])], reads=[I["rwkv_w_a2"]], writes=[wa2])
            P.dma("sp", [(lmat[:], I["c_lmat32"][:, :])], reads=[I["c_lmat32"]], writes=[lmat])
            P.dma("sp", [(umat[:], I["c_umat32"][:, :])], reads=[I["c_umat32"]], writes=[umat])
            P.dma("sp", [(ind[:], I["c_ind32"][:, :])], reads=[I["c_ind32"]], writes=[ind])
            cur = [P.sb([128, NCc], F32, "rcur") for _ in range(2)]
            prv = P.sb([128, NCc], F32, "rprv")
            dd = P.sb([128, NCc], F32, "rdd")
            th = P.sb([128, 96], F32, "rth")
            thT = P.sb([96, 128], F32, "rthT")
            alT = P.sb([96, 128], F32, "ralT")
            pz = [P.ps([128, 2, 512], F32, "rpz") for _ in range(2)]
            ptr = [P.ps([64, 4, 128], F32, "rptr") for _ in range(2)]
            ptT2 = P.ps([96, 2, 128], F32, "rptT")
            pdec = P.ps([64, 16, 4], F32, "rpdec")
            zs = P.sb([128, 1024], F32, "rzs")
            aa = P.sb([128, 1024], F32, "raa")
            kk = P.sb([128, 1024], F32, "rkk")
            kkn = P.sb([128, 1024], F32, "rkkn")
            kkan = P.sb([128, 1024], F32, "rkkan")
            k2 = P.sb([128, 1024], F32, "rk2")
            ssq = P.sb([128, 16], F32, "rssq")
            tt = P.sb([128, 1024], F32, "rtt")
            E1 = P.sb([128, 1024], F32, "rE1")
            Q = P.sb([128, 4, 1024], F32, "rQ")
            o3 = [P.sb([128, 3, 1024], F32, "ro3") for _ in range(2)]
            bon = [P.sb([128, 16], F32, "rbon") for _ in range(2)]
            XTs = P.sb([64, 16, 4, 128], F32, "rXTs")
            decs = P.sb([64, 16, 4], F32, "rdecs")

            def loadt(i):
                P.dma("sp", [(cur[i % 2][:], tm[i * 128:(i + 1) * 128, c0:c0 + NCc])], reads=[tm], writes=[cur[i % 2]])
            loadt(0)
            for i in range(NT):
                if i + 1 < NT:
                    loadt(i + 1)
                cu, o3i, bn = cur[i % 2], o3[i % 2], bon[i % 2]
                if i == 0:
                    V("pool", lambda e: e.memset(prv[0:1, :], 0.0), [], [prv])
                    P.dma("sp", [(prv[1:128, :], tm[0:127, c0:c0 + NCc])], reads=[tm], writes=[prv])
                else:
                    P.dma("sp", [(prv[:], tm[i * 128 - 1:i * 128 + 127, c0:c0 + NCc])], reads=[tm], writes=[prv])
                V("dve", lambda e: e.tensor_tensor(dd[:], prv[:], cu[:], ALU.subtract), [prv, cu], [dd])
                V("pool", lambda e: e.tensor_tensor(dd[:], dd[:], mub[:], ALU.mult), [dd, mub], [dd])
                V("dve", lambda e: e.tensor_tensor(cu[:], cu[:], dd[:], ALU.add), [cu, dd], [cu])
                rr_, k_, v_ = cu[:, 0:1024], cu[:, 1024:2048], cu[:, 2048:3072]
                V("act", lambda e: e.activation(th[:], cu[:, 3072:3168], AF.Tanh), [cu], [th])
                V("pe", lambda e: e.transpose(ptT2[:, 0, :], th[:], ident_f[:]), [th, ident_f], [ptT2])
                V("pe", lambda e: e.transpose(ptT2[:, 1, :], cu[:, 3168:3264], ident_f[:]), [cu, ident_f], [ptT2])
                V("act", lambda e: e.copy(thT[:], ptT2[:, 0, :]), [ptT2], [thT])
                V("dve", lambda e: e.tensor_copy(alT[:], ptT2[:, 1, :]), [ptT2], [alT])
                for n2 in range(2):
                    V("pe", lambda e: e.matmul(pz[0][:, n2, :], thT[:], ww2[:, n2 * 512:(n2 + 1) * 512], start=True, stop=True), [thT, ww2], [pz[0]])
                    V("pe", lambda e: e.matmul(pz[1][:, n2, :], alT[:], wa2[:, n2 * 512:(n2 + 1) * 512], start=True, stop=True), [alT, wa2], [pz[1]])
                V("dve", lambda e: e.tensor_tensor(zs[:], pz[0][:].rearrange("p a b -> p (a b)"), w0b[:], ALU.add), [pz[0], w0b], [zs])
                V("act", lambda e: e.activation(zs[:], zs[:], AF.Sigmoid), [zs], [zs])
                V("dve", lambda e: e.tensor_scalar(zs[:], zs[:], -0.606531, None, op0=ALU.mult), [zs], [zs])
                V("dve", lambda e: e.tensor_tensor(aa[:], pz[1][:].rearrange("p a b -> p (a b)"), a0b[:], ALU.add), [pz[1], a0b], [aa])
                V("act", lambda e: e.activation(aa[:], aa[:], AF.Sigmoid), [aa], [aa])
                V("dve", lambda e: e.tensor_tensor(kk[:], k_, kkb[:], ALU.mult), [cu, kkb], [kk])
                V("act", lambda e: e.activation(tt[:], kk[:], AF.Square), [kk], [tt])
                V("dve", lambda e: e.tensor_reduce(ssq[:], tt[:].rearrange("p (h k) -> p h k", k=64), AX.X, ALU.add), [tt], [ssq])
                V("act", lambda e: e.activation(ssq[:], ssq[:], AF.Sqrt, bias=1e-12, scale=1.0), [ssq], [ssq])
                V("dve", lambda e: e.reciprocal(ssq[:], ssq[:]), [ssq], [ssq])
                V("dve", lambda e: e.tensor_tensor(kkn[:].rearrange("p (h k) -> p h k", k=64), kk[:].rearrange("p (h k) -> p h k", k=64),
                                                   ssq[:, :].unsqueeze(2).to_broadcast([128, 16, 64]), ALU.mult), [kk, ssq], [kkn])
                V("dve", lambda e: e.scalar_tensor_tensor(kkan[:], kkn[:], -1.0, aa[:], op0=ALU.mult, op1=ALU.mult), [kkn, aa], [kkan])
                V("pool", lambda e: e.tensor_tensor(tt[:], aa[:], kab[:], ALU.mult), [aa, kab], [tt])
                V("pool", lambda e: e.tensor_tensor(tt[:], tt[:], omka[:], ALU.add), [tt, omka], [tt])
                V("dve", lambda e: e.tensor_tensor(k2[:], k_, tt[:], ALU.mult), [cu, tt], [k2])
                V("pool", lambda e: e.tensor_tensor(tt[:], rr_, rkb[:], ALU.mult), [cu, rkb], [tt])
                V("dve", lambda e: e.tensor_tensor(tt[:], tt[:], k2[:], ALU.mult), [tt, k2], [tt])
                V("dve", lambda e: e.tensor_reduce(bn[:], tt[:].rearrange("p (h k) -> p h k", k=64), AX.X, ALU.add), [tt], [bn])
                for n2 in range(2):
                    V("pe", lambda e: e.matmul(pz[0][:, n2, :], lmat[:], zs[:, n2 * 512:(n2 + 1) * 512], start=True, stop=True), [lmat, zs], [pz[0]])
                    V("pe", lambda e: e.matmul(pz[1][:, n2, :], umat[:], zs[:, n2 * 512:(n2 + 1) * 512], start=True, stop=True), [umat, zs], [pz[1]])
                cwf = pz[0][:].rearrange("p a b -> p (a b)")
                gf = pz[1][:].rearrange("p a b -> p (a b)")
                for n2 in range(2):
                    V("act", lambda e: e.activation(E1[:, n2 * 512:(n2 + 1) * 512], pz[0][:, n2, :], AF.Exp), [pz[0]], [E1])
                V("dve", lambda e: e.tensor_tensor(Q[:, 3, :], rr_, E1[:], ALU.mult), [cu, E1], [Q])
                for n2 in range(2):
                    V("act", lambda e: e.activation(E1[:, n2 * 512:(n2 + 1) * 512], pz[0][:, n2, :], AF.Exp, scale=-1.0), [pz[0]], [E1])
                V("dve", lambda e: e.tensor_tensor(Q[:, 0, :], kkan[:], E1[:], ALU.mult), [kkan, E1], [Q])
                V("pool", lambda e: e.tensor_tensor(Q[:, 1, :], k2[:], E1[:], ALU.mult), [k2, E1], [Q])
                V("dve", lambda e: e.tensor_tensor(tt[:], cwf, zs[:], ALU.subtract), [pz[0], zs], [tt])
                V("act", lambda e: e.activation(E1[:], tt[:], AF.Exp), [tt], [E1])
                V("dve", lambda e: e.tensor_tensor(Q[:, 2, :], kkn[:], E1[:], ALU.mult), [kkn, E1], [Q])
                for n2 in range(2):
                    V("act", lambda e: e.activation(E1[:, n2 * 512:(n2 + 1) * 512], pz[1][:, n2, :], AF.Exp), [pz[1]], [E1])
                V("dve", lambda e: e.tensor_tensor(o3i[:, 0, :], kkan[:], E1[:], ALU.mult), [kkan, E1], [o3i])
                V("pool", lambda e: e.tensor_tensor(o3i[:, 1, :], k2[:], E1[:], ALU.mult), [k2, E1], [o3i])
                V("pool", lambda e: e.tensor_copy(o3i[:, 2, :], v_), [cu], [o3i])
                for h in range(16):
                    V("pe", lambda e: e.matmul(pdec[:, h, :], zs[:, h * 64:(h + 1) * 64], ind[:], start=True, stop=True), [zs, ind], [pdec])
                V("act", lambda e: e.activation(decs[:], pdec[:], AF.Exp), [pdec], [decs])
                P.dma("sp", [(rwdec[:, :, i * 4:(i + 1) * 4], decs[:])], reads=[decs], writes=[rwdec])
                for h in range(16):
                    pt = ptr[h % 2]
                    for q in range(4):
                        V("pe", lambda e: e.transpose(pt[:, q, :], Q[:, q, h * 64:(h + 1) * 64], ident_f[:]), [Q, ident_f], [pt])
                    evac(h, XTs[:, h, :, :], pt[:], [pt], [XTs])
                P.dma("sp", [(rwxt.t[:, :, i, c4 * 128:(c4 + 1) * 128].rearrange("k h (q t) -> k h q t", t=32), XTs[:, :, :, c4 * 32:(c4 + 1) * 32]) for c4 in range(4)],
                      reads=[XTs], writes=[rwxt])
                P.dma("sp", [(rwtm2[i * 128:(i + 1) * 128, :, :], o3i[:])], reads=[o3i], writes=[rwtm2])
                P.dma("sp", [(rwbon[i * 128:(i + 1) * 128, :], bn[:])], reads=[bn], writes=[rwbon])

    def phase_rws2(l):
        with P.scope():
            mtp = P.sb([64, 64], F32, "wmtp")
            mn = P.sb([32, 32], F32, "wmn")
            ones64 = P.sb([64, 64], F32, "wones")
            onesm = P.sb([64, 64], F32, "wonesm")
            P.dma("sp", [(mtp[:], I["c_m32tp"][:, :])], reads=[I["c_m32tp"]], writes=[mtp])
            P.dma("sp", [(mn[:], I["c_m32n"][:, :])], reads=[I["c_m32n"]], writes=[mn])
            V("pool", lambda e: e.memset(ones64[:], 1.0), [], [ones64])
            V("pool", lambda e: e.memset(onesm[:], 1.0 / 64.0), [], [onesm])

            def hv(name, off=0):
                t = P.sb([64, 16], F32, "w" + name)
                src = I[name]
                P.dma("sp", [(t[:], src.t[l, off:off + 1024].rearrange("(h v) -> v h", v=64))], reads=[src], writes=[t], allow_slow_non_contiguous=True)
                return t
            lng, lnb = hv("rwkv_lnx_g"), hv("rwkv_lnx_b")
            muv, mug = hv("rwkv_mu", 2048), hv("rwkv_mu", 3264)
            decall = P.sb([64, 16, NC32], F32, "wdec")
            P.dma("sp", [(decall[:], rwdec[:, :, :])], reads=[rwdec], writes=[decall])
            H = P.sb([64, 16, 64], F32, "wH")
            V("pool", lambda e: e.memset(H[:], 0.0), [], [H])
            XT = [P.sb([64, 16, 4, 4, 32], F32, "wXT") for _ in range(2)]
            BK = [P.sb([64, 16, 64], F32, "wBK") for _ in range(2)]
            UV = [P.sb([64, 16, 64], F32, "wUV") for _ in range(2)]
            MT = P.sb([64, 16, 64], F32, "wMT")
            Pm = [P.sb([32, 16, 32], F32, "wPm") for _ in range(2)]
            PTm = [P.sb([32, 16, 32], F32, "wPTm") for _ in range(2)]
            Zs = P.sb([32, 16, 64], F32, "wZs")
            pTP = P.ps([64, 16, 64], F32, "wpTP")
            pZ = P.ps([64, 16, 64], F32, "wpZ")
            pN = P.ps([64, 16, 32], F32, "wpN")
            pP2 = P.ps([32, 16, 32], F32, "wpP2")
            pP2T = P.ps([32, 16, 32], F32, "wpP2T")
            Yt = P.sb([64, 16, 64], F32, "wYt")
            vch = [P.sb([64, 16, 65], F32, "wvch") for _ in range(2)]
            gch = [P.sb([64, 16, 65], F32, "wgch") for _ in range(2)]
            s2 = [P.sb([64, 16], F32, "ws2") for _ in range(2)]
            vl = P.sb([64, 16, 64], F32, "wvl")
            gl = P.sb([64, 16, 64], F32, "wgl")
            yc = P.sb([64, 16, 64], F32, "wyc")
            sq = P.sb([64, 16, 64], F32, "wsq")
            rstd = P.sb([64, 16, 64], F32, "wrstd")
            tA = P.sb([64, 16, 64], F32, "wtA")
            rh = [P.sb([64, 64], F32, "wrh") for _ in range(2)]
            yb = [P.sb([64, 16, 64], BF16, "wyb") for _ in range(2)]
            rv, rg = FM_ROWS["rw_v"], FM_ROWS["rw_gate"]

            def b3(t2):
                return t2[:, :].unsqueeze(2).to_broadcast([64, 16, 64])

            def fl(b, np_=64):
                return b[0:np_, :, :].rearrange("p h t -> p (h t)")

            def loadtile(i):
                P.dma("sp", [(XT[i % 2][:].rearrange("k h c q t -> k h (c q t)"), rwxt.t[:, :, i, :])], reads=[rwxt], writes=[XT[i % 2]])

            def loadchunk(c):
                t0 = c * 32
                P.dma("sp", [(BK[c % 2][0:32, :, :].rearrange("p h k -> p (h k)"), rwtm2.t[t0:t0 + 32, 0, :]),
                             (BK[c % 2][32:64, :, :].rearrange("p h k -> p (h k)"), rwtm2.t[t0:t0 + 32, 1, :])], reads=[rwtm2], writes=[BK[c % 2]])
                P.dma("sp", [(UV[c % 2][32:64, :, :].rearrange("p h k -> p (h k)"), rwtm2.t[t0:t0 + 32, 2, :])], reads=[rwtm2], writes=[UV[c % 2]])

            def loadepi(g):
                t0 = g * 64
                P.dma("sp", [(vch[g % 2][:], fm.t[rv:rv + 1024, PAD + t0 - 1:PAD + t0 + 64].rearrange("(h v) t -> v h t", v=64))], reads=[fm], writes=[vch[g % 2]])
                P.dma("sp", [(gch[g % 2][:], fm.t[rg:rg + 1024, PAD + t0 - 1:PAD + t0 + 64].rearrange("(h v) t -> v h t", v=64))], reads=[fm], writes=[gch[g % 2]])
                P.dma("sp", [(s2[g % 2][:], rwbon[t0:t0 + 64, :])], reads=[rwbon], writes=[s2[g % 2]])
            loadtile(0)
            loadchunk(0)
            loadepi(0)
            ke = 0
            for i in range(NT):
                if i + 1 < NT:
                    loadtile(i + 1)
                X = XT[i % 2]
                for cc in range(4):
                    c = i * 4 + cc
                    if c + 1 < NC32:
                        loadchunk(c + 1)
                    BKc, UVc = BK[c % 2], UV[c % 2]
                    for h in range(16):
                        V("pe", lambda e: e.matmul(pTP[:, h, :], X[:, h, cc, 0:2, :].rearrange("k q t -> k (q t)"), X[:, h, cc, 2:4, :].rearrange("k q t -> k (q t)"),
                                                   start=True, stop=True), [X], [pTP])
                    for h in range(16):
                        V("pe", lambda e: e.matmul(pN[0:32, h, :], X[:, h, cc, 2, :], X[:, h, cc, 0, :], start=True, stop=True), [X], [pN])
                    V("dve", lambda e: e.tensor_tensor(MT[:], pTP[:], mtp[:, :].unsqueeze(1).to_broadcast([64, 16, 64]), ALU.mult), [pTP, mtp], [MT])
                    V("dve", lambda e: e.tensor_tensor(Pm[0][:], pN[0:32, :, :], mn[:, :].unsqueeze(1).to_broadcast([32, 16, 32]), ALU.mult), [pN, mn], [Pm[0]])
                    for h in range(16):
                        V("pe", lambda e: e.matmul(pZ[0:32, h, :], X[:, h, cc, 2, :], H[:, h, :], start=True, stop=False), [X, H], [pZ])
                        V("pe", lambda e: e.matmul(pZ[0:32, h, :], MT[32:64, h, 0:32], UVc[32:64, h, :], start=False, stop=True), [MT, UVc], [pZ])
                    for h2 in range(2):
                        V("act", lambda e: e.copy(Zs[:, h2 * 8:(h2 + 1) * 8, :], pZ[0:32, h2 * 8:(h2 + 1) * 8, :]), [pZ], [Zs])
                    for lv in range(5):
                        if lv == 0:
                            PTb, PTv = MT, (lambda h: MT[0:32, h, 0:32])
                        else:
                            PTb, PTv = PTm[lv % 2], (lambda h, b=PTm[lv % 2]: b[:, h, :])
                        Pb = Pm[lv % 2]
                        for h in range(16):
                            V("pe", lambda e: e.matmul(pZ[0:32, h, :], PTv(h), Zs[:, h, :], start=True, stop=True), [PTb, Zs], [pZ])
                        if lv < 4:
                            for h in range(16):
                                V("pe", lambda e: e.matmul(pP2[:, h, :], PTv(h), Pb[:, h, :], start=True, stop=True), [PTb, Pb], [pP2])
                                V("pe", lambda e: e.matmul(pP2T[:, h, :], Pb[:, h, :], PTv(h), start=True, stop=True), [PTb, Pb], [pP2T])
                            V("dve", lambda e: e.tensor_tensor(Zs[:], Zs[:], pZ[0:32, :, :], ALU.add), [Zs, pZ], [Zs])
                            V("act", lambda e: e.copy(Pm[(lv + 1) % 2][:], pP2[:]), [pP2], [Pm[(lv + 1) % 2]])
                            V("dve", lambda e: e.tensor_copy(PTm[(lv + 1) % 2][:], pP2T[:]), [pP2T], [PTm[(lv + 1) % 2]])
                        else:
                            V("dve", lambda e: e.tensor_tensor(UVc[0:32, :, :], Zs[:], pZ[0:32, :, :], ALU.add), [Zs, pZ], [UVc])
                    for h in range(16):
                        V("pe", lambda e: e.matmul(pN[:, h, :], H[:, h, :], X[:, h, cc, 3, :], start=True, stop=False), [H, X], [pN])
                        V("pe", lambda e: e.matmul(pN[:, h, :], UVc[:, h, :], MT[:, h, 32:64], start=False, stop=True), [UVc, MT], [pN])
                    V("act", lambda e: e.copy(Yt[:, :, (cc % 2) * 32:(cc % 2) * 32 + 32], pN[:]), [pN], [Yt])
                    for h in range(16):
                        V("pe", lambda e: e.matmul(pTP[:, h, :], BKc[:, h, :], UVc[:, h, :], start=True, stop=True), [BKc, UVc], [pTP])
                    V("dve", lambda e: e.tensor_tensor(H[:], H[:], decall[:, :, c:c + 1].to_broadcast([64, 16, 64]), ALU.mult), [H, decall], [H])
                    V("dve", lambda e: e.tensor_tensor(H[:], H[:], pTP[:], ALU.add), [H, pTP], [H])
                    if cc % 2 == 1:
                        g = c // 2
                        t0 = g * 64
                        if g + 1 < NCH:
                            loadepi(g + 1)
                        vc, gc, s2c = vch[g % 2], gch[g % 2], s2[g % 2]
                        for (src, dst, mu_) in ((vc, vl, muv), (gc, gl, mug)):
                            V("pool", lambda e: e.tensor_tensor(dst[:], src[:, :, 0:64], src[:, :, 1:65], ALU.subtract), [src], [dst])
                            V("pool", lambda e: e.tensor_tensor(dst[:], dst[:], b3(mu_), ALU.mult), [dst, mu_], [dst])
                            V("pool", lambda e: e.tensor_tensor(dst[:], dst[:], src[:, :, 1:65], ALU.add), [dst, src], [dst])
                        for n2 in range(2):
                            V("pe", lambda e: e.matmul(fl(pTP)[:, n2 * 512:(n2 + 1) * 512], onesm[:], fl(Yt)[:, n2 * 512:(n2 + 1) * 512], start=True, stop=True), [onesm, Yt], [pTP])
                        V("dve", lambda e: e.tensor_tensor(yc[:], Yt[:], pTP[:], ALU.subtract), [Yt, pTP], [yc])
                        V("act", lambda e: e.activation(sq[:], yc[:], AF.Square), [yc], [sq])
                        for n2 in range(2):
                            V("pe", lambda e: e.matmul(fl(pZ)[:, n2 * 512:(n2 + 1) * 512], onesm[:], fl(sq)[:, n2 * 512:(n2 + 1) * 512], start=True, stop=True), [onesm, sq], [pZ])
                        for h2 in range(2):
                            V("act", lambda e: e.activation(rstd[:, h2 * 8:(h2 + 1) * 8, :], pZ[:, h2 * 8:(h2 + 1) * 8, :], AF.Sqrt, bias=64e-5, scale=1.0), [pZ], [rstd])
                        V("dve", lambda e: e.reciprocal(rstd[:], rstd[:]), [rstd], [rstd])
                        V("dve", lambda e: e.tensor_tensor(yc[:], yc[:], rstd[:], ALU.mult), [yc, rstd], [yc])
                        V("dve", lambda e: e.tensor_tensor(yc[:], yc[:], b3(lng), ALU.mult), [yc, lng], [yc])
                        V("dve", lambda e: e.tensor_tensor(yc[:], yc[:], b3(lnb), ALU.add), [yc, lnb], [yc])
                        for h in range(16):
                            rhh = rh[h % 2]
                            V("pool", lambda e: e.tensor_scalar(rhh[:], ident_f[0:64, 0:64], s2c[:, h:h + 1], None, op0=ALU.mult), [ident_f, s2c], [rhh])
                            V("pe", lambda e: e.matmul(pTP[:, h, :], ones64[:], rhh[:], start=True, stop=True), [ones64, rhh], [pTP])
                        V("dve", lambda e: e.tensor_tensor(tA[:], pTP[:], vl[:], ALU.mult), [pTP, vl], [tA])
                        V("dve", lambda e: e.tensor_tensor(yc[:], yc[:], tA[:], ALU.add), [yc, tA], [yc])
                        V("act", lambda e: e.activation(gl[:], gl[:], AF.Silu), [gl], [gl])
                        ybc = yb[g % 2]
                        V("dve", lambda e: e.tensor_tensor(ybc[:], yc[:], gl[:], ALU.mult), [yc, gl], [ybc])
                        P.dma("sp", [(ysT.t[2048:3072, t0:t0 + 64].rearrange("(h v) t -> v h t", v=64), ybc[:])], reads=[ybc], writes=[ysT])

    PH = {"tables": phase_tables, "proj": phase_proj, "rope": phase_rope, "gla": phase_gla, "sgu": phase_sgu,
          "m1": phase_m1, "m2": phase_m2, "dsa1": phase_dsa1, "dsa2": phase_dsa2, "rwp": phase_rwp, "rws": phase_rws, "rwp2": phase_rwp2, "rws2": phase_rws2}
    plan = dbg.get("plan")
    if plan is None:
        plan = [("tables",)]
        for l in range(L):
            plan += [("proj", l, l), ("rope", l), ("gla", l), ("dsa1", l), ("dsa2", l), ("rwp2", l), ("rws2", l), ("sgu", l), ("m1", l), ("m2", l, l, l + 1)]
    for st in plan:
        nm = st[0]
        if nm == "tables":
            phase_tables()
        elif nm == "proj":
            phase_proj(st[1], xcur[st[2]])
        elif nm == "m2":
            phase_m2(st[1], xcur[st[2]], xcur[st[3]])
        else:
            PH[nm](st[1])
    P.barrier()
    es.close()
    global LAST_P
    LAST_P = P
    return nc


def make_inputs(inputs, b):
    m = {"x": np.ascontiguousarray(inputs["x"][b]), "pos": np.ascontiguousarray(inputs["positions"][b:b + 1]).astype(np.int32)}
    for k, v in inputs.items():
        if k in ("x", "positions"):
            continue
        m[k] = np.ascontiguousarray(v)
    m.update(host_consts())
    return m


def kernel(**inputs):
    nc = build_nc()
    in_maps = [make_inputs(inputs, c % 4) for c in range(8)]
    res = run_bass_kernel_spmd(nc, in_maps, core_ids=list(range(8)))
    return np.stack([res.results[c]["y"] for c in range(4)], axis=0).astype(np.float32)
```

```python
import numpy as np
from contextlib import ExitStack, contextmanager
import concourse.bass as bass
import concourse.mybir as mybir
from concourse.bass_utils import run_bass_kernel_spmd

F32 = mybir.dt.float32
BF16 = mybir.dt.bfloat16
I32 = mybir.dt.int32
ALU = mybir.AluOpType
AF = mybir.ActivationFunctionType
AX = mybir.AxisListType

D = 2048
S = 4096
L = 2
NIN = 23320
PAD = 64
TP = S + PAD
NEG = -1.0e30


class Buf:
    __slots__ = ("t", "w", "r", "sem", "semkey", "semv", "name", "acc", "wl", "psum")

    def __init__(self, t, name, acc=False):
        self.t = t
        self.name = name
        self.acc = acc
        self.psum = False
        self.wl = []
        self.w = None
        self.r = []
        self.sem = None
        self.semkey = None
        self.semv = 0

    def __getitem__(self, k):
        return self.t[k]


class Prog:
    def __init__(self, nc, es):
        self.nc = nc
        self.es = es
        self.eng = {"pe": nc.tensor, "act": nc.scalar, "dve": nc.vector, "pool": nc.gpsimd, "sp": nc.sync}
        self.esem = {}
        self.ecnt = {}
        self.ekey = {}
        self.eepoch = {}
        for e in ("pe", "act", "dve", "pool"):
            self.esem[e] = es.enter_context(nc.semaphore("e_" + e))
            self.ecnt[e] = 0
            self.eepoch[e] = 0
            self.ekey[e] = e + "#0"
        self.nwait = 0
        self.seen = {e: {} for e in self.eng}
        self.nbuf = 0
        self.scopes = [es]
        self.scope_bufs = [[]]
        self.sempool = []
        self.allsems = {}
        self.nsem = 0
        self.ninst = 0

    @contextmanager
    def scope(self):
        es = ExitStack()
        self.scopes.append(es)
        self.scope_bufs.append([])
        yield
        self.barrier()
        for b in self.scope_bufs.pop():
            if b.sem is not None:
                self.sempool.append((b.sem, b.semkey, b.semv))
        self.scopes.pop()
        es.close()

    def sb(self, shape, dt, name=None):
        self.nbuf += 1
        name = (name or "sb") + "_%d" % self.nbuf
        t = self.scopes[-1].enter_context(self.nc.sbuf_tensor(name, list(shape), dt))
        b = Buf(t, name)
        self.scope_bufs[-1].append(b)
        return b

    def ps(self, shape, dt, name=None):
        self.nbuf += 1
        name = (name or "ps") + "_%d" % self.nbuf
        t = self.scopes[-1].enter_context(self.nc.psum_tensor(name, list(shape), dt))
        b = Buf(t, name)
        b.psum = True
        self.scope_bufs[-1].append(b)
        return b

    def dram(self, name, shape, dt, kind="Internal"):
        t = self.nc.dram_tensor(name, list(shape), dt, kind=kind).ap()
        return Buf(t, name, acc=True)

    def _wait(self, e, ev, raw=True):
        if ev is None:
            return
        key, sem, val = ev
        if e == "pe" and key.startswith("pe#"):
            return
        if self.seen[e].get(key, 0) >= val:
            return
        self.eng[e].wait_ge(sem, val)
        self.nwait += 1
        self.seen[e][key] = val

    def _deps(self, e, reads, writes):
        for b in reads:
            self._wait(e, b.w, True)
            for ev in b.wl:
                self._wait(e, ev, True)
            if b.psum:
                for ev in b.r:
                    if not ev[0].startswith(e + "#"):
                        self._wait(e, ev, False)
        for b in writes:
            if not b.acc:
                self._wait(e, b.w, False)
            for ev in b.r:
                self._wait(e, ev, False)

    @staticmethod
    def _compact(evs):
        best = {}
        for ev in evs:
            k = ev[0]
            if k not in best or best[k][2] < ev[2]:
                best[k] = ev
        return list(best.values())

    def _record(self, ev, reads, writes):
        for b in writes:
            if b.acc:
                b.wl.append(ev)
                if len(b.wl) > 16:
                    b.wl = self._compact(b.wl)
            else:
                b.w = ev
            b.r = []
        for b in reads:
            if b not in writes:
                b.r.append(ev)
                if len(b.r) > 16:
                    b.r = self._compact(b.r)

    def op(self, e, fn, reads=(), writes=()):
        self._deps(e, reads, writes)
        ins = fn(self.eng[e])
        self.ecnt[e] += 1
        self.ninst += 1
        ins.then_inc(self.esem[e], 1)
        self._record((self.ekey[e], self.esem[e], self.ecnt[e]), reads, writes)
        return ins

    def _getsem(self, b):
        if b.sem is None:
            if self.sempool:
                b.sem, b.semkey, b.semv = self.sempool.pop()
            else:
                self.nsem += 1
                b.semkey = "d%d" % self.nsem
                b.sem = self.es.enter_context(self.nc.semaphore(b.semkey))
                b.semv = 0
            self.allsems[b.semkey] = b

    def dma(self, q, pairs, reads=(), writes=(), sembuf=None, **kw):
        self._deps(q, reads, writes)
        sb = sembuf
        if sb is None:
            cands = [b for b in list(writes) + list(reads) if not b.acc]
            sb = cands[0] if cands else (writes[0] if writes else reads[0])
        self._getsem(sb)
        self._wait(q, (sb.semkey, sb.sem, sb.semv))
        for (o, i) in pairs:
            self.eng[q].dma_start(out=o, in_=i, **kw).then_inc(sb.sem, 16)
            sb.semv += 16
            self.ninst += 1
        self._record((sb.semkey, sb.sem, sb.semv), reads, writes)

    def barrier(self):
        evs = [(self.ekey[c], self.esem[c], self.ecnt[c], c) for c in self.esem]
        for k, b in self.allsems.items():
            evs.append((k, b.sem, b.semv, None))
        for (sem, key, v) in self.sempool:
            evs.append((key, sem, v, None))
        for e in self.eng:
            for ev in evs:
                if ev[3] != e and ev[2] > 0:
                    if self.seen[e].get(ev[0], 0) < ev[2]:
                        self.eng[e].wait_ge(ev[1], ev[2])
                        self.nwait += 1
                        self.seen[e][ev[0]] = ev[2]
        for c in list(self.esem):
            if self.ecnt[c] > 12000:
                self.eepoch[c] += 1
                self.ekey[c] = "%s#%d" % (c, self.eepoch[c])
                self.esem[c] = self.es.enter_context(self.nc.semaphore("e_%s_%d" % (c, self.eepoch[c])))
                self.ecnt[c] = 0


SEGS = [
    (0, 512, "F", "gla_q"), (512, 512, "T", "gla_k"), (1024, 1024, "T", "gla_v"),
    (2048, 16, "F", "gla_g"), (2064, 1024, "F", "gla_gate"),
    (3088, 1024, "F", "dsa_q"), (4112, 1024, "F", "dsa_k"), (5136, 1024, "T", "dsa_v"),
    (6160, 512, "F", "dsa_iq"), (6672, 64, "F", "dsa_ik"), (6736, 8, "T", "dsa_iw"),
    (6744, 1024, "F", "dsa_gate"),
    (7768, 1024, "T", "rw_r"), (8792, 1024, "T", "rw_k"), (9816, 1024, "T", "rw_vT"), (9816, 1024, "F", "rw_v"),
    (10840, 96, "T", "rw_wlr"), (10936, 96, "T", "rw_alr"), (11032, 1024, "F", "rw_gate"),
    (12056, 1024, "F", "sgu_u"), (13080, 1024, "T", "sgu_zv"), (14104, 1024, "F", "sgu_gate"),
    (15128, 8192, "H", "pm"),
]
FM_ROWS = {}
TM_COLS = {}
_r = 0
_c = 0
for (_c0, _n, _m, _nm) in SEGS:
    if _m == "F":
        FM_ROWS[_nm] = _r
        _r += _n
    elif _m == "T":
        TM_COLS[_nm] = _c
        _c += _n
NFM = _r
NTM = _c


def host_consts():
    c = {}
    c["c_ident"] = np.eye(128, dtype=np.float32)
    s = np.arange(64)
    c["c_ustrict"] = (s[:, None] > s[None, :]).astype(np.float32)
    i = np.arange(128)
    c["c_sgumask"] = ((i[None, :] // 64) <= (i[:, None] // 64)).astype(np.float32)
    inv_a = (500000.0 ** (-(np.arange(0, 32, 2, dtype=np.float32) / np.float32(32)))).astype(np.float32)
    inv_i = (500000.0 ** (-(np.arange(0, 16, 2, dtype=np.float32) / np.float32(16)))).astype(np.float32)
    inv = np.zeros((128, 2), np.float32)
    inv[:, 0] = inv_a[i % 16]
    inv[:, 1] = inv_i[i % 8]
    c["c_inv"] = inv
    sel = np.zeros((128, 64, 128), np.float32)
    for tl in range(64):
        sel[tl, tl, 0:64] = 1.0
        sel[64 + tl, tl, 64:128] = 1.0
    c["c_sel"] = sel
    c["c_blk"] = ((i[:, None] // 64) == (i[None, :] // 64)).astype(np.float32)
    c["c_i2"] = ((i[:, None] % 64) == s[None, :]).astype(np.float32)
    j = np.arange(64)
    sidx = j % 32
    m = np.zeros((64, 64), np.float32)
    m[:, 0:32] = (sidx[:, None] < np.arange(32)[None, :])
    m[:, 32:64] = (sidx[:, None] <= np.arange(32)[None, :])
    c["c_m32tp"] = m
    t32 = np.arange(32)
    c["c_m32n"] = (t32[None, :] < t32[:, None]).astype(np.float32)
    blk32 = (i[:, None] // 32) == (i[None, :] // 32)
    c["c_lmat32"] = (blk32 & (i[:, None] <= i[None, :])).astype(np.float32)
    c["c_umat32"] = (blk32 & (i[:, None] > i[None, :])).astype(np.float32)
    c["c_ind32"] = ((i[:, None] // 32) == np.arange(4)[None, :]).astype(np.float32)
    return c


def build_nc(S=4096, dbg=None):
    dbg = dbg or {}
    TP = S + PAD
    NT = S // 128
    NB = S // 512
    NCH = S // 64
    KTOP = min(256, S // 4)
    nc = bass.Bass("TRN2", target_bir_lowering=False)
    es = ExitStack()
    P = Prog(nc, es)
    I = {}

    def inp(name, shape, dt=F32):
        I[name] = P.dram(name, shape, dt, kind="ExternalInput")
        return I[name]

    x_in = inp("x", [S, D])
    pos_in = inp("pos", [1, S], I32)
    inp("norm_pre", [L, D]); inp("norm_post", [L, D]); inp("w_in", [L, D, NIN])
    inp("gla_w_g2", [L, 16, 512]); inp("gla_b_g", [L, 512]); inp("gla_head_g", [L, 256])
    inp("rwkv_mu", [L, 4288]); inp("rwkv_w0", [L, 1024]); inp("rwkv_w_w2", [L, 96, 1024])
    inp("rwkv_a0", [L, 1024]); inp("rwkv_w_a2", [L, 96, 1024]); inp("rwkv_k_k", [L, 1024])
    inp("rwkv_k_a", [L, 1024]); inp("rwkv_r_k", [L, 16, 64]); inp("rwkv_lnx_g", [L, 1024])
    inp("rwkv_lnx_b", [L, 1024]); inp("sgu_ln_g", [L, 1024]); inp("sgu_ln_b", [L, 1024])
    inp("sgu_w_s", [L, 8, 128, 128]); inp("sgu_b_s", [L, 8, 128]); inp("w_proj", [L, 4, 1024, D])
    inp("w_out", [L, D, D])
    inp("c_ident", [128, 128]); inp("c_ustrict", [64, 64]); inp("c_sgumask", [128, 128])
    inp("c_inv", [128, 2]); inp("c_sel", [128, 64, 128]); inp("c_blk", [128, 128]); inp("c_i2", [128, 64])
    inp("c_m32tp", [64, 64]); inp("c_m32n", [32, 32]); inp("c_lmat32", [128, 128]); inp("c_umat32", [128, 128]); inp("c_ind32", [128, 4])
    y_out = P.dram("y", [S, D], F32, kind="ExternalOutput")

    ext_in = dbg.get("ext_in", ())
    ext_out = dbg.get("ext_out", ())

    def scr(name, shape, dt):
        kind = "ExternalInput" if name in ext_in else ("ExternalOutput" if name in ext_out else "Internal")
        return P.dram(name, shape, dt, kind=kind)
    fm = scr("fm", [NFM, TP], F32)
    tm = scr("tm", [S, NTM], F32)
    pmT = scr("pmT", [8192, S], BF16)
    ysT = scr("ysT", [4096, S], BF16)
    mT = scr("mT", [D, S], BF16)
    tabs = scr("tabs", [4, 128, S], F32)
    maskT = scr("maskT", [NT, S, 128], BF16)
    rwtm = scr("rwtm", [S, 5, 1024], F32)
    rwbon = scr("rwbon", [S, 16], F32)
    NC32 = S // 32
    rwtm2 = scr("rwtm2", [S, 3, 1024], F32)
    rwxt = scr("rwxt", [64, 16, NT, 512], F32)
    rwdec = scr("rwdec", [64, 16, NC32], F32)
    xcur = [x_in, scr("x1", [S, D], F32), y_out]

    if "fm_in" in dbg:
        pass

    ident_f = P.sb([128, 128], F32, "identf")
    ident_b = P.sb([128, 128], BF16, "identb")
    ones_b = P.sb([128, 128], BF16, "onesb")
    zeros_f = P.sb([128, PAD], F32, "zerosf")
    P.dma("sp", [(ident_f[:], I["c_ident"][:, :])], reads=[I["c_ident"]], writes=[ident_f])
    P.op("dve", lambda e: e.tensor_copy(ident_b[:], ident_f[:]), [ident_f], [ident_b])
    P.op("pool", lambda e: e.memset(ones_b[:], 1.0), [], [ones_b])
    P.op("pool", lambda e: e.memset(zeros_f[:], 0.0), [], [zeros_f])
    for nm in ("rw_v", "rw_gate"):
        for r8 in range(8):
            r0 = FM_ROWS[nm] + r8 * 128
            P.dma("sp", [(fm[r0:r0 + 128, 0:PAD], zeros_f[:])], reads=[zeros_f], writes=[fm])

    def evac(i, out_ap, in_ap, reads, writes):
        if i % 2 == 0:
            P.op("act", lambda e: e.copy(out_ap, in_ap), reads, writes)
        else:
            P.op("dve", lambda e: e.tensor_copy(out_ap, in_ap), reads, writes)

    def V(e, fn, reads, writes):
        P.op(e, fn, reads, writes)

    def phase_tables():
        with P.scope():
            posi = P.sb([128, S], I32, "posi")
            posf = P.sb([128, S], F32, "posf")
            inv = P.sb([128, 2], F32, "inv")
            ang = P.sb([128, S], F32, "ang")
            kf = P.sb([128, S], F32, "kf")
            ki = P.sb([128, S], I32, "ki")
            r1 = P.sb([128, S], F32, "r1")
            y = P.sb([128, S], F32, "y")
            m = P.sb([128, S], F32, "m")
            P.dma("sp", [(posi[:], pos_in[0:1, :].to_broadcast([128, S]))], reads=[pos_in], writes=[posi])
            P.dma("sp", [(inv[:], I["c_inv"][:, :])], reads=[I["c_inv"]], writes=[inv])
            V("dve", lambda e: e.tensor_copy(posf[:], posi[:]), [posi], [posf])
            for which in range(2):
                V("dve", lambda e: e.tensor_scalar(ang[:], posf[:], inv[:, which:which + 1], None, op0=ALU.mult), [posf, inv], [ang])
                V("dve", lambda e: e.tensor_scalar(kf[:], ang[:], float(1.0 / (2 * np.pi)), None, op0=ALU.mult), [ang], [kf])
                V("dve", lambda e: e.tensor_copy(ki[:], kf[:]), [kf], [ki])
                V("dve", lambda e: e.tensor_copy(kf[:], ki[:]), [ki], [kf])
                V("dve", lambda e: e.scalar_tensor_tensor(r1[:], kf[:], -6.28125, ang[:], op0=ALU.mult, op1=ALU.add), [kf, ang], [r1])
                V("dve", lambda e: e.scalar_tensor_tensor(ang[:], kf[:], -0.0019353071795864769, r1[:], op0=ALU.mult, op1=ALU.add), [kf, r1], [ang])
                for cs in range(2):
                    sh = float(np.pi / 2) if cs == 0 else 0.0
                    V("dve", lambda e: e.tensor_scalar(y[:], ang[:], sh, None, op0=ALU.add), [ang], [y])
                    V("dve", lambda e: e.tensor_scalar(m[:], y[:], float(np.pi), None, op0=ALU.is_gt), [y], [m])
                    V("dve", lambda e: e.scalar_tensor_tensor(y[:], m[:], float(-2 * np.pi), y[:], op0=ALU.mult, op1=ALU.add), [m, y], [y])
                    V("dve", lambda e: e.tensor_scalar(m[:], y[:], float(-np.pi), None, op0=ALU.is_lt), [y], [m])
                    V("dve", lambda e: e.scalar_tensor_tensor(y[:], m[:], float(2 * np.pi), y[:], op0=ALU.mult, op1=ALU.add), [m, y], [y])
                    V("dve", lambda e: e.tensor_scalar(y[:], y[:], float(np.pi), float(-np.pi), op0=ALU.min, op1=ALU.max), [y], [y])
                    V("act", lambda e: e.activation(r1[:], y[:], AF.Sin), [y], [r1])
                    P.dma("sp", [(tabs.t[which * 2 + cs, :, :], r1[:])], reads=[r1], writes=[tabs])

    def phase_proj(l, xin):
        with P.scope():
            hT = P.sb([128, 16, S], BF16, "hT")
            with P.scope():
                gbc = P.sb([128, D], F32, "gbc")
                P.dma("sp", [(gbc[:], I["norm_pre"][l:l + 1, :].to_broadcast([128, D]))], reads=[I["norm_pre"]], writes=[gbc])
                xt = [P.sb([128, D], F32, "xt") for _ in range(2)]
                junk = P.sb([128, D], BF16, "junk")
                hb = [P.sb([128, D], BF16, "hb") for _ in range(2)]
                ss = [P.sb([128, 1], F32, "ss") for _ in range(2)]
                rs = [P.sb([128, 1], F32, "rs") for _ in range(2)]
                pst = [P.ps([128, 4, 128], BF16, "pst") for _ in range(4)]
                P.dma("sp", [(xt[0][:], xin[0:128, :])], reads=[xin], writes=[xt[0]])
                for i in range(NT):
                    if i + 1 < NT:
                        P.dma("sp", [(xt[(i + 1) % 2][:], xin[(i + 1) * 128:(i + 2) * 128, :])], reads=[xin], writes=[xt[(i + 1) % 2]])
                    xi, si, ri, hi = xt[i % 2], ss[i % 2], rs[i % 2], hb[i % 2]
                    V("act", lambda e: e.activation(junk[:], xi[:], AF.Square, accum_out=si[:]), [xi], [junk, si])
                    V("act", lambda e: e.activation(ri[:], si[:], AF.Sqrt, bias=1e-6, scale=1.0 / D), [si], [ri])
                    V("dve", lambda e: e.reciprocal(ri[:], ri[:]), [ri], [ri])
                    V("dve", lambda e: e.scalar_tensor_tensor(hi[:], xi[:], ri[:, 0:1], gbc[:], op0=ALU.mult, op1=ALU.mult), [xi, ri, gbc], [hi])
                    for g4 in range(4):
                        pt = pst[g4]
                        for j in range(4):
                            kc = g4 * 4 + j
                            V("pe", lambda e: e.transpose(pt[:, j, :], hi[:, kc * 128:(kc + 1) * 128], ident_b[:]), [hi, ident_b], [pt])
                        evac(g4, hT[:, g4 * 4:(g4 + 1) * 4, i * 128:(i + 1) * 128], pt[:], [pt], [hT])
            with P.scope():
                wf = [P.sb([128, 16, 128], F32, "wf") for _ in range(2)]
                wb = [P.sb([128, 16, 128], BF16, "wb") for _ in range(2)]
                stg = [P.sb([128, S], F32, "stg") for _ in range(2)]
                stgh = [P.sb([128, S], BF16, "stgh") for _ in range(2)]
                pp = [P.ps([128, 512], F32, "pp") for _ in range(6)]
                slabs = []
                for (c0, n, mode, nm) in SEGS:
                    off = 0
                    while off < n:
                        m = min(128, n - off)
                        slabs.append((c0 + off, m, mode, nm, off))
                        off += m
                w_l = I["w_in"]

                def load(s):
                    c0, m, mode, nm, off = slabs[s]
                    src = w_l.t[l, :, c0:c0 + m].rearrange("(kc p) m -> p kc m", p=128)
                    P.dma("sp", [(wf[s % 2][:, k4 * 4:(k4 + 1) * 4, 0:m], src[:, k4 * 4:(k4 + 1) * 4, :]) for k4 in range(4)],
                          reads=[w_l], writes=[wf[s % 2]])

                def cast(s):
                    c0, m, mode, nm, off = slabs[s]
                    V("pool", lambda e: e.tensor_copy(wb[s % 2][:, :, 0:m], wf[s % 2][:, :, 0:m]), [wf[s % 2]], [wb[s % 2]])

                load(0)
                cast(0)
                if len(slabs) > 1:
                    load(1)
                pi = 0
                for s in range(len(slabs)):
                    c0, m, mode, nm, off = slabs[s]
                    if s + 1 < len(slabs):
                        cast(s + 1)
                    if s + 2 < len(slabs):
                        load(s + 2)
                    w_s = wb[s % 2]
                    if mode in ("F", "H"):
                        st = stg[s % 2] if mode == "F" else stgh[s % 2]
                        for tb in range(NB):
                            ps = pp[pi % 6]
                            for kc in range(16):
                                V("pe", lambda e: e.matmul(ps[0:m, :], w_s[:, kc, 0:m], hT[:, kc, tb * 512:(tb + 1) * 512],
                                                           start=(kc == 0), stop=(kc == 15)), [w_s, hT], [ps])
                            evac(pi, st[0:m, tb * 512:(tb + 1) * 512], ps[0:m, :], [ps], [st])
                            pi += 1
                        if mode == "F":
                            r0 = FM_ROWS[nm] + off
                            P.dma("sp", [(fm[r0:r0 + m, PAD:TP], st[0:m, :])], reads=[st], writes=[fm])
                        else:
                            P.dma("sp", [(pmT[off:off + m, :], st[0:m, :])], reads=[st], writes=[pmT])
                    else:
                        st = stg[s % 2]
                        st3 = st[:].rearrange("p (t m) -> p t m", m=128)
                        for t4 in range(NT // 4):
                            ps = pp[pi % 6]
                            ps3 = ps[:].rearrange("p (t m) -> p t m", m=128)
                            for j in range(4):
                                tt = t4 * 4 + j
                                for kc in range(16):
                                    V("pe", lambda e: e.matmul(ps3[:, j, 0:m], hT[:, kc, tt * 128:(tt + 1) * 128], w_s[:, kc, 0:m],
                                                               start=(kc == 0), stop=(kc == 15)), [w_s, hT], [ps])
                            evac(pi, st3[:, t4 * 4:(t4 + 1) * 4, 0:m], ps3[:, :, 0:m], [ps], [st])
                            pi += 1
                        cc = TM_COLS[nm] + off
                        P.dma("sp", [(tm.t[:, cc:cc + m].rearrange("(t p) m -> p t m", p=128), st3[:, 0:NT, 0:m])], reads=[st], writes=[tm])

    def phase_rope(l):
        with P.scope():
            A = P.sb([128, S], F32, "ropeA")
            B = P.sb([128, S], F32, "ropeB")
            C = P.sb([128, S], F32, "ropeC")
            Sn = P.sb([128, S], F32, "ropeS")
            t1 = P.sb([128, S], F32, "ropet1")
            t2 = P.sb([128, S], F32, "ropet2")
            A2 = P.sb([128, S], F32, "ropeA2")
            B2 = P.sb([128, S], F32, "ropeB2")
            for (nm, nh, hd, half, tab) in (("dsa_q", 8, 128, 16, 0), ("dsa_k", 8, 128, 16, 0), ("dsa_iq", 8, 64, 8, 2), ("dsa_ik", 1, 64, 8, 2)):
                np_ = nh * half
                r0 = FM_ROWS[nm]
                rows = fm.t[r0:r0 + nh * hd, PAD:TP].rearrange("(h r) t -> h r t", r=hd)
                P.dma("sp", [(C[0:np_, :], tabs.t[tab, 0:np_, :]), (Sn[0:np_, :], tabs.t[tab + 1, 0:np_, :])], reads=[tabs], writes=[C, Sn], sembuf=C)
                P.dma("sp", [(A[0:np_, :], rows[:, 0:half, :]), (B[0:np_, :], rows[:, half:2 * half, :])], reads=[fm], writes=[A, B], sembuf=A)
                V("dve", lambda e: e.tensor_tensor(t1[0:np_, :], A[0:np_, :], C[0:np_, :], ALU.mult), [A, C], [t1])
                V("pool", lambda e: e.tensor_tensor(t2[0:np_, :], B[0:np_, :], Sn[0:np_, :], ALU.mult), [B, Sn], [t2])
                V("dve", lambda e: e.tensor_tensor(A2[0:np_, :], t1[0:np_, :], t2[0:np_, :], ALU.subtract), [t1, t2], [A2])
                V("dve", lambda e: e.tensor_tensor(t1[0:np_, :], B[0:np_, :], C[0:np_, :], ALU.mult), [B, C], [t1])
                V("pool", lambda e: e.tensor_tensor(t2[0:np_, :], A[0:np_, :], Sn[0:np_, :], ALU.mult), [A, Sn], [t2])
                V("dve", lambda e: e.tensor_tensor(B2[0:np_, :], t1[0:np_, :], t2[0:np_, :], ALU.add), [t1, t2], [B2])
                P.dma("sp", [(rows[:, 0:half, :], A2[0:np_, :]), (rows[:, half:2 * half, :], B2[0:np_, :])], reads=[A2, B2], writes=[fm], sembuf=A2)

    def phase_gla(l):
        with P.scope():
            qT = P.sb([128, 4, S], BF16, "gqT")
            oT = P.sb([128, 8, S], BF16, "goT")
            stg = [P.sb([128, S], F32, "gstg") for _ in range(2)]
            glr = P.sb([17, S], F32, "glr")
            wg = P.sb([17, 512], F32, "gwg")
            ust = P.sb([64, 64], F32, "gust")
            ones64 = P.sb([64, 1], F32, "gones")
            hg = P.sb([128, 2], F32, "ghg")
            state = P.sb([128, 4, 256], F32, "gstate")
            state_b = P.sb([128, 4, 256], BF16, "gstateb")
            kc_ = [P.sb([64, 512], F32, "gk") for _ in range(2)]
            vc_ = [P.sb([64, 1024], F32, "gv") for _ in range(2)]
            vb = P.sb([64, 1024], BF16, "gvb")
            ee = P.sb([64, 512], F32, "gee")
            lsp = P.sb([64, 512], F32, "glsp")
            dkk = P.sb([64, 512], F32, "gdk")
            kdec = P.sb([64, 512], BF16, "gkdec")
            dec = P.sb([128, 4], F32, "gdec")
            pz = P.ps([64, 512], F32, "gpz")
            pG = P.ps([64, 512], F32, "gpG")
            ptot = P.ps([128, 4], F32, "gptot")
            pkv = [P.ps([128, 2, 256], F32, "gpkv") for _ in range(2)]
            po = P.ps([128, 8, 64], F32, "gpo")
            V("pool", lambda e: e.memset(glr[:], 1.0), [], [glr])
            V("pool", lambda e: e.memset(ones64[:], 1.0), [], [ones64])
            V("pool", lambda e: e.memset(state[:], 0.0), [], [state])
            r0 = FM_ROWS["gla_g"]
            P.dma("sp", [(glr[0:16, :], fm[r0:r0 + 16, PAD:TP])], reads=[fm], writes=[glr])
            P.dma("sp", [(wg[0:16, :], I["gla_w_g2"].t[l, :, :]), (wg[16:17, :], I["gla_b_g"][l:l + 1, :])], reads=[I["gla_w_g2"], I["gla_b_g"]], writes=[wg])
            P.dma("sp", [(ust[:], I["c_ustrict"][:, :])], reads=[I["c_ustrict"]], writes=[ust])
            P.dma("sp", [(hg[:], I["gla_head_g"].t[l, :].rearrange("(a p) -> p a", p=128))], reads=[I["gla_head_g"]], writes=[hg], allow_slow_non_contiguous=True)
            r0 = FM_ROWS["gla_q"]
            for h in range(4):
                st = stg[h % 2]
                P.dma("sp", [(st[:], fm[r0 + h * 128:r0 + (h + 1) * 128, PAD:TP])], reads=[fm], writes=[st])
                V("pool", lambda e: e.tensor_copy(qT[:, h, :], st[:]), [st], [qT])
            ck = TM_COLS["gla_k"]
            cv = TM_COLS["gla_v"]

            def loadkv(c):
                P.dma("sp", [(kc_[c % 2][:], tm[c * 64:(c + 1) * 64, ck:ck + 512])], reads=[tm], writes=[kc_[c % 2]])
                P.dma("sp", [(vc_[c % 2][:], tm[c * 64:(c + 1) * 64, cv:cv + 1024])], reads=[tm], writes=[vc_[c % 2]])
            loadkv(0)
            for c in range(NCH):
                if c + 1 < NCH:
                    loadkv(c + 1)
                t0 = c * 64
                kk_, vv_ = kc_[c % 2], vc_[c % 2]
                V("pe", lambda e: e.matmul(pz[:], glr[:, t0:t0 + 64], wg[:], start=True, stop=True), [glr, wg], [pz])
                V("act", lambda e: e.activation(ee[:], pz[:], AF.Exp, scale=-1.0), [pz], [ee])
                V("act", lambda e: e.activation(lsp[:], ee[:], AF.Ln, bias=1.0), [ee], [lsp])
                V("pe", lambda e: e.matmul(pG[:], ust[:], lsp[:], start=True, stop=True), [ust, lsp], [pG])
                for h in range(4):
                    V("pe", lambda e: e.matmul(ptot[:, h:h + 1], lsp[:, h * 128:(h + 1) * 128], ones64[:], start=True, stop=True), [lsp, ones64], [ptot])
                V("act", lambda e: e.activation(dkk[:], pG[:], AF.Exp, scale=-1.0 / 16.0), [pG], [dkk])
                V("act", lambda e: e.activation(dec[:], ptot[:], AF.Exp, scale=-1.0 / 16.0), [ptot], [dec])
                V("dve", lambda e: e.tensor_tensor(kdec[:], kk_[:], dkk[:], ALU.mult), [kk_, dkk], [kdec])
                V("pool", lambda e: e.tensor_copy(vb[:], vv_[:]), [vv_], [vb])
                for h in range(4):
                    pk = pkv[h // 2]
                    V("pe", lambda e: e.matmul(pk[:, h % 2, :], kdec[:, h * 128:(h + 1) * 128], vb[:, h * 256:(h + 1) * 256], start=True, stop=True), [kdec, vb], [pk])
                for h in range(4):
                    pk = pkv[h // 2]
                    V("dve", lambda e: e.scalar_tensor_tensor(state[:, h, :], state[:, h, :], dec[:, h:h + 1], pk[:, h % 2, :], op0=ALU.mult, op1=ALU.add), [state, dec, pk], [state])
                V("act", lambda e: e.copy(state_b[:], state[:]), [state], [state_b])
                for h in range(4):
                    for vh in range(2):
                        V("pe", lambda e: e.matmul(po[:, h * 2 + vh, :], state_b[:, h, vh * 128:(vh + 1) * 128], qT[:, h, t0:t0 + 64], start=True, stop=True), [state_b, qT], [po])
                evac(c, oT[:, :, t0:t0 + 64], po[:], [po], [oT])
            sq = P.sb([128, 2, 512], BF16, "gsq")
            pms = [P.ps([128, 512], F32, "gpms") for _ in range(2)]
            rstd = P.sb([128, 512], F32, "grstd")
            yy = P.sb([128, 512], F32, "gyy")
            sg = P.sb([128, 512], F32, "gsg")
            yo = [P.sb([128, S], BF16, "gyo") for _ in range(2)]
            sc = float(128.0 ** -0.5)
            hgs = P.sb([128, 2], F32, "ghgs")
            V("dve", lambda e: e.tensor_scalar(hgs[:], hg[:], sc, None, op0=ALU.mult), [hg], [hgs])
            rg = FM_ROWS["gla_gate"]
            it = 0
            for h in range(4):
                for vh in range(2):
                    gt = stg[it % 2]
                    yob = yo[it % 2]
                    P.dma("sp", [(gt[:], fm[rg + h * 256 + vh * 128:rg + h * 256 + (vh + 1) * 128, PAD:TP])], reads=[fm], writes=[gt])
                    for tb in range(NB):
                        ts_ = slice(tb * 512, (tb + 1) * 512)
                        pm_ = pms[(it * NB + tb) % 2]
                        if vh == 0:
                            for v2 in range(2):
                                V("act", lambda e: e.activation(sq[:, v2, :], oT[:, h * 2 + v2, ts_], AF.Square), [oT], [sq])
                        else:
                            for v2 in range(2):
                                V("act", lambda e: e.activation(sq[:, v2, :], oT[:, h * 2 + v2, ts_], AF.Square), [oT], [sq])
                        for v2 in range(2):
                            V("pe", lambda e: e.matmul(pm_[:], ones_b[:], sq[:, v2, :], start=(v2 == 0), stop=(v2 == 1)), [ones_b, sq], [pm_])
                        V("act", lambda e: e.activation(rstd[:], pm_[:], AF.Sqrt, bias=1e-6, scale=sc * sc / 256.0), [pm_], [rstd])
                        V("dve", lambda e: e.reciprocal(rstd[:], rstd[:]), [rstd], [rstd])
                        V("act", lambda e: e.activation(sg[:], gt[:, ts_], AF.Silu), [gt], [sg])
                        V("dve", lambda e: e.tensor_tensor(yy[:], oT[:, h * 2 + vh, ts_], rstd[:], ALU.mult), [oT, rstd], [yy])
                        V("dve", lambda e: e.scalar_tensor_tensor(yob[:, ts_], yy[:], hgs[:, vh:vh + 1], sg[:], op0=ALU.mult, op1=ALU.mult), [yy, hgs, sg], [yob])
                    ro = h * 256 + vh * 128
                    P.dma("sp", [(ysT[ro:ro + 128, :], yob[:])], reads=[yob], writes=[ysT])
                    it += 1

    def phase_sgu(l):
        with P.scope():
            lng = P.sb([128, 1024], F32, "slng")
            lnb = P.sb([128, 1024], F32, "slnb")
            bsb = P.sb([128, 8, 128], F32, "sbsb")
            msk = P.sb([128, 128], F32, "smsk")
            wsT = P.sb([128, 8, 128], BF16, "swsT")
            wtmp = P.sb([128, 128], F32, "swtmp")
            pT = P.ps([128, 128], F32, "spT")
            P.dma("sp", [(lng[:], I["sgu_ln_g"][l:l + 1, :].to_broadcast([128, 1024]))], reads=[I["sgu_ln_g"]], writes=[lng])
            P.dma("sp", [(lnb[:], I["sgu_ln_b"][l:l + 1, :].to_broadcast([128, 1024]))], reads=[I["sgu_ln_b"]], writes=[lnb])
            P.dma("sp", [(bsb[:].rearrange("p g i -> p (g i)"), I["sgu_b_s"].t[l:l + 1, :, :].rearrange("a g i -> a (g i)").to_broadcast([128, 1024]))], reads=[I["sgu_b_s"]], writes=[bsb])
            P.dma("sp", [(msk[:], I["c_sgumask"][:, :])], reads=[I["c_sgumask"]], writes=[msk])
            for g in range(8):
                P.dma("sp", [(wtmp[:], I["sgu_w_s"].t[l, g, :, :])], reads=[I["sgu_w_s"]], writes=[wtmp])
                V("dve", lambda e: e.tensor_tensor(wtmp[:], wtmp[:], msk[:], ALU.mult), [wtmp, msk], [wtmp])
                V("pe", lambda e: e.transpose(pT[:], wtmp[:], ident_f[:]), [wtmp, ident_f], [pT])
                V("act", lambda e: e.copy(wsT[:, g, :], pT[:]), [pT], [wsT])
            zv = [P.sb([128, 1024], F32, "szv") for _ in range(2)]
            uu = [P.sb([128, 8, 128], F32, "suu") for _ in range(2)]
            gg = [P.sb([128, 8, 128], F32, "sgg") for _ in range(2)]
            g1 = P.sb([128, 1024], F32, "sg1")
            junk = P.sb([128, 1024], BF16, "sjunk")
            vn = P.sb([128, 1024], F32, "svn")
            vln = P.sb([128, 1024], BF16, "svln")
            s1 = P.sb([128, 1], F32, "ss1"); s2 = P.sb([128, 1], F32, "ss2"); mm = P.sb([128, 1], F32, "smm")
            msq = P.sb([128, 1], F32, "smsq"); var = P.sb([128, 1], F32, "svar"); rstd = P.sb([128, 1], F32, "srstd")
            nmr = P.sb([128, 1], F32, "snmr")
            psv = [P.ps([128, 4, 128], F32, "spsv") for _ in range(2)]
            t1 = P.sb([128, 8, 128], F32, "st1")
            ug = P.sb([128, 8, 128], F32, "sug")
            sg = P.sb([128, 8, 128], F32, "ssg")
            yb = [P.sb([128, 8, 128], BF16, "syb") for _ in range(2)]
            cz = TM_COLS["sgu_zv"]
            ru = FM_ROWS["sgu_u"]
            rgt = FM_ROWS["sgu_gate"]

            def loadc(c):
                P.dma("sp", [(zv[c % 2][:], tm[c * 128:(c + 1) * 128, cz:cz + 1024])], reads=[tm], writes=[zv[c % 2]])
                P.dma("sp", [(uu[c % 2][:], fm.t[ru:ru + 1024, PAD + c * 128:PAD + (c + 1) * 128].rearrange("(g d) t -> d g t", d=128))], reads=[fm], writes=[uu[c % 2]])
                P.dma("sp", [(gg[c % 2][:], fm.t[rgt:rgt + 1024, PAD + c * 128:PAD + (c + 1) * 128].rearrange("(g d) t -> d g t", d=128))], reads=[fm], writes=[gg[c % 2]])
            loadc(0)
            for c in range(NT):
                if c + 1 < NT:
                    loadc(c + 1)
                z, u_, g_ = zv[c % 2], uu[c % 2], gg[c % 2]
                ybc = yb[c % 2]
                V("act", lambda e: e.activation(g1[:], z[:], AF.Gelu), [z], [g1])
                V("dve", lambda e: e.tensor_reduce(s1[:], g1[:], AX.X, ALU.add), [g1], [s1])
                V("act", lambda e: e.activation(junk[:], g1[:], AF.Square, accum_out=s2[:]), [g1], [junk, s2])
                V("dve", lambda e: e.tensor_scalar(mm[:], s1[:], 1.0 / 1024.0, None, op0=ALU.mult), [s1], [mm])
                V("dve", lambda e: e.tensor_tensor(msq[:], mm[:], mm[:], ALU.mult), [mm], [msq])
                V("dve", lambda e: e.scalar_tensor_tensor(var[:], s2[:], 1.0 / 1024.0, msq[:], op0=ALU.mult, op1=ALU.subtract), [s2, msq], [var])
                V("act", lambda e: e.activation(rstd[:], var[:], AF.Sqrt, bias=1e-5, scale=1.0), [var], [rstd])
                V("dve", lambda e: e.reciprocal(rstd[:], rstd[:]), [rstd], [rstd])
                V("dve", lambda e: e.scalar_tensor_tensor(nmr[:], mm[:], -1.0, rstd[:], op0=ALU.mult, op1=ALU.mult), [mm, rstd], [nmr])
                V("act", lambda e: e.activation(vn[:], g1[:], AF.Identity, bias=nmr[:, 0:1], scale=rstd[:, 0:1]), [g1, nmr, rstd], [vn])
                V("dve", lambda e: e.tensor_tensor(vn[:], vn[:], lng[:], ALU.mult), [vn, lng], [vn])
                V("dve", lambda e: e.tensor_tensor(vln[:], vn[:], lnb[:], ALU.add), [vn, lnb], [vln])
                for g in range(8):
                    pv = psv[g // 4]
                    V("pe", lambda e: e.matmul(pv[:, g % 4, :], vln[:, g * 128:(g + 1) * 128], wsT[:, g, :], start=True, stop=True), [vln, wsT], [pv])
                for g2 in range(2):
                    V("dve", lambda e: e.tensor_tensor(t1[:, g2 * 4:(g2 + 1) * 4, :], psv[g2][:], bsb[:, g2 * 4:(g2 + 1) * 4, :], ALU.add), [psv[g2], bsb], [t1])
                V("act", lambda e: e.activation(ug[:], u_[:], AF.Gelu), [u_], [ug])
                V("act", lambda e: e.activation(sg[:], g_[:], AF.Silu), [g_], [sg])
                V("pool", lambda e: e.tensor_tensor(ug[:], ug[:], sg[:], ALU.mult), [ug, sg], [ug])
                V("dve", lambda e: e.tensor_tensor(ybc[:], t1[:], ug[:], ALU.mult), [t1, ug], [ybc])
                P.dma("sp", [(ysT.t[3072:4096, c * 128:(c + 1) * 128].rearrange("(g d) t -> d g t", d=128), ybc[:])], reads=[ybc], writes=[ysT])

    def phase_m1(l):
        with P.scope():
            wp = P.sb([128, 32, D], BF16, "mwp")
            wst = [P.sb([128, 2048], F32, "mwst") for _ in range(2)]
            wpj = I["w_proj"]
            k = 0
            for g in range(4):
                for wc in range(8):
                    st = wst[k % 2]
                    P.dma("sp", [(st[:], wpj.t[l, g, wc * 128:(wc + 1) * 128, :])], reads=[wpj], writes=[st])
                    if k % 2 == 0:
                        V("pool", lambda e: e.tensor_copy(wp[:, g * 8 + wc, :], st[:]), [st], [wp])
                    else:
                        V("dve", lambda e: e.tensor_copy(wp[:, g * 8 + wc, :], st[:]), [st], [wp])
                    k += 1
            TB = 256
            yt = [P.sb([128, 32, TB], BF16, "myt") for _ in range(2)]
            pmt = [P.sb([128, 4, TB], BF16, "mpm") for _ in range(3)]
            sig = P.sb([128, 4, TB], F32, "msig")
            acc = P.sb([128, TB], F32, "macc")
            tmp = P.sb([128, TB], F32, "mtmp")
            mo = [P.sb([128, 16, TB], BF16, "mmo") for _ in range(2)]
            pps = [P.ps([128, TB], F32, "mpp") for _ in range(6)]
            nblk = S // TB

            def loady(b):
                P.dma("sp", [(yt[b % 2][:, q * 8:(q + 1) * 8, :], ysT.t[q * 1024:(q + 1) * 1024, b * TB:(b + 1) * TB].rearrange("(c p) t -> p c t", p=128)) for q in range(4)],
                      reads=[ysT], writes=[yt[b % 2]])
            loady(0)
            pi = 0
            it = 0
            for b in range(nblk):
                if b + 1 < nblk:
                    loady(b + 1)
                ytb = yt[b % 2]
                mob = mo[b % 2]
                for dc in range(16):
                    pmb = pmt[it % 3]
                    it += 1
                    P.dma("sp", [(pmb[:], pmT.t[:, b * TB:(b + 1) * TB].rearrange("(g r) t -> r g t", g=4)[dc * 128:(dc + 1) * 128, :, :])], reads=[pmT], writes=[pmb])
                    V("act", lambda e: e.activation(sig[:], pmb[:], AF.Sigmoid), [pmb], [sig])
                    for g in range(4):
                        ps = pps[pi % 6]
                        pi += 1
                        for wc in range(8):
                            V("pe", lambda e: e.matmul(ps[:], wp[:, g * 8 + wc, dc * 128:(dc + 1) * 128], ytb[:, g * 8 + wc, :], start=(wc == 0), stop=(wc == 7)), [wp, ytb], [ps])
                        if g == 0:
                            V("dve", lambda e: e.tensor_tensor(acc[:], ps[:], sig[:, g, :], ALU.mult), [ps, sig], [acc])
                        elif g < 3:
                            V("dve", lambda e: e.tensor_tensor(tmp[:], ps[:], sig[:, g, :], ALU.mult), [ps, sig], [tmp])
                            V("pool", lambda e: e.tensor_tensor(acc[:], acc[:], tmp[:], ALU.add), [acc, tmp], [acc])
                        else:
                            V("dve", lambda e: e.tensor_tensor(tmp[:], ps[:], sig[:, g, :], ALU.mult), [ps, sig], [tmp])
                            V("pool", lambda e: e.tensor_tensor(mob[:, dc, :], acc[:], tmp[:], ALU.add), [acc, tmp], [mob])
                P.dma("sp", [(mT.t[:, b * TB:(b + 1) * TB].rearrange("(c p) t -> p c t", p=128), mob[:])], reads=[mob], writes=[mT])

    def phase_m2(l, xin, xout):
        with P.scope():
            wo = P.sb([128, 16, D], BF16, "owo")
            wst = [P.sb([128, 2048], F32, "owst") for _ in range(2)]
            for dc in range(16):
                st = wst[dc % 2]
                P.dma("sp", [(st[:], I["w_out"].t[l, dc * 128:(dc + 1) * 128, :])], reads=[I["w_out"]], writes=[st])
                if dc % 2 == 0:
                    V("pool", lambda e: e.tensor_copy(wo[:, dc, :], st[:]), [st], [wo])
                else:
                    V("dve", lambda e: e.tensor_copy(wo[:, dc, :], st[:]), [st], [wo])
            gbc = P.sb([128, D], F32, "ogbc")
            P.dma("sp", [(gbc[:], I["norm_post"][l:l + 1, :].to_broadcast([128, D]))], reads=[I["norm_post"]], writes=[gbc])
            mt = [P.sb([128, 16, 128], BF16, "omt") for _ in range(2)]
            xt = [P.sb([128, D], F32, "oxt") for _ in range(2)]
            ot = P.sb([128, D], F32, "oot")
            junk = P.sb([128, D], BF16, "ojunk")
            res = [P.sb([128, D], F32, "ores") for _ in range(2)]
            ssq = P.sb([128, 1], F32, "ossq")
            rs = P.sb([128, 1], F32, "ors")
            pps = [P.ps([128, 512], F32, "opp") for _ in range(4)]

            def loadt(i):
                P.dma("sp", [(mt[i % 2][:], mT.t[:, i * 128:(i + 1) * 128].rearrange("(c p) t -> p c t", p=128))], reads=[mT], writes=[mt[i % 2]])
                P.dma("sp", [(xt[i % 2][:], xin[i * 128:(i + 1) * 128, :])], reads=[xin], writes=[xt[i % 2]])
            loadt(0)
            for i in range(NT):
                if i + 1 < NT:
                    loadt(i + 1)
                mti, xti, rsi = mt[i % 2], xt[i % 2], res[i % 2]
                for nb in range(4):
                    ps = pps[nb]
                    for dc in range(16):
                        V("pe", lambda e: e.matmul(ps[:], mti[:, dc, :], wo[:, dc, nb * 512:(nb + 1) * 512], start=(dc == 0), stop=(dc == 15)), [mti, wo], [ps])
                    evac(nb, ot[:, nb * 512:(nb + 1) * 512], ps[:], [ps], [ot])
                V("act", lambda e: e.activation(junk[:], ot[:], AF.Square, accum_out=ssq[:]), [ot], [junk, ssq])
                V("act", lambda e: e.activation(rs[:], ssq[:], AF.Sqrt, bias=1e-6, scale=1.0 / D), [ssq], [rs])
                V("dve", lambda e: e.reciprocal(rs[:], rs[:]), [rs], [rs])
                V("dve", lambda e: e.scalar_tensor_tensor(ot[:], ot[:], rs[:, 0:1], gbc[:], op0=ALU.mult, op1=ALU.mult), [ot, rs, gbc], [ot])
                V("pool", lambda e: e.tensor_tensor(rsi[:], ot[:], xti[:], ALU.add), [ot, xti], [rsi])
                P.dma("sp", [(xout[i * 128:(i + 1) * 128, :], rsi[:])], reads=[rsi], writes=[xout])


    def phase_dsa1(l):
        with P.scope():
            iqT = P.sb([64, 8, S], BF16, "diqT")
            ikT = P.sb([64, S], BF16, "dikT")
            iw = P.sb([128, NT, 8], F32, "diw")
            stg = [P.sb([64, S], F32, "dstg") for _ in range(2)]
            score = P.sb([128, S], F32, "dscore")
            work = P.sb([128, S], F32, "dwork")
            maskb = P.sb([128, S], BF16, "dmaskb")
            mts = [P.sb([128, NT, 128], BF16, "dmts") for _ in range(2)]
            rl = [P.sb([128, 512], F32, "drl") for _ in range(2)]
            m8 = P.sb([128, 8], F32, "dm8")
            thr = P.sb([128, 1], F32, "dthr")
            blo = P.sb([128, 1], F32, "dblo"); bw0 = P.sb([128, 1], F32, "dbw0"); bmid = P.sb([128, 1], F32, "dbmid")
            bcnt = P.sb([128, 1], F32, "dbcnt"); bge = P.sb([128, 1], F32, "dbge"); bstp = P.sb([128, 1], F32, "dbstp")
            pps = [P.ps([128, 512], F32, "dpp") for _ in range(4)]
            ptr = [P.ps([128, 4, 128], BF16, "dptr") for _ in range(2)]
            r0 = FM_ROWS["dsa_iq"]
            for h in range(8):
                st = stg[h % 2]
                P.dma("sp", [(st[:], fm[r0 + h * 64:r0 + (h + 1) * 64, PAD:TP])], reads=[fm], writes=[st])
                V("pool", lambda e: e.tensor_copy(iqT[:, h, :], st[:]), [st], [iqT])
            r0 = FM_ROWS["dsa_ik"]
            P.dma("sp", [(stg[0][:], fm[r0:r0 + 64, PAD:TP])], reads=[fm], writes=[stg[0]])
            V("pool", lambda e: e.tensor_copy(ikT[:], stg[0][:]), [stg[0]], [ikT])
            cw = TM_COLS["dsa_iw"]
            P.dma("sp", [(iw[:], tm.t[:, cw:cw + 8].rearrange("(t p) c -> p t c", p=128))], reads=[tm], writes=[iw])
            V("dve", lambda e: e.tensor_scalar(iw[:], iw[:], float(512.0 ** -0.5), None, op0=ALU.mult), [iw], [iw])
            k = 0
            kt = 0
            for qt in range(NT):
                nk = (qt + 1) * 128
                for sb in range((nk + 511) // 512):
                    w = min(512, nk - sb * 512)
                    cs = slice(sb * 512, sb * 512 + w)
                    for h in range(8):
                        ps = pps[k % 4]
                        rt = rl[k % 2]
                        k += 1
                        V("pe", lambda e: e.matmul(ps[:, 0:w], iqT[:, h, qt * 128:(qt + 1) * 128], ikT[:, cs], start=True, stop=True), [iqT, ikT], [ps])
                        V("act", lambda e: e.activation(rt[:, 0:w], ps[:, 0:w], AF.Relu), [ps], [rt])
                        if h == 0:
                            V("dve", lambda e: e.tensor_scalar(score[:, cs], rt[:, 0:w], iw[:, qt, 0:1], None, op0=ALU.mult), [rt, iw], [score])
                        else:
                            V("dve", lambda e: e.scalar_tensor_tensor(score[:, cs], rt[:, 0:w], iw[:, qt, h:h + 1], score[:, cs], op0=ALU.mult, op1=ALU.add), [rt, iw, score], [score])
                V("pool", lambda e: e.memset(score[0:64, nk - 64:nk], NEG), [], [score])
                if nk <= KTOP:
                    V("pool", lambda e: e.memset(thr[:], NEG / 2), [], [thr])
                else:
                    V("dve", lambda e: e.max(out=m8[:], in_=score[:, 0:nk]), [score], [m8])
                    V("dve", lambda e: e.tensor_reduce(blo[:], score[:, 0:nk - 64], AX.X, ALU.min), [score], [blo])
                    V("dve", lambda e: e.tensor_tensor(bw0[:], m8[:, 0:1], blo[:], ALU.subtract), [m8, blo], [bw0])
                    for it in range(24):
                        f = float(2.0 ** -(it + 1))
                        V("dve", lambda e: e.scalar_tensor_tensor(bmid[:], bw0[:], f, blo[:], op0=ALU.mult, op1=ALU.add), [bw0, blo], [bmid])
                        V("dve", lambda e: e.memset(bcnt[:], 0.0), [], [bcnt])
                        V("dve", lambda e: e.tensor_scalar(maskb[:, 0:nk], score[:, 0:nk], bmid[:, 0:1], 0.0, op0=ALU.is_ge, op1=ALU.add, accum_out=bcnt[:]), [score, bmid], [maskb, bcnt])
                        V("dve", lambda e: e.tensor_scalar(bge[:], bcnt[:], float(KTOP) - 0.5, None, op0=ALU.is_ge), [bcnt], [bge])
                        V("dve", lambda e: e.tensor_tensor(bstp[:], bge[:], bw0[:], ALU.mult), [bge, bw0], [bstp])
                        V("dve", lambda e: e.scalar_tensor_tensor(blo[:], bstp[:], f, blo[:], op0=ALU.mult, op1=ALU.add), [bstp, blo], [blo])
                    V("dve", lambda e: e.tensor_copy(thr[:], blo[:]), [blo], [thr])
                V("dve", lambda e: e.tensor_scalar(maskb[:, 0:nk], score[:, 0:nk], thr[:, 0:1], None, op0=ALU.is_ge), [score, thr], [maskb])
                mtb = mts[qt % 2]
                for j4 in range((qt + 4) // 4):
                    n = min(4, qt + 1 - j4 * 4)
                    pt = ptr[kt % 2]
                    kt += 1
                    for jj in range(n):
                        j = j4 * 4 + jj
                        V("pe", lambda e: e.transpose(pt[:, jj, :], maskb[:, j * 128:(j + 1) * 128], ident_b[:]), [maskb, ident_b], [pt])
                    evac(kt, mtb[:, j4 * 4:j4 * 4 + n, :], pt[:, 0:n, :], [pt], [mtb])
                P.dma("sp", [(maskT.t[qt, 0:nk, :].rearrange("(j p) t -> p j t", p=128), mtb[:, 0:qt + 1, :])], reads=[mtb], writes=[maskT])

    def phase_dsa2(l):
        with P.scope():
            stg = [P.sb([128, S], F32, "astg") for _ in range(2)]
            kT = P.sb([128, S], BF16, "akT")
            qT = P.sb([128, S], BF16, "aqT")
            Vh = P.sb([128, NT, 128], BF16, "aVh")
            sg = P.sb([128, S], F32, "asg")
            mk = [P.sb([128, 4, 128], BF16, "amk") for _ in range(3)]
            ex = [P.sb([128, 4, 128], BF16, "aex") for _ in range(2)]
            ptt = [P.sb([128, 4, 128], BF16, "aptt") for _ in range(2)]
            pl = [P.ps([128, 4, 128], F32, "apl") for _ in range(2)]
            po = [P.ps([128, 512], F32, "apo") for _ in range(2)]
            pd = [P.ps([128, 512], F32, "apd") for _ in range(2)]
            rden = P.sb([128, 128], F32, "arden")
            o1 = P.sb([128, 128], F32, "ao1")
            yo = [P.sb([128, S], BF16, "ayo") for _ in range(2)]
            sc = float(128.0 ** -0.5)
            rk, rq, rg = FM_ROWS["dsa_k"], FM_ROWS["dsa_q"], FM_ROWS["dsa_gate"]
            cv = TM_COLS["dsa_v"]
            kb = 0
            for h in range(8):
                P.dma("sp", [(stg[0][:], fm[rk + h * 128:rk + (h + 1) * 128, PAD:TP])], reads=[fm], writes=[stg[0]])
                V("pool", lambda e: e.tensor_copy(kT[:], stg[0][:]), [stg[0]], [kT])
                P.dma("sp", [(stg[1][:], fm[rq + h * 128:rq + (h + 1) * 128, PAD:TP])], reads=[fm], writes=[stg[1]])
                V("pool", lambda e: e.tensor_copy(qT[:], stg[1][:]), [stg[1]], [qT])
                P.dma("sp", [(stg[0][:].rearrange("p (j v) -> p j v", v=128), tm.t[:, cv + h * 128:cv + (h + 1) * 128].rearrange("(j p) v -> p j v", p=128))], reads=[tm], writes=[stg[0]])
                V("pool", lambda e: e.tensor_copy(Vh[:], stg[0][:].rearrange("p (j v) -> p j v", v=128)), [stg[0]], [Vh])
                P.dma("sp", [(stg[1][:], fm[rg + h * 128:rg + (h + 1) * 128, PAD:TP])], reads=[fm], writes=[stg[1]])
                V("act", lambda e: e.activation(sg[:], stg[1][:], AF.Silu), [stg[1]], [sg])
                yob = yo[h % 2]
                for qt in range(NT):
                    nj = qt + 1
                    po_, pd_ = po[qt % 2], pd[qt % 2]
                    qs = slice(qt * 128, (qt + 1) * 128)
                    for j4 in range((nj + 3) // 4):
                        n = min(4, nj - j4 * 4)
                        mkb, plb, exb, ptb = mk[kb % 3], pl[kb % 2], ex[kb % 2], ptt[kb % 2]
                        kb += 1
                        P.dma("sp", [(mkb[:, 0:n, :], maskT.t[qt, j4 * 512:j4 * 512 + n * 128, :].rearrange("(j p) t -> p j t", p=128))], reads=[maskT], writes=[mkb])
                        for jj in range(n):
                            j = j4 * 4 + jj
                            V("pe", lambda e: e.matmul(plb[:, jj, :], kT[:, j * 128:(j + 1) * 128], qT[:, qs], start=True, stop=True), [kT, qT], [plb])
                        V("act", lambda e: e.activation(exb[:, 0:n, :], plb[:, 0:n, :], AF.Exp, scale=sc), [plb], [exb])
                        V("dve", lambda e: e.tensor_tensor(ptb[:, 0:n, :], exb[:, 0:n, :], mkb[:, 0:n, :], ALU.mult), [exb, mkb], [ptb])
                        for jj in range(n):
                            j = j4 * 4 + jj
                            V("pe", lambda e: e.matmul(po_[:, 0:128], Vh[:, j, :], ptb[:, jj, :], start=(j == 0), stop=(j == nj - 1)), [Vh, ptb], [po_])
                            V("pe", lambda e: e.matmul(pd_[:, 0:128], ones_b[:], ptb[:, jj, :], start=(j == 0), stop=(j == nj - 1)), [ones_b, ptb], [pd_])
                    V("dve", lambda e: e.reciprocal(rden[:], pd_[:, 0:128]), [pd_], [rden])
                    V("dve", lambda e: e.tensor_tensor(o1[:], po_[:, 0:128], rden[:], ALU.mult), [po_, rden], [o1])
                    V("pool", lambda e: e.tensor_tensor(yob[:, qs], o1[:], sg[:, qs], ALU.mult), [o1, sg], [yob])
                P.dma("sp", [(ysT[1024 + h * 128:1024 + (h + 1) * 128, :], yob[:])], reads=[yob], writes=[ysT])

    def phase_rwp(l):
        with P.scope():
            c0 = TM_COLS["rw_r"]
            mub = P.sb([128, 2240], F32, "rmub")
            mu = I["rwkv_mu"]
            P.dma("sp", [(mub[:, 0:2048], mu[l:l + 1, 0:2048].to_broadcast([128, 2048])), (mub[:, 2048:2240], mu[l:l + 1, 3072:3264].to_broadcast([128, 192]))], reads=[mu], writes=[mub])

            def bc(name, n=1024):
                t = P.sb([128, n], F32, "rb" + name)
                src = I[name]
                if name == "rwkv_r_k":
                    ap = src.t[l:l + 1, :, :].rearrange("a h k -> a (h k)").to_broadcast([128, n])
                else:
                    ap = src[l:l + 1, :].to_broadcast([128, n])
                P.dma("sp", [(t[:], ap)], reads=[src], writes=[t])
                return t
            kkb, kab, w0b, a0b, rkb = bc("rwkv_k_k"), bc("rwkv_k_a"), bc("rwkv_w0"), bc("rwkv_a0"), bc("rwkv_r_k")
            omka = P.sb([128, 1024], F32, "romka")
            V("dve", lambda e: e.tensor_scalar(omka[:], kab[:], -1.0, 1.0, op0=ALU.mult, op1=ALU.add), [kab], [omka])
            ww2 = P.sb([96, 1024], F32, "rww2")
            wa2 = P.sb([96, 1024], F32, "rwa2")
            P.dma("sp", [(ww2[:], I["rwkv_w_w2"].t[l, :, :])], reads=[I["rwkv_w_w2"]], writes=[ww2])
            P.dma("sp", [(wa2[:], I["rwkv_w_a2"].t[l, :, :])], reads=[I["rwkv_w_a2"]], writes=[wa2])
            cur = [P.sb([128, 2240], F32, "rcur") for _ in range(2)]
            prv = [P.sb([128, 2240], F32, "rprv") for _ in range(2)]
            dd = P.sb([128, 2240], F32, "rdd")
            th = P.sb([128, 96], F32, "rth")
            thT = P.sb([96, 128], F32, "rthT")
            alT = P.sb([96, 128], F32, "ralT")
            ptT = [P.ps([96, 128], F32, "rptT") for _ in range(2)]
            pz = [P.ps([128, 2, 512], F32, "rpz") for _ in range(2)]
            zs = P.sb([128, 1024], F32, "rzs")
            aa = P.sb([128, 1024], F32, "raa")
            kk = P.sb([128, 1024], F32, "rkk")
            sq = P.sb([128, 1024], F32, "rsq")
            ssq = P.sb([128, 16], F32, "rssq")
            tt = P.sb([128, 1024], F32, "rtt")
            out5 = [P.sb([128, 5, 1024], F32, "rout5") for _ in range(2)]
            bon = [P.sb([128, 16], F32, "rbon") for _ in range(2)]

            def loadt(i):
                P.dma("sp", [(cur[i % 2][:], tm[i * 128:(i + 1) * 128, c0:c0 + 2240])], reads=[tm], writes=[cur[i % 2]])
                if i == 0:
                    V("pool", lambda e: e.memset(prv[0][0:1, :], 0.0), [], [prv[0]])
                    P.dma("sp", [(prv[0][1:128, :], tm[0:127, c0:c0 + 2240])], reads=[tm], writes=[prv[0]])
                else:
                    P.dma("sp", [(prv[i % 2][:], tm[i * 128 - 1:i * 128 + 127, c0:c0 + 2240])], reads=[tm], writes=[prv[i % 2]])
            loadt(0)
            for i in range(NT):
                if i + 1 < NT:
                    loadt(i + 1)
                cu, pr, o5, bn = cur[i % 2], prv[i % 2], out5[i % 2], bon[i % 2]
                V("dve", lambda e: e.tensor_tensor(dd[:], pr[:], cu[:], ALU.subtract), [pr, cu], [dd])
                V("pool", lambda e: e.tensor_tensor(dd[:], dd[:], mub[:], ALU.mult), [dd, mub], [dd])
                V("dve", lambda e: e.tensor_tensor(cu[:], cu[:], dd[:], ALU.add), [cu, dd], [cu])
                rr_, k_ = cu[:, 0:1024], cu[:, 1024:2048]
                V("act", lambda e: e.activation(th[:], cu[:, 2048:2144], AF.Tanh), [cu], [th])
                V("pe", lambda e: e.transpose(ptT[0][:], th[:], ident_f[:]), [th, ident_f], [ptT[0]])
                V("act", lambda e: e.copy(thT[:], ptT[0][:]), [ptT[0]], [thT])
                V("pe", lambda e: e.transpose(ptT[1][:], cu[:, 2144:2240], ident_f[:]), [cu, ident_f], [ptT[1]])
                V("dve", lambda e: e.tensor_copy(alT[:], ptT[1][:]), [ptT[1]], [alT])
                for n2 in range(2):
                    V("pe", lambda e: e.matmul(pz[0][:, n2, :], thT[:], ww2[:, n2 * 512:(n2 + 1) * 512], start=True, stop=True), [thT, ww2], [pz[0]])
                    V("pe", lambda e: e.matmul(pz[1][:, n2, :], alT[:], wa2[:, n2 * 512:(n2 + 1) * 512], start=True, stop=True), [alT, wa2], [pz[1]])
                V("dve", lambda e: e.tensor_tensor(zs[:], pz[0][:].rearrange("p a b -> p (a b)"), w0b[:], ALU.add), [pz[0], w0b], [zs])
                V("act", lambda e: e.activation(zs[:], zs[:], AF.Sigmoid), [zs], [zs])
                V("act", lambda e: e.activation(o5[:, 0, :], zs[:], AF.Exp, scale=-0.606531), [zs], [o5])
                V("dve", lambda e: e.tensor_tensor(aa[:], pz[1][:].rearrange("p a b -> p (a b)"), a0b[:], ALU.add), [pz[1], a0b], [aa])
                V("act", lambda e: e.activation(aa[:], aa[:], AF.Sigmoid), [aa], [aa])
                V("dve", lambda e: e.tensor_tensor(kk[:], k_, kkb[:], ALU.mult), [cu, kkb], [kk])
                V("act", lambda e: e.activation(sq[:], kk[:], AF.Square), [kk], [sq])
                V("dve", lambda e: e.tensor_reduce(ssq[:], sq[:].rearrange("p (h k) -> p h k", k=64), AX.X, ALU.add), [sq], [ssq])
                V("act", lambda e: e.activation(ssq[:], ssq[:], AF.Sqrt, bias=1e-12, scale=1.0), [ssq], [ssq])
                V("dve", lambda e: e.reciprocal(ssq[:], ssq[:]), [ssq], [ssq])
                V("dve", lambda e: e.tensor_tensor(o5[:, 1, :].rearrange("p (h k) -> p h k", k=64), kk[:].rearrange("p (h k) -> p h k", k=64),
                                                   ssq[:, :].unsqueeze(2).to_broadcast([128, 16, 64]), ALU.mult), [kk, ssq], [o5])
                V("dve", lambda e: e.scalar_tensor_tensor(o5[:, 2, :], o5[:, 1, :], -1.0, aa[:], op0=ALU.mult, op1=ALU.mult), [o5, aa], [o5])
                V("pool", lambda e: e.tensor_tensor(tt[:], aa[:], kab[:], ALU.mult), [aa, kab], [tt])
                V("pool", lambda e: e.tensor_tensor(tt[:], tt[:], omka[:], ALU.add), [tt, omka], [tt])
                V("dve", lambda e: e.tensor_tensor(o5[:, 3, :], k_, tt[:], ALU.mult), [cu, tt], [o5])
                V("pool", lambda e: e.tensor_copy(o5[:, 4, :], rr_), [cu], [o5])
                V("pool", lambda e: e.tensor_tensor(tt[:], rr_, rkb[:], ALU.mult), [cu, rkb], [tt])
                V("dve", lambda e: e.tensor_tensor(tt[:], tt[:], o5[:, 3, :], ALU.mult), [tt, o5], [tt])
                V("dve", lambda e: e.tensor_reduce(bn[:], tt[:].rearrange("p (h k) -> p h k", k=64), AX.X, ALU.add), [tt], [bn])
                P.dma("sp", [(rwtm[i * 128:(i + 1) * 128, :, :], o5[:])], reads=[o5], writes=[rwtm])
                P.dma("sp", [(rwbon[i * 128:(i + 1) * 128, :], bn[:])], reads=[bn], writes=[rwbon])

    def phase_rws(l):
        with P.scope():
            sel = P.sb([128, 64, 128], F32, "wsel")
            blk = P.sb([128, 128], F32, "wblk")
            blkm = P.sb([128, 128], F32, "wblkm")
            i2 = P.sb([128, 64], F32, "wi2")
            P.dma("sp", [(sel[:], I["c_sel"][:, :, :])], reads=[I["c_sel"]], writes=[sel])
            P.dma("sp", [(blk[:], I["c_blk"][:, :])], reads=[I["c_blk"]], writes=[blk])
            P.dma("sp", [(i2[:], I["c_i2"][:, :])], reads=[I["c_i2"]], writes=[i2])
            V("dve", lambda e: e.tensor_scalar(blkm[:], blk[:], 1.0 / 64.0, None, op0=ALU.mult), [blk], [blkm])

            def hv(name, off=0):
                t = P.sb([128, 8], F32, "w" + name)
                src = I[name]
                P.dma("sp", [(t[hf * 64:(hf + 1) * 64, :], src.t[l, off + hf * 512:off + (hf + 1) * 512].rearrange("(h v) -> v h", v=64)) for hf in range(2)],
                      reads=[src], writes=[t], allow_slow_non_contiguous=True)
                return t
            lng, lnb = hv("rwkv_lnx_g"), hv("rwkv_lnx_b")
            muv, mug = hv("rwkv_mu", 2048), hv("rwkv_mu", 3264)
            St = P.sb([128, 8, 64], F32, "wS")
            V("pool", lambda e: e.memset(St[:], 0.0), [], [St])
            X = [P.sb([128, 5, 512], F32, "wX") for _ in range(2)]
            vch = [P.sb([128, 8, 65], F32, "wvch") for _ in range(2)]
            gch = [P.sb([128, 8, 65], F32, "wgch") for _ in range(2)]
            s2 = [P.sb([128, 8], F32, "ws2") for _ in range(2)]
            vl = P.sb([128, 8, 64], F32, "wvl")
            gl = P.sb([128, 8, 64], F32, "wgl")
            bcs = [[P.sb([128, 512], F32, "wbc") for _ in range(5)] for _ in range(2)]
            pb = [P.ps([128, 512], F32, "wpb") for _ in range(5)]
            pq = [P.ps([128, 512], F32, "wpq") for _ in range(2)]
            Y = P.sb([128, 8, 64], F32, "wY")
            tA = P.sb([128, 8, 64], F32, "wtA")
            tB = [P.sb([128, 8, 64], F32, "wtB") for _ in range(2)]
            sa = P.sb([128, 8], F32, "wsa")
            mean = P.sb([128, 512], F32, "wmean")
            yc = P.sb([128, 8, 64], F32, "wyc")
            sq = P.sb([128, 512], F32, "wsq")
            rstd = P.sb([128, 512], F32, "wrstd")
            rh = [P.sb([128, 64], F32, "wrh") for _ in range(2)]
            yb = [P.sb([128, 8, 64], BF16, "wyb") for _ in range(2)]
            rv, rg = FM_ROWS["rw_v"], FM_ROWS["rw_gate"]

            def b3(t2):
                return t2[:, :].unsqueeze(2).to_broadcast([128, 8, 64])

            def loadc(c):
                t0 = c * 64
                P.dma("sp", [(X[c % 2][hf * 64:(hf + 1) * 64, :, :], rwtm.t[t0:t0 + 64, :, hf * 512:(hf + 1) * 512]) for hf in range(2)], reads=[rwtm], writes=[X[c % 2]])
                P.dma("sp", [(vch[c % 2][hf * 64:(hf + 1) * 64, :, :], fm.t[rv + hf * 512:rv + (hf + 1) * 512, PAD + t0 - 1:PAD + t0 + 64].rearrange("(h v) t -> v h t", v=64)) for hf in range(2)],
                      reads=[fm], writes=[vch[c % 2]])
                P.dma("sp", [(gch[c % 2][hf * 64:(hf + 1) * 64, :, :], fm.t[rg + hf * 512:rg + (hf + 1) * 512, PAD + t0 - 1:PAD + t0 + 64].rearrange("(h v) t -> v h t", v=64)) for hf in range(2)],
                      reads=[fm], writes=[gch[c % 2]])
                P.dma("sp", [(s2[c % 2][hf * 64:(hf + 1) * 64, :], rwbon[t0:t0 + 64, hf * 8:(hf + 1) * 8]) for hf in range(2)], reads=[rwbon], writes=[s2[c % 2]])
            loadc(0)
            step = 0
            for c in range(NCH):
                if c + 1 < NCH:
                    loadc(c + 1)
                t0 = c * 64
                Xc, vc, gc, s2c = X[c % 2], vch[c % 2], gch[c % 2], s2[c % 2]
                for (src, dst, mu_) in ((vc, vl, muv), (gc, gl, mug)):
                    V("pool", lambda e: e.tensor_tensor(dst[:], src[:, :, 0:64], src[:, :, 1:65], ALU.subtract), [src], [dst])
                    V("pool", lambda e: e.tensor_tensor(dst[:], dst[:], b3(mu_), ALU.mult), [dst, mu_], [dst])
                    V("pool", lambda e: e.tensor_tensor(dst[:], dst[:], src[:, :, 1:65], ALU.add), [dst, src], [dst])
                for tl in range(64):
                    bset = bcs[step % 2]
                    tBs = tB[step % 2]
                    step += 1
                    for p in range(5):
                        V("pe", lambda e: e.matmul(pb[p][:], sel[:, tl, :], Xc[:, p, :], start=True, stop=True), [sel, Xc], [pb[p]])
                        V("act", lambda e: e.copy(bset[p][:], pb[p][:]), [pb[p]], [bset[p]])
                    wB, kkB, kkanB, k2B, rB = [b[:].rearrange("p (h k) -> p h k", k=64) for b in bset]
                    V("pool", lambda e: e.tensor_tensor(tBs[:], k2B, vl[:, :, tl:tl + 1].to_broadcast([128, 8, 64]), ALU.mult), [bset[3], vl], [tBs])
                    V("dve", lambda e: e.tensor_tensor(tA[:], St[:], kkB, ALU.mult), [St, bset[1]], [tA])
                    V("dve", lambda e: e.tensor_reduce(sa[:], tA[:], AX.X, ALU.add), [tA], [sa])
                    V("dve", lambda e: e.tensor_tensor(St[:], St[:], wB, ALU.mult), [St, bset[0]], [St])
                    V("dve", lambda e: e.tensor_tensor(tA[:], kkanB, b3(sa), ALU.mult), [bset[2], sa], [tA])
                    V("dve", lambda e: e.tensor_tensor(St[:], St[:], tA[:], ALU.add), [St, tA], [St])
                    V("dve", lambda e: e.tensor_tensor(St[:], St[:], tBs[:], ALU.add), [St, tBs], [St])
                    V("dve", lambda e: e.tensor_tensor(tA[:], St[:], rB, ALU.mult), [St, bset[4]], [tA])
                    V("dve", lambda e: e.tensor_reduce(Y[:, :, tl], tA[:], AX.X, ALU.add), [tA], [Y])
                Yf = Y[:].rearrange("p h t -> p (h t)")
                V("pe", lambda e: e.matmul(pq[0][:], blkm[:], Yf, start=True, stop=True), [blkm, Y], [pq[0]])
                V("dve", lambda e: e.tensor_tensor(yc[:].rearrange("p h t -> p (h t)"), Yf, pq[0][:], ALU.subtract), [Y, pq[0]], [yc])
                V("act", lambda e: e.activation(sq[:], yc[:].rearrange("p h t -> p (h t)"), AF.Square), [yc], [sq])
                V("pe", lambda e: e.matmul(pq[1][:], blkm[:], sq[:], start=True, stop=True), [blkm, sq], [pq[1]])
                V("act", lambda e: e.activation(rstd[:], pq[1][:], AF.Sqrt, bias=64e-5, scale=1.0), [pq[1]], [rstd])
                V("dve", lambda e: e.reciprocal(rstd[:], rstd[:]), [rstd], [rstd])
                V("dve", lambda e: e.tensor_tensor(yc[:].rearrange("p h t -> p (h t)"), yc[:].rearrange("p h t -> p (h t)"), rstd[:], ALU.mult), [yc, rstd], [yc])
                V("dve", lambda e: e.tensor_tensor(yc[:], yc[:], b3(lng), ALU.mult), [yc, lng], [yc])
                V("dve", lambda e: e.tensor_tensor(yc[:], yc[:], b3(lnb), ALU.add), [yc, lnb], [yc])
                for h8 in range(8):
                    rhh = rh[h8 % 2]
                    V("pool", lambda e: e.tensor_scalar(rhh[:], i2[:], s2c[:, h8:h8 + 1], None, op0=ALU.mult), [i2, s2c], [rhh])
                    V("pe", lambda e: e.matmul(pq[0][:, h8 * 64:(h8 + 1) * 64], blk[:], rhh[:], start=True, stop=True), [blk, rhh], [pq[0]])
                V("dve", lambda e: e.tensor_tensor(tA[:].rearrange("p h t -> p (h t)"), pq[0][:], vl[:].rearrange("p h t -> p (h t)"), ALU.mult), [pq[0], vl], [tA])
                V("dve", lambda e: e.tensor_tensor(yc[:], yc[:], tA[:], ALU.add), [yc, tA], [yc])
                V("act", lambda e: e.activation(gl[:], gl[:], AF.Silu), [gl], [gl])
                ybc = yb[c % 2]
                V("dve", lambda e: e.tensor_tensor(ybc[:], yc[:], gl[:], ALU.mult), [yc, gl], [ybc])
                P.dma("sp", [(ysT.t[2048 + hf * 512:2048 + (hf + 1) * 512, t0:t0 + 64].rearrange("(h v) t -> v h t", v=64), ybc[hf * 64:(hf + 1) * 64, :, :]) for hf in range(2)],
                      reads=[ybc], writes=[ysT])


    def phase_rwp2(l):
        with P.scope():
            c0 = TM_COLS["rw_r"]
            NCc = 3264
            mub = P.sb([128, NCc], F32, "rmub")
            mu = I["rwkv_mu"]
            P.dma("sp", [(mub[:], mu[l:l + 1, 0:NCc].to_broadcast([128, NCc]))], reads=[mu], writes=[mub])

            def bc(name, n=1024):
                t = P.sb([128, n], F32, "rb" + name)
                src = I[name]
                if name == "rwkv_r_k":
                    ap = src.t[l:l + 1, :, :].rearrange("a h k -> a (h k)").to_broadcast([128, n])
                else:
                    ap = src[l:l + 1, :].to_broadcast([128, n])
                P.dma("sp", [(t[:], ap)], reads=[src], writes=[t])
                return t
            kkb, kab, w0b, a0b, rkb = bc("rwkv_k_k"), bc("rwkv_k_a"), bc("rwkv_w0"), bc("rwkv_a0"), bc("rwkv_r_k")
            omka = P.sb([128, 1024], F32, "romka")
            V("dve", lambda e: e.tensor_scalar(omka[:], kab[:], -1.0, 1.0, op0=ALU.mult, op1=ALU.add), [kab], [omka])
            ww2 = P.sb([96, 1024], F32, "rww2")
            wa2 = P.sb([96, 1024], F32, "rwa2")
            lmat = P.sb([128, 128], F32, "rlmat")
            umat = P.sb([128, 128], F32, "rumat")
            ind = P.sb([128, 4], F32, "rind")
            P.dma("sp", [(ww2[:], I["rwkv_w_w2"].t[l, :, :])], reads=[I["rwkv_w_w2"]], writes=[ww2])
            P.dma("sp", [(wa2[:], I["rwkv_w_a2"].t[l, :, :])], reads=[I["rwkv_w_a2"]], writes=[wa2])
            P.dma("sp", [(lmat[:], I["c_lmat32"][:, :])], reads=[I["c_lmat32"]], writes=[lmat])
            P.dma("sp", [(umat[:], I["c_umat32"][:, :])], reads=[I["c_umat32"]], writes=[umat])
            P.dma("sp", [(ind[:], I["c_ind32"][:, :])], reads=[I["c_ind32"]], writes=[ind])
            cur = [P.sb([128, NCc], F32, "rcur") for _ in range(2)]
            prv = P.sb([128, NCc], F32, "rprv")
            dd = P.sb([128, NCc], F32, "rdd")
            th = P.sb([128, 96], F32, "rth")
            thT = P.sb([96, 128], F32, "rthT")
            alT = P.sb([96, 128], F32, "ralT")
            pz = [P.ps([128, 2, 512], F32, "rpz") for _ in range(2)]
            ptr = [P.ps([64, 4, 128], F32, "rptr") for _ in range(2)]
            ptT2 = P.ps([96, 2, 128], F32, "rptT")
            pdec = P.ps([64, 16, 4], F32, "rpdec")
            zs = P.sb([128, 1024], F32, "rzs")
            aa = P.sb([128, 1024], F32, "raa")
            kk = P.sb([128, 1024], F32, "rkk")
            kkn = P.sb([128, 1024], F32, "rkkn")
            kkan = P.sb([128, 1024], F32, "rkkan")
            k2 = P.sb([128, 1024], F32, "rk2")
            ssq = P.sb([128, 16], F32, "rssq")
            tt = P.sb([128, 1024], F32, "rtt")
            E1 = P.sb([128, 1024], F32, "rE1")
            Q = P.sb([128, 4, 1024], F32, "rQ")
            o3 = [P.sb([128, 3, 1024], F32, "ro3") for _ in range(2)]
            bon = [P.sb([128, 16], F32, "rbon") for _ in range(2)]
            XTs = P.sb([64, 16, 4, 128], F32, "rXTs")
            decs = P.sb([64, 16, 4], F32, "rdecs")

            def loadt(i):
                P.dma("sp", [(cur[i % 2][:], tm[i * 128:(i + 1) * 128, c0:c0 + NCc])], reads=[tm], writes=[cur[i % 2]])
            loadt(0)
            for i in range(NT):
                if i + 1 < NT:
                    loadt(i + 1)
                cu, o3i, bn = cur[i % 2], o3[i % 2], bon[i % 2]
                if i == 0:
                    V("pool", lambda e: e.memset(prv[0:1, :], 0.0), [], [prv])
                    P.dma("sp", [(prv[1:128, :], tm[0:127, c0:c0 + NCc])], reads=[tm], writes=[prv])
                else:
                    P.dma("sp", [(prv[:], tm[i * 128 - 1:i * 128 + 127, c0:c0 + NCc])], reads=[tm], writes=[prv])
                V("dve", lambda e: e.tensor_tensor(dd[:], prv[:], cu[:], ALU.subtract), [prv, cu], [dd])
                V("pool", lambda e: e.tensor_tensor(dd[:], dd[:], mub[:], ALU.mult), [dd, mub], [dd])
                V("dve", lambda e: e.tensor_tensor(cu[:], cu[:], dd[:], ALU.add), [cu, dd], [cu])
                rr_, k_, v_ = cu[:, 0:1024], cu[:, 1024:2048], cu[:, 2048:3072]
                V("act", lambda e: e.activation(th[:], cu[:, 3072:3168], AF.Tanh), [cu], [th])
                V("pe", lambda e: e.transpose(ptT2[:, 0, :], th[:], ident_f[:]), [th, ident_f], [ptT2])
                V("pe", lambda e: e.transpose(ptT2[:, 1, :], cu[:, 3168:3264], ident_f[:]), [cu, ident_f], [ptT2])
                V("act", lambda e: e.copy(thT[:], ptT2[:, 0, :]), [ptT2], [thT])
                V("dve", lambda e: e.tensor_copy(alT[:], ptT2[:, 1, :]), [ptT2], [alT])
                for n2 in range(2):
                    V("pe", lambda e: e.matmul(pz[0][:, n2, :], thT[:], ww2[:, n2 * 512:(n2 + 1) * 512], start=True, stop=True), [thT, ww2], [pz[0]])
                    V("pe", lambda e: e.matmul(pz[1][:, n2, :], alT[:], wa2[:, n2 * 512:(n2 + 1) * 512], start=True, stop=True), [alT, wa2], [pz[1]])
                V("dve", lambda e: e.tensor_tensor(zs[:], pz[0][:].rearrange("p a b -> p (a b)"), w0b[:], ALU.add), [pz[0], w0b], [zs])
                V("act", lambda e: e.activation(zs[:], zs[:], AF.Sigmoid), [zs], [zs])
                V("dve", lambda e: e.tensor_scalar(zs[:], zs[:], -0.606531, None, op0=ALU.mult), [zs], [zs])
                V("dve", lambda e: e.tensor_tensor(aa[:], pz[1][:].rearrange("p a b -> p (a b)"), a0b[:], ALU.add), [pz[1], a0b], [aa])
                V("act", lambda e: e.activation(aa[:], aa[:], AF.Sigmoid), [aa], [aa])
                V("dve", lambda e: e.tensor_tensor(kk[:], k_, kkb[:], ALU.mult), [cu, kkb], [kk])
                V("act", lambda e: e.activation(tt[:], kk[:], AF.Square), [kk], [tt])
                V("dve", lambda e: e.tensor_reduce(ssq[:], tt[:].rearrange("p (h k) -> p h k", k=64), AX.X, ALU.add), [tt], [ssq])
                V("act", lambda e: e.activation(ssq[:], ssq[:], AF.Sqrt, bias=1e-12, scale=1.0), [ssq], [ssq])
                V("dve", lambda e: e.reciprocal(ssq[:], ssq[:]), [ssq], [ssq])
                V("dve", lambda e: e.tensor_tensor(kkn[:].rearrange("p (h k) -> p h k", k=64), kk[:].rearrange("p (h k) -> p h k", k=64),
                                                   ssq[:, :].unsqueeze(2).to_broadcast([128, 16, 64]), ALU.mult), [kk, ssq], [kkn])
                V("dve", lambda e: e.scalar_tensor_tensor(kkan[:], kkn[:], -1.0, aa[:], op0=ALU.mult, op1=ALU.mult), [kkn, aa], [kkan])
                V("pool", lambda e: e.tensor_tensor(tt[:], aa[:], kab[:], ALU.mult), [aa, kab], [tt])
                V("pool", lambda e: e.tensor_tensor(tt[:], tt[:], omka[:], ALU.add), [tt, omka], [tt])
                V("dve", lambda e: e.tensor_tensor(k2[:], k_, tt[:], ALU.mult), [cu, tt], [k2])
                V("pool", lambda e: e.tensor_tensor(tt[:], rr_, rkb[:], ALU.mult), [cu, rkb], [tt])
                V("dve", lambda e: e.tensor_tensor(tt[:], tt[:], k2[:], ALU.mult), [tt, k2], [tt])
                V("dve", lambda e: e.tensor_reduce(bn[:], tt[:].rearrange("p (h k) -> p h k", k=64), AX.X, ALU.add), [tt], [bn])
                for n2 in range(2):
                    V("pe", lambda e: e.matmul(pz[0][:, n2, :], lmat[:], zs[:, n2 * 512:(n2 + 1) * 512], start=True, stop=True), [lmat, zs], [pz[0]])
                    V("pe", lambda e: e.matmul(pz[1][:, n2, :], umat[:], zs[:, n2 * 512:(n2 + 1) * 512], start=True, stop=True), [umat, zs], [pz[1]])
                cwf = pz[0][:].rearrange("p a b -> p (a b)")
                gf = pz[1][:].rearrange("p a b -> p (a b)")
                for n2 in range(2):
                    V("act", lambda e: e.activation(E1[:, n2 * 512:(n2 + 1) * 512], pz[0][:, n2, :], AF.Exp), [pz[0]], [E1])
                V("dve", lambda e: e.tensor_tensor(Q[:, 3, :], rr_, E1[:], ALU.mult), [cu, E1], [Q])
                for n2 in range(2):
                    V("act", lambda e: e.activation(E1[:, n2 * 512:(n2 + 1) * 512], pz[0][:, n2, :], AF.Exp, scale=-1.0), [pz[0]], [E1])
                V("dve", lambda e: e.tensor_tensor(Q[:, 0, :], kkan[:], E1[:], ALU.mult), [kkan, E1], [Q])
                V("pool", lambda e: e.tensor_tensor(Q[:, 1, :], k2[:], E1[:], ALU.mult), [k2, E1], [Q])
                V("dve", lambda e: e.tensor_tensor(tt[:], cwf, zs[:], ALU.subtract), [pz[0], zs], [tt])
                V("act", lambda e: e.activation(E1[:], tt[:], AF.Exp), [tt], [E1])
                V("dve", lambda e: e.tensor_tensor(Q[:, 2, :], kkn[:], E1[:], ALU.mult), [kkn, E1], [Q])
                for n2 in range(2):
                    V("act", lambda e: e.activation(E1[:, n2 * 512:(n2 + 1) * 512], pz[1][:, n2, :], AF.Exp), [pz[1]], [E1])
                V("dve", lambda e: e.tensor_tensor(o3i[:, 0, :], kkan[:], E1[:], ALU.mult), [kkan, E1], [o3i])
                V("pool", lambda e: e.tensor_tensor(o3i[:, 1, :], k2[:], E1[:], ALU.mult), [k2, E1], [o3i])
                V("pool", lambda e: e.tensor_copy(o3i[:, 2, :], v_), [cu], [o3i])
                for h in range(16):
                    V("pe", lambda e: e.matmul(pdec[:, h, :], zs[:, h * 64:(h + 1) * 64], ind[:], start=True, stop=True), [zs, ind], [pdec])
                V("act", lambda e: e.activation(decs[:], pdec[:], AF.Exp), [pdec], [decs])
                P.dma("sp", [(rwdec[:, :, i * 4:(i + 1) * 4], decs[:])], reads=[decs], writes=[rwdec])
                for h in range(16):
                    pt = ptr[h % 2]
                    for q in range(4):
                        V("pe", lambda e: e.transpose(pt[:, q, :], Q[:, q, h * 64:(h + 1) * 64], ident_f[:]), [Q, ident_f], [pt])
                    evac(h, XTs[:, h, :, :], pt[:], [pt], [XTs])
                P.dma("sp", [(rwxt.t[:, :, i, c4 * 128:(c4 + 1) * 128].rearrange("k h (q t) -> k h q t", t=32), XTs[:, :, :, c4 * 32:(c4 + 1) * 32]) for c4 in range(4)],
                      reads=[XTs], writes=[rwxt])
                P.dma("sp", [(rwtm2[i * 128:(i + 1) * 128, :, :], o3i[:])], reads=[o3i], writes=[rwtm2])
                P.dma("sp", [(rwbon[i * 128:(i + 1) * 128, :], bn[:])], reads=[bn], writes=[rwbon])

    def phase_rws2(l):
        with P.scope():
            mtp = P.sb([64, 64], F32, "wmtp")
            mn = P.sb([32, 32], F32, "wmn")
            ones64 = P.sb([64, 64], F32, "wones")
            onesm = P.sb([64, 64], F32, "wonesm")
            P.dma("sp", [(mtp[:], I["c_m32tp"][:, :])], reads=[I["c_m32tp"]], writes=[mtp])
            P.dma("sp", [(mn[:], I["c_m32n"][:, :])], reads=[I["c_m32n"]], writes=[mn])
            V("pool", lambda e: e.memset(ones64[:], 1.0), [], [ones64])
            V("pool", lambda e: e.memset(onesm[:], 1.0 / 64.0), [], [onesm])

            def hv(name, off=0):
                t = P.sb([64, 16], F32, "w" + name)
                src = I[name]
                P.dma("sp", [(t[:], src.t[l, off:off + 1024].rearrange("(h v) -> v h", v=64))], reads=[src], writes=[t], allow_slow_non_contiguous=True)
                return t
            lng, lnb = hv("rwkv_lnx_g"), hv("rwkv_lnx_b")
            muv, mug = hv("rwkv_mu", 2048), hv("rwkv_mu", 3264)
            decall = P.sb([64, 16, NC32], F32, "wdec")
            P.dma("sp", [(decall[:], rwdec[:, :, :])], reads=[rwdec], writes=[decall])
            H = P.sb([64, 16, 64], F32, "wH")
            V("pool", lambda e: e.memset(H[:], 0.0), [], [H])
            XT = [P.sb([64, 16, 4, 4, 32], F32, "wXT") for _ in range(2)]
            BK = [P.sb([64, 16, 64], F32, "wBK") for _ in range(2)]
            UV = [P.sb([64, 16, 64], F32, "wUV") for _ in range(2)]
            MT = P.sb([64, 16, 64], F32, "wMT")
            Pm = [P.sb([32, 16, 32], F32, "wPm") for _ in range(2)]
            PTm = [P.sb([32, 16, 32], F32, "wPTm") for _ in range(2)]
            Zs = P.sb([32, 16, 64], F32, "wZs")
            pTP = P.ps([64, 16, 64], F32, "wpTP")
            pZ = P.ps([64, 16, 64], F32, "wpZ")
            pN = P.ps([64, 16, 32], F32, "wpN")
            pP2 = P.ps([32, 16, 32], F32, "wpP2")
            pP2T = P.ps([32, 16, 32], F32, "wpP2T")
            Yt = P.sb([64, 16, 64], F32, "wYt")
            vch = [P.sb([64, 16, 65], F32, "wvch") for _ in range(2)]
            gch = [P.sb([64, 16, 65], F32, "wgch") for _ in range(2)]
            s2 = [P.sb([64, 16], F32, "ws2") for _ in range(2)]
            vl = P.sb([64, 16, 64], F32, "wvl")
            gl = P.sb([64, 16, 64], F32, "wgl")
            yc = P.sb([64, 16, 64], F32, "wyc")
            sq = P.sb([64, 16, 64], F32, "wsq")
            rstd = P.sb([64, 16, 64], F32, "wrstd")
            tA = P.sb([64, 16, 64], F32, "wtA")
            rh = [P.sb([64, 64], F32, "wrh") for _ in range(2)]
            yb = [P.sb([64, 16, 64], BF16, "wyb") for _ in range(2)]
            rv, rg = FM_ROWS["rw_v"], FM_ROWS["rw_gate"]

            def b3(t2):
                return t2[:, :].unsqueeze(2).to_broadcast([64, 16, 64])

            def fl(b, np_=64):
                return b[0:np_, :, :].rearrange("p h t -> p (h t)")

            def loadtile(i):
                P.dma("sp", [(XT[i % 2][:].rearrange("k h c q t -> k h (c q t)"), rwxt.t[:, :, i, :])], reads=[rwxt], writes=[XT[i % 2]])

            def loadchunk(c):
                t0 = c * 32
                P.dma("sp", [(BK[c % 2][0:32, :, :].rearrange("p h k -> p (h k)"), rwtm2.t[t0:t0 + 32, 0, :]),
                             (BK[c % 2][32:64, :, :].rearrange("p h k -> p (h k)"), rwtm2.t[t0:t0 + 32, 1, :])], reads=[rwtm2], writes=[BK[c % 2]])
                P.dma("sp", [(UV[c % 2][32:64, :, :].rearrange("p h k -> p (h k)"), rwtm2.t[t0:t0 + 32, 2, :])], reads=[rwtm2], writes=[UV[c % 2]])

            def loadepi(g):
                t0 = g * 64
                P.dma("sp", [(vch[g % 2][:], fm.t[rv:rv + 1024, PAD + t0 - 1:PAD + t0 + 64].rearrange("(h v) t -> v h t", v=64))], reads=[fm], writes=[vch[g % 2]])
                P.dma("sp", [(gch[g % 2][:], fm.t[rg:rg + 1024, PAD + t0 - 1:PAD + t0 + 64].rearrange("(h v) t -> v h t", v=64))], reads=[fm], writes=[gch[g % 2]])
                P.dma("sp", [(s2[g % 2][:], rwbon[t0:t0 + 64, :])], reads=[rwbon], writes=[s2[g % 2]])
            loadtile(0)
            loadchunk(0)
            loadepi(0)
            ke = 0
            for i in range(NT):
                if i + 1 < NT:
                    loadtile(i + 1)
                X = XT[i % 2]
                for cc in range(4):
                    c = i * 4 + cc
                    if c + 1 < NC32:
                        loadchunk(c + 1)
                    BKc, UVc = BK[c % 2], UV[c % 2]
                    for h in range(16):
                        V("pe", lambda e: e.matmul(pTP[:, h, :], X[:, h, cc, 0:2, :].rearrange("k q t -> k (q t)"), X[:, h, cc, 2:4, :].rearrange("k q t -> k (q t)"),
                                                   start=True, stop=True), [X], [pTP])
                    for h in range(16):
                        V("pe", lambda e: e.matmul(pN[0:32, h, :], X[:, h, cc, 2, :], X[:, h, cc, 0, :], start=True, stop=True), [X], [pN])
                    V("dve", lambda e: e.tensor_tensor(MT[:], pTP[:], mtp[:, :].unsqueeze(1).to_broadcast([64, 16, 64]), ALU.mult), [pTP, mtp], [MT])
                    V("dve", lambda e: e.tensor_tensor(Pm[0][:], pN[0:32, :, :], mn[:, :].unsqueeze(1).to_broadcast([32, 16, 32]), ALU.mult), [pN, mn], [Pm[0]])
                    for h in range(16):
                        V("pe", lambda e: e.matmul(pZ[0:32, h, :], X[:, h, cc, 2, :], H[:, h, :], start=True, stop=False), [X, H], [pZ])
                        V("pe", lambda e: e.matmul(pZ[0:32, h, :], MT[32:64, h, 0:32], UVc[32:64, h, :], start=False, stop=True), [MT, UVc], [pZ])
                    for h2 in range(2):
                        V("act", lambda e: e.copy(Zs[:, h2 * 8:(h2 + 1) * 8, :], pZ[0:32, h2 * 8:(h2 + 1) * 8, :]), [pZ], [Zs])
                    for lv in range(5):
                        if lv == 0:
                            PTb, PTv = MT, (lambda h: MT[0:32, h, 0:32])
                        else:
                            PTb, PTv = PTm[lv % 2], (lambda h, b=PTm[lv % 2]: b[:, h, :])
                        Pb = Pm[lv % 2]
                        for h in range(16):
                            V("pe", lambda e: e.matmul(pZ[0:32, h, :], PTv(h), Zs[:, h, :], start=True, stop=True), [PTb, Zs], [pZ])
                        if lv < 4:
                            for h in range(16):
                                V("pe", lambda e: e.matmul(pP2[:, h, :], PTv(h), Pb[:, h, :], start=True, stop=True), [PTb, Pb], [pP2])
                                V("pe", lambda e: e.matmul(pP2T[:, h, :], Pb[:, h, :], PTv(h), start=True, stop=True), [PTb, Pb], [pP2T])
                            V("dve", lambda e: e.tensor_tensor(Zs[:], Zs[:], pZ[0:32, :, :], ALU.add), [Zs, pZ], [Zs])
                            V("act", lambda e: e.copy(Pm[(lv + 1) % 2][:], pP2[:]), [pP2], [Pm[(lv + 1) % 2]])
                            V("dve", lambda e: e.tensor_copy(PTm[(lv + 1) % 2][:], pP2T[:]), [pP2T], [PTm[(lv + 1) % 2]])
                        else:
                            V("dve", lambda e: e.tensor_tensor(UVc[0:32, :, :], Zs[:], pZ[0:32, :, :], ALU.add), [Zs, pZ], [UVc])
                    for h in range(16):
                        V("pe", lambda e: e.matmul(pN[:, h, :], H[:, h, :], X[:, h, cc, 3, :], start=True, stop=False), [H, X], [pN])
                        V("pe", lambda e: e.matmul(pN[:, h, :], UVc[:, h, :], MT[:, h, 32:64], start=False, stop=True), [UVc, MT], [pN])
                    V("act", lambda e: e.copy(Yt[:, :, (cc % 2) * 32:(cc % 2) * 32 + 32], pN[:]), [pN], [Yt])
                    for h in range(16):
                        V("pe", lambda e: e.matmul(pTP[:, h, :], BKc[:, h, :], UVc[:, h, :], start=True, stop=True), [BKc, UVc], [pTP])
                    V("dve", lambda e: e.tensor_tensor(H[:], H[:], decall[:, :, c:c + 1].to_broadcast([64, 16, 64]), ALU.mult), [H, decall], [H])
                    V("dve", lambda e: e.tensor_tensor(H[:], H[:], pTP[:], ALU.add), [H, pTP], [H])
                    if cc % 2 == 1:
                        g = c // 2
                        t0 = g * 64
                        if g + 1 < NCH:
                            loadepi(g + 1)
                        vc, gc, s2c = vch[g % 2], gch[g % 2], s2[g % 2]
                        for (src, dst, mu_) in ((vc, vl, muv), (gc, gl, mug)):
                            V("pool", lambda e: e.tensor_tensor(dst[:], src[:, :, 0:64], src[:, :, 1:65], ALU.subtract), [src], [dst])
                            V("pool", lambda e: e.tensor_tensor(dst[:], dst[:], b3(mu_), ALU.mult), [dst, mu_], [dst])
                            V("pool", lambda e: e.tensor_tensor(dst[:], dst[:], src[:, :, 1:65], ALU.add), [dst, src], [dst])
                        for n2 in range(2):
                            V("pe", lambda e: e.matmul(fl(pTP)[:, n2 * 512:(n2 + 1) * 512], onesm[:], fl(Yt)[:, n2 * 512:(n2 + 1) * 512], start=True, stop=True), [onesm, Yt], [pTP])
                        V("dve", lambda e: e.tensor_tensor(yc[:], Yt[:], pTP[:], ALU.subtract), [Yt, pTP], [yc])
                        V("act", lambda e: e.activation(sq[:], yc[:], AF.Square), [yc], [sq])
                        for n2 in range(2):
                            V("pe", lambda e: e.matmul(fl(pZ)[:, n2 * 512:(n2 + 1) * 512], onesm[:], fl(sq)[:, n2 * 512:(n2 + 1) * 512], start=True, stop=True), [onesm, sq], [pZ])
                        for h2 in range(2):
                            V("act", lambda e: e.activation(rstd[:, h2 * 8:(h2 + 1) * 8, :], pZ[:, h2 * 8:(h2 + 1) * 8, :], AF.Sqrt, bias=64e-5, scale=1.0), [pZ], [rstd])
                        V("dve", lambda e: e.reciprocal(rstd[:], rstd[:]), [rstd], [rstd])
                        V("dve", lambda e: e.tensor_tensor(yc[:], yc[:], rstd[:], ALU.mult), [yc, rstd], [yc])
                        V("dve", lambda e: e.tensor_tensor(yc[:], yc[:], b3(lng), ALU.mult), [yc, lng], [yc])
                        V("dve", lambda e: e.tensor_tensor(yc[:], yc[:], b3(lnb), ALU.add), [yc, lnb], [yc])
                        for h in range(16):
                            rhh = rh[h % 2]
                            V("pool", lambda e: e.tensor_scalar(rhh[:], ident_f[0:64, 0:64], s2c[:, h:h + 1], None, op0=ALU.mult), [ident_f, s2c], [rhh])
                            V("pe", lambda e: e.matmul(pTP[:, h, :], ones64[:], rhh[:], start=True, stop=True), [ones64, rhh], [pTP])
                        V("dve", lambda e: e.tensor_tensor(tA[:], pTP[:], vl[:], ALU.mult), [pTP, vl], [tA])
                        V("dve", lambda e: e.tensor_tensor(yc[:], yc[:], tA[:], ALU.add), [yc, tA], [yc])
                        V("act", lambda e: e.activation(gl[:], gl[:], AF.Silu), [gl], [gl])
                        ybc = yb[g % 2]
                        V("dve", lambda e: e.tensor_tensor(ybc[:], yc[:], gl[:], ALU.mult), [yc, gl], [ybc])
                        P.dma("sp", [(ysT.t[2048:3072, t0:t0 + 64].rearrange("(h v) t -> v h t", v=64), ybc[:])], reads=[ybc], writes=[ysT])

    PH = {"tables": phase_tables, "proj": phase_proj, "rope": phase_rope, "gla": phase_gla, "sgu": phase_sgu,
          "m1": phase_m1, "m2": phase_m2, "dsa1": phase_dsa1, "dsa2": phase_dsa2, "rwp": phase_rwp, "rws": phase_rws, "rwp2": phase_rwp2, "rws2": phase_rws2}
    plan = dbg.get("plan")
    if plan is None:
        plan = [("tables",)]
        for l in range(L):
            plan += [("proj", l, l), ("rope", l), ("gla", l), ("dsa1", l), ("dsa2", l), ("rwp2", l), ("rws2", l), ("sgu", l), ("m1", l), ("m2", l, l, l + 1)]
    for st in plan:
        nm = st[0]
        if nm == "tables":
            phase_tables()
        elif nm == "proj":
            phase_proj(st[1], xcur[st[2]])
        elif nm == "m2":
            phase_m2(st[1], xcur[st[2]], xcur[st[3]])
        else:
            PH[nm](st[1])
    P.barrier()
    es.close()
    global LAST_P
    LAST_P = P
    return nc


def make_inputs(inputs, b):
    m = {"x": np.ascontiguousarray(inputs["x"][b]), "pos": np.ascontiguousarray(inputs["positions"][b:b + 1]).astype(np.int32)}
    for k, v in inputs.items():
        if k in ("x", "positions"):
            continue
        m[k] = np.ascontiguousarray(v)
    m.update(host_consts())
    return m


def kernel(**inputs):
    nc = build_nc()
    in_maps = [make_inputs(inputs, c % 4) for c in range(8)]
    res = run_bass_kernel_spmd(nc, in_maps, core_ids=list(range(8)))
    return np.stack([res.results[c]["y"] for c in range(4)], axis=0).astype(np.float32)
```

```python
import numpy as np
from contextlib import ExitStack, contextmanager
import concourse.bass as bass
import concourse.mybir as mybir
from concourse.bass_utils import run_bass_kernel_spmd

F32 = mybir.dt.float32
BF16 = mybir.dt.bfloat16
I32 = mybir.dt.int32
ALU = mybir.AluOpType
AF = mybir.ActivationFunctionType
AX = mybir.AxisListType

D = 2048
S = 4096
L = 2
NIN = 23320
PAD = 64
TP = S + PAD
NEG = -1.0e30


class Buf:
    __slots__ = ("t", "w", "r", "sem", "semkey", "semv", "name", "acc", "wl", "psum")

    def __init__(self, t, name, acc=False):
        self.t = t
        self.name = name
        self.acc = acc
        self.psum = False
        self.wl = []
        self.w = None
        self.r = []
        self.sem = None
        self.semkey = None
        self.semv = 0

    def __getitem__(self, k):
        return self.t[k]


class Prog:
    def __init__(self, nc, es):
        self.nc = nc
        self.es = es
        self.eng = {"pe": nc.tensor, "act": nc.scalar, "dve": nc.vector, "pool": nc.gpsimd, "sp": nc.sync}
        self.esem = {}
        self.ecnt = {}
        self.ekey = {}
        self.eepoch = {}
        for e in ("pe", "act", "dve", "pool"):
            self.esem[e] = es.enter_context(nc.semaphore("e_" + e))
            self.ecnt[e] = 0
            self.eepoch[e] = 0
            self.ekey[e] = e + "#0"
        self.nwait = 0
        self.seen = {e: {} for e in self.eng}
        self.nbuf = 0
        self.scopes = [es]
        self.scope_bufs = [[]]
        self.sempool = []
        self.allsems = {}
        self.nsem = 0
        self.ninst = 0

    @contextmanager
    def scope(self):
        es = ExitStack()
        self.scopes.append(es)
        self.scope_bufs.append([])
        yield
        self.barrier()
        for b in self.scope_bufs.pop():
            if b.sem is not None:
                self.sempool.append((b.sem, b.semkey, b.semv))
        self.scopes.pop()
        es.close()

    def sb(self, shape, dt, name=None):
        self.nbuf += 1
        name = (name or "sb") + "_%d" % self.nbuf
        t = self.scopes[-1].enter_context(self.nc.sbuf_tensor(name, list(shape), dt))
        b = Buf(t, name)
        self.scope_bufs[-1].append(b)
        return b

    def ps(self, shape, dt, name=None):
        self.nbuf += 1
        name = (name or "ps") + "_%d" % self.nbuf
        t = self.scopes[-1].enter_context(self.nc.psum_tensor(name, list(shape), dt))
        b = Buf(t, name)
        b.psum = True
        self.scope_bufs[-1].append(b)
        return b

    def dram(self, name, shape, dt, kind="Internal"):
        t = self.nc.dram_tensor(name, list(shape), dt, kind=kind).ap()
        return Buf(t, name, acc=True)

    def _wait(self, e, ev, raw=True):
        if ev is None:
            return
        key, sem, val = ev
        if e == "pe" and key.startswith("pe#"):
            return
        if self.seen[e].get(key, 0) >= val:
            return
        self.eng[e].wait_ge(sem, val)
        self.nwait += 1
        self.seen[e][key] = val

    def _deps(self, e, reads, writes):
        for b in reads:
            self._wait(e, b.w, True)
            for ev in b.wl:
                self._wait(e, ev, True)
            if b.psum:
                for ev in b.r:
                    if not ev[0].startswith(e + "#"):
                        self._wait(e, ev, False)
        for b in writes:
            if not b.acc:
                self._wait(e, b.w, False)
            for ev in b.r:
                self._wait(e, ev, False)

    @staticmethod
    def _compact(evs):
        best = {}
        for ev in evs:
            k = ev[0]
            if k not in best or best[k][2] < ev[2]:
                best[k] = ev
        return list(best.values())

    def _record(self, ev, reads, writes):
        for b in writes:
            if b.acc:
                b.wl.append(ev)
                if len(b.wl) > 16:
                    b.wl = self._compact(b.wl)
            else:
                b.w = ev
            b.r = []
        for b in reads:
            if b not in writes:
                b.r.append(ev)
                if len(b.r) > 16:
                    b.r = self._compact(b.r)

    def op(self, e, fn, reads=(), writes=()):
        self._deps(e, reads, writes)
        ins = fn(self.eng[e])
        self.ecnt[e] += 1
        self.ninst += 1
        ins.then_inc(self.esem[e], 1)
        self._record((self.ekey[e], self.esem[e], self.ecnt[e]), reads, writes)
        return ins

    def _getsem(self, b):
        if b.sem is None:
            if self.sempool:
                b.sem, b.semkey, b.semv = self.sempool.pop()
            else:
                self.nsem += 1
                b.semkey = "d%d" % self.nsem
                b.sem = self.es.enter_context(self.nc.semaphore(b.semkey))
                b.semv = 0
            self.allsems[b.semkey] = b

    def dma(self, q, pairs, reads=(), writes=(), sembuf=None, **kw):
        self._deps(q, reads, writes)
        sb = sembuf
        if sb is None:
            cands = [b for b in list(writes) + list(reads) if not b.acc]
            sb = cands[0] if cands else (writes[0] if writes else reads[0])
        self._getsem(sb)
        self._wait(q, (sb.semkey, sb.sem, sb.semv))
        for (o, i) in pairs:
            self.eng[q].dma_start(out=o, in_=i, **kw).then_inc(sb.sem, 16)
            sb.semv += 16
            self.ninst += 1
        self._record((sb.semkey, sb.sem, sb.semv), reads, writes)

    def barrier(self):
        evs = [(self.ekey[c], self.esem[c], self.ecnt[c], c) for c in self.esem]
        for k, b in self.allsems.items():
            evs.append((k, b.sem, b.semv, None))
        for (sem, key, v) in self.sempool:
            evs.append((key, sem, v, None))
        for e in self.eng:
            for ev in evs:
                if ev[3] != e and ev[2] > 0:
                    if self.seen[e].get(ev[0], 0) < ev[2]:
                        self.eng[e].wait_ge(ev[1], ev[2])
                        self.nwait += 1
                        self.seen[e][ev[0]] = ev[2]
        for c in list(self.esem):
            if self.ecnt[c] > 12000:
                self.eepoch[c] += 1
                self.ekey[c] = "%s#%d" % (c, self.eepoch[c])
                self.esem[c] = self.es.enter_context(self.nc.semaphore("e_%s_%d" % (c, self.eepoch[c])))
                self.ecnt[c] = 0


SEGS = [
    (0, 512, "F", "gla_q"), (512, 512, "T", "gla_k"), (1024, 1024, "T", "gla_v"),
    (2048, 16, "F", "gla_g"), (2064, 1024, "F", "gla_gate"),
    (3088, 1024, "F", "dsa_q"), (4112, 1024, "F", "dsa_k"), (5136, 1024, "T", "dsa_v"),
    (6160, 512, "F", "dsa_iq"), (6672, 64, "F", "dsa_ik"), (6736, 8, "T", "dsa_iw"),
    (6744, 1024, "F", "dsa_gate"),
    (7768, 1024, "T", "rw_r"), (8792, 1024, "T", "rw_k"), (9816, 1024, "T", "rw_vT"), (9816, 1024, "F", "rw_v"),
    (10840, 96, "T", "rw_wlr"), (10936, 96, "T", "rw_alr"), (11032, 1024, "F", "rw_gate"),
    (12056, 1024, "F", "sgu_u"), (13080, 1024, "T", "sgu_zv"), (14104, 1024, "F", "sgu_gate"),
    (15128, 8192, "H", "pm"),
]
FM_ROWS = {}
TM_COLS = {}
_r = 0
_c = 0
for (_c0, _n, _m, _nm) in SEGS:
    if _m == "F":
        FM_ROWS[_nm] = _r
        _r += _n
    elif _m == "T":
        TM_COLS[_nm] = _c
        _c += _n
NFM = _r
NTM = _c


def host_consts():
    c = {}
    c["c_ident"] = np.eye(128, dtype=np.float32)
    s = np.arange(64)
    c["c_ustrict"] = (s[:, None] > s[None, :]).astype(np.float32)
    i = np.arange(128)
    c["c_sgumask"] = ((i[None, :] // 64) <= (i[:, None] // 64)).astype(np.float32)
    inv_a = (500000.0 ** (-(np.arange(0, 32, 2, dtype=np.float32) / np.float32(32)))).astype(np.float32)
    inv_i = (500000.0 ** (-(np.arange(0, 16, 2, dtype=np.float32) / np.float32(16)))).astype(np.float32)
    inv = np.zeros((128, 2), np.float32)
    inv[:, 0] = inv_a[i % 16]
    inv[:, 1] = inv_i[i % 8]
    c["c_inv"] = inv
    sel = np.zeros((128, 64, 128), np.float32)
    for tl in range(64):
        sel[tl, tl, 0:64] = 1.0
        sel[64 + tl, tl, 64:128] = 1.0
    c["c_sel"] = sel
    c["c_blk"] = ((i[:, None] // 64) == (i[None, :] // 64)).astype(np.float32)
    c["c_i2"] = ((i[:, None] % 64) == s[None, :]).astype(np.float32)
    j = np.arange(64)
    sidx = j % 32
    m = np.zeros((64, 64), np.float32)
    m[:, 0:32] = (sidx[:, None] < np.arange(32)[None, :])
    m[:, 32:64] = (sidx[:, None] <= np.arange(32)[None, :])
    c["c_m32tp"] = m
    t32 = np.arange(32)
    c["c_m32n"] = (t32[None, :] < t32[:, None]).astype(np.float32)
    blk32 = (i[:, None] // 32) == (i[None, :] // 32)
    c["c_lmat32"] = (blk32 & (i[:, None] <= i[None, :])).astype(np.float32)
    c["c_umat32"] = (blk32 & (i[:, None] > i[None, :])).astype(np.float32)
    c["c_ind32"] = ((i[:, None] // 32) == np.arange(4)[None, :]).astype(np.float32)
    return c


def build_nc(S=4096, dbg=None):
    dbg = dbg or {}
    TP = S + PAD
    NT = S // 128
    NB = S // 512
    NCH = S // 64
    KTOP = min(256, S // 4)
    nc = bass.Bass("TRN2", target_bir_lowering=False)
    es = ExitStack()
    P = Prog(nc, es)
    I = {}

    def inp(name, shape, dt=F32):
        I[name] = P.dram(name, shape, dt, kind="ExternalInput")
        return I[name]

    x_in = inp("x", [S, D])
    pos_in = inp("pos", [1, S], I32)
    inp("norm_pre", [L, D]); inp("norm_post", [L, D]); inp("w_in", [L, D, NIN])
    inp("gla_w_g2", [L, 16, 512]); inp("gla_b_g", [L, 512]); inp("gla_head_g", [L, 256])
    inp("rwkv_mu", [L, 4288]); inp("rwkv_w0", [L, 1024]); inp("rwkv_w_w2", [L, 96, 1024])
    inp("rwkv_a0", [L, 1024]); inp("rwkv_w_a2", [L, 96, 1024]); inp("rwkv_k_k", [L, 1024])
    inp("rwkv_k_a", [L, 1024]); inp("rwkv_r_k", [L, 16, 64]); inp("rwkv_lnx_g", [L, 1024])
    inp("rwkv_lnx_b", [L, 1024]); inp("sgu_ln_g", [L, 1024]); inp("sgu_ln_b", [L, 1024])
    inp("sgu_w_s", [L, 8, 128, 128]); inp("sgu_b_s", [L, 8, 128]); inp("w_proj", [L, 4, 1024, D])
    inp("w_out", [L, D, D])
    inp("c_ident", [128, 128]); inp("c_ustrict", [64, 64]); inp("c_sgumask", [128, 128])
    inp("c_inv", [128, 2]); inp("c_sel", [128, 64, 128]); inp("c_blk", [128, 128]); inp("c_i2", [128, 64])
    inp("c_m32tp", [64, 64]); inp("c_m32n", [32, 32]); inp("c_lmat32", [128, 128]); inp("c_umat32", [128, 128]); inp("c_ind32", [128, 4])
    y_out = P.dram("y", [S, D], F32, kind="ExternalOutput")

    ext_in = dbg.get("ext_in", ())
    ext_out = dbg.get("ext_out", ())

    def scr(name, shape, dt):
        kind = "ExternalInput" if name in ext_in else ("ExternalOutput" if name in ext_out else "Internal")
        return P.dram(name, shape, dt, kind=kind)
    fm = scr("fm", [NFM, TP], F32)
    tm = scr("tm", [S, NTM], F32)
    pmT = scr("pmT", [8192, S], BF16)
    ysT = scr("ysT", [4096, S], BF16)
    mT = scr("mT", [D, S], BF16)
    tabs = scr("tabs", [4, 128, S], F32)
    maskT = scr("maskT", [NT, S, 128], BF16)
    rwtm = scr("rwtm", [S, 5, 1024], F32)
    rwbon = scr("rwbon", [S, 16], F32)
    NC32 = S // 32
    rwtm2 = scr("rwtm2", [S, 3, 1024], F32)
    rwxt = scr("rwxt", [64, 16, NT, 512], F32)
    rwdec = scr("rwdec", [64, 16, NC32], F32)
    xcur = [x_in, scr("x1", [S, D], F32), y_out]

    if "fm_in" in dbg:
        pass

    ident_f = P.sb([128, 128], F32, "identf")
    ident_b = P.sb([128, 128], BF16, "identb")
    ones_b = P.sb([128, 128], BF16, "onesb")
    zeros_f = P.sb([128, PAD], F32, "zerosf")
    P.dma("sp", [(ident_f[:], I["c_ident"][:, :])], reads=[I["c_ident"]], writes=[ident_f])
    P.op("dve", lambda e: e.tensor_copy(ident_b[:], ident_f[:]), [ident_f], [ident_b])
    P.op("pool", lambda e: e.memset(ones_b[:], 1.0), [], [ones_b])
    P.op("pool", lambda e: e.memset(zeros_f[:], 0.0), [], [zeros_f])
    for nm in ("rw_v", "rw_gate"):
        for r8 in range(8):
            r0 = FM_ROWS[nm] + r8 * 128
            P.dma("sp", [(fm[r0:r0 + 128, 0:PAD], zeros_f[:])], reads=[zeros_f], writes=[fm])

    def evac(i, out_ap, in_ap, reads, writes):
        if i % 2 == 0:
            P.op("act", lambda e: e.copy(out_ap, in_ap), reads, writes)
        else:
            P.op("dve", lambda e: e.tensor_copy(out_ap, in_ap), reads, writes)

    def V(e, fn, reads, writes):
        P.op(e, fn, reads, writes)

    def phase_tables():
        with P.scope():
            posi = P.sb([128, S], I32, "posi")
            posf = P.sb([128, S], F32, "posf")
            inv = P.sb([128, 2], F32, "inv")
            ang = P.sb([128, S], F32, "ang")
            kf = P.sb([128, S], F32, "kf")
            ki = P.sb([128, S], I32, "ki")
            r1 = P.sb([128, S], F32, "r1")
            y = P.sb([128, S], F32, "y")
            m = P.sb([128, S], F32, "m")
            P.dma("sp", [(posi[:], pos_in[0:1, :].to_broadcast([128, S]))], reads=[pos_in], writes=[posi])
            P.dma("sp", [(inv[:], I["c_inv"][:, :])], reads=[I["c_inv"]], writes=[inv])
            V("dve", lambda e: e.tensor_copy(posf[:], posi[:]), [posi], [posf])
            for which in range(2):
                V("dve", lambda e: e.tensor_scalar(ang[:], posf[:], inv[:, which:which + 1], None, op0=ALU.mult), [posf, inv], [ang])
                V("dve", lambda e: e.tensor_scalar(kf[:], ang[:], float(1.0 / (2 * np.pi)), None, op0=ALU.mult), [ang], [kf])
                V("dve", lambda e: e.tensor_copy(ki[:], kf[:]), [kf], [ki])
                V("dve", lambda e: e.tensor_copy(kf[:], ki[:]), [ki], [kf])
                V("dve", lambda e: e.scalar_tensor_tensor(r1[:], kf[:], -6.28125, ang[:], op0=ALU.mult, op1=ALU.add), [kf, ang], [r1])
                V("dve", lambda e: e.scalar_tensor_tensor(ang[:], kf[:], -0.0019353071795864769, r1[:], op0=ALU.mult, op1=ALU.add), [kf, r1], [ang])
                for cs in range(2):
                    sh = float(np.pi / 2) if cs == 0 else 0.0
                    V("dve", lambda e: e.tensor_scalar(y[:], ang[:], sh, None, op0=ALU.add), [ang], [y])
                    V("dve", lambda e: e.tensor_scalar(m[:], y[:], float(np.pi), None, op0=ALU.is_gt), [y], [m])
                    V("dve", lambda e: e.scalar_tensor_tensor(y[:], m[:], float(-2 * np.pi), y[:], op0=ALU.mult, op1=ALU.add), [m, y], [y])
                    V("dve", lambda e: e.tensor_scalar(m[:], y[:], float(-np.pi), None, op0=ALU.is_lt), [y], [m])
                    V("dve", lambda e: e.scalar_tensor_tensor(y[:], m[:], float(2 * np.pi), y[:], op0=ALU.mult, op1=ALU.add), [m, y], [y])
                    V("dve", lambda e: e.tensor_scalar(y[:], y[:], float(np.pi), float(-np.pi), op0=ALU.min, op1=ALU.max), [y], [y])
                    V("act", lambda e: e.activation(r1[:], y[:], AF.Sin), [y], [r1])
                    P.dma("sp", [(tabs.t[which * 2 + cs, :, :], r1[:])], reads=[r1], writes=[tabs])

    def phase_proj(l, xin):
        with P.scope():
            hT = P.sb([128, 16, S], BF16, "hT")
            with P.scope():
                gbc = P.sb([128, D], F32, "gbc")
                P.dma("sp", [(gbc[:], I["norm_pre"][l:l + 1, :].to_broadcast([128, D]))], reads=[I["norm_pre"]], writes=[gbc])
                xt = [P.sb([128, D], F32, "xt") for _ in range(2)]
                junk = P.sb([128, D], BF16, "junk")
                hb = [P.sb([128, D], BF16, "hb") for _ in range(2)]
                ss = [P.sb([128, 1], F32, "ss") for _ in range(2)]
                rs = [P.sb([128, 1], F32, "rs") for _ in range(2)]
                pst = [P.ps([128, 4, 128], BF16, "pst") for _ in range(4)]
                P.dma("sp", [(xt[0][:], xin[0:128, :])], reads=[xin], writes=[xt[0]])
                for i in range(NT):
                    if i + 1 < NT:
                        P.dma("sp", [(xt[(i + 1) % 2][:], xin[(i + 1) * 128:(i + 2) * 128, :])], reads=[xin], writes=[xt[(i + 1) % 2]])
                    xi, si, ri, hi = xt[i % 2], ss[i % 2], rs[i % 2], hb[i % 2]
                    V("act", lambda e: e.activation(junk[:], xi[:], AF.Square, accum_out=si[:]), [xi], [junk, si])
                    V("act", lambda e: e.activation(ri[:], si[:], AF.Sqrt, bias=1e-6, scale=1.0 / D), [si], [ri])
                    V("dve", lambda e: e.reciprocal(ri[:], ri[:]), [ri], [ri])
                    V("dve", lambda e: e.scalar_tensor_tensor(hi[:], xi[:], ri[:, 0:1], gbc[:], op0=ALU.mult, op1=ALU.mult), [xi, ri, gbc], [hi])
                    for g4 in range(4):
                        pt = pst[g4]
                        for j in range(4):
                            kc = g4 * 4 + j
                            V("pe", lambda e: e.transpose(pt[:, j, :], hi[:, kc * 128:(kc + 1) * 128], ident_b[:]), [hi, ident_b], [pt])
                        evac(g4, hT[:, g4 * 4:(g4 + 1) * 4, i * 128:(i + 1) * 128], pt[:], [pt], [hT])
            with P.scope():
                wf = [P.sb([128, 16, 128], F32, "wf") for _ in range(2)]
                wb = [P.sb([128, 16, 128], BF16, "wb") for _ in range(2)]
                stg = [P.sb([128, S], F32, "stg") for _ in range(2)]
                stgh = [P.sb([128, S], BF16, "stgh") for _ in range(2)]
                pp = [P.ps([128, 512], F32, "pp") for _ in range(6)]
                slabs = []
                for (c0, n, mode, nm) in SEGS:
                    off = 0
                    while off < n:
                        m = min(128, n - off)
                        slabs.append((c0 + off, m, mode, nm, off))
                        off += m
                w_l = I["w_in"]

                def load(s):
                    c0, m, mode, nm, off = slabs[s]
                    src = w_l.t[l, :, c0:c0 + m].rearrange("(kc p) m -> p kc m", p=128)
                    P.dma("sp", [(wf[s % 2][:, k4 * 4:(k4 + 1) * 4, 0:m], src[:, k4 * 4:(k4 + 1) * 4, :]) for k4 in range(4)],
                          reads=[w_l], writes=[wf[s % 2]])

                def cast(s):
                    c0, m, mode, nm, off = slabs[s]
                    V("pool", lambda e: e.tensor_copy(wb[s % 2][:, :, 0:m], wf[s % 2][:, :, 0:m]), [wf[s % 2]], [wb[s % 2]])

                load(0)
                cast(0)
                if len(slabs) > 1:
                    load(1)
                pi = 0
                for s in range(len(slabs)):
                    c0, m, mode, nm, off = slabs[s]
                    if s + 1 < len(slabs):
                        cast(s + 1)
                    if s + 2 < len(slabs):
                        load(s + 2)
                    w_s = wb[s % 2]
                    if mode in ("F", "H"):
                        st = stg[s % 2] if mode == "F" else stgh[s % 2]
                        for tb in range(NB):
                            ps = pp[pi % 6]
                            for kc in range(16):
                                V("pe", lambda e: e.matmul(ps[0:m, :], w_s[:, kc, 0:m], hT[:, kc, tb * 512:(tb + 1) * 512],
                                                           start=(kc == 0), stop=(kc == 15)), [w_s, hT], [ps])
                            evac(pi, st[0:m, tb * 512:(tb + 1) * 512], ps[0:m, :], [ps], [st])
                            pi += 1
                        if mode == "F":
                            r0 = FM_ROWS[nm] + off
                            P.dma("sp", [(fm[r0:r0 + m, PAD:TP], st[0:m, :])], reads=[st], writes=[fm])
                        else:
                            P.dma("sp", [(pmT[off:off + m, :], st[0:m, :])], reads=[st], writes=[pmT])
                    else:
                        st = stg[s % 2]
                        st3 = st[:].rearrange("p (t m) -> p t m", m=128)
                        for t4 in range(NT // 4):
                            ps = pp[pi % 6]
                            ps3 = ps[:].rearrange("p (t m) -> p t m", m=128)
                            for j in range(4):
                                tt = t4 * 4 + j
                                for kc in range(16):
                                    V("pe", lambda e: e.matmul(ps3[:, j, 0:m], hT[:, kc, tt * 128:(tt + 1) * 128], w_s[:, kc, 0:m],
                                                               start=(kc == 0), stop=(kc == 15)), [w_s, hT], [ps])
                            evac(pi, st3[:, t4 * 4:(t4 + 1) * 4, 0:m], ps3[:, :, 0:m], [ps], [st])
                            pi += 1
                        cc = TM_COLS[nm] + off
                        P.dma("sp", [(tm.t[:, cc:cc + m].rearrange("(t p) m -> p t m", p=128), st3[:, 0:NT, 0:m])], reads=[st], writes=[tm])

    def phase_rope(l):
        with P.scope():
            A = P.sb([128, S], F32, "ropeA")
            B = P.sb([128, S], F32, "ropeB")
            C = P.sb([128, S], F32, "ropeC")
            Sn = P.sb([128, S], F32, "ropeS")
            t1 = P.sb([128, S], F32, "ropet1")
            t2 = P.sb([128, S], F32, "ropet2")
            A2 = P.sb([128, S], F32, "ropeA2")
            B2 = P.sb([128, S], F32, "ropeB2")
            for (nm, nh, hd, half, tab) in (("dsa_q", 8, 128, 16, 0), ("dsa_k", 8, 128, 16, 0), ("dsa_iq", 8, 64, 8, 2), ("dsa_ik", 1, 64, 8, 2)):
                np_ = nh * half
                r0 = FM_ROWS[nm]
                rows = fm.t[r0:r0 + nh * hd, PAD:TP].rearrange("(h r) t -> h r t", r=hd)
                P.dma("sp", [(C[0:np_, :], tabs.t[tab, 0:np_, :]), (Sn[0:np_, :], tabs.t[tab + 1, 0:np_, :])], reads=[tabs], writes=[C, Sn], sembuf=C)
                P.dma("sp", [(A[0:np_, :], rows[:, 0:half, :]), (B[0:np_, :], rows[:, half:2 * half, :])], reads=[fm], writes=[A, B], sembuf=A)
                V("dve", lambda e: e.tensor_tensor(t1[0:np_, :], A[0:np_, :], C[0:np_, :], ALU.mult), [A, C], [t1])
                V("pool", lambda e: e.tensor_tensor(t2[0:np_, :], B[0:np_, :], Sn[0:np_, :], ALU.mult), [B, Sn], [t2])
                V("dve", lambda e: e.tensor_tensor(A2[0:np_, :], t1[0:np_, :], t2[0:np_, :], ALU.subtract), [t1, t2], [A2])
                V("dve", lambda e: e.tensor_tensor(t1[0:np_, :], B[0:np_, :], C[0:np_, :], ALU.mult), [B, C], [t1])
                V("pool", lambda e: e.tensor_tensor(t2[0:np_, :], A[0:np_, :], Sn[0:np_, :], ALU.mult), [A, Sn], [t2])
                V("dve", lambda e: e.tensor_tensor(B2[0:np_, :], t1[0:np_, :], t2[0:np_, :], ALU.add), [t1, t2], [B2])
                P.dma("sp", [(rows[:, 0:half, :], A2[0:np_, :]), (rows[:, half:2 * half, :], B2[0:np_, :])], reads=[A2, B2], writes=[fm], sembuf=A2)

    def phase_gla(l):
        with P.scope():
            qT = P.sb([128, 4, S], BF16, "gqT")
            oT = P.sb([128, 8, S], BF16, "goT")
            stg = [P.sb([128, S], F32, "gstg") for _ in range(2)]
            glr = P.sb([17, S], F32, "glr")
            wg = P.sb([17, 512], F32, "gwg")
            ust = P.sb([64, 64], F32, "gust")
            ones64 = P.sb([64, 1], F32, "gones")
            hg = P.sb([128, 2], F32, "ghg")
            state = P.sb([128, 4, 256], F32, "gstate")
            state_b = P.sb([128, 4, 256], BF16, "gstateb")
            kc_ = [P.sb([64, 512], F32, "gk") for _ in range(2)]
            vc_ = [P.sb([64, 1024], F32, "gv") for _ in range(2)]
            vb = P.sb([64, 1024], BF16, "gvb")
            ee = P.sb([64, 512], F32, "gee")
            lsp = P.sb([64, 512], F32, "glsp")
            dkk = P.sb([64, 512], F32, "gdk")
            kdec = P.sb([64, 512], BF16, "gkdec")
            dec = P.sb([128, 4], F32, "gdec")
            pz = P.ps([64, 512], F32, "gpz")
            pG = P.ps([64, 512], F32, "gpG")
            ptot = P.ps([128, 4], F32, "gptot")
            pkv = [P.ps([128, 2, 256], F32, "gpkv") for _ in range(2)]
            po = P.ps([128, 8, 64], F32, "gpo")
            V("pool", lambda e: e.memset(glr[:], 1.0), [], [glr])
            V("pool", lambda e: e.memset(ones64[:], 1.0), [], [ones64])
            V("pool", lambda e: e.memset(state[:], 0.0), [], [state])
            r0 = FM_ROWS["gla_g"]
            P.dma("sp", [(glr[0:16, :], fm[r0:r0 + 16, PAD:TP])], reads=[fm], writes=[glr])
            P.dma("sp", [(wg[0:16, :], I["gla_w_g2"].t[l, :, :]), (wg[16:17, :], I["gla_b_g"][l:l + 1, :])], reads=[I["gla_w_g2"], I["gla_b_g"]], writes=[wg])
            P.dma("sp", [(ust[:], I["c_ustrict"][:, :])], reads=[I["c_ustrict"]], writes=[ust])
            P.dma("sp", [(hg[:], I["gla_head_g"].t[l, :].rearrange("(a p) -> p a", p=128))], reads=[I["gla_head_g"]], writes=[hg], allow_slow_non_contiguous=True)
            r0 = FM_ROWS["gla_q"]
            for h in range(4):
                st = stg[h % 2]
                P.dma("sp", [(st[:], fm[r0 + h * 128:r0 + (h + 1) * 128, PAD:TP])], reads=[fm], writes=[st])
                V("pool", lambda e: e.tensor_copy(qT[:, h, :], st[:]), [st], [qT])
            ck = TM_COLS["gla_k"]
            cv = TM_COLS["gla_v"]

            def loadkv(c):
                P.dma("sp", [(kc_[c % 2][:], tm[c * 64:(c + 1) * 64, ck:ck + 512])], reads=[tm], writes=[kc_[c % 2]])
                P.dma("sp", [(vc_[c % 2][:], tm[c * 64:(c + 1) * 64, cv:cv + 1024])], reads=[tm], writes=[vc_[c % 2]])
            loadkv(0)
            for c in range(NCH):
                if c + 1 < NCH:
                    loadkv(c + 1)
                t0 = c * 64
                kk_, vv_ = kc_[c % 2], vc_[c % 2]
                V("pe", lambda e: e.matmul(pz[:], glr[:, t0:t0 + 64], wg[:], start=True, stop=True), [glr, wg], [pz])
                V("act", lambda e: e.activation(ee[:], pz[:], AF.Exp, scale=-1.0), [pz], [ee])
                V("act", lambda e: e.activation(lsp[:], ee[:], AF.Ln, bias=1.0), [ee], [lsp])
                V("pe", lambda e: e.matmul(pG[:], ust[:], lsp[:], start=True, stop=True), [ust, lsp], [pG])
                for h in range(4):
                    V("pe", lambda e: e.matmul(ptot[:, h:h + 1], lsp[:, h * 128:(h + 1) * 128], ones64[:], start=True, stop=True), [lsp, ones64], [ptot])
                V("act", lambda e: e.activation(dkk[:], pG[:], AF.Exp, scale=-1.0 / 16.0), [pG], [dkk])
                V("act", lambda e: e.activation(dec[:], ptot[:], AF.Exp, scale=-1.0 / 16.0), [ptot], [dec])
                V("dve", lambda e: e.tensor_tensor(kdec[:], kk_[:], dkk[:], ALU.mult), [kk_, dkk], [kdec])
                V("pool", lambda e: e.tensor_copy(vb[:], vv_[:]), [vv_], [vb])
                for h in range(4):
                    pk = pkv[h // 2]
                    V("pe", lambda e: e.matmul(pk[:, h % 2, :], kdec[:, h * 128:(h + 1) * 128], vb[:, h * 256:(h + 1) * 256], start=True, stop=True), [kdec, vb], [pk])
                for h in range(4):
                    pk = pkv[h // 2]
                    V("dve", lambda e: e.scalar_tensor_tensor(state[:, h, :], state[:, h, :], dec[:, h:h + 1], pk[:, h % 2, :], op0=ALU.mult, op1=ALU.add), [state, dec, pk], [state])
                V("act", lambda e: e.copy(state_b[:], state[:]), [state], [state_b])
                for h in range(4):
                    for vh in range(2):
                        V("pe", lambda e: e.matmul(po[:, h * 2 + vh, :], state_b[:, h, vh * 128:(vh + 1) * 128], qT[:, h, t0:t0 + 64], start=True, stop=True), [state_b, qT], [po])
                evac(c, oT[:, :, t0:t0 + 64], po[:], [po], [oT])
            sq = P.sb([128, 2, 512], BF16, "gsq")
            pms = [P.ps([128, 512], F32, "gpms") for _ in range(2)]
            rstd = P.sb([128, 512], F32, "grstd")
            yy = P.sb([128, 512], F32, "gyy")
            sg = P.sb([128, 512], F32, "gsg")
            yo = [P.sb([128, S], BF16, "gyo") for _ in range(2)]
            sc = float(128.0 ** -0.5)
            hgs = P.sb([128, 2], F32, "ghgs")
            V("dve", lambda e: e.tensor_scalar(hgs[:], hg[:], sc, None, op0=ALU.mult), [hg], [hgs])
            rg = FM_ROWS["gla_gate"]
            it = 0
            for h in range(4):
                for vh in range(2):
                    gt = stg[it % 2]
                    yob = yo[it % 2]
                    P.dma("sp", [(gt[:], fm[rg + h * 256 + vh * 128:rg + h * 256 + (vh + 1) * 128, PAD:TP])], reads=[fm], writes=[gt])
                    for tb in range(NB):
                        ts_ = slice(tb * 512, (tb + 1) * 512)
                        pm_ = pms[(it * NB + tb) % 2]
                        if vh == 0:
                            for v2 in range(2):
                                V("act", lambda e: e.activation(sq[:, v2, :], oT[:, h * 2 + v2, ts_], AF.Square), [oT], [sq])
                        else:
                            for v2 in range(2):
                                V("act", lambda e: e.activation(sq[:, v2, :], oT[:, h * 2 + v2, ts_], AF.Square), [oT], [sq])
                        for v2 in range(2):
                            V("pe", lambda e: e.matmul(pm_[:], ones_b[:], sq[:, v2, :], start=(v2 == 0), stop=(v2 == 1)), [ones_b, sq], [pm_])
                        V("act", lambda e: e.activation(rstd[:], pm_[:], AF.Sqrt, bias=1e-6, scale=sc * sc / 256.0), [pm_], [rstd])
                        V("dve", lambda e: e.reciprocal(rstd[:], rstd[:]), [rstd], [rstd])
                        V("act", lambda e: e.activation(sg[:], gt[:, ts_], AF.Silu), [gt], [sg])
                        V("dve", lambda e: e.tensor_tensor(yy[:], oT[:, h * 2 + vh, ts_], rstd[:], ALU.mult), [oT, rstd], [yy])
                        V("dve", lambda e: e.scalar_tensor_tensor(yob[:, ts_], yy[:], hgs[:, vh:vh + 1], sg[:], op0=ALU.mult, op1=ALU.mult), [yy, hgs, sg], [yob])
                    ro = h * 256 + vh * 128
                    P.dma("sp", [(ysT[ro:ro + 128, :], yob[:])], reads=[yob], writes=[ysT])
                    it += 1

    def phase_sgu(l):
        with P.scope():
            lng = P.sb([128, 1024], F32, "slng")
            lnb = P.sb([128, 1024], F32, "slnb")
            bsb = P.sb([128, 8, 128], F32, "sbsb")
            msk = P.sb([128, 128], F32, "smsk")
            wsT = P.sb([128, 8, 128], BF16, "swsT")
            wtmp = P.sb([128, 128], F32, "swtmp")
            pT = P.ps([128, 128], F32, "spT")
            P.dma("sp", [(lng[:], I["sgu_ln_g"][l:l + 1, :].to_broadcast([128, 1024]))], reads=[I["sgu_ln_g"]], writes=[lng])
            P.dma("sp", [(lnb[:], I["sgu_ln_b"][l:l + 1, :].to_broadcast([128, 1024]))], reads=[I["sgu_ln_b"]], writes=[lnb])
            P.dma("sp", [(bsb[:].rearrange("p g i -> p (g i)"), I["sgu_b_s"].t[l:l + 1, :, :].rearrange("a g i -> a (g i)").to_broadcast([128, 1024]))], reads=[I["sgu_b_s"]], writes=[bsb])
            P.dma("sp", [(msk[:], I["c_sgumask"][:, :])], reads=[I["c_sgumask"]], writes=[msk])
            for g in range(8):
                P.dma("sp", [(wtmp[:], I["sgu_w_s"].t[l, g, :, :])], reads=[I["sgu_w_s"]], writes=[wtmp])
                V("dve", lambda e: e.tensor_tensor(wtmp[:], wtmp[:], msk[:], ALU.mult), [wtmp, msk], [wtmp])
                V("pe", lambda e: e.transpose(pT[:], wtmp[:], ident_f[:]), [wtmp, ident_f], [pT])
                V("act", lambda e: e.copy(wsT[:, g, :], pT[:]), [pT], [wsT])
            zv = [P.sb([128, 1024], F32, "szv") for _ in range(2)]
            uu = [P.sb([128, 8, 128], F32, "suu") for _ in range(2)]
            gg = [P.sb([128, 8, 128], F32, "sgg") for _ in range(2)]
            g1 = P.sb([128, 1024], F32, "sg1")
            junk = P.sb([128, 1024], BF16, "sjunk")
            vn = P.sb([128, 1024], F32, "svn")
            vln = P.sb([128, 1024], BF16, "svln")
            s1 = P.sb([128, 1], F32, "ss1"); s2 = P.sb([128, 1], F32, "ss2"); mm = P.sb([128, 1], F32, "smm")
            msq = P.sb([128, 1], F32, "smsq"); var = P.sb([128, 1], F32, "svar"); rstd = P.sb([128, 1], F32, "srstd")
            nmr = P.sb([128, 1], F32, "snmr")
            psv = [P.ps([128, 4, 128], F32, "spsv") for _ in range(2)]
            t1 = P.sb([128, 8, 128], F32, "st1")
            ug = P.sb([128, 8, 128], F32, "sug")
            sg = P.sb([128, 8, 128], F32, "ssg")
            yb = [P.sb([128, 8, 128], BF16, "syb") for _ in range(2)]
            cz = TM_COLS["sgu_zv"]
            ru = FM_ROWS["sgu_u"]
            rgt = FM_ROWS["sgu_gate"]

            def loadc(c):
                P.dma("sp", [(zv[c % 2][:], tm[c * 128:(c + 1) * 128, cz:cz + 1024])], reads=[tm], writes=[zv[c % 2]])
                P.dma("sp", [(uu[c % 2][:], fm.t[ru:ru + 1024, PAD + c * 128:PAD + (c + 1) * 128].rearrange("(g d) t -> d g t", d=128))], reads=[fm], writes=[uu[c % 2]])
                P.dma("sp", [(gg[c % 2][:], fm.t[rgt:rgt + 1024, PAD + c * 128:PAD + (c + 1) * 128].rearrange("(g d) t -> d g t", d=128))], reads=[fm], writes=[gg[c % 2]])
            loadc(0)
            for c in range(NT):
                if c + 1 < NT:
                    loadc(c + 1)
                z, u_, g_ = zv[c % 2], uu[c % 2], gg[c % 2]
                ybc = yb[c % 2]
                V("act", lambda e: e.activation(g1[:], z[:], AF.Gelu), [z], [g1])
                V("dve", lambda e: e.tensor_reduce(s1[:], g1[:], AX.X, ALU.add), [g1], [s1])
                V("act", lambda e: e.activation(junk[:], g1[:], AF.Square, accum_out=s2[:]), [g1], [junk, s2])
                V("dve", lambda e: e.tensor_scalar(mm[:], s1[:], 1.0 / 1024.0, None, op0=ALU.mult), [s1], [mm])
                V("dve", lambda e: e.tensor_tensor(msq[:], mm[:], mm[:], ALU.mult), [mm], [msq])
                V("dve", lambda e: e.scalar_tensor_tensor(var[:], s2[:], 1.0 / 1024.0, msq[:], op0=ALU.mult, op1=ALU.subtract), [s2, msq], [var])
                V("act", lambda e: e.activation(rstd[:], var[:], AF.Sqrt, bias=1e-5, scale=1.0), [var], [rstd])
                V("dve", lambda e: e.reciprocal(rstd[:], rstd[:]), [rstd], [rstd])
                V("dve", lambda e: e.scalar_tensor_tensor(nmr[:], mm[:], -1.0, rstd[:], op0=ALU.mult, op1=ALU.mult), [mm, rstd], [nmr])
                V("act", lambda e: e.activation(vn[:], g1[:], AF.Identity, bias=nmr[:, 0:1], scale=rstd[:, 0:1]), [g1, nmr, rstd], [vn])
                V("dve", lambda e: e.tensor_tensor(vn[:], vn[:], lng[:], ALU.mult), [vn, lng], [vn])
                V("dve", lambda e: e.tensor_tensor(vln[:], vn[:], lnb[:], ALU.add), [vn, lnb], [vln])
                for g in range(8):
                    pv = psv[g // 4]
                    V("pe", lambda e: e.matmul(pv[:, g % 4, :], vln[:, g * 128:(g + 1) * 128], wsT[:, g, :], start=True, stop=True), [vln, wsT], [pv])
                for g2 in range(2):
                    V("dve", lambda e: e.tensor_tensor(t1[:, g2 * 4:(g2 + 1) * 4, :], psv[g2][:], bsb[:, g2 * 4:(g2 + 1) * 4, :], ALU.add), [psv[g2], bsb], [t1])
                V("act", lambda e: e.activation(ug[:], u_[:], AF.Gelu), [u_], [ug])
                V("act", lambda e: e.activation(sg[:], g_[:], AF.Silu), [g_], [sg])
                V("pool", lambda e: e.tensor_tensor(ug[:], ug[:], sg[:], ALU.mult), [ug, sg], [ug])
                V("dve", lambda e: e.tensor_tensor(ybc[:], t1[:], ug[:], ALU.mult), [t1, ug], [ybc])
                P.dma("sp", [(ysT.t[3072:4096, c * 128:(c + 1) * 128].rearrange("(g d) t -> d g t", d=128), ybc[:])], reads=[ybc], writes=[ysT])

    def phase_m1(l):
        with P.scope():
            wp = P.sb([128, 32, D], BF16, "mwp")
            wst = [P.sb([128, 2048], F32, "mwst") for _ in range(2)]
            wpj = I["w_proj"]
            k = 0
            for g in range(4):
                for wc in range(8):
                    st = wst[k % 2]
                    P.dma("sp", [(st[:], wpj.t[l, g, wc * 128:(wc + 1) * 128, :])], reads=[wpj], writes=[st])
                    if k % 2 == 0:
                        V("pool", lambda e: e.tensor_copy(wp[:, g * 8 + wc, :], st[:]), [st], [wp])
                    else:
                        V("dve", lambda e: e.tensor_copy(wp[:, g * 8 + wc, :], st[:]), [st], [wp])
                    k += 1
            TB = 256
            yt = [P.sb([128, 32, TB], BF16, "myt") for _ in range(2)]
            pmt = [P.sb([128, 4, TB], BF16, "mpm") for _ in range(3)]
            sig = P.sb([128, 4, TB], F32, "msig")
            acc = P.sb([128, TB], F32, "macc")
            tmp = P.sb([128, TB], F32, "mtmp")
            mo = [P.sb([128, 16, TB], BF16, "mmo") for _ in range(2)]
            pps = [P.ps([128, TB], F32, "mpp") for _ in range(6)]
            nblk = S // TB

            def loady(b):
                P.dma("sp", [(yt[b % 2][:, q * 8:(q + 1) * 8, :], ysT.t[q * 1024:(q + 1) * 1024, b * TB:(b + 1) * TB].rearrange("(c p) t -> p c t", p=128)) for q in range(4)],
                      reads=[ysT], writes=[yt[b % 2]])
            loady(0)
            pi = 0
            it = 0
            for b in range(nblk):
                if b + 1 < nblk:
                    loady(b + 1)
                ytb = yt[b % 2]
                mob = mo[b % 2]
                for dc in range(16):
                    pmb = pmt[it % 3]
                    it += 1
                    P.dma("sp", [(pmb[:], pmT.t[:, b * TB:(b + 1) * TB].rearrange("(g r) t -> r g t", g=4)[dc * 128:(dc + 1) * 128, :, :])], reads=[pmT], writes=[pmb])
                    V("act", lambda e: e.activation(sig[:], pmb[:], AF.Sigmoid), [pmb], [sig])
                    for g in range(4):
                        ps = pps[pi % 6]
                        pi += 1
                        for wc in range(8):
                            V("pe", lambda e: e.matmul(ps[:], wp[:, g * 8 + wc, dc * 128:(dc + 1) * 128], ytb[:, g * 8 + wc, :], start=(wc == 0), stop=(wc == 7)), [wp, ytb], [ps])
                        if g == 0:
                            V("dve", lambda e: e.tensor_tensor(acc[:], ps[:], sig[:, g, :], ALU.mult), [ps, sig], [acc])
                        elif g < 3:
                            V("dve", lambda e: e.tensor_tensor(tmp[:], ps[:], sig[:, g, :], ALU.mult), [ps, sig], [tmp])
                            V("pool", lambda e: e.tensor_tensor(acc[:], acc[:], tmp[:], ALU.add), [acc, tmp], [acc])
                        else:
                            V("dve", lambda e: e.tensor_tensor(tmp[:], ps[:], sig[:, g, :], ALU.mult), [ps, sig], [tmp])
                            V("pool", lambda e: e.tensor_tensor(mob[:, dc, :], acc[:], tmp[:], ALU.add), [acc, tmp], [mob])
                P.dma("sp", [(mT.t[:, b * TB:(b + 1) * TB].rearrange("(c p) t -> p c t", p=128), mob[:])], reads=[mob], writes=[mT])

    def phase_m2(l, xin, xout):
        with P.scope():
            wo = P.sb([128, 16, D], BF16, "owo")
            wst = [P.sb([128, 2048], F32, "owst") for _ in range(2)]
            for dc in range(16):
                st = wst[dc % 2]
                P.dma("sp", [(st[:], I["w_out"].t[l, dc * 128:(dc + 1) * 128, :])], reads=[I["w_out"]], writes=[st])
                if dc % 2 == 0:
                    V("pool", lambda e: e.tensor_copy(wo[:, dc, :], st[:]), [st], [wo])
                else:
                    V("dve", lambda e: e.tensor_copy(wo[:, dc, :], st[:]), [st], [wo])
            gbc = P.sb([128, D], F32, "ogbc")
            P.dma("sp", [(gbc[:], I["norm_post"][l:l + 1, :].to_broadcast([128, D]))], reads=[I["norm_post"]], writes=[gbc])
            mt = [P.sb([128, 16, 128], BF16, "omt") for _ in range(2)]
            xt = [P.sb([128, D], F32, "oxt") for _ in range(2)]
            ot = P.sb([128, D], F32, "oot")
            junk = P.sb([128, D], BF16, "ojunk")
            res = [P.sb([128, D], F32, "ores") for _ in range(2)]
            ssq = P.sb([128, 1], F32, "ossq")
            rs = P.sb([128, 1], F32, "ors")
            pps = [P.ps([128, 512], F32, "opp") for _ in range(4)]

            def loadt(i):
                P.dma("sp", [(mt[i % 2][:], mT.t[:, i * 128:(i + 1) * 128].rearrange("(c p) t -> p c t", p=128))], reads=[mT], writes=[mt[i % 2]])
                P.dma("sp", [(xt[i % 2][:], xin[i * 128:(i + 1) * 128, :])], reads=[xin], writes=[xt[i % 2]])
            loadt(0)
            for i in range(NT):
                if i + 1 < NT:
                    loadt(i + 1)
                mti, xti, rsi = mt[i % 2], xt[i % 2], res[i % 2]
                for nb in range(4):
                    ps = pps[nb]
                    for dc in range(16):
                        V("pe", lambda e: e.matmul(ps[:], mti[:, dc, :], wo[:, dc, nb * 512:(nb + 1) * 512], start=(dc == 0), stop=(dc == 15)), [mti, wo], [ps])
                    evac(nb, ot[:, nb * 512:(nb + 1) * 512], ps[:], [ps], [ot])
                V("act", lambda e: e.activation(junk[:], ot[:], AF.Square, accum_out=ssq[:]), [ot], [junk, ssq])
                V("act", lambda e: e.activation(rs[:], ssq[:], AF.Sqrt, bias=1e-6, scale=1.0 / D), [ssq], [rs])
                V("dve", lambda e: e.reciprocal(rs[:], rs[:]), [rs], [rs])
                V("dve", lambda e: e.scalar_tensor_tensor(ot[:], ot[:], rs[:, 0:1], gbc[:], op0=ALU.mult, op1=ALU.mult), [ot, rs, gbc], [ot])
                V("pool", lambda e: e.tensor_tensor(rsi[:], ot[:], xti[:], ALU.add), [ot, xti], [rsi])
                P.dma("sp", [(xout[i * 128:(i + 1) * 128, :], rsi[:])], reads=[rsi], writes=[xout])


    def phase_dsa1(l):
        with P.scope():
            iqT = P.sb([64, 8, S], BF16, "diqT")
            ikT = P.sb([64, S], BF16, "dikT")
            iw = P.sb([128, NT, 8], F32, "diw")
            stg = [P.sb([64, S], F32, "dstg") for _ in range(2)]
            score = P.sb([128, S], F32, "dscore")
            work = P.sb([128, S], F32, "dwork")
            maskb = P.sb([128, S], BF16, "dmaskb")
            mts = [P.sb([128, NT, 128], BF16, "dmts") for _ in range(2)]
            rl = [P.sb([128, 512], F32, "drl") for _ in range(2)]
            m8 = P.sb([128, 8], F32, "dm8")
            thr = P.sb([128, 1], F32, "dthr")
            blo = P.sb([128, 1], F32, "dblo"); bw0 = P.sb([128, 1], F32, "dbw0"); bmid = P.sb([128, 1], F32, "dbmid")
            bcnt = P.sb([128, 1], F32, "dbcnt"); bge = P.sb([128, 1], F32, "dbge"); bstp = P.sb([128, 1], F32, "dbstp")
            pps = [P.ps([128, 512], F32, "dpp") for _ in range(4)]
            ptr = [P.ps([128, 4, 128], BF16, "dptr") for _ in range(2)]
            r0 = FM_ROWS["dsa_iq"]
            for h in range(8):
                st = stg[h % 2]
                P.dma("sp", [(st[:], fm[r0 + h * 64:r0 + (h + 1) * 64, PAD:TP])], reads=[fm], writes=[st])
                V("pool", lambda e: e.tensor_copy(iqT[:, h, :], st[:]), [st], [iqT])
            r0 = FM_ROWS["dsa_ik"]
            P.dma("sp", [(stg[0][:], fm[r0:r0 + 64, PAD:TP])], reads=[fm], writes=[stg[0]])
            V("pool", lambda e: e.tensor_copy(ikT[:], stg[0][:]), [stg[0]], [ikT])
            cw = TM_COLS["dsa_iw"]
            P.dma("sp", [(iw[:], tm.t[:, cw:cw + 8].rearrange("(t p) c -> p t c", p=128))], reads=[tm], writes=[iw])
            V("dve", lambda e: e.tensor_scalar(iw[:], iw[:], float(512.0 ** -0.5), None, op0=ALU.mult), [iw], [iw])
            k = 0
            kt = 0
            for qt in range(NT):
                nk = (qt + 1) * 128
                for sb in range((nk + 511) // 512):
                    w = min(512, nk - sb * 512)
                    cs = slice(sb * 512, sb * 512 + w)
                    for h in range(8):
                        ps = pps[k % 4]
                        rt = rl[k % 2]
                        k += 1
                        V("pe", lambda e: e.matmul(ps[:, 0:w], iqT[:, h, qt * 128:(qt + 1) * 128], ikT[:, cs], start=True, stop=True), [iqT, ikT], [ps])
                        V("act", lambda e: e.activation(rt[:, 0:w], ps[:, 0:w], AF.Relu), [ps], [rt])
                        if h == 0:
                            V("dve", lambda e: e.tensor_scalar(score[:, cs], rt[:, 0:w], iw[:, qt, 0:1], None, op0=ALU.mult), [rt, iw], [score])
                        else:
                            V("dve", lambda e: e.scalar_tensor_tensor(score[:, cs], rt[:, 0:w], iw[:, qt, h:h + 1], score[:, cs], op0=ALU.mult, op1=ALU.add), [rt, iw, score], [score])
                V("pool", lambda e: e.memset(score[0:64, nk - 64:nk], NEG), [], [score])
                if nk <= KTOP:
                    V("pool", lambda e: e.memset(thr[:], NEG / 2), [], [thr])
                else:
                    V("dve", lambda e: e.max(out=m8[:], in_=score[:, 0:nk]), [score], [m8])
                    V("dve", lambda e: e.tensor_reduce(blo[:], score[:, 0:nk - 64], AX.X, ALU.min), [score], [blo])
                    V("dve", lambda e: e.tensor_tensor(bw0[:], m8[:, 0:1], blo[:], ALU.subtract), [m8, blo], [bw0])
                    for it in range(24):
                        f = float(2.0 ** -(it + 1))
                        V("dve", lambda e: e.scalar_tensor_tensor(bmid[:], bw0[:], f, blo[:], op0=ALU.mult, op1=ALU.add), [bw0, blo], [bmid])
                        V("dve", lambda e: e.memset(bcnt[:], 0.0), [], [bcnt])
                        V("dve", lambda e: e.tensor_scalar(maskb[:, 0:nk], score[:, 0:nk], bmid[:, 0:1], 0.0, op0=ALU.is_ge, op1=ALU.add, accum_out=bcnt[:]), [score, bmid], [maskb, bcnt])
                        V("dve", lambda e: e.tensor_scalar(bge[:], bcnt[:], float(KTOP) - 0.5, None, op0=ALU.is_ge), [bcnt], [bge])
                        V("dve", lambda e: e.tensor_tensor(bstp[:], bge[:], bw0[:], ALU.mult), [bge, bw0], [bstp])
                        V("dve", lambda e: e.scalar_tensor_tensor(blo[:], bstp[:], f, blo[:], op0=ALU.mult, op1=ALU.add), [bstp, blo], [blo])
                    V("dve", lambda e: e.tensor_copy(thr[:], blo[:]), [blo], [thr])
                V("dve", lambda e: e.tensor_scalar(maskb[:, 0:nk], score[:, 0:nk], thr[:, 0:1], None, op0=ALU.is_ge), [score, thr], [maskb])
                mtb = mts[qt % 2]
                for j4 in range((qt + 4) // 4):
                    n = min(4, qt + 1 - j4 * 4)
                    pt = ptr[kt % 2]
                    kt += 1
                    for jj in range(n):
                        j = j4 * 4 + jj
                        V("pe", lambda e: e.transpose(pt[:, jj, :], maskb[:, j * 128:(j + 1) * 128], ident_b[:]), [maskb, ident_b], [pt])
                    evac(kt, mtb[:, j4 * 4:j4 * 4 + n, :], pt[:, 0:n, :], [pt], [mtb])
                P.dma("sp", [(maskT.t[qt, 0:nk, :].rearrange("(j p) t -> p j t", p=128), mtb[:, 0:qt + 1, :])], reads=[mtb], writes=[maskT])

    def phase_dsa2(l):
        with P.scope():
            stg = [P.sb([128, S], F32, "astg") for _ in range(2)]
            kT = P.sb([128, S], BF16, "akT")
            qT = P.sb([128, S], BF16, "aqT")
            Vh = P.sb([128, NT, 128], BF16, "aVh")
            sg = P.sb([128, S], F32, "asg")
            mk = [P.sb([128, 4, 128], BF16, "amk") for _ in range(3)]
            ex = [P.sb([128, 4, 128], BF16, "aex") for _ in range(2)]
            ptt = [P.sb([128, 4, 128], BF16, "aptt") for _ in range(2)]
            pl = [P.ps([128, 4, 128], F32, "apl") for _ in range(2)]
            po = [P.ps([128, 512], F32, "apo") for _ in range(2)]
            pd = [P.ps([128, 512], F32, "apd") for _ in range(2)]
            rden = P.sb([128, 128], F32, "arden")
            o1 = P.sb([128, 128], F32, "ao1")
            yo = [P.sb([128, S], BF16, "ayo") for _ in range(2)]
            sc = float(128.0 ** -0.5)
            rk, rq, rg = FM_ROWS["dsa_k"], FM_ROWS["dsa_q"], FM_ROWS["dsa_gate"]
            cv = TM_COLS["dsa_v"]
            kb = 0
            for h in range(8):
                P.dma("sp", [(stg[0][:], fm[rk + h * 128:rk + (h + 1) * 128, PAD:TP])], reads=[fm], writes=[stg[0]])
                V("pool", lambda e: e.tensor_copy(kT[:], stg[0][:]), [stg[0]], [kT])
                P.dma("sp", [(stg[1][:], fm[rq + h * 128:rq + (h + 1) * 128, PAD:TP])], reads=[fm], writes=[stg[1]])
                V("pool", lambda e: e.tensor_copy(qT[:], stg[1][:]), [stg[1]], [qT])
                P.dma("sp", [(stg[0][:].rearrange("p (j v) -> p j v", v=128), tm.t[:, cv + h * 128:cv + (h + 1) * 128].rearrange("(j p) v -> p j v", p=128))], reads=[tm], writes=[stg[0]])
                V("pool", lambda e: e.tensor_copy(Vh[:], stg[0][:].rearrange("p (j v) -> p j v", v=128)), [stg[0]], [Vh])
                P.dma("sp", [(stg[1][:], fm[rg + h * 128:rg + (h + 1) * 128, PAD:TP])], reads=[fm], writes=[stg[1]])
                V("act", lambda e: e.activation(sg[:], stg[1][:], AF.Silu), [stg[1]], [sg])
                yob = yo[h % 2]
                for qt in range(NT):
                    nj = qt + 1
                    po_, pd_ = po[qt % 2], pd[qt % 2]
                    qs = slice(qt * 128, (qt + 1) * 128)
                    for j4 in range((nj + 3) // 4):
                        n = min(4, nj - j4 * 4)
                        mkb, plb, exb, ptb = mk[kb % 3], pl[kb % 2], ex[kb % 2], ptt[kb % 2]
                        kb += 1
                        P.dma("sp", [(mkb[:, 0:n, :], maskT.t[qt, j4 * 512:j4 * 512 + n * 128, :].rearrange("(j p) t -> p j t", p=128))], reads=[maskT], writes=[mkb])
                        for jj in range(n):
                            j = j4 * 4 + jj
                            V("pe", lambda e: e.matmul(plb[:, jj, :], kT[:, j * 128:(j + 1) * 128], qT[:, qs], start=True, stop=True), [kT, qT], [plb])
                        V("act", lambda e: e.activation(exb[:, 0:n, :], plb[:, 0:n, :], AF.Exp, scale=sc), [plb], [exb])
                        V("dve", lambda e: e.tensor_tensor(ptb[:, 0:n, :], exb[:, 0:n, :], mkb[:, 0:n, :], ALU.mult), [exb, mkb], [ptb])
                        for jj in range(n):
                            j = j4 * 4 + jj
                            V("pe", lambda e: e.matmul(po_[:, 0:128], Vh[:, j, :], ptb[:, jj, :], start=(j == 0), stop=(j == nj - 1)), [Vh, ptb], [po_])
                            V("pe", lambda e: e.matmul(pd_[:, 0:128], ones_b[:], ptb[:, jj, :], start=(j == 0), stop=(j == nj - 1)), [ones_b, ptb], [pd_])
                    V("dve", lambda e: e.reciprocal(rden[:], pd_[:, 0:128]), [pd_], [rden])
                    V("dve", lambda e: e.tensor_tensor(o1[:], po_[:, 0:128], rden[:], ALU.mult), [po_, rden], [o1])
                    V("pool", lambda e: e.tensor_tensor(yob[:, qs], o1[:], sg[:, qs], ALU.mult), [o1, sg], [yob])
                P.dma("sp", [(ysT[1024 + h * 128:1024 + (h + 1) * 128, :], yob[:])], reads=[yob], writes=[ysT])

    def phase_rwp(l):
        with P.scope():
            c0 = TM_COLS["rw_r"]
            mub = P.sb([128, 2240], F32, "rmub")
            mu = I["rwkv_mu"]
            P.dma("sp", [(mub[:, 0:2048], mu[l:l + 1, 0:2048].to_broadcast([128, 2048])), (mub[:, 2048:2240], mu[l:l + 1, 3072:3264].to_broadcast([128, 192]))], reads=[mu], writes=[mub])

            def bc(name, n=1024):
                t = P.sb([128, n], F32, "rb" + name)
                src = I[name]
                if name == "rwkv_r_k":
                    ap = src.t[l:l + 1, :, :].rearrange("a h k -> a (h k)").to_broadcast([128, n])
                else:
                    ap = src[l:l + 1, :].to_broadcast([128, n])
                P.dma("sp", [(t[:], ap)], reads=[src], writes=[t])
                return t
            kkb, kab, w0b, a0b, rkb = bc("rwkv_k_k"), bc("rwkv_k_a"), bc("rwkv_w0"), bc("rwkv_a0"), bc("rwkv_r_k")
            omka = P.sb([128, 1024], F32, "romka")
            V("dve", lambda e: e.tensor_scalar(omka[:], kab[:], -1.0, 1.0, op0=ALU.mult, op1=ALU.add), [kab], [omka])
            ww2 = P.sb([96, 1024], F32, "rww2")
            wa2 = P.sb([96, 1024], F32, "rwa2")
            P.dma("sp", [(ww2[:], I["rwkv_w_w2"].t[l, :, :])], reads=[I["rwkv_w_w2"]], writes=[ww2])
            P.dma("sp", [(wa2[:], I["rwkv_w_a2"].t[l, :, :])], reads=[I["rwkv_w_a2"]], writes=[wa2])
            cur = [P.sb([128, 2240], F32, "rcur") for _ in range(2)]
            prv = [P.sb([128, 2240], F32, "rprv") for _ in range(2)]
            dd = P.sb([128, 2240], F32, "rdd")
            th = P.sb([128, 96], F32, "rth")
            thT = P.sb([96, 128], F32, "rthT")
            alT = P.sb([96, 128], F32, "ralT")
            ptT = [P.ps([96, 128], F32, "rptT") for _ in range(2)]
            pz = [P.ps([128, 2, 512], F32, "rpz") for _ in range(2)]
            zs = P.sb([128, 1024], F32, "rzs")
            aa = P.sb([128, 1024], F32, "raa")
            kk = P.sb([128, 1024], F32, "rkk")
            sq = P.sb([128, 1024], F32, "rsq")
            ssq = P.sb([128, 16], F32, "rssq")
            tt = P.sb([128, 1024], F32, "rtt")
            out5 = [P.sb([128, 5, 1024], F32, "rout5") for _ in range(2)]
            bon = [P.sb([128, 16], F32, "rbon") for _ in range(2)]

            def loadt(i):
                P.dma("sp", [(cur[i % 2][:], tm[i * 128:(i + 1) * 128, c0:c0 + 2240])], reads=[tm], writes=[cur[i % 2]])
                if i == 0:
                    V("pool", lambda e: e.memset(prv[0][0:1, :], 0.0), [], [prv[0]])
                    P.dma("sp", [(prv[0][1:128, :], tm[0:127, c0:c0 + 2240])], reads=[tm], writes=[prv[0]])
                else:
                    P.dma("sp", [(prv[i % 2][:], tm[i * 128 - 1:i * 128 + 127, c0:c0 + 2240])], reads=[tm], writes=[prv[i % 2]])
            loadt(0)
            for i in range(NT):
                if i + 1 < NT:
                    loadt(i + 1)
                cu, pr, o5, bn = cur[i % 2], prv[i % 2], out5[i % 2], bon[i % 2]
                V("dve", lambda e: e.tensor_tensor(dd[:], pr[:], cu[:], ALU.subtract), [pr, cu], [dd])
                V("pool", lambda e: e.tensor_tensor(dd[:], dd[:], mub[:], ALU.mult), [dd, mub], [dd])
                V("dve", lambda e: e.tensor_tensor(cu[:], cu[:], dd[:], ALU.add), [cu, dd], [cu])
                rr_, k_ = cu[:, 0:1024], cu[:, 1024:2048]
                V("act", lambda e: e.activation(th[:], cu[:, 2048:2144], AF.Tanh), [cu], [th])
                V("pe", lambda e: e.transpose(ptT[0][:], th[:], ident_f[:]), [th, ident_f], [ptT[0]])
                V("act", lambda e: e.copy(thT[:], ptT[0][:]), [ptT[0]], [thT])
                V("pe", lambda e: e.transpose(ptT[1][:], cu[:, 2144:2240], ident_f[:]), [cu, ident_f], [ptT[1]])
                V("dve", lambda e: e.tensor_copy(alT[:], ptT[1][:]), [ptT[1]], [alT])
                for n2 in range(2):
                    V("pe", lambda e: e.matmul(pz[0][:, n2, :], thT[:], ww2[:, n2 * 512:(n2 + 1) * 512], start=True, stop=True), [thT, ww2], [pz[0]])
                    V("pe", lambda e: e.matmul(pz[1][:, n2, :], alT[:], wa2[:, n2 * 512:(n2 + 1) * 512], start=True, stop=True), [alT, wa2], [pz[1]])
                V("dve", lambda e: e.tensor_tensor(zs[:], pz[0][:].rearrange("p a b -> p (a b)"), w0b[:], ALU.add), [pz[0], w0b], [zs])
                V("act", lambda e: e.activation(zs[:], zs[:], AF.Sigmoid), [zs], [zs])
                V("act", lambda e: e.activation(o5[:, 0, :], zs[:], AF.Exp, scale=-0.606531), [zs], [o5])
                V("dve", lambda e: e.tensor_tensor(aa[:], pz[1][:].rearrange("p a b -> p (a b)"), a0b[:], ALU.add), [pz[1], a0b], [aa])
                V("act", lambda e: e.activation(aa[:], aa[:], AF.Sigmoid), [aa], [aa])
                V("dve", lambda e: e.tensor_tensor(kk[:], k_, kkb[:], ALU.mult), [cu, kkb], [kk])
                V("act", lambda e: e.activation(sq[:], kk[:], AF.Square), [kk], [sq])
                V("dve", lambda e: e.tensor_reduce(ssq[:], sq[:].rearrange("p (h k) -> p h k", k=64), AX.X, ALU.add), [sq], [ssq])
                V("act", lambda e: e.activation(ssq[:], ssq[:], AF.Sqrt, bias=1e-12, scale=1.0), [ssq], [ssq])
                V("dve", lambda e: e.reciprocal(ssq[:], ssq[:]), [ssq], [ssq])
                V("dve", lambda e: e.tensor_tensor(o5[:, 1, :].rearrange("p (h k) -> p h k", k=64), kk[:].rearrange("p (h k) -> p h k", k=64),
                                                   ssq[:, :].unsqueeze(2).to_broadcast([128, 16, 64]), ALU.mult), [kk, ssq], [o5])
                V("dve", lambda e: e.scalar_tensor_tensor(o5[:, 2, :], o5[:, 1, :], -1.0, aa[:], op0=ALU.mult, op1=ALU.mult), [o5, aa], [o5])
                V("pool", lambda e: e.tensor_tensor(tt[:], aa[:], kab[:], ALU.mult), [aa, kab], [tt])
                V("pool", lambda e: e.tensor_tensor(tt[:], tt[:], omka[:], ALU.add), [tt, omka], [tt])
                V("dve", lambda e: e.tensor_tensor(o5[:, 3, :], k_, tt[:], ALU.mult), [cu, tt], [o5])
                V("pool", lambda e: e.tensor_copy(o5[:, 4, :], rr_), [cu], [o5])
                V("pool", lambda e: e.tensor_tensor(tt[:], rr_, rkb[:], ALU.mult), [cu, rkb], [tt])
                V("dve", lambda e: e.tensor_tensor(tt[:], tt[:], o5[:, 3, :], ALU.mult), [tt, o5], [tt])
                V("dve", lambda e: e.tensor_reduce(bn[:], tt[:].rearrange("p (h k) -> p h k", k=64), AX.X, ALU.add), [tt], [bn])
                P.dma("sp", [(rwtm[i * 128:(i + 1) * 128, :, :], o5[:])], reads=[o5], writes=[rwtm])
                P.dma("sp", [(rwbon[i * 128:(i + 1) * 128, :], bn[:])], reads=[bn], writes=[rwbon])

    def phase_rws(l):
        with P.scope():
            sel = P.sb([128, 64, 128], F32, "wsel")
            blk = P.sb([128, 128], F32, "wblk")
            blkm = P.sb([128, 128], F32, "wblkm")
            i2 = P.sb([128, 64], F32, "wi2")
            P.dma("sp", [(sel[:], I["c_sel"][:, :, :])], reads=[I["c_sel"]], writes=[sel])
            P.dma("sp", [(blk[:], I["c_blk"][:, :])], reads=[I["c_blk"]], writes=[blk])
            P.dma("sp", [(i2[:], I["c_i2"][:, :])], reads=[I["c_i2"]], writes=[i2])
            V("dve", lambda e: e.tensor_scalar(blkm[:], blk[:], 1.0 / 64.0, None, op0=ALU.mult), [blk], [blkm])

            def hv(name, off=0):
                t = P.sb([128, 8], F32, "w" + name)
                src = I[name]
                P.dma("sp", [(t[hf * 64:(hf + 1) * 64, :], src.t[l, off + hf * 512:off + (hf + 1) * 512].rearrange("(h v) -> v h", v=64)) for hf in range(2)],
                      reads=[src], writes=[t], allow_slow_non_contiguous=True)
                return t
            lng, lnb = hv("rwkv_lnx_g"), hv("rwkv_lnx_b")
            muv, mug = hv("rwkv_mu", 2048), hv("rwkv_mu", 3264)
            St = P.sb([128, 8, 64], F32, "wS")
            V("pool", lambda e: e.memset(St[:], 0.0), [], [St])
            X = [P.sb([128, 5, 512], F32, "wX") for _ in range(2)]
            vch = [P.sb([128, 8, 65], F32, "wvch") for _ in range(2)]
            gch = [P.sb([128, 8, 65], F32, "wgch") for _ in range(2)]
            s2 = [P.sb([128, 8], F32, "ws2") for _ in range(2)]
            vl = P.sb([128, 8, 64], F32, "wvl")
            gl = P.sb([128, 8, 64], F32, "wgl")
            bcs = [[P.sb([128, 512], F32, "wbc") for _ in range(5)] for _ in range(2)]
            pb = [P.ps([128, 512], F32, "wpb") for _ in range(5)]
            pq = [P.ps([128, 512], F32, "wpq") for _ in range(2)]
            Y = P.sb([128, 8, 64], F32, "wY")
            tA = P.sb([128, 8, 64], F32, "wtA")
            tB = [P.sb([128, 8, 64], F32, "wtB") for _ in range(2)]
            sa = P.sb([128, 8], F32, "wsa")
            mean = P.sb([128, 512], F32, "wmean")
            yc = P.sb([128, 8, 64], F32, "wyc")
            sq = P.sb([128, 512], F32, "wsq")
            rstd = P.sb([128, 512], F32, "wrstd")
            rh = [P.sb([128, 64], F32, "wrh") for _ in range(2)]
            yb = [P.sb([128, 8, 64], BF16, "wyb") for _ in range(2)]
            rv, rg = FM_ROWS["rw_v"], FM_ROWS["rw_gate"]

            def b3(t2):
                return t2[:, :].unsqueeze(2).to_broadcast([128, 8, 64])

            def loadc(c):
                t0 = c * 64
                P.dma("sp", [(X[c % 2][hf * 64:(hf + 1) * 64, :, :], rwtm.t[t0:t0 + 64, :, hf * 512:(hf + 1) * 512]) for hf in range(2)], reads=[rwtm], writes=[X[c % 2]])
                P.dma("sp", [(vch[c % 2][hf * 64:(hf + 1) * 64, :, :], fm.t[rv + hf * 512:rv + (hf + 1) * 512, PAD + t0 - 1:PAD + t0 + 64].rearrange("(h v) t -> v h t", v=64)) for hf in range(2)],
                      reads=[fm], writes=[vch[c % 2]])
                P.dma("sp", [(gch[c % 2][hf * 64:(hf + 1) * 64, :, :], fm.t[rg + hf * 512:rg + (hf + 1) * 512, PAD + t0 - 1:PAD + t0 + 64].rearrange("(h v) t -> v h t", v=64)) for hf in range(2)],
                      reads=[fm], writes=[gch[c % 2]])
                P.dma("sp", [(s2[c % 2][hf * 64:(hf + 1) * 64, :], rwbon[t0:t0 + 64, hf * 8:(hf + 1) * 8]) for hf in range(2)], reads=[rwbon], writes=[s2[c % 2]])
            loadc(0)
            step = 0
            for c in range(NCH):
                if c + 1 < NCH:
                    loadc(c + 1)
                t0 = c * 64
                Xc, vc, gc, s2c = X[c % 2], vch[c % 2], gch[c % 2], s2[c % 2]
                for (src, dst, mu_) in ((vc, vl, muv), (gc, gl, mug)):
                    V("pool", lambda e: e.tensor_tensor(dst[:], src[:, :, 0:64], src[:, :, 1:65], ALU.subtract), [src], [dst])
                    V("pool", lambda e: e.tensor_tensor(dst[:], dst[:], b3(mu_), ALU.mult), [dst, mu_], [dst])
                    V("pool", lambda e: e.tensor_tensor(dst[:], dst[:], src[:, :, 1:65], ALU.add), [dst, src], [dst])
                for tl in range(64):
                    bset = bcs[step % 2]
                    tBs = tB[step % 2]
                    step += 1
                    for p in range(5):
                        V("pe", lambda e: e.matmul(pb[p][:], sel[:, tl, :], Xc[:, p, :], start=True, stop=True), [sel, Xc], [pb[p]])
                        V("act", lambda e: e.copy(bset[p][:], pb[p][:]), [pb[p]], [bset[p]])
                    wB, kkB, kkanB, k2B, rB = [b[:].rearrange("p (h k) -> p h k", k=64) for b in bset]
                    V("pool", lambda e: e.tensor_tensor(tBs[:], k2B, vl[:, :, tl:tl + 1].to_broadcast([128, 8, 64]), ALU.mult), [bset[3], vl], [tBs])
                    V("dve", lambda e: e.tensor_tensor(tA[:], St[:], kkB, ALU.mult), [St, bset[1]], [tA])
                    V("dve", lambda e: e.tensor_reduce(sa[:], tA[:], AX.X, ALU.add), [tA], [sa])
                    V("dve", lambda e: e.tensor_tensor(St[:], St[:], wB, ALU.mult), [St, bset[0]], [St])
                    V("dve", lambda e: e.tensor_tensor(tA[:], kkanB, b3(sa), ALU.mult), [bset[2], sa], [tA])
                    V("dve", lambda e: e.tensor_tensor(St[:], St[:], tA[:], ALU.add), [St, tA], [St])
                    V("dve", lambda e: e.tensor_tensor(St[:], St[:], tBs[:], ALU.add), [St, tBs], [St])
                    V("dve", lambda e: e.tensor_tensor(tA[:], St[:], rB, ALU.mult), [St, bset[4]], [tA])
                    V("dve", lambda e: e.tensor_reduce(Y[:, :, tl], tA[:], AX.X, ALU.add), [tA], [Y])
                Yf = Y[:].rearrange("p h t -> p (h t)")
                V("pe", lambda e: e.matmul(pq[0][:], blkm[:], Yf, start=True, stop=True), [blkm, Y], [pq[0]])
                V("dve", lambda e: e.tensor_tensor(yc[:].rearrange("p h t -> p (h t)"), Yf, pq[0][:], ALU.subtract), [Y, pq[0]], [yc])
                V("act", lambda e: e.activation(sq[:], yc[:].rearrange("p h t -> p (h t)"), AF.Square), [yc], [sq])
                V("pe", lambda e: e.matmul(pq[1][:], blkm[:], sq[:], start=True, stop=True), [blkm, sq], [pq[1]])
                V("act", lambda e: e.activation(rstd[:], pq[1][:], AF.Sqrt, bias=64e-5, scale=1.0), [pq[1]], [rstd])
                V("dve", lambda e: e.reciprocal(rstd[:], rstd[:]), [rstd], [rstd])
                V("dve", lambda e: e.tensor_tensor(yc[:].rearrange("p h t -> p (h t)"), yc[:].rearrange("p h t -> p (h t)"), rstd[:], ALU.mult), [yc, rstd], [yc])
                V("dve", lambda e: e.tensor_tensor(yc[:], yc[:], b3(lng), ALU.mult), [yc, lng], [yc])
                V("dve", lambda e: e.tensor_tensor(yc[:], yc[:], b3(lnb), ALU.add), [yc, lnb], [yc])
                for h8 in range(8):
                    rhh = rh[h8 % 2]
                    V("pool", lambda e: e.tensor_scalar(rhh[:], i2[:], s2c[:, h8:h8 + 1], None, op0=ALU.mult), [i2, s2c], [rhh])
                    V("pe", lambda e: e.matmul(pq[0][:, h8 * 64:(h8 + 1) * 64], blk[:], rhh[:], start=True, stop=True), [blk, rhh], [pq[0]])
                V("dve", lambda e: e.tensor_tensor(tA[:].rearrange("p h t -> p (h t)"), pq[0][:], vl[:].rearrange("p h t -> p (h t)"), ALU.mult), [pq[0], vl], [tA])
                V("dve", lambda e: e.tensor_tensor(yc[:], yc[:], tA[:], ALU.add), [yc, tA], [yc])
                V("act", lambda e: e.activation(gl[:], gl[:], AF.Silu), [gl], [gl])
                ybc = yb[c % 2]
                V("dve", lambda e: e.tensor_tensor(ybc[:], yc[:], gl[:], ALU.mult), [yc, gl], [ybc])
                P.dma("sp", [(ysT.t[2048 + hf * 512:2048 + (hf + 1) * 512, t0:t0 + 64].rearrange("(h v) t -> v h t", v=64), ybc[hf * 64:(hf + 1) * 64, :, :]) for hf in range(2)],
                      reads=[ybc], writes=[ysT])


    def phase_rwp2(l):
        with P.scope():
            c0 = TM_COLS["rw_r"]
            NCc = 3264
            mub = P.sb([128, NCc], F32, "rmub")
            mu = I["rwkv_mu"]
            P.dma("sp", [(mub[:], mu[l:l + 1, 0:NCc].to_broadcast([128, NCc]))], reads=[mu], writes=[mub])

            def bc(name, n=1024):
                t = P.sb([128, n], F32, "rb" + name)
                src = I[name]
                if name == "rwkv_r_k":
                    ap = src.t[l:l + 1, :, :].rearrange("a h k -> a (h k)").to_broadcast([128, n])
                else:
                    ap = src[l:l + 1, :].to_broadcast([128, n])
                P.dma("sp", [(t[:], ap)], reads=[src], writes=[t])
                return t
            kkb, kab, w0b, a0b, rkb = bc("rwkv_k_k"), bc("rwkv_k_a"), bc("rwkv_w0"), bc("rwkv_a0"), bc("rwkv_r_k")
            omka = P.sb([128, 1024], F32, "romka")
            V("dve", lambda e: e.tensor_scalar(omka[:], kab[:], -1.0, 1.0, op0=ALU.mult, op1=ALU.add), [kab], [omka])
            ww2 = P.sb([96, 1024], F32, "rww2")
            wa2 = P.sb([96, 1024], F32, "rwa2")
            lmat = P.sb([128, 128], F32, "rlmat")
            umat = P.sb([128, 128], F32, "rumat")
            ind = P.sb([128, 4], F32, "rind")
            P.dma("sp", [(ww2[:], I["rwkv_w_w2"].t[l, :, :])], reads=[I["rwkv_w_w2"]], writes=[ww2])
            P.dma("sp", [(wa2[:], I["rwkv_w_a2"].t[l, :, :])], reads=[I["rwkv_w_a2"]], writes=[wa2])
            P.dma("sp", [(lmat[:], I["c_lmat32"][:, :])], reads=[I["c_lmat32"]], writes=[lmat])
            P.dma("sp", [(umat[:], I["c_umat32"][:, :])], reads=[I["c_umat32"]], writes=[umat])
            P.dma("sp", [(ind[:], I["c_ind32"][:, :])], reads=[I["c_ind32"]], writes=[ind])
            cur = [P.sb([128, NCc], F32, "rcur") for _ in range(2)]
            prv = P.sb([128, NCc], F32, "rprv")
            dd = P.sb([128, NCc], F32, "rdd")
            th = P.sb([128, 96], F32, "rth")
            thT = P.sb([96, 128], F32, "rthT")
            alT = P.sb([96, 128], F32, "ralT")
            pz = [P.ps([128, 2, 512], F32, "rpz") for _ in range(2)]
            ptr = [P.ps([64, 4, 128], F32, "rptr") for _ in range(2)]
            ptT2 = P.ps([96, 2, 128], F32, "rptT")
            pdec = P.ps([64, 16, 4], F32, "rpdec")
            zs = P.sb([128, 1024], F32, "rzs")
            aa = P.sb([128, 1024], F32, "raa")
            kk = P.sb([128, 1024], F32, "rkk")
            kkn = P.sb([128, 1024], F32, "rkkn")
            kkan = P.sb([128, 1024], F32, "rkkan")
            k2 = P.sb([128, 1024], F32, "rk2")
            ssq = P.sb([128, 16], F32, "rssq")
            tt = P.sb([128, 1024], F32, "rtt")
            E1 = P.sb([128, 1024], F32, "rE1")
            Q = P.sb([128, 4, 1024], F32, "rQ")
            o3 = [P.sb([128, 3, 1024], F32, "ro3") for _ in range(2)]
            bon = [P.sb([128, 16], F32, "rbon") for _ in range(2)]
            XTs = P.sb([64, 16, 4, 128], F32, "rXTs")
            decs = P.sb([64, 16, 4], F32, "rdecs")

            def loadt(i):
                P.dma("sp", [(cur[i % 2][:], tm[i * 128:(i + 1) * 128, c0:c0 + NCc])], reads=[tm], writes=[cur[i % 2]])
            loadt(0)
            for i in range(NT):
                if i + 1 < NT:
                    loadt(i + 1)
                cu, o3i, bn = cur[i % 2], o3[i % 2], bon[i % 2]
                if i == 0:
                    V("pool", lambda e: e.memset(prv[0:1, :], 0.0), [], [prv])
                    P.dma("sp", [(prv[1:128, :], tm[0:127, c0:c0 + NCc])], reads=[tm], writes=[prv])
                else:
                    P.dma("sp", [(prv[:], tm[i * 128 - 1:i * 128 + 127, c0:c0 + NCc])], reads=[tm], writes=[prv])
                V("dve", lambda e: e.tensor_tensor(dd[:], prv[:], cu[:], ALU.subtract), [prv, cu], [dd])
                V("pool", lambda e: e.tensor_tensor(dd[:], dd[:], mub[:], ALU.mult), [dd, mub], [dd])
                V("dve", lambda e: e.tensor_tensor(cu[:], cu[:], dd[:], ALU.add), [cu, dd], [cu])
                rr_, k_, v_ = cu[:, 0:1024], cu[:, 1024:2048], cu[:, 2048:3072]
                V("act", lambda e: e.activation(th[:], cu[:, 3072:3168], AF.Tanh), [cu], [th])
                V("pe", lambda e: e.transpose(ptT2[:, 0, :], th[:], ident_f[:]), [th, ident_f], [ptT2])
                V("pe", lambda e: e.transpose(ptT2[:, 1, :], cu[:, 3168:3264], ident_f[:]), [cu, ident_f], [ptT2])
                V("act", lambda e: e.copy(thT[:], ptT2[:, 0, :]), [ptT2], [thT])
                V("dve", lambda e: e.tensor_copy(alT[:], ptT2[:, 1, :]), [ptT2], [alT])
                for n2 in range(2):
                    V("pe", lambda e: e.matmul(pz[0][:, n2, :], thT[:], ww2[:, n2 * 512:(n2 + 1) * 512], start=True, stop=True), [thT, ww2], [pz[0]])
                    V("pe", lambda e: e.matmul(pz[1][:, n2, :], alT[:], wa2[:, n2 * 512:(n2 + 1) * 512], start=True, stop=True), [alT, wa2], [pz[1]])
                V("dve", lambda e: e.tensor_tensor(zs[:], pz[0][:].rearrange("p a b -> p (a b)"), w0b[:], ALU.add), [pz[0], w0b], [zs])
                V("act", lambda e: e.activation(zs[:], zs[:], AF.Sigmoid), [zs], [zs])
                V("dve", lambda e: e.tensor_scalar(zs[:], zs[:], -0.606531, None, op0=ALU.mult), [zs], [zs])
                V("dve", lambda e: e.tensor_tensor(aa[:], pz[1][:].rearrange("p a b -> p (a b)"), a0b[:], ALU.add), [pz[1], a0b], [aa])
                V("act", lambda e: e.activation(aa[:], aa[:], AF.Sigmoid), [aa], [aa])
                V("dve", lambda e: e.tensor_tensor(kk[:], k_, kkb[:], ALU.mult), [cu, kkb], [kk])
                V("act", lambda e: e.activation(tt[:], kk[:], AF.Square), [kk], [tt])
                V("dve", lambda e: e.tensor_reduce(ssq[:], tt[:].rearrange("p (h k) -> p h k", k=64), AX.X, ALU.add), [tt], [ssq])
                V("act", lambda e: e.activation(ssq[:], ssq[:], AF.Sqrt, bias=1e-12, scale=1.0), [ssq], [ssq])
                V("dve", lambda e: e.reciprocal(ssq[:], ssq[:]), [ssq], [ssq])
                V("dve", lambda e: e.tensor_tensor(kkn[:].rearrange("p (h k) -> p h k", k=64), kk[:].rearrange("p (h k) -> p h k", k=64),
                                                   ssq[:, :].unsqueeze(2).to_broadcast([128, 16, 64]), ALU.mult), [kk, ssq], [kkn])
                V("dve", lambda e: e.scalar_tensor_tensor(kkan[:], kkn[:], -1.0, aa[:], op0=ALU.mult, op1=ALU.mult), [kkn, aa], [kkan])
                V("pool", lambda e: e.tensor_tensor(tt[:], aa[:], kab[:], ALU.mult), [aa, kab], [tt])
                V("pool", lambda e: e.tensor_tensor(tt[:], tt[:], omka[:], ALU.add), [tt, omka], [tt])
                V("dve", lambda e: e.tensor_tensor(k2[:], k_, tt[:], ALU.mult), [cu, tt], [k2])
                V("pool", lambda e: e.tensor_tensor(tt[:], rr_, rkb[:], ALU.mult), [cu, rkb], [tt])
                V("dve", lambda e: e.tensor_tensor(tt[:], tt[:], k2[:], ALU.mult), [tt, k2], [tt])
                V("dve", lambda e: e.tensor_reduce(bn[:], tt[:].rearrange("p (h k) -> p h k", k=64), AX.X, ALU.add), [tt], [bn])
                for n2 in range(2):
                    V("pe", lambda e: e.matmul(pz[0][:, n2, :], lmat[:], zs[:, n2 * 512:(n2 + 1) * 512], start=True, stop=True), [lmat, zs], [pz[0]])
                    V("pe", lambda e: e.matmul(pz[1][:, n2, :], umat[:], zs[:, n2 * 512:(n2 + 1) * 512], start=True, stop=True), [umat, zs], [pz[1]])
                cwf = pz[0][:].rearrange("p a b -> p (a b)")
                gf = pz[1][:].rearrange("p a b -> p (a b)")
                for n2 in range(2):
                    V("act", lambda e: e.activation(E1[:, n2 * 512:(n2 + 1) * 512], pz[0][:, n2, :], AF.Exp), [pz[0]], [E1])
                V("dve", lambda e: e.tensor_tensor(Q[:, 3, :], rr_, E1[:], ALU.mult), [cu, E1], [Q])
                for n2 in range(2):
                    V("act", lambda e: e.activation(E1[:, n2 * 512:(n2 + 1) * 512], pz[0][:, n2, :], AF.Exp, scale=-1.0), [pz[0]], [E1])
                V("dve", lambda e: e.tensor_tensor(Q[:, 0, :], kkan[:], E1[:], ALU.mult), [kkan, E1], [Q])
                V("pool", lambda e: e.tensor_tensor(Q[:, 1, :], k2[:], E1[:], ALU.mult), [k2, E1], [Q])
                V("dve", lambda e: e.tensor_tensor(tt[:], cwf, zs[:], ALU.subtract), [pz[0], zs], [tt])
                V("act", lambda e: e.activation(E1[:], tt[:], AF.Exp), [tt], [E1])
                V("dve", lambda e: e.tensor_tensor(Q[:, 2, :], kkn[:], E1[:], ALU.mult), [kkn, E1], [Q])
                for n2 in range(2):
                    V("act", lambda e: e.activation(E1[:, n2 * 512:(n2 + 1) * 512], pz[1][:, n2, :], AF.Exp), [pz[1]], [E1])
                V("dve", lambda e: e.tensor_tensor(o3i[:, 0, :], kkan[:], E1[:], ALU.mult), [kkan, E1], [o3i])
                V("pool", lambda e: e.tensor_tensor(o3i[:, 1, :], k2[:], E1[:], ALU.mult), [k2, E1], [o3i])
                V("pool", lambda e: e.tensor_copy(o3i[:, 2, :], v_), [cu], [o3i])
                for h in range(16):
                    V("pe", lambda e: e.matmul(pdec[:, h, :], zs[:, h * 64:(h + 1) * 64], ind[:], start=True, stop=True), [zs, ind], [pdec])
                V("act", lambda e: e.activation(decs[:], pdec[:], AF.Exp), [pdec], [decs])
                P.dma("sp", [(rwdec[:, :, i * 4:(i + 1) * 4], decs[:])], reads=[decs], writes=[rwdec])
                for h in range(16):
                    pt = ptr[h % 2]
                    for q in range(4):
                        V("pe", lambda e: e.transpose(pt[:, q, :], Q[:, q, h * 64:(h + 1) * 64], ident_f[:]), [Q, ident_f], [pt])
                    evac(h, XTs[:, h, :, :], pt[:], [pt], [XTs])
                P.dma("sp", [(rwxt.t[:, :, i, c4 * 128:(c4 + 1) * 128].rearrange("k h (q t) -> k h q t", t=32), XTs[:, :, :, c4 * 32:(c4 + 1) * 32]) for c4 in range(4)],
                      reads=[XTs], writes=[rwxt])
                P.dma("sp", [(rwtm2[i * 128:(i + 1) * 128, :, :], o3i[:])], reads=[o3i], writes=[rwtm2])
                P.dma("sp", [(rwbon[i * 128:(i + 1) * 128, :], bn[:])], reads=[bn], writes=[rwbon])

    def phase_rws2(l):
        with P.scope():
            mtp = P.sb([64, 64], F32, "wmtp")
            mn = P.sb([32, 32], F32, "wmn")
            ones64 = P.sb([64, 64], F32, "wones")
            onesm = P.sb([64, 64], F32, "wonesm")
            P.dma("sp", [(mtp[:], I["c_m32tp"][:, :])], reads=[I["c_m32tp"]], writes=[mtp])
            P.dma("sp", [(mn[:], I["c_m32n"][:, :])], reads=[I["c_m32n"]], writes=[mn])
            V("pool", lambda e: e.memset(ones64[:], 1.0), [], [ones64])
            V("pool", lambda e: e.memset(onesm[:], 1.0 / 64.0), [], [onesm])

            def hv(name, off=0):
                t = P.sb([64, 16], F32, "w" + name)
                src = I[name]
                P.dma("sp", [(t[:], src.t[l, off:off + 1024].rearrange("(h v) -> v h", v=64))], reads=[src], writes=[t], allow_slow_non_contiguous=True)
                return t
            lng, lnb = hv("rwkv_lnx_g"), hv("rwkv_lnx_b")
            muv, mug = hv("rwkv_mu", 2048), hv("rwkv_mu", 3264)
            decall = P.sb([64, 16, NC32], F32, "wdec")
            P.dma("sp", [(decall[:], rwdec[:, :, :])], reads=[rwdec], writes=[decall])
            H = P.sb([64, 16, 64], F32, "wH")
            V("pool", lambda e: e.memset(H[:], 0.0), [], [H])
            XT = [P.sb([64, 16, 4, 4, 32], F32, "wXT") for _ in range(2)]
            BK = [P.sb([64, 16, 64], F32, "wBK") for _ in range(2)]
            UV = [P.sb([64, 16, 64], F32, "wUV") for _ in range(2)]
            MT = P.sb([64, 16, 64], F32, "wMT")
            Pm = [P.sb([32, 16, 32], F32, "wPm") for _ in range(2)]
            PTm = [P.sb([32, 16, 32], F32, "wPTm") for _ in range(2)]
            Zs = P.sb([32, 16, 64], F32, "wZs")
            pTP = P.ps([64, 16, 64], F32, "wpTP")
            pZ = P.ps([64, 16, 64], F32, "wpZ")
            pN = P.ps([64, 16, 32], F32, "wpN")
            pP2 = P.ps([32, 16, 32], F32, "wpP2")
            pP2T = P.ps([32, 16, 32], F32, "wpP2T")
            Yt = P.sb([64, 16, 64], F32, "wYt")
            vch = [P.sb([64, 16, 65], F32, "wvch") for _ in range(2)]
            gch = [P.sb([64, 16, 65], F32, "wgch") for _ in range(2)]
            s2 = [P.sb([64, 16], F32, "ws2") for _ in range(2)]
            vl = P.sb([64, 16, 64], F32, "wvl")
            gl = P.sb([64, 16, 64], F32, "wgl")
            yc = P.sb([64, 16, 64], F32, "wyc")
            sq = P.sb([64, 16, 64], F32, "wsq")
            rstd = P.sb([64, 16, 64], F32, "wrstd")
            tA = P.sb([64, 16, 64], F32, "wtA")
            rh = [P.sb([64, 64], F32, "wrh") for _ in range(2)]
            yb = [P.sb([64, 16, 64], BF16, "wyb") for _ in range(2)]
            rv, rg = FM_ROWS["rw_v"], FM_ROWS["rw_gate"]

            def b3(t2):
                return t2[:, :].unsqueeze(2).to_broadcast([64, 16, 64])

            def fl(b, np_=64):
                return b[0:np_, :, :].rearrange("p h t -> p (h t)")

            def loadtile(i):
                P.dma("sp", [(XT[i % 2][:].rearrange("k h c q t -> k h (c q t)"), rwxt.t[:, :, i, :])], reads=[rwxt], writes=[XT[i % 2]])

            def loadchunk(c):
                t0 = c * 32
                P.dma("sp", [(BK[c % 2][0:32, :, :].rearrange("p h k -> p (h k)"), rwtm2.t[t0:t0 + 32, 0, :]),
                             (BK[c % 2][32:64, :, :].rearrange("p h k -> p (h k)"), rwtm2.t[t0:t0 + 32, 1, :])], reads=[rwtm2], writes=[BK[c % 2]])
                P.dma("sp", [(UV[c % 2][32:64, :, :].rearrange("p h k -> p (h k)"), rwtm2.t[t0:t0 + 32, 2, :])], reads=[rwtm2], writes=[UV[c % 2]])

            def loadepi(g):
                t0 = g * 64
                P.dma("sp", [(vch[g % 2][:], fm.t[rv:rv + 1024, PAD + t0 - 1:PAD + t0 + 64].rearrange("(h v) t -> v h t", v=64))], reads=[fm], writes=[vch[g % 2]])
                P.dma("sp", [(gch[g % 2][:], fm.t[rg:rg + 1024, PAD + t0 - 1:PAD + t0 + 64].rearrange("(h v) t -> v h t", v=64))], reads=[fm], writes=[gch[g % 2]])
                P.dma("sp", [(s2[g % 2][:], rwbon[t0:t0 + 64, :])], reads=[rwbon], writes=[s2[g % 2]])
            loadtile(0)
            loadchunk(0)
            loadepi(0)
            ke = 0
            for i in range(NT):
                if i + 1 < NT:
                    loadtile(i + 1)
                X = XT[i % 2]
                for cc in range(4):
                    c = i * 4 + cc
                    if c + 1 < NC32:
                        loadchunk(c + 1)
                    BKc, UVc = BK[c % 2], UV[c % 2]
                    for h in range(16):
                        V("pe", lambda e: e.matmul(pTP[:, h, :], X[:, h, cc, 0:2, :].rearrange("k q t -> k (q t)"), X[:, h, cc, 2:4, :].rearrange("k q t -> k (q t)"),
                                                   start=True, stop=True), [X], [pTP])
                    for h in range(16):
                        V("pe", lambda e: e.matmul(pN[0:32, h, :], X[:, h, cc, 2, :], X[:, h, cc, 0, :], start=True, stop=True), [X], [pN])
                    V("dve", lambda e: e.tensor_tensor(MT[:], pTP[:], mtp[:, :].unsqueeze(1).to_broadcast([64, 16, 64]), ALU.mult), [pTP, mtp], [MT])
                    V("dve", lambda e: e.tensor_tensor(Pm[0][:], pN[0:32, :, :], mn[:, :].unsqueeze(1).to_broadcast([32, 16, 32]), ALU.mult), [pN, mn], [Pm[0]])
                    for h in range(16):
                        V("pe", lambda e: e.matmul(pZ[0:32, h, :], X[:, h, cc, 2, :], H[:, h, :], start=True, stop=False), [X, H], [pZ])
                        V("pe", lambda e: e.matmul(pZ[0:32, h, :], MT[32:64, h, 0:32], UVc[32:64, h, :], start=False, stop=True), [MT, UVc], [pZ])
                    for h2 in range(2):
                        V("act", lambda e: e.copy(Zs[:, h2 * 8:(h2 + 1) * 8, :], pZ[0:32, h2 * 8:(h2 + 1) * 8, :]), [pZ], [Zs])
                    for lv in range(5):
                        if lv == 0:
                            PTb, PTv = MT, (lambda h: MT[0:32, h, 0:32])
                        else:
                            PTb, PTv = PTm[lv % 2], (lambda h, b=PTm[lv % 2]: b[:, h, :])
                        Pb = Pm[lv % 2]
                        for h in range(16):
                            V("pe", lambda e: e.matmul(pZ[0:32, h, :], PTv(h), Zs[:, h, :], start=True, stop=True), [PTb, Zs], [pZ])
                        if lv < 4:
                            for h in range(16):
                                V("pe", lambda e: e.matmul(pP2[:, h, :], PTv(h), Pb[:, h, :], start=True, stop=True), [PTb, Pb], [pP2])
                                V("pe", lambda e: e.matmul(pP2T[:, h, :], Pb[:, h, :], PTv(h), start=True, stop=True), [PTb, Pb], [pP2T])
                            V("dve", lambda e: e.tensor_tensor(Zs[:], Zs[:], pZ[0:32, :, :], ALU.add), [Zs, pZ], [Zs])
                            V("act", lambda e: e.copy(Pm[(lv + 1) % 2][:], pP2[:]), [pP2], [Pm[(lv + 1) % 2]])
                            V("dve", lambda e: e.tensor_copy(PTm[(lv + 1) % 2][:], pP2T[:]), [pP2T], [PTm[(lv + 1) % 2]])
                        else:
                            V("dve", lambda e: e.tensor_tensor(UVc[0:32, :, :], Zs[:], pZ[0:32, :, :], ALU.add), [Zs, pZ], [UVc])
                    for h in range(16):
                        V("pe", lambda e: e.matmul(pN[:, h, :], H[:, h, :], X[:, h, cc, 3, :], start=True, stop=False), [H, X], [pN])
                        V("pe", lambda e: e.matmul(pN[:, h, :], UVc[:, h, :], MT[:, h, 32:64], start=False, stop=True), [UVc, MT], [pN])
                    V("act", lambda e: e.copy(Yt[:, :, (cc % 2) * 32:(cc % 2) * 32 + 32], pN[:]), [pN], [Yt])
                    for h in range(16):
                        V("pe", lambda e: e.matmul(pTP[:, h, :], BKc[:, h, :], UVc[:, h, :], start=True, stop=True), [BKc, UVc], [pTP])
                    V("dve", lambda e: e.tensor_tensor(H[:], H[:], decall[:, :, c:c + 1].to_broadcast([64, 16, 64]), ALU.mult), [H, decall], [H])
                    V("dve", lambda e: e.tensor_tensor(H[:], H[:], pTP[:], ALU.add), [H, pTP], [H])
                    if cc % 2 == 1:
                        g = c // 2
                        t0 = g * 64
                        if g + 1 < NCH:
                            loadepi(g + 1)
                        vc, gc, s2c = vch[g % 2], gch[g % 2], s2[g % 2]
                        for (src, dst, mu_) in ((vc, vl, muv), (gc, gl, mug)):
                            V("pool", lambda e: e.tensor_tensor(dst[:], src[:, :, 0:64], src[:, :, 1:65], ALU.subtract), [src], [dst])
                            V("pool", lambda e: e.tensor_tensor(dst[:], dst[:], b3(mu_), ALU.mult), [dst, mu_], [dst])
                            V("pool", lambda e: e.tensor_tensor(dst[:], dst[:], src[:, :, 1:65], ALU.add), [dst, src], [dst])
                        for n2 in range(2):
                            V("pe", lambda e: e.matmul(fl(pTP)[:, n2 * 512:(n2 + 1) * 512], onesm[:], fl(Yt)[:, n2 * 512:(n2 + 1) * 512], start=True, stop=True), [onesm, Yt], [pTP])
                        V("dve", lambda e: e.tensor_tensor(yc[:], Yt[:], pTP[:], ALU.subtract), [Yt, pTP], [yc])
                        V("act", lambda e: e.activation(sq[:], yc[:], AF.Square), [yc], [sq])
                        for n2 in range(2):
                            V("pe", lambda e: e.matmul(fl(pZ)[:, n2 * 512:(n2 + 1) * 512], onesm[:], fl(sq)[:, n2 * 512:(n2 + 1) * 512], start=True, stop=True), [onesm, sq], [pZ])
                        for h2 in range(2):
                            V("act", lambda e: e.activation(rstd[:, h2 * 8:(h2 + 1) * 8, :], pZ[:, h2 * 8:(h2 + 1) * 8, :], AF.Sqrt, bias=64e-5, scale=1.0), [pZ], [rstd])
                        V("dve", lambda e: e.reciprocal(rstd[:], rstd[:]), [rstd], [rstd])
                        V("dve", lambda e: e.tensor_tensor(yc[:], yc[:], rstd[:], ALU.mult), [yc, rstd], [yc])
                        V("dve", lambda e: e.tensor_tensor(yc[:], yc[:], b3(lng), ALU.mult), [yc, lng], [yc])
                        V("dve", lambda e: e.tensor_tensor(yc[:], yc[:], b3(lnb), ALU.add), [yc, lnb], [yc])
                        for h in range(16):
                            rhh = rh[h % 2]
                            V("pool", lambda e: e.tensor_scalar(rhh[:], ident_f[0:64, 0:64], s2c[:, h:h + 1], None, op0=ALU.mult), [ident_f, s2c], [rhh])
                            V("pe", lambda e: e.matmul(pTP[:, h, :], ones64[:], rhh[:], start=True, stop=True), [ones64, rhh], [pTP])
                        V("dve", lambda e: e.tensor_tensor(tA[:], pTP[:], vl[:], ALU.mult), [pTP, vl], [tA])
                        V("dve", lambda e: e.tensor_tensor(yc[:], yc[:], tA[:], ALU.add), [yc, tA], [yc])
                        V("act", lambda e: e.activation(gl[:], gl[:], AF.Silu), [gl], [gl])
                        ybc = yb[g % 2]
                        V("dve", lambda e: e.tensor_tensor(ybc[:], yc[:], gl[:], ALU.mult), [yc, gl], [ybc])
                        P.dma("sp", [(ysT.t[2048:3072, t0:t0 + 64].rearrange("(h v) t -> v h t", v=64), ybc[:])], reads=[ybc], writes=[ysT])


    def phase_rws3(l):
        with P.scope():
            HG = 8
            mtp = P.sb([64, 64], F32, "wmtp")
            mn = P.sb([32, 32], F32, "wmn")
            ones64 = P.sb([64, 64], F32, "wones")
            onesm = P.sb([64, 64], F32, "wonesm")
            P.dma("sp", [(mtp[:], I["c_m32tp"][:, :])], reads=[I["c_m32tp"]], writes=[mtp])
            P.dma("sp", [(mn[:], I["c_m32n"][:, :])], reads=[I["c_m32n"]], writes=[mn])
            V("pool", lambda e: e.memset(ones64[:], 1.0), [], [ones64])
            V("pool", lambda e: e.memset(onesm[:], 1.0 / 64.0), [], [onesm])

            def hv(name, off=0):
                t = P.sb([64, 16], F32, "w" + name)
                src = I[name]
                P.dma("sp", [(t[:], src.t[l, off:off + 1024].rearrange("(h v) -> v h", v=64))], reads=[src], writes=[t], allow_slow_non_contiguous=True)
                return t
            lng, lnb = hv("rwkv_lnx_g"), hv("rwkv_lnx_b")
            muv, mug = hv("rwkv_mu", 2048), hv("rwkv_mu", 3264)
            decall = P.sb([64, 16, NC32], F32, "wdec")
            P.dma("sp", [(decall[:], rwdec[:, :, :])], reads=[rwdec], writes=[decall])
            XT = [P.sb([64, 16, 4, 4, 32], F32, "wXT") for _ in range(2)]
            vch = [P.sb([64, 16, 65], F32, "wvch") for _ in range(2)]
            gch = [P.sb([64, 16, 65], F32, "wgch") for _ in range(2)]
            s2 = [P.sb([64, 16], F32, "ws2") for _ in range(2)]
            vl = P.sb([64, 16, 64], F32, "wvl")
            gl = P.sb([64, 16, 64], F32, "wgl")
            rh = [P.sb([64, 64], F32, "wrh") for _ in range(4)]
            H, MT, PP, Zs, BK, UV, Yt, yc, sq, rstd, tA, yb = [], [], [], [], [], [], [], [], [], [], [], []
            pTP, pZ, pN, pPP = [], [], [], []
            for g in range(2):
                H.append(P.sb([64, HG, 64], F32, "wH"))
                V("pool", lambda e: e.memset(H[g][:], 0.0), [], [H[g]])
                MT.append(P.sb([64, HG, 64], F32, "wMT"))
                PP.append([P.sb([32, 2, HG, 32], F32, "wPP") for _ in range(2)])
                Zs.append(P.sb([32, HG, 64], F32, "wZs"))
                BK.append([P.sb([64, HG, 64], F32, "wBK") for _ in range(2)])
                UV.append([P.sb([64, HG, 64], F32, "wUV") for _ in range(2)])
                Yt.append(P.sb([64, HG, 64], F32, "wYt"))
                yc.append(P.sb([64, HG, 64], F32, "wyc"))
                sq.append(P.sb([64, HG, 64], F32, "wsq"))
                rstd.append(P.sb([64, HG, 64], F32, "wrstd"))
                tA.append(P.sb([64, HG, 64], F32, "wtA"))
                yb.append([P.sb([64, HG, 64], BF16, "wyb") for _ in range(2)])
                pTP.append(P.ps([64, HG, 64], F32, "wpTP"))
                pZ.append(P.ps([64, HG, 64], F32, "wpZ"))
                pN.append(P.ps([64, HG, 32], F32, "wpN"))
                pPP.append(P.ps([32, 2, HG, 32], F32, "wpPP"))
            rv, rg = FM_ROWS["rw_v"], FM_ROWS["rw_gate"]

            def b3(t2, g):
                return t2[:, g * HG:(g + 1) * HG].unsqueeze(2).to_broadcast([64, HG, 64])

            def fl(b):
                return b[:, :, :].rearrange("p h t -> p (h t)")

            def loadtile(i):
                P.dma("sp", [(XT[i % 2][:].rearrange("k h c q t -> k h (c q t)"), rwxt.t[:, :, i, :])], reads=[rwxt], writes=[XT[i % 2]])

            def loadchunk(c):
                t0 = c * 32
                for g in range(2):
                    cs = slice(g * 512, (g + 1) * 512)
                    P.dma("sp", [(BK[g][c % 2][0:32, :, :].rearrange("p h k -> p (h k)"), rwtm2.t[t0:t0 + 32, 0, cs]),
                                 (BK[g][c % 2][32:64, :, :].rearrange("p h k -> p (h k)"), rwtm2.t[t0:t0 + 32, 1, cs])], reads=[rwtm2], writes=[BK[g][c % 2]])
                    P.dma("sp", [(UV[g][c % 2][32:64, :, :].rearrange("p h k -> p (h k)"), rwtm2.t[t0:t0 + 32, 2, cs])], reads=[rwtm2], writes=[UV[g][c % 2]])

            def loadepi(gi):
                t0 = gi * 64
                P.dma("sp", [(vch[gi % 2][:], fm.t[rv:rv + 1024, PAD + t0 - 1:PAD + t0 + 64].rearrange("(h v) t -> v h t", v=64))], reads=[fm], writes=[vch[gi % 2]])
                P.dma("sp", [(gch[gi % 2][:], fm.t[rg:rg + 1024, PAD + t0 - 1:PAD + t0 + 64].rearrange("(h v) t -> v h t", v=64))], reads=[fm], writes=[gch[gi % 2]])
                P.dma("sp", [(s2[gi % 2][:], rwbon[t0:t0 + 64, :])], reads=[rwbon], writes=[s2[gi % 2]])
            loadtile(0)
            loadchunk(0)
            loadepi(0)
            GR = (0, 1)
            for i in range(NT):
                if i + 1 < NT:
                    loadtile(i + 1)
                X = XT[i % 2]
                for cc in range(4):
                    c = i * 4 + cc
                    if c + 1 < NC32:
                        loadchunk(c + 1)
                    for g in GR:
                        for hh in range(HG):
                            h = g * HG + hh
                            V("pe", lambda e: e.matmul(pTP[g][:, hh, :], X[:, h, cc, 0:2, :].rearrange("k q t -> k (q t)"), X[:, h, cc, 2:4, :].rearrange("k q t -> k (q t)"),
                                                       start=True, stop=True), [X], [pTP[g]])
                        for hh in range(HG):
                            h = g * HG + hh
                            V("pe", lambda e: e.matmul(pN[g][0:32, hh, :], X[:, h, cc, 2, :], X[:, h, cc, 0, :], start=True, stop=True), [X], [pN[g]])
                    for g in GR:
                        V("dve", lambda e: e.tensor_tensor(MT[g][:], pTP[g][:], mtp[:, :].unsqueeze(1).to_broadcast([64, HG, 64]), ALU.mult), [pTP[g], mtp], [MT[g]])
                        V("dve", lambda e: e.tensor_tensor(PP[g][0][:, 0, :, :], pN[g][0:32, :, :], mn[:, :].unsqueeze(1).to_broadcast([32, HG, 32]), ALU.mult), [pN[g], mn], [PP[g][0]])
                    for g in GR:
                        UVc = UV[g][c % 2]
                        for hh in range(HG):
                            h = g * HG + hh
                            V("pe", lambda e: e.matmul(pZ[g][0:32, hh, :], X[:, h, cc, 2, :], H[g][:, hh, :], start=True, stop=False), [X, H[g]], [pZ[g]])
                            V("pe", lambda e: e.matmul(pZ[g][0:32, hh, :], MT[g][32:64, hh, 0:32], UVc[32:64, hh, :], start=False, stop=True), [MT[g], UVc], [pZ[g]])
                    for g in GR:
                        V("act", lambda e: e.copy(Zs[g][:], pZ[g][0:32, :, :]), [pZ[g]], [Zs[g]])
                    for lv in range(5):
                        for g in GR:
                            UVc = UV[g][c % 2]
                            if lv == 0:
                                PTb, PTv = MT[g], (lambda hh, b=MT[g]: b[0:32, hh, 0:32])
                            else:
                                PTb, PTv = PP[g][lv % 2], (lambda hh, b=PP[g][lv % 2]: b[:, 1, hh, :])
                            Pb = PP[g][lv % 2]
                            for hh in range(HG):
                                V("pe", lambda e: e.matmul(pZ[g][0:32, hh, :], PTv(hh), Zs[g][:, hh, :], start=True, stop=True), [PTb, Zs[g]], [pZ[g]])
                            if lv < 4:
                                for hh in range(HG):
                                    V("pe", lambda e: e.matmul(pPP[g][:, 0, hh, :], PTv(hh), Pb[:, 0, hh, :], start=True, stop=True), [PTb, Pb], [pPP[g]])
                                    V("pe", lambda e: e.matmul(pPP[g][:, 1, hh, :], Pb[:, 0, hh, :], PTv(hh), start=True, stop=True), [PTb, Pb], [pPP[g]])
                        for g in GR:
                            UVc = UV[g][c % 2]
                            if lv < 4:
                                V("dve", lambda e: e.tensor_tensor(Zs[g][:], Zs[g][:], pZ[g][0:32, :, :], ALU.add), [Zs[g], pZ[g]], [Zs[g]])
                                V("act", lambda e: e.copy(PP[g][(lv + 1) % 2][:], pPP[g][:]), [pPP[g]], [PP[g][(lv + 1) % 2]])
                            else:
                                V("dve", lambda e: e.tensor_tensor(UVc[0:32, :, :], Zs[g][:], pZ[g][0:32, :, :], ALU.add), [Zs[g], pZ[g]], [UVc])
                    for g in GR:
                        UVc = UV[g][c % 2]
                        for hh in range(HG):
                            h = g * HG + hh
                            V("pe", lambda e: e.matmul(pN[g][:, hh, :], H[g][:, hh, :], X[:, h, cc, 3, :], start=True, stop=False), [H[g], X], [pN[g]])
                            V("pe", lambda e: e.matmul(pN[g][:, hh, :], UVc[:, hh, :], MT[g][:, hh, 32:64], start=False, stop=True), [UVc, MT[g]], [pN[g]])
                    for g in GR:
                        V("act", lambda e: e.copy(Yt[g][:, :, (cc % 2) * 32:(cc % 2) * 32 + 32], pN[g][:]), [pN[g]], [Yt[g]])
                    for g in GR:
                        UVc, BKc = UV[g][c % 2], BK[g][c % 2]
                        for hh in range(HG):
                            V("pe", lambda e: e.matmul(pTP[g][:, hh, :], BKc[:, hh, :], UVc[:, hh, :], start=True, stop=True), [BKc, UVc], [pTP[g]])
                    for g in GR:
                        V("dve", lambda e: e.tensor_tensor(H[g][:], H[g][:], decall[:, g * HG:(g + 1) * HG, c:c + 1].to_broadcast([64, HG, 64]), ALU.mult), [H[g], decall], [H[g]])
                        V("dve", lambda e: e.tensor_tensor(H[g][:], H[g][:], pTP[g][:], ALU.add), [H[g], pTP[g]], [H[g]])
                    if cc % 2 == 1:
                        gi = c // 2
                        t0 = gi * 64
                        if gi + 1 < NCH:
                            loadepi(gi + 1)
                        vc, gc, s2c = vch[gi % 2], gch[gi % 2], s2[gi % 2]
                        for (src, dst, mu_) in ((vc, vl, muv), (gc, gl, mug)):
                            V("pool", lambda e: e.tensor_tensor(dst[:], src[:, :, 0:64], src[:, :, 1:65], ALU.subtract), [src], [dst])
                            V("pool", lambda e: e.tensor_tensor(dst[:], dst[:], mu_[:, :].unsqueeze(2).to_broadcast([64, 16, 64]), ALU.mult), [dst, mu_], [dst])
                            V("pool", lambda e: e.tensor_tensor(dst[:], dst[:], src[:, :, 1:65], ALU.add), [dst, src], [dst])
                        V("act", lambda e: e.activation(gl[:], gl[:], AF.Silu), [gl], [gl])
                        for g in GR:
                            V("pe", lambda e: e.matmul(fl(pTP[g]), onesm[:], fl(Yt[g]), start=True, stop=True), [onesm, Yt[g]], [pTP[g]])
                        for g in GR:
                            V("dve", lambda e: e.tensor_tensor(yc[g][:], Yt[g][:], pTP[g][:], ALU.subtract), [Yt[g], pTP[g]], [yc[g]])
                            V("act", lambda e: e.activation(sq[g][:], yc[g][:], AF.Square), [yc[g]], [sq[g]])
                        for g in GR:
                            V("pe", lambda e: e.matmul(fl(pZ[g]), onesm[:], fl(sq[g]), start=True, stop=True), [onesm, sq[g]], [pZ[g]])
                        k4 = 0
                        for g in GR:
                            for hh in range(HG):
                                h = g * HG + hh
                                rhh = rh[k4 % 4]
                                k4 += 1
                                V("pool", lambda e: e.tensor_scalar(rhh[:], ident_f[0:64, 0:64], s2c[:, h:h + 1], None, op0=ALU.mult), [ident_f, s2c], [rhh])
                                V("pe", lambda e: e.matmul(pTP[g][:, hh, :], ones64[:], rhh[:], start=True, stop=True), [ones64, rhh], [pTP[g]])
                        for g in GR:
                            hs = slice(g * HG, (g + 1) * HG)
                            V("act", lambda e: e.activation(rstd[g][:], pZ[g][:], AF.Sqrt, bias=64e-5, scale=1.0), [pZ[g]], [rstd[g]])
                            V("dve", lambda e: e.reciprocal(rstd[g][:], rstd[g][:]), [rstd[g]], [rstd[g]])
                            V("dve", lambda e: e.tensor_tensor(yc[g][:], yc[g][:], rstd[g][:], ALU.mult), [yc[g], rstd[g]], [yc[g]])
                            V("dve", lambda e: e.tensor_tensor(yc[g][:], yc[g][:], b3(lng, g), ALU.mult), [yc[g], lng], [yc[g]])
                            V("dve", lambda e: e.tensor_tensor(yc[g][:], yc[g][:], b3(lnb, g), ALU.add), [yc[g], lnb], [yc[g]])
                            V("dve", lambda e: e.tensor_tensor(tA[g][:], pTP[g][:], vl[:, hs, :], ALU.mult), [pTP[g], vl], [tA[g]])
                            V("dve", lambda e: e.tensor_tensor(yc[g][:], yc[g][:], tA[g][:], ALU.add), [yc[g], tA[g]], [yc[g]])
                            ybc = yb[g][gi % 2]
                            V("dve", lambda e: e.tensor_tensor(ybc[:], yc[g][:], gl[:, hs, :], ALU.mult), [yc[g], gl], [ybc])
                            P.dma("sp", [(ysT.t[2048 + g * 512:2048 + (g + 1) * 512, t0:t0 + 64].rearrange("(h v) t -> v h t", v=64), ybc[:])], reads=[ybc], writes=[ysT])

    PH = {"tables": phase_tables, "proj": phase_proj, "rope": phase_rope, "gla": phase_gla, "sgu": phase_sgu,
          "m1": phase_m1, "m2": phase_m2, "dsa1": phase_dsa1, "dsa2": phase_dsa2, "rwp": phase_rwp, "rws": phase_rws, "rwp2": phase_rwp2, "rws2": phase_rws2, "rws3": phase_rws3}
    plan = dbg.get("plan")
    if plan is None:
        plan = [("tables",)]
        for l in range(L):
            plan += [("proj", l, l), ("rope", l), ("gla", l), ("dsa1", l), ("dsa2", l), ("rwp2", l), ("rws3", l), ("sgu", l), ("m1", l), ("m2", l, l, l + 1)]
    for st in plan:
        nm = st[0]
        if nm == "tables":
            phase_tables()
        elif nm == "proj":
            phase_proj(st[1], xcur[st[2]])
        elif nm == "m2":
            phase_m2(st[1], xcur[st[2]], xcur[st[3]])
        else:
            PH[nm](st[1])
    P.barrier()
    es.close()
    global LAST_P
    LAST_P = P
    return nc


def make_inputs(inputs, b):
    m = {"x": np.ascontiguousarray(inputs["x"][b]), "pos": np.ascontiguousarray(inputs["positions"][b:b + 1]).astype(np.int32)}
    for k, v in inputs.items():
        if k in ("x", "positions"):
            continue
        m[k] = np.ascontiguousarray(v)
    m.update(host_consts())
    return m


def kernel(**inputs):
    nc = build_nc()
    in_maps = [make_inputs(inputs, c % 4) for c in range(8)]
    res = run_bass_kernel_spmd(nc, in_maps, core_ids=list(range(8)))
    return np.stack([res.results[c]["y"] for c in range(4)], axis=0).astype(np.float32)
```

```python
import numpy as np
from contextlib import ExitStack, contextmanager
import concourse.bass as bass
import concourse.mybir as mybir
from concourse.bass_utils import run_bass_kernel_spmd

F32 = mybir.dt.float32
BF16 = mybir.dt.bfloat16
I32 = mybir.dt.int32
ALU = mybir.AluOpType
AF = mybir.ActivationFunctionType
AX = mybir.AxisListType

D = 2048
S = 4096
L = 2
NIN = 23320
PAD = 64
TP = S + PAD
NEG = -1.0e30


class Buf:
    __slots__ = ("t", "w", "r", "sem", "semkey", "semv", "name", "acc", "wl", "psum")

    def __init__(self, t, name, acc=False):
        self.t = t
        self.name = name
        self.acc = acc
        self.psum = False
        self.wl = []
        self.w = None
        self.r = []
        self.sem = None
        self.semkey = None
        self.semv = 0

    def __getitem__(self, k):
        return self.t[k]


class Prog:
    def __init__(self, nc, es):
        self.nc = nc
        self.es = es
        self.eng = {"pe": nc.tensor, "act": nc.scalar, "dve": nc.vector, "pool": nc.gpsimd, "sp": nc.sync}
        self.esem = {}
        self.ecnt = {}
        self.ekey = {}
        self.eepoch = {}
        for e in ("pe", "act", "dve", "pool"):
            self.esem[e] = es.enter_context(nc.semaphore("e_" + e))
            self.ecnt[e] = 0
            self.eepoch[e] = 0
            self.ekey[e] = e + "#0"
        self.nwait = 0
        self.seen = {e: {} for e in self.eng}
        self.nbuf = 0
        self.scopes = [es]
        self.scope_bufs = [[]]
        self.sempool = []
        self.allsems = {}
        self.nsem = 0
        self.ninst = 0

    @contextmanager
    def scope(self):
        es = ExitStack()
        self.scopes.append(es)
        self.scope_bufs.append([])
        yield
        self.barrier()
        for b in self.scope_bufs.pop():
            if b.sem is not None:
                self.sempool.append((b.sem, b.semkey, b.semv))
        self.scopes.pop()
        es.close()

    def sb(self, shape, dt, name=None):
        self.nbuf += 1
        name = (name or "sb") + "_%d" % self.nbuf
        t = self.scopes[-1].enter_context(self.nc.sbuf_tensor(name, list(shape), dt))
        b = Buf(t, name)
        self.scope_bufs[-1].append(b)
        return b

    def ps(self, shape, dt, name=None):
        self.nbuf += 1
        name = (name or "ps") + "_%d" % self.nbuf
        t = self.scopes[-1].enter_context(self.nc.psum_tensor(name, list(shape), dt))
        b = Buf(t, name)
        b.psum = True
        self.scope_bufs[-1].append(b)
        return b

    def dram(self, name, shape, dt, kind="Internal"):
        t = self.nc.dram_tensor(name, list(shape), dt, kind=kind).ap()
        return Buf(t, name, acc=True)

    def _wait(self, e, ev, raw=True):
        if ev is None:
            return
        key, sem, val = ev
        if e == "pe" and key.startswith("pe#"):
            return
        if self.seen[e].get(key, 0) >= val:
            return
        self.eng[e].wait_ge(sem, val)
        self.nwait += 1
        self.seen[e][key] = val

    def _deps(self, e, reads, writes):
        for b in reads:
            self._wait(e, b.w, True)
            for ev in b.wl:
                self._wait(e, ev, True)
            if b.psum:
                for ev in b.r:
                    if not ev[0].startswith(e + "#"):
                        self._wait(e, ev, False)
        for b in writes:
            if not b.acc:
                self._wait(e, b.w, False)
            for ev in b.r:
                self._wait(e, ev, False)

    @staticmethod
    def _compact(evs):
        best = {}
        for ev in evs:
            k = ev[0]
            if k not in best or best[k][2] < ev[2]:
                best[k] = ev
        return list(best.values())

    def _record(self, ev, reads, writes):
        for b in writes:
            if b.acc:
                b.wl.append(ev)
                if len(b.wl) > 16:
                    b.wl = self._compact(b.wl)
            else:
                b.w = ev
            b.r = []
        for b in reads:
            if b not in writes:
                b.r.append(ev)
                if len(b.r) > 16:
                    b.r = self._compact(b.r)

    def op(self, e, fn, reads=(), writes=()):
        self._deps(e, reads, writes)
        ins = fn(self.eng[e])
        self.ecnt[e] += 1
        self.ninst += 1
        ins.then_inc(self.esem[e], 1)
        self._record((self.ekey[e], self.esem[e], self.ecnt[e]), reads, writes)
        return ins

    def _getsem(self, b):
        if b.sem is None:
            if self.sempool:
                b.sem, b.semkey, b.semv = self.sempool.pop()
            else:
                self.nsem += 1
                b.semkey = "d%d" % self.nsem
                b.sem = self.es.enter_context(self.nc.semaphore(b.semkey))
                b.semv = 0
            self.allsems[b.semkey] = b

    def dma(self, q, pairs, reads=(), writes=(), sembuf=None, **kw):
        self._deps(q, reads, writes)
        sb = sembuf
        if sb is None:
            cands = [b for b in list(writes) + list(reads) if not b.acc]
            sb = cands[0] if cands else (writes[0] if writes else reads[0])
        self._getsem(sb)
        self._wait(q, (sb.semkey, sb.sem, sb.semv))
        for (o, i) in pairs:
            self.eng[q].dma_start(out=o, in_=i, **kw).then_inc(sb.sem, 16)
            sb.semv += 16
            self.ninst += 1
        self._record((sb.semkey, sb.sem, sb.semv), reads, writes)

    def barrier(self):
        evs = [(self.ekey[c], self.esem[c], self.ecnt[c], c) for c in self.esem]
        for k, b in self.allsems.items():
            evs.append((k, b.sem, b.semv, None))
        for (sem, key, v) in self.sempool:
            evs.append((key, sem, v, None))
        for e in self.eng:
            for ev in evs:
                if ev[3] != e and ev[2] > 0:
                    if self.seen[e].get(ev[0], 0) < ev[2]:
                        self.eng[e].wait_ge(ev[1], ev[2])
                        self.nwait += 1
                        self.seen[e][ev[0]] = ev[2]
        for c in list(self.esem):
            if self.ecnt[c] > 12000:
                self.eepoch[c] += 1
                self.ekey[c] = "%s#%d" % (c, self.eepoch[c])
                self.esem[c] = self.es.enter_context(self.nc.semaphore("e_%s_%d" % (c, self.eepoch[c])))
                self.ecnt[c] = 0


SEGS = [
    (0, 512, "F", "gla_q"), (512, 512, "T", "gla_k"), (1024, 1024, "T", "gla_v"),
    (2048, 16, "F", "gla_g"), (2064, 1024, "F", "gla_gate"),
    (3088, 1024, "F", "dsa_q"), (4112, 1024, "F", "dsa_k"), (5136, 1024, "T", "dsa_v"),
    (6160, 512, "F", "dsa_iq"), (6672, 64, "F", "dsa_ik"), (6736, 8, "T", "dsa_iw"),
    (6744, 1024, "F", "dsa_gate"),
    (7768, 1024, "T", "rw_r"), (8792, 1024, "T", "rw_k"), (9816, 1024, "T", "rw_vT"), (9816, 1024, "F", "rw_v"),
    (10840, 96, "T", "rw_wlr"), (10936, 96, "T", "rw_alr"), (11032, 1024, "F", "rw_gate"),
    (12056, 1024, "F", "sgu_u"), (13080, 1024, "T", "sgu_zv"), (14104, 1024, "F", "sgu_gate"),
    (15128, 8192, "H", "pm"),
]
FM_ROWS = {}
TM_COLS = {}
_r = 0
_c = 0
for (_c0, _n, _m, _nm) in SEGS:
    if _m == "F":
        FM_ROWS[_nm] = _r
        _r += _n
    elif _m == "T":
        TM_COLS[_nm] = _c
        _c += _n
NFM = _r
NTM = _c


def host_consts():
    c = {}
    c["c_ident"] = np.eye(128, dtype=np.float32)
    s = np.arange(64)
    c["c_ustrict"] = (s[:, None] > s[None, :]).astype(np.float32)
    i = np.arange(128)
    c["c_sgumask"] = ((i[None, :] // 64) <= (i[:, None] // 64)).astype(np.float32)
    inv_a = (500000.0 ** (-(np.arange(0, 32, 2, dtype=np.float32) / np.float32(32)))).astype(np.float32)
    inv_i = (500000.0 ** (-(np.arange(0, 16, 2, dtype=np.float32) / np.float32(16)))).astype(np.float32)
    inv = np.zeros((128, 2), np.float32)
    inv[:, 0] = inv_a[i % 16]
    inv[:, 1] = inv_i[i % 8]
    c["c_inv"] = inv
    sel = np.zeros((128, 64, 128), np.float32)
    for tl in range(64):
        sel[tl, tl, 0:64] = 1.0
        sel[64 + tl, tl, 64:128] = 1.0
    c["c_sel"] = sel
    c["c_blk"] = ((i[:, None] // 64) == (i[None, :] // 64)).astype(np.float32)
    c["c_i2"] = ((i[:, None] % 64) == s[None, :]).astype(np.float32)
    j = np.arange(64)
    sidx = j % 32
    m = np.zeros((64, 64), np.float32)
    m[:, 0:32] = (sidx[:, None] < np.arange(32)[None, :])
    m[:, 32:64] = (sidx[:, None] <= np.arange(32)[None, :])
    c["c_m32tp"] = m
    t32 = np.arange(32)
    c["c_m32n"] = (t32[None, :] < t32[:, None]).astype(np.float32)
    blk32 = (i[:, None] // 32) == (i[None, :] // 32)
    c["c_lmat32"] = (blk32 & (i[:, None] <= i[None, :])).astype(np.float32)
    c["c_umat32"] = (blk32 & (i[:, None] > i[None, :])).astype(np.float32)
    c["c_ind32"] = ((i[:, None] // 32) == np.arange(4)[None, :]).astype(np.float32)
    return c


def build_nc(S=4096, dbg=None):
    dbg = dbg or {}
    TP = S + PAD
    NT = S // 128
    NB = S // 512
    NCH = S // 64
    KTOP = min(256, S // 4)
    nc = bass.Bass("TRN2", target_bir_lowering=False)
    es = ExitStack()
    P = Prog(nc, es)
    I = {}

    def inp(name, shape, dt=F32):
        I[name] = P.dram(name, shape, dt, kind="ExternalInput")
        return I[name]

    x_in = inp("x", [S, D])
    pos_in = inp("pos", [1, S], I32)
    inp("norm_pre", [L, D]); inp("norm_post", [L, D]); inp("w_in", [L, D, NIN])
    inp("gla_w_g2", [L, 16, 512]); inp("gla_b_g", [L, 512]); inp("gla_head_g", [L, 256])
    inp("rwkv_mu", [L, 4288]); inp("rwkv_w0", [L, 1024]); inp("rwkv_w_w2", [L, 96, 1024])
    inp("rwkv_a0", [L, 1024]); inp("rwkv_w_a2", [L, 96, 1024]); inp("rwkv_k_k", [L, 1024])
    inp("rwkv_k_a", [L, 1024]); inp("rwkv_r_k", [L, 16, 64]); inp("rwkv_lnx_g", [L, 1024])
    inp("rwkv_lnx_b", [L, 1024]); inp("sgu_ln_g", [L, 1024]); inp("sgu_ln_b", [L, 1024])
    inp("sgu_w_s", [L, 8, 128, 128]); inp("sgu_b_s", [L, 8, 128]); inp("w_proj", [L, 4, 1024, D])
    inp("w_out", [L, D, D])
    inp("c_ident", [128, 128]); inp("c_ustrict", [64, 64]); inp("c_sgumask", [128, 128])
    inp("c_inv", [128, 2]); inp("c_sel", [128, 64, 128]); inp("c_blk", [128, 128]); inp("c_i2", [128, 64])
    inp("c_m32tp", [64, 64]); inp("c_m32n", [32, 32]); inp("c_lmat32", [128, 128]); inp("c_umat32", [128, 128]); inp("c_ind32", [128, 4])
    y_out = P.dram("y", [S, D], F32, kind="ExternalOutput")

    ext_in = dbg.get("ext_in", ())
    ext_out = dbg.get("ext_out", ())

    def scr(name, shape, dt):
        kind = "ExternalInput" if name in ext_in else ("ExternalOutput" if name in ext_out else "Internal")
        return P.dram(name, shape, dt, kind=kind)
    fm = scr("fm", [NFM, TP], F32)
    tm = scr("tm", [S, NTM], F32)
    pmT = scr("pmT", [8192, S], BF16)
    ysT = scr("ysT", [4096, S], BF16)
    mT = scr("mT", [D, S], BF16)
    tabs = scr("tabs", [4, 128, S], F32)
    maskT = scr("maskT", [NT, S, 128], BF16)
    rwtm = scr("rwtm", [S, 5, 1024], F32)
    rwbon = scr("rwbon", [S, 16], F32)
    NC32 = S // 32
    rwtm2 = scr("rwtm2", [S, 3, 1024], F32)
    rwxt = scr("rwxt", [64, 16, NT, 512], F32)
    rwdec = scr("rwdec", [64, 16, NC32], F32)
    xcur = [x_in, scr("x1", [S, D], F32), y_out]

    if "fm_in" in dbg:
        pass

    ident_f = P.sb([128, 128], F32, "identf")
    ident_b = P.sb([128, 128], BF16, "identb")
    ones_b = P.sb([128, 128], BF16, "onesb")
    zeros_f = P.sb([128, PAD], F32, "zerosf")
    P.dma("sp", [(ident_f[:], I["c_ident"][:, :])], reads=[I["c_ident"]], writes=[ident_f])
    P.op("dve", lambda e: e.tensor_copy(ident_b[:], ident_f[:]), [ident_f], [ident_b])
    P.op("pool", lambda e: e.memset(ones_b[:], 1.0), [], [ones_b])
    P.op("pool", lambda e: e.memset(zeros_f[:], 0.0), [], [zeros_f])
    for nm in ("rw_v", "rw_gate"):
        for r8 in range(8):
            r0 = FM_ROWS[nm] + r8 * 128
            P.dma("sp", [(fm[r0:r0 + 128, 0:PAD], zeros_f[:])], reads=[zeros_f], writes=[fm])

    def evac(i, out_ap, in_ap, reads, writes):
        if i % 2 == 0:
            P.op("act", lambda e: e.copy(out_ap, in_ap), reads, writes)
        else:
            P.op("dve", lambda e: e.tensor_copy(out_ap, in_ap), reads, writes)

    def V(e, fn, reads, writes):
        P.op(e, fn, reads, writes)

    def phase_tables():
        with P.scope():
            posi = P.sb([128, S], I32, "posi")
            posf = P.sb([128, S], F32, "posf")
            inv = P.sb([128, 2], F32, "inv")
            ang = P.sb([128, S], F32, "ang")
            kf = P.sb([128, S], F32, "kf")
            ki = P.sb([128, S], I32, "ki")
            r1 = P.sb([128, S], F32, "r1")
            y = P.sb([128, S], F32, "y")
            m = P.sb([128, S], F32, "m")
            P.dma("sp", [(posi[:], pos_in[0:1, :].to_broadcast([128, S]))], reads=[pos_in], writes=[posi])
            P.dma("sp", [(inv[:], I["c_inv"][:, :])], reads=[I["c_inv"]], writes=[inv])
            V("dve", lambda e: e.tensor_copy(posf[:], posi[:]), [posi], [posf])
            for which in range(2):
                V("dve", lambda e: e.tensor_scalar(ang[:], posf[:], inv[:, which:which + 1], None, op0=ALU.mult), [posf, inv], [ang])
                V("dve", lambda e: e.tensor_scalar(kf[:], ang[:], float(1.0 / (2 * np.pi)), None, op0=ALU.mult), [ang], [kf])
                V("dve", lambda e: e.tensor_copy(ki[:], kf[:]), [kf], [ki])
                V("dve", lambda e: e.tensor_copy(kf[:], ki[:]), [ki], [kf])
                V("dve", lambda e: e.scalar_tensor_tensor(r1[:], kf[:], -6.28125, ang[:], op0=ALU.mult, op1=ALU.add), [kf, ang], [r1])
                V("dve", lambda e: e.scalar_tensor_tensor(ang[:], kf[:], -0.0019353071795864769, r1[:], op0=ALU.mult, op1=ALU.add), [kf, r1], [ang])
                for cs in range(2):
                    sh = float(np.pi / 2) if cs == 0 else 0.0
                    V("dve", lambda e: e.tensor_scalar(y[:], ang[:], sh, None, op0=ALU.add), [ang], [y])
                    V("dve", lambda e: e.tensor_scalar(m[:], y[:], float(np.pi), None, op0=ALU.is_gt), [y], [m])
                    V("dve", lambda e: e.scalar_tensor_tensor(y[:], m[:], float(-2 * np.pi), y[:], op0=ALU.mult, op1=ALU.add), [m, y], [y])
                    V("dve", lambda e: e.tensor_scalar(m[:], y[:], float(-np.pi), None, op0=ALU.is_lt), [y], [m])
                    V("dve", lambda e: e.scalar_tensor_tensor(y[:], m[:], float(2 * np.pi), y[:], op0=ALU.mult, op1=ALU.add), [m, y], [y])
                    V("dve", lambda e: e.tensor_scalar(y[:], y[:], float(np.pi), float(-np.pi), op0=ALU.min, op1=ALU.max), [y], [y])
                    V("act", lambda e: e.activation(r1[:], y[:], AF.Sin), [y], [r1])
                    P.dma("sp", [(tabs.t[which * 2 + cs, :, :], r1[:])], reads=[r1], writes=[tabs])

    def phase_proj(l, xin):
        with P.scope():
            hT = P.sb([128, 16, S], BF16, "hT")
            with P.scope():
                gbc = P.sb([128, D], F32, "gbc")
                P.dma("sp", [(gbc[:], I["norm_pre"][l:l + 1, :].to_broadcast([128, D]))], reads=[I["norm_pre"]], writes=[gbc])
                xt = [P.sb([128, D], F32, "xt") for _ in range(2)]
                junk = P.sb([128, D], BF16, "junk")
                hb = [P.sb([128, D], BF16, "hb") for _ in range(2)]
                ss = [P.sb([128, 1], F32, "ss") for _ in range(2)]
                rs = [P.sb([128, 1], F32, "rs") for _ in range(2)]
                pst = [P.ps([128, 4, 128], BF16, "pst") for _ in range(4)]
                P.dma("sp", [(xt[0][:], xin[0:128, :])], reads=[xin], writes=[xt[0]])
                for i in range(NT):
                    if i + 1 < NT:
                        P.dma("sp", [(xt[(i + 1) % 2][:], xin[(i + 1) * 128:(i + 2) * 128, :])], reads=[xin], writes=[xt[(i + 1) % 2]])
                    xi, si, ri, hi = xt[i % 2], ss[i % 2], rs[i % 2], hb[i % 2]
                    V("act", lambda e: e.activation(junk[:], xi[:], AF.Square, accum_out=si[:]), [xi], [junk, si])
                    V("act", lambda e: e.activation(ri[:], si[:], AF.Sqrt, bias=1e-6, scale=1.0 / D), [si], [ri])
                    V("dve", lambda e: e.reciprocal(ri[:], ri[:]), [ri], [ri])
                    V("dve", lambda e: e.scalar_tensor_tensor(hi[:], xi[:], ri[:, 0:1], gbc[:], op0=ALU.mult, op1=ALU.mult), [xi, ri, gbc], [hi])
                    for g4 in range(4):
                        pt = pst[g4]
                        for j in range(4):
                            kc = g4 * 4 + j
                            V("pe", lambda e: e.transpose(pt[:, j, :], hi[:, kc * 128:(kc + 1) * 128], ident_b[:]), [hi, ident_b], [pt])
                        evac(g4, hT[:, g4 * 4:(g4 + 1) * 4, i * 128:(i + 1) * 128], pt[:], [pt], [hT])
            with P.scope():
                wf = [P.sb([128, 16, 128], F32, "wf") for _ in range(2)]
                wb = [P.sb([128, 16, 128], BF16, "wb") for _ in range(2)]
                stg = [P.sb([128, S], F32, "stg") for _ in range(2)]
                stgh = [P.sb([128, S], BF16, "stgh") for _ in range(2)]
                pp = [P.ps([128, 512], F32, "pp") for _ in range(6)]
                slabs = []
                for (c0, n, mode, nm) in SEGS:
                    off = 0
                    while off < n:
                        m = min(128, n - off)
                        slabs.append((c0 + off, m, mode, nm, off))
                        off += m
                w_l = I["w_in"]

                def load(s):
                    c0, m, mode, nm, off = slabs[s]
                    src = w_l.t[l, :, c0:c0 + m].rearrange("(kc p) m -> p kc m", p=128)
                    P.dma("sp", [(wf[s % 2][:, k4 * 4:(k4 + 1) * 4, 0:m], src[:, k4 * 4:(k4 + 1) * 4, :]) for k4 in range(4)],
                          reads=[w_l], writes=[wf[s % 2]])

                def cast(s):
                    c0, m, mode, nm, off = slabs[s]
                    V("pool", lambda e: e.tensor_copy(wb[s % 2][:, :, 0:m], wf[s % 2][:, :, 0:m]), [wf[s % 2]], [wb[s % 2]])

                load(0)
                cast(0)
                if len(slabs) > 1:
                    load(1)
                pi = 0
                for s in range(len(slabs)):
                    c0, m, mode, nm, off = slabs[s]
                    if s + 1 < len(slabs):
                        cast(s + 1)
                    if s + 2 < len(slabs):
                        load(s + 2)
                    w_s = wb[s % 2]
                    if mode in ("F", "H"):
                        st = stg[s % 2] if mode == "F" else stgh[s % 2]
                        for tb in range(NB):
                            ps = pp[pi % 6]
                            for kc in range(16):
                                V("pe", lambda e: e.matmul(ps[0:m, :], w_s[:, kc, 0:m], hT[:, kc, tb * 512:(tb + 1) * 512],
                                                           start=(kc == 0), stop=(kc == 15)), [w_s, hT], [ps])
                            evac(pi, st[0:m, tb * 512:(tb + 1) * 512], ps[0:m, :], [ps], [st])
                            pi += 1
                        if mode == "F":
                            r0 = FM_ROWS[nm] + off
                            P.dma("sp", [(fm[r0:r0 + m, PAD:TP], st[0:m, :])], reads=[st], writes=[fm])
                        else:
                            P.dma("sp", [(pmT[off:off + m, :], st[0:m, :])], reads=[st], writes=[pmT])
                    else:
                        st = stg[s % 2]
                        st3 = st[:].rearrange("p (t m) -> p t m", m=128)
                        for t4 in range(NT // 4):
                            ps = pp[pi % 6]
                            ps3 = ps[:].rearrange("p (t m) -> p t m", m=128)
                            for j in range(4):
                                tt = t4 * 4 + j
                                for kc in range(16):
                                    V("pe", lambda e: e.matmul(ps3[:, j, 0:m], hT[:, kc, tt * 128:(tt + 1) * 128], w_s[:, kc, 0:m],
                                                               start=(kc == 0), stop=(kc == 15)), [w_s, hT], [ps])
                            evac(pi, st3[:, t4 * 4:(t4 + 1) * 4, 0:m], ps3[:, :, 0:m], [ps], [st])
                            pi += 1
                        cc = TM_COLS[nm] + off
                        P.dma("sp", [(tm.t[:, cc:cc + m].rearrange("(t p) m -> p t m", p=128), st3[:, 0:NT, 0:m])], reads=[st], writes=[tm])

    def phase_rope(l):
        with P.scope():
            A = P.sb([128, S], F32, "ropeA")
            B = P.sb([128, S], F32, "ropeB")
            C = P.sb([128, S], F32, "ropeC")
            Sn = P.sb([128, S], F32, "ropeS")
            t1 = P.sb([128, S], F32, "ropet1")
            t2 = P.sb([128, S], F32, "ropet2")
            A2 = P.sb([128, S], F32, "ropeA2")
            B2 = P.sb([128, S], F32, "ropeB2")
            for (nm, nh, hd, half, tab) in (("dsa_q", 8, 128, 16, 0), ("dsa_k", 8, 128, 16, 0), ("dsa_iq", 8, 64, 8, 2), ("dsa_ik", 1, 64, 8, 2)):
                np_ = nh * half
                r0 = FM_ROWS[nm]
                rows = fm.t[r0:r0 + nh * hd, PAD:TP].rearrange("(h r) t -> h r t", r=hd)
                P.dma("sp", [(C[0:np_, :], tabs.t[tab, 0:np_, :]), (Sn[0:np_, :], tabs.t[tab + 1, 0:np_, :])], reads=[tabs], writes=[C, Sn], sembuf=C)
                P.dma("sp", [(A[0:np_, :], rows[:, 0:half, :]), (B[0:np_, :], rows[:, half:2 * half, :])], reads=[fm], writes=[A, B], sembuf=A)
                V("dve", lambda e: e.tensor_tensor(t1[0:np_, :], A[0:np_, :], C[0:np_, :], ALU.mult), [A, C], [t1])
                V("pool", lambda e: e.tensor_tensor(t2[0:np_, :], B[0:np_, :], Sn[0:np_, :], ALU.mult), [B, Sn], [t2])
                V("dve", lambda e: e.tensor_tensor(A2[0:np_, :], t1[0:np_, :], t2[0:np_, :], ALU.subtract), [t1, t2], [A2])
                V("dve", lambda e: e.tensor_tensor(t1[0:np_, :], B[0:np_, :], C[0:np_, :], ALU.mult), [B, C], [t1])
                V("pool", lambda e: e.tensor_tensor(t2[0:np_, :], A[0:np_, :], Sn[0:np_, :], ALU.mult), [A, Sn], [t2])
                V("dve", lambda e: e.tensor_tensor(B2[0:np_, :], t1[0:np_, :], t2[0:np_, :], ALU.add), [t1, t2], [B2])
                P.dma("sp", [(rows[:, 0:half, :], A2[0:np_, :]), (rows[:, half:2 * half, :], B2[0:np_, :])], reads=[A2, B2], writes=[fm], sembuf=A2)

    def phase_gla(l):
        with P.scope():
            qT = P.sb([128, 4, S], BF16, "gqT")
            oT = P.sb([128, 8, S], BF16, "goT")
            stg = [P.sb([128, S], F32, "gstg") for _ in range(2)]
            glr = P.sb([17, S], F32, "glr")
            wg = P.sb([17, 512], F32, "gwg")
            ust = P.sb([64, 64], F32, "gust")
            ones64 = P.sb([64, 1], F32, "gones")
            hg = P.sb([128, 2], F32, "ghg")
            state = P.sb([128, 4, 256], F32, "gstate")
            state_b = P.sb([128, 4, 256], BF16, "gstateb")
            kc_ = [P.sb([64, 512], F32, "gk") for _ in range(2)]
            vc_ = [P.sb([64, 1024], F32, "gv") for _ in range(2)]
            vb = P.sb([64, 1024], BF16, "gvb")
            ee = P.sb([64, 512], F32, "gee")
            lsp = P.sb([64, 512], F32, "glsp")
            dkk = P.sb([64, 512], F32, "gdk")
            kdec2 = [P.sb([64, 512], BF16, "gkdec") for _ in range(2)]
            vb2 = [P.sb([64, 1024], BF16, "gvb2") for _ in range(2)]
            dec2 = [P.sb([128, 4], F32, "gdec") for _ in range(2)]
            pz = P.ps([64, 512], F32, "gpz")
            pG = P.ps([64, 512], F32, "gpG")
            ptot = P.ps([128, 4], F32, "gptot")
            pkv = [P.ps([128, 2, 256], F32, "gpkv") for _ in range(2)]
            po = P.ps([128, 8, 64], F32, "gpo")
            V("pool", lambda e: e.memset(glr[:], 1.0), [], [glr])
            V("pool", lambda e: e.memset(ones64[:], 1.0), [], [ones64])
            V("pool", lambda e: e.memset(state[:], 0.0), [], [state])
            r0 = FM_ROWS["gla_g"]
            P.dma("sp", [(glr[0:16, :], fm[r0:r0 + 16, PAD:TP])], reads=[fm], writes=[glr])
            P.dma("sp", [(wg[0:16, :], I["gla_w_g2"].t[l, :, :]), (wg[16:17, :], I["gla_b_g"][l:l + 1, :])], reads=[I["gla_w_g2"], I["gla_b_g"]], writes=[wg])
            P.dma("sp", [(ust[:], I["c_ustrict"][:, :])], reads=[I["c_ustrict"]], writes=[ust])
            P.dma("sp", [(hg[:], I["gla_head_g"].t[l, :].rearrange("(a p) -> p a", p=128))], reads=[I["gla_head_g"]], writes=[hg], allow_slow_non_contiguous=True)
            r0 = FM_ROWS["gla_q"]
            for h in range(4):
                st = stg[h % 2]
                P.dma("sp", [(st[:], fm[r0 + h * 128:r0 + (h + 1) * 128, PAD:TP])], reads=[fm], writes=[st])
                V("pool", lambda e: e.tensor_copy(qT[:, h, :], st[:]), [st], [qT])
            ck = TM_COLS["gla_k"]
            cv = TM_COLS["gla_v"]

            def loadkv(c):
                P.dma("sp", [(kc_[c % 2][:], tm[c * 64:(c + 1) * 64, ck:ck + 512])], reads=[tm], writes=[kc_[c % 2]])
                P.dma("sp", [(vc_[c % 2][:], tm[c * 64:(c + 1) * 64, cv:cv + 1024])], reads=[tm], writes=[vc_[c % 2]])
            loadkv(0)

            def part1(c):
                t0 = c * 64
                kk_, vv_ = kc_[c % 2], vc_[c % 2]
                kd, vbb, dc_ = kdec2[c % 2], vb2[c % 2], dec2[c % 2]
                V("pe", lambda e: e.matmul(pz[:], glr[:, t0:t0 + 64], wg[:], start=True, stop=True), [glr, wg], [pz])
                V("act", lambda e: e.activation(ee[:], pz[:], AF.Exp, scale=-1.0), [pz], [ee])
                V("act", lambda e: e.activation(lsp[:], ee[:], AF.Ln, bias=1.0), [ee], [lsp])
                V("pe", lambda e: e.matmul(pG[:], ust[:], lsp[:], start=True, stop=True), [ust, lsp], [pG])
                for h in range(4):
                    V("pe", lambda e: e.matmul(ptot[:, h:h + 1], lsp[:, h * 128:(h + 1) * 128], ones64[:], start=True, stop=True), [lsp, ones64], [ptot])
                V("act", lambda e: e.activation(dkk[:], pG[:], AF.Exp, scale=-1.0 / 16.0), [pG], [dkk])
                V("act", lambda e: e.activation(dc_[:], ptot[:], AF.Exp, scale=-1.0 / 16.0), [ptot], [dc_])
                V("dve", lambda e: e.tensor_tensor(kd[:], kk_[:], dkk[:], ALU.mult), [kk_, dkk], [kd])
                V("pool", lambda e: e.tensor_copy(vbb[:], vv_[:]), [vv_], [vbb])

            def part2(c):
                t0 = c * 64
                kd, vbb, dc_ = kdec2[c % 2], vb2[c % 2], dec2[c % 2]
                for h in range(4):
                    pk = pkv[h // 2]
                    V("pe", lambda e: e.matmul(pk[:, h % 2, :], kd[:, h * 128:(h + 1) * 128], vbb[:, h * 256:(h + 1) * 256], start=True, stop=True), [kd, vbb], [pk])
                for h in range(4):
                    pk = pkv[h // 2]
                    V("dve", lambda e: e.scalar_tensor_tensor(state[:, h, :], state[:, h, :], dc_[:, h:h + 1], pk[:, h % 2, :], op0=ALU.mult, op1=ALU.add), [state, dc_, pk], [state])
                V("act", lambda e: e.copy(state_b[:], state[:]), [state], [state_b])
                for h in range(4):
                    for vh in range(2):
                        V("pe", lambda e: e.matmul(po[:, h * 2 + vh, :], state_b[:, h, vh * 128:(vh + 1) * 128], qT[:, h, t0:t0 + 64], start=True, stop=True), [state_b, qT], [po])
                evac(c, oT[:, :, t0:t0 + 64], po[:], [po], [oT])
            part1(0)
            for c in range(NCH):
                if c + 1 < NCH:
                    loadkv(c + 1)
                    part1(c + 1)
                part2(c)
            sq = P.sb([128, 2, 512], BF16, "gsq")
            pms = [P.ps([128, 512], F32, "gpms") for _ in range(2)]
            rstd = P.sb([128, 512], F32, "grstd")
            yy = P.sb([128, 512], F32, "gyy")
            sg = P.sb([128, 512], F32, "gsg")
            yo = [P.sb([128, S], BF16, "gyo") for _ in range(2)]
            sc = float(128.0 ** -0.5)
            hgs = P.sb([128, 2], F32, "ghgs")
            V("dve", lambda e: e.tensor_scalar(hgs[:], hg[:], sc, None, op0=ALU.mult), [hg], [hgs])
            rg = FM_ROWS["gla_gate"]
            it = 0
            for h in range(4):
                for vh in range(2):
                    gt = stg[it % 2]
                    yob = yo[it % 2]
                    P.dma("sp", [(gt[:], fm[rg + h * 256 + vh * 128:rg + h * 256 + (vh + 1) * 128, PAD:TP])], reads=[fm], writes=[gt])
                    for tb in range(NB):
                        ts_ = slice(tb * 512, (tb + 1) * 512)
                        pm_ = pms[(it * NB + tb) % 2]
                        if vh == 0:
                            for v2 in range(2):
                                V("act", lambda e: e.activation(sq[:, v2, :], oT[:, h * 2 + v2, ts_], AF.Square), [oT], [sq])
                        else:
                            for v2 in range(2):
                                V("act", lambda e: e.activation(sq[:, v2, :], oT[:, h * 2 + v2, ts_], AF.Square), [oT], [sq])
                        for v2 in range(2):
                            V("pe", lambda e: e.matmul(pm_[:], ones_b[:], sq[:, v2, :], start=(v2 == 0), stop=(v2 == 1)), [ones_b, sq], [pm_])
                        V("act", lambda e: e.activation(rstd[:], pm_[:], AF.Sqrt, bias=1e-6, scale=sc * sc / 256.0), [pm_], [rstd])
                        V("dve", lambda e: e.reciprocal(rstd[:], rstd[:]), [rstd], [rstd])
                        V("act", lambda e: e.activation(sg[:], gt[:, ts_], AF.Silu), [gt], [sg])
                        V("dve", lambda e: e.tensor_tensor(yy[:], oT[:, h * 2 + vh, ts_], rstd[:], ALU.mult), [oT, rstd], [yy])
                        V("dve", lambda e: e.scalar_tensor_tensor(yob[:, ts_], yy[:], hgs[:, vh:vh + 1], sg[:], op0=ALU.mult, op1=ALU.mult), [yy, hgs, sg], [yob])
                    ro = h * 256 + vh * 128
                    P.dma("sp", [(ysT[ro:ro + 128, :], yob[:])], reads=[yob], writes=[ysT])
                    it += 1

    def phase_sgu(l):
        with P.scope():
            lng = P.sb([128, 1024], F32, "slng")
            lnb = P.sb([128, 1024], F32, "slnb")
            bsb = P.sb([128, 8, 128], F32, "sbsb")
            msk = P.sb([128, 128], F32, "smsk")
            wsT = P.sb([128, 8, 128], BF16, "swsT")
            wtmp = P.sb([128, 128], F32, "swtmp")
            pT = P.ps([128, 128], F32, "spT")
            P.dma("sp", [(lng[:], I["sgu_ln_g"][l:l + 1, :].to_broadcast([128, 1024]))], reads=[I["sgu_ln_g"]], writes=[lng])
            P.dma("sp", [(lnb[:], I["sgu_ln_b"][l:l + 1, :].to_broadcast([128, 1024]))], reads=[I["sgu_ln_b"]], writes=[lnb])
            P.dma("sp", [(bsb[:].rearrange("p g i -> p (g i)"), I["sgu_b_s"].t[l:l + 1, :, :].rearrange("a g i -> a (g i)").to_broadcast([128, 1024]))], reads=[I["sgu_b_s"]], writes=[bsb])
            P.dma("sp", [(msk[:], I["c_sgumask"][:, :])], reads=[I["c_sgumask"]], writes=[msk])
            for g in range(8):
                P.dma("sp", [(wtmp[:], I["sgu_w_s"].t[l, g, :, :])], reads=[I["sgu_w_s"]], writes=[wtmp])
                V("dve", lambda e: e.tensor_tensor(wtmp[:], wtmp[:], msk[:], ALU.mult), [wtmp, msk], [wtmp])
                V("pe", lambda e: e.transpose(pT[:], wtmp[:], ident_f[:]), [wtmp, ident_f], [pT])
                V("act", lambda e: e.copy(wsT[:, g, :], pT[:]), [pT], [wsT])
            zv = [P.sb([128, 1024], F32, "szv") for _ in range(2)]
            uu = [P.sb([128, 8, 128], F32, "suu") for _ in range(2)]
            gg = [P.sb([128, 8, 128], F32, "sgg") for _ in range(2)]
            g1 = P.sb([128, 1024], F32, "sg1")
            junk = P.sb([128, 1024], BF16, "sjunk")
            vn = P.sb([128, 1024], F32, "svn")
            vln = P.sb([128, 1024], BF16, "svln")
            s1 = P.sb([128, 1], F32, "ss1"); s2 = P.sb([128, 1], F32, "ss2"); mm = P.sb([128, 1], F32, "smm")
            msq = P.sb([128, 1], F32, "smsq"); var = P.sb([128, 1], F32, "svar"); rstd = P.sb([128, 1], F32, "srstd")
            nmr = P.sb([128, 1], F32, "snmr")
            psv = [P.ps([128, 4, 128], F32, "spsv") for _ in range(2)]
            t1 = P.sb([128, 8, 128], F32, "st1")
            ug = P.sb([128, 8, 128], F32, "sug")
            sg = P.sb([128, 8, 128], F32, "ssg")
            yb = [P.sb([128, 8, 128], BF16, "syb") for _ in range(2)]
            cz = TM_COLS["sgu_zv"]
            ru = FM_ROWS["sgu_u"]
            rgt = FM_ROWS["sgu_gate"]

            def loadc(c):
                P.dma("sp", [(zv[c % 2][:], tm[c * 128:(c + 1) * 128, cz:cz + 1024])], reads=[tm], writes=[zv[c % 2]])
                P.dma("sp", [(uu[c % 2][:], fm.t[ru:ru + 1024, PAD + c * 128:PAD + (c + 1) * 128].rearrange("(g d) t -> d g t", d=128))], reads=[fm], writes=[uu[c % 2]])
                P.dma("sp", [(gg[c % 2][:], fm.t[rgt:rgt + 1024, PAD + c * 128:PAD + (c + 1) * 128].rearrange("(g d) t -> d g t", d=128))], reads=[fm], writes=[gg[c % 2]])
            loadc(0)
            for c in range(NT):
                if c + 1 < NT:
                    loadc(c + 1)
                z, u_, g_ = zv[c % 2], uu[c % 2], gg[c % 2]
                ybc = yb[c % 2]
                V("act", lambda e: e.activation(g1[:], z[:], AF.Gelu), [z], [g1])
                V("dve", lambda e: e.tensor_reduce(s1[:], g1[:], AX.X, ALU.add), [g1], [s1])
                V("act", lambda e: e.activation(junk[:], g1[:], AF.Square, accum_out=s2[:]), [g1], [junk, s2])
                V("dve", lambda e: e.tensor_scalar(mm[:], s1[:], 1.0 / 1024.0, None, op0=ALU.mult), [s1], [mm])
                V("dve", lambda e: e.tensor_tensor(msq[:], mm[:], mm[:], ALU.mult), [mm], [msq])
                V("dve", lambda e: e.scalar_tensor_tensor(var[:], s2[:], 1.0 / 1024.0, msq[:], op0=ALU.mult, op1=ALU.subtract), [s2, msq], [var])
                V("act", lambda e: e.activation(rstd[:], var[:], AF.Sqrt, bias=1e-5, scale=1.0), [var], [rstd])
                V("dve", lambda e: e.reciprocal(rstd[:], rstd[:]), [rstd], [rstd])
                V("dve", lambda e: e.scalar_tensor_tensor(nmr[:], mm[:], -1.0, rstd[:], op0=ALU.mult, op1=ALU.mult), [mm, rstd], [nmr])
                V("act", lambda e: e.activation(vn[:], g1[:], AF.Identity, bias=nmr[:, 0:1], scale=rstd[:, 0:1]), [g1, nmr, rstd], [vn])
                V("dve", lambda e: e.tensor_tensor(vn[:], vn[:], lng[:], ALU.mult), [vn, lng], [vn])
                V("dve", lambda e: e.tensor_tensor(vln[:], vn[:], lnb[:], ALU.add), [vn, lnb], [vln])
                for g in range(8):
                    pv = psv[g // 4]
                    V("pe", lambda e: e.matmul(pv[:, g % 4, :], vln[:, g * 128:(g + 1) * 128], wsT[:, g, :], start=True, stop=True), [vln, wsT], [pv])
                for g2 in range(2):
                    V("dve", lambda e: e.tensor_tensor(t1[:, g2 * 4:(g2 + 1) * 4, :], psv[g2][:], bsb[:, g2 * 4:(g2 + 1) * 4, :], ALU.add), [psv[g2], bsb], [t1])
                V("act", lambda e: e.activation(ug[:], u_[:], AF.Gelu), [u_], [ug])
                V("act", lambda e: e.activation(sg[:], g_[:], AF.Silu), [g_], [sg])
                V("pool", lambda e: e.tensor_tensor(ug[:], ug[:], sg[:], ALU.mult), [ug, sg], [ug])
                V("dve", lambda e: e.tensor_tensor(ybc[:], t1[:], ug[:], ALU.mult), [t1, ug], [ybc])
                P.dma("sp", [(ysT.t[3072:4096, c * 128:(c + 1) * 128].rearrange("(g d) t -> d g t", d=128), ybc[:])], reads=[ybc], writes=[ysT])

    def phase_m1(l):
        with P.scope():
            wp = P.sb([128, 32, D], BF16, "mwp")
            wst = [P.sb([128, 2048], F32, "mwst") for _ in range(2)]
            wpj = I["w_proj"]
            k = 0
            for g in range(4):
                for wc in range(8):
                    st = wst[k % 2]
                    P.dma("sp", [(st[:], wpj.t[l, g, wc * 128:(wc + 1) * 128, :])], reads=[wpj], writes=[st])
                    if k % 2 == 0:
                        V("pool", lambda e: e.tensor_copy(wp[:, g * 8 + wc, :], st[:]), [st], [wp])
                    else:
                        V("dve", lambda e: e.tensor_copy(wp[:, g * 8 + wc, :], st[:]), [st], [wp])
                    k += 1
            TB = 256
            yt = [P.sb([128, 32, TB], BF16, "myt") for _ in range(2)]
            pmt = [P.sb([128, 4, TB], BF16, "mpm") for _ in range(3)]
            sig = P.sb([128, 4, TB], F32, "msig")
            acc = P.sb([128, TB], F32, "macc")
            tmp = P.sb([128, TB], F32, "mtmp")
            mo = [P.sb([128, 16, TB], BF16, "mmo") for _ in range(2)]
            pps = [P.ps([128, TB], F32, "mpp") for _ in range(6)]
            nblk = S // TB

            def loady(b):
                P.dma("sp", [(yt[b % 2][:, q * 8:(q + 1) * 8, :], ysT.t[q * 1024:(q + 1) * 1024, b * TB:(b + 1) * TB].rearrange("(c p) t -> p c t", p=128)) for q in range(4)],
                      reads=[ysT], writes=[yt[b % 2]])
            loady(0)
            pi = 0
            it = 0
            for b in range(nblk):
                if b + 1 < nblk:
                    loady(b + 1)
                ytb = yt[b % 2]
                mob = mo[b % 2]
                for dc in range(16):
                    pmb = pmt[it % 3]
                    it += 1
                    P.dma("sp", [(pmb[:], pmT.t[:, b * TB:(b + 1) * TB].rearrange("(g r) t -> r g t", g=4)[dc * 128:(dc + 1) * 128, :, :])], reads=[pmT], writes=[pmb])
                    V("act", lambda e: e.activation(sig[:], pmb[:], AF.Sigmoid), [pmb], [sig])
                    for g in range(4):
                        ps = pps[pi % 6]
                        pi += 1
                        for wc in range(8):
                            V("pe", lambda e: e.matmul(ps[:], wp[:, g * 8 + wc, dc * 128:(dc + 1) * 128], ytb[:, g * 8 + wc, :], start=(wc == 0), stop=(wc == 7)), [wp, ytb], [ps])
                        if g == 0:
                            V("dve", lambda e: e.tensor_tensor(acc[:], ps[:], sig[:, g, :], ALU.mult), [ps, sig], [acc])
                        elif g < 3:
                            V("dve", lambda e: e.tensor_tensor(tmp[:], ps[:], sig[:, g, :], ALU.mult), [ps, sig], [tmp])
                            V("pool", lambda e: e.tensor_tensor(acc[:], acc[:], tmp[:], ALU.add), [acc, tmp], [acc])
                        else:
                            V("dve", lambda e: e.tensor_tensor(tmp[:], ps[:], sig[:, g, :], ALU.mult), [ps, sig], [tmp])
                            V("pool", lambda e: e.tensor_tensor(mob[:, dc, :], acc[:], tmp[:], ALU.add), [acc, tmp], [mob])
                P.dma("sp", [(mT.t[:, b * TB:(b + 1) * TB].rearrange("(c p) t -> p c t", p=128), mob[:])], reads=[mob], writes=[mT])

    def phase_m2(l, xin, xout):
        with P.scope():
            wo = P.sb([128, 16, D], BF16, "owo")
            wst = [P.sb([128, 2048], F32, "owst") for _ in range(2)]
            for dc in range(16):
                st = wst[dc % 2]
                P.dma("sp", [(st[:], I["w_out"].t[l, dc * 128:(dc + 1) * 128, :])], reads=[I["w_out"]], writes=[st])
                if dc % 2 == 0:
                    V("pool", lambda e: e.tensor_copy(wo[:, dc, :], st[:]), [st], [wo])
                else:
                    V("dve", lambda e: e.tensor_copy(wo[:, dc, :], st[:]), [st], [wo])
            gbc = P.sb([128, D], F32, "ogbc")
            P.dma("sp", [(gbc[:], I["norm_post"][l:l + 1, :].to_broadcast([128, D]))], reads=[I["norm_post"]], writes=[gbc])
            mt = [P.sb([128, 16, 128], BF16, "omt") for _ in range(2)]
            xt = [P.sb([128, D], F32, "oxt") for _ in range(2)]
            ot = P.sb([128, D], F32, "oot")
            junk = P.sb([128, D], BF16, "ojunk")
            res = [P.sb([128, D], F32, "ores") for _ in range(2)]
            ssq = P.sb([128, 1], F32, "ossq")
            rs = P.sb([128, 1], F32, "ors")
            pps = [P.ps([128, 512], F32, "opp") for _ in range(4)]

            def loadt(i):
                P.dma("sp", [(mt[i % 2][:], mT.t[:, i * 128:(i + 1) * 128].rearrange("(c p) t -> p c t", p=128))], reads=[mT], writes=[mt[i % 2]])
                P.dma("sp", [(xt[i % 2][:], xin[i * 128:(i + 1) * 128, :])], reads=[xin], writes=[xt[i % 2]])
            loadt(0)
            for i in range(NT):
                if i + 1 < NT:
                    loadt(i + 1)
                mti, xti, rsi = mt[i % 2], xt[i % 2], res[i % 2]
                for nb in range(4):
                    ps = pps[nb]
                    for dc in range(16):
                        V("pe", lambda e: e.matmul(ps[:], mti[:, dc, :], wo[:, dc, nb * 512:(nb + 1) * 512], start=(dc == 0), stop=(dc == 15)), [mti, wo], [ps])
                    evac(nb, ot[:, nb * 512:(nb + 1) * 512], ps[:], [ps], [ot])
                V("act", lambda e: e.activation(junk[:], ot[:], AF.Square, accum_out=ssq[:]), [ot], [junk, ssq])
                V("act", lambda e: e.activation(rs[:], ssq[:], AF.Sqrt, bias=1e-6, scale=1.0 / D), [ssq], [rs])
                V("dve", lambda e: e.reciprocal(rs[:], rs[:]), [rs], [rs])
                V("dve", lambda e: e.scalar_tensor_tensor(ot[:], ot[:], rs[:, 0:1], gbc[:], op0=ALU.mult, op1=ALU.mult), [ot, rs, gbc], [ot])
                V("pool", lambda e: e.tensor_tensor(rsi[:], ot[:], xti[:], ALU.add), [ot, xti], [rsi])
                P.dma("sp", [(xout[i * 128:(i + 1) * 128, :], rsi[:])], reads=[rsi], writes=[xout])


    def phase_dsa1(l):
        with P.scope():
            iqT = P.sb([64, 8, S], BF16, "diqT")
            ikT = P.sb([64, S], BF16, "dikT")
            iw = P.sb([128, NT, 8], F32, "diw")
            stg = [P.sb([64, S], F32, "dstg") for _ in range(2)]
            score = P.sb([128, S], F32, "dscore")
            work = P.sb([128, S], F32, "dwork")
            maskb = P.sb([128, S], BF16, "dmaskb")
            mts = [P.sb([128, NT, 128], BF16, "dmts") for _ in range(2)]
            rl = [P.sb([128, 512], F32, "drl") for _ in range(2)]
            m8 = P.sb([128, 8], F32, "dm8")
            thr = P.sb([128, 1], F32, "dthr")
            blo = P.sb([128, 1], F32, "dblo"); bw0 = P.sb([128, 1], F32, "dbw0"); bmid = P.sb([128, 1], F32, "dbmid")
            bcnt = P.sb([128, 1], F32, "dbcnt"); bge = P.sb([128, 1], F32, "dbge"); bstp = P.sb([128, 1], F32, "dbstp")
            pps = [P.ps([128, 512], F32, "dpp") for _ in range(4)]
            ptr = [P.ps([128, 4, 128], BF16, "dptr") for _ in range(2)]
            r0 = FM_ROWS["dsa_iq"]
            for h in range(8):
                st = stg[h % 2]
                P.dma("sp", [(st[:], fm[r0 + h * 64:r0 + (h + 1) * 64, PAD:TP])], reads=[fm], writes=[st])
                V("pool", lambda e: e.tensor_copy(iqT[:, h, :], st[:]), [st], [iqT])
            r0 = FM_ROWS["dsa_ik"]
            P.dma("sp", [(stg[0][:], fm[r0:r0 + 64, PAD:TP])], reads=[fm], writes=[stg[0]])
            V("pool", lambda e: e.tensor_copy(ikT[:], stg[0][:]), [stg[0]], [ikT])
            cw = TM_COLS["dsa_iw"]
            P.dma("sp", [(iw[:], tm.t[:, cw:cw + 8].rearrange("(t p) c -> p t c", p=128))], reads=[tm], writes=[iw])
            V("dve", lambda e: e.tensor_scalar(iw[:], iw[:], float(512.0 ** -0.5), None, op0=ALU.mult), [iw], [iw])
            k = 0
            kt = 0
            for qt in range(NT):
                nk = (qt + 1) * 128
                for sb in range((nk + 511) // 512):
                    w = min(512, nk - sb * 512)
                    cs = slice(sb * 512, sb * 512 + w)
                    for h in range(8):
                        ps = pps[k % 4]
                        rt = rl[k % 2]
                        k += 1
                        V("pe", lambda e: e.matmul(ps[:, 0:w], iqT[:, h, qt * 128:(qt + 1) * 128], ikT[:, cs], start=True, stop=True), [iqT, ikT], [ps])
                        V("act", lambda e: e.activation(rt[:, 0:w], ps[:, 0:w], AF.Relu), [ps], [rt])
                        if h == 0:
                            V("dve", lambda e: e.tensor_scalar(score[:, cs], rt[:, 0:w], iw[:, qt, 0:1], None, op0=ALU.mult), [rt, iw], [score])
                        else:
                            V("dve", lambda e: e.scalar_tensor_tensor(score[:, cs], rt[:, 0:w], iw[:, qt, h:h + 1], score[:, cs], op0=ALU.mult, op1=ALU.add), [rt, iw, score], [score])
                V("pool", lambda e: e.memset(score[0:64, nk - 64:nk], NEG), [], [score])
                if nk <= KTOP:
                    V("pool", lambda e: e.memset(thr[:], NEG / 2), [], [thr])
                else:
                    V("dve", lambda e: e.max(out=m8[:], in_=score[:, 0:nk]), [score], [m8])
                    V("dve", lambda e: e.tensor_reduce(blo[:], score[:, 0:nk - 64], AX.X, ALU.min), [score], [blo])
                    V("dve", lambda e: e.tensor_tensor(bw0[:], m8[:, 0:1], blo[:], ALU.subtract), [m8, blo], [bw0])
                    for it in range(24):
                        f = float(2.0 ** -(it + 1))
                        V("dve", lambda e: e.scalar_tensor_tensor(bmid[:], bw0[:], f, blo[:], op0=ALU.mult, op1=ALU.add), [bw0, blo], [bmid])
                        V("dve", lambda e: e.memset(bcnt[:], 0.0), [], [bcnt])
                        V("dve", lambda e: e.tensor_scalar(maskb[:, 0:nk], score[:, 0:nk], bmid[:, 0:1], 0.0, op0=ALU.is_ge, op1=ALU.add, accum_out=bcnt[:]), [score, bmid], [maskb, bcnt])
                        V("dve", lambda e: e.tensor_scalar(bge[:], bcnt[:], float(KTOP) - 0.5, None, op0=ALU.is_ge), [bcnt], [bge])
                        V("dve", lambda e: e.tensor_tensor(bstp[:], bge[:], bw0[:], ALU.mult), [bge, bw0], [bstp])
                        V("dve", lambda e: e.scalar_tensor_tensor(blo[:], bstp[:], f, blo[:], op0=ALU.mult, op1=ALU.add), [bstp, blo], [blo])
                    V("dve", lambda e: e.tensor_copy(thr[:], blo[:]), [blo], [thr])
                V("dve", lambda e: e.tensor_scalar(maskb[:, 0:nk], score[:, 0:nk], thr[:, 0:1], None, op0=ALU.is_ge), [score, thr], [maskb])
                mtb = mts[qt % 2]
                for j4 in range((qt + 4) // 4):
                    n = min(4, qt + 1 - j4 * 4)
                    pt = ptr[kt % 2]
                    kt += 1
                    for jj in range(n):
                        j = j4 * 4 + jj
                        V("pe", lambda e: e.transpose(pt[:, jj, :], maskb[:, j * 128:(j + 1) * 128], ident_b[:]), [maskb, ident_b], [pt])
                    evac(kt, mtb[:, j4 * 4:j4 * 4 + n, :], pt[:, 0:n, :], [pt], [mtb])
                P.dma("sp", [(maskT.t[qt, 0:nk, :].rearrange("(j p) t -> p j t", p=128), mtb[:, 0:qt + 1, :])], reads=[mtb], writes=[maskT])

    def phase_dsa2(l):
        with P.scope():
            stg = [P.sb([128, S], F32, "astg") for _ in range(2)]
            kT = P.sb([128, S], BF16, "akT")
            qT = P.sb([128, S], BF16, "aqT")
            Vh = P.sb([128, NT, 128], BF16, "aVh")
            sg = P.sb([128, S], F32, "asg")
            mk = [P.sb([128, 4, 128], BF16, "amk") for _ in range(3)]
            ex = [P.sb([128, 4, 128], BF16, "aex") for _ in range(2)]
            ptt = [P.sb([128, 4, 128], BF16, "aptt") for _ in range(2)]
            pl = [P.ps([128, 4, 128], F32, "apl") for _ in range(2)]
            po = [P.ps([128, 512], F32, "apo") for _ in range(2)]
            pd = [P.ps([128, 512], F32, "apd") for _ in range(2)]
            rden = P.sb([128, 128], F32, "arden")
            o1 = P.sb([128, 128], F32, "ao1")
            yo = [P.sb([128, S], BF16, "ayo") for _ in range(2)]
            sc = float(128.0 ** -0.5)
            rk, rq, rg = FM_ROWS["dsa_k"], FM_ROWS["dsa_q"], FM_ROWS["dsa_gate"]
            cv = TM_COLS["dsa_v"]
            kb = 0
            for h in range(8):
                P.dma("sp", [(stg[0][:], fm[rk + h * 128:rk + (h + 1) * 128, PAD:TP])], reads=[fm], writes=[stg[0]])
                V("pool", lambda e: e.tensor_copy(kT[:], stg[0][:]), [stg[0]], [kT])
                P.dma("sp", [(stg[1][:], fm[rq + h * 128:rq + (h + 1) * 128, PAD:TP])], reads=[fm], writes=[stg[1]])
                V("pool", lambda e: e.tensor_copy(qT[:], stg[1][:]), [stg[1]], [qT])
                P.dma("sp", [(stg[0][:].rearrange("p (j v) -> p j v", v=128), tm.t[:, cv + h * 128:cv + (h + 1) * 128].rearrange("(j p) v -> p j v", p=128))], reads=[tm], writes=[stg[0]])
                V("pool", lambda e: e.tensor_copy(Vh[:], stg[0][:].rearrange("p (j v) -> p j v", v=128)), [stg[0]], [Vh])
                P.dma("sp", [(stg[1][:], fm[rg + h * 128:rg + (h + 1) * 128, PAD:TP])], reads=[fm], writes=[stg[1]])
                V("act", lambda e: e.activation(sg[:], stg[1][:], AF.Silu), [stg[1]], [sg])
                yob = yo[h % 2]
                groups = []
                for qt in range(NT):
                    nj = qt + 1
                    ng = (nj + 3) // 4
                    for j4 in range(ng):
                        groups.append((qt, j4, min(4, nj - j4 * 4), j4 == ng - 1))

                def front(i):
                    qt, j4, n, last = groups[i]
                    kk_ = kb + i
                    mkb, plb = mk[kk_ % 3], pl[kk_ % 2]
                    qs = slice(qt * 128, (qt + 1) * 128)
                    P.dma("sp", [(mkb[:, 0:n, :], maskT.t[qt, j4 * 512:j4 * 512 + n * 128, :].rearrange("(j p) t -> p j t", p=128))], reads=[maskT], writes=[mkb])
                    for jj in range(n):
                        j = j4 * 4 + jj
                        V("pe", lambda e: e.matmul(plb[:, jj, :], kT[:, j * 128:(j + 1) * 128], qT[:, qs], start=True, stop=True), [kT, qT], [plb])

                def back(i):
                    qt, j4, n, last = groups[i]
                    kk_ = kb + i
                    nj = qt + 1
                    mkb, plb, exb, ptb = mk[kk_ % 3], pl[kk_ % 2], ex[kk_ % 2], ptt[kk_ % 2]
                    po_, pd_ = po[qt % 2], pd[qt % 2]
                    qs = slice(qt * 128, (qt + 1) * 128)
                    V("act", lambda e: e.activation(exb[:, 0:n, :], plb[:, 0:n, :], AF.Exp, scale=sc), [plb], [exb])
                    V("dve", lambda e: e.tensor_tensor(ptb[:, 0:n, :], exb[:, 0:n, :], mkb[:, 0:n, :], ALU.mult), [exb, mkb], [ptb])
                    for jj in range(n):
                        j = j4 * 4 + jj
                        V("pe", lambda e: e.matmul(po_[:, 0:128], Vh[:, j, :], ptb[:, jj, :], start=(j == 0), stop=(j == nj - 1)), [Vh, ptb], [po_])
                        V("pe", lambda e: e.matmul(pd_[:, 0:128], ones_b[:], ptb[:, jj, :], start=(j == 0), stop=(j == nj - 1)), [ones_b, ptb], [pd_])
                    if last:
                        V("dve", lambda e: e.reciprocal(rden[:], pd_[:, 0:128]), [pd_], [rden])
                        V("dve", lambda e: e.tensor_tensor(o1[:], po_[:, 0:128], rden[:], ALU.mult), [po_, rden], [o1])
                        V("pool", lambda e: e.tensor_tensor(yob[:, qs], o1[:], sg[:, qs], ALU.mult), [o1, sg], [yob])
                front(0)
                for i in range(len(groups)):
                    if i + 1 < len(groups):
                        front(i + 1)
                    back(i)
                kb += len(groups)
                P.dma("sp", [(ysT[1024 + h * 128:1024 + (h + 1) * 128, :], yob[:])], reads=[yob], writes=[ysT])

    def phase_rwp(l):
        with P.scope():
            c0 = TM_COLS["rw_r"]
            mub = P.sb([128, 2240], F32, "rmub")
            mu = I["rwkv_mu"]
            P.dma("sp", [(mub[:, 0:2048], mu[l:l + 1, 0:2048].to_broadcast([128, 2048])), (mub[:, 2048:2240], mu[l:l + 1, 3072:3264].to_broadcast([128, 192]))], reads=[mu], writes=[mub])

            def bc(name, n=1024):
                t = P.sb([128, n], F32, "rb" + name)
                src = I[name]
                if name == "rwkv_r_k":
                    ap = src.t[l:l + 1, :, :].rearrange("a h k -> a (h k)").to_broadcast([128, n])
                else:
                    ap = src[l:l + 1, :].to_broadcast([128, n])
                P.dma("sp", [(t[:], ap)], reads=[src], writes=[t])
                return t
            kkb, kab, w0b, a0b, rkb = bc("rwkv_k_k"), bc("rwkv_k_a"), bc("rwkv_w0"), bc("rwkv_a0"), bc("rwkv_r_k")
            omka = P.sb([128, 1024], F32, "romka")
            V("dve", lambda e: e.tensor_scalar(omka[:], kab[:], -1.0, 1.0, op0=ALU.mult, op1=ALU.add), [kab], [omka])
            ww2 = P.sb([96, 1024], F32, "rww2")
            wa2 = P.sb([96, 1024], F32, "rwa2")
            P.dma("sp", [(ww2[:], I["rwkv_w_w2"].t[l, :, :])], reads=[I["rwkv_w_w2"]], writes=[ww2])
            P.dma("sp", [(wa2[:], I["rwkv_w_a2"].t[l, :, :])], reads=[I["rwkv_w_a2"]], writes=[wa2])
            cur = [P.sb([128, 2240], F32, "rcur") for _ in range(2)]
            prv = [P.sb([128, 2240], F32, "rprv") for _ in range(2)]
            dd = P.sb([128, 2240], F32, "rdd")
            th = P.sb([128, 96], F32, "rth")
            thT = P.sb([96, 128], F32, "rthT")
            alT = P.sb([96, 128], F32, "ralT")
            ptT = [P.ps([96, 128], F32, "rptT") for _ in range(2)]
            pz = [P.ps([128, 2, 512], F32, "rpz") for _ in range(2)]
            zs = P.sb([128, 1024], F32, "rzs")
            aa = P.sb([128, 1024], F32, "raa")
            kk = P.sb([128, 1024], F32, "rkk")
            sq = P.sb([128, 1024], F32, "rsq")
            ssq = P.sb([128, 16], F32, "rssq")
            tt = P.sb([128, 1024], F32, "rtt")
            out5 = [P.sb([128, 5, 1024], F32, "rout5") for _ in range(2)]
            bon = [P.sb([128, 16], F32, "rbon") for _ in range(2)]

            def loadt(i):
                P.dma("sp", [(cur[i % 2][:], tm[i * 128:(i + 1) * 128, c0:c0 + 2240])], reads=[tm], writes=[cur[i % 2]])
                if i == 0:
                    V("pool", lambda e: e.memset(prv[0][0:1, :], 0.0), [], [prv[0]])
                    P.dma("sp", [(prv[0][1:128, :], tm[0:127, c0:c0 + 2240])], reads=[tm], writes=[prv[0]])
                else:
                    P.dma("sp", [(prv[i % 2][:], tm[i * 128 - 1:i * 128 + 127, c0:c0 + 2240])], reads=[tm], writes=[prv[i % 2]])
            loadt(0)
            for i in range(NT):
                if i + 1 < NT:
                    loadt(i + 1)
                cu, pr, o5, bn = cur[i % 2], prv[i % 2], out5[i % 2], bon[i % 2]
                V("dve", lambda e: e.tensor_tensor(dd[:], pr[:], cu[:], ALU.subtract), [pr, cu], [dd])
                V("pool", lambda e: e.tensor_tensor(dd[:], dd[:], mub[:], ALU.mult), [dd, mub], [dd])
                V("dve", lambda e: e.tensor_tensor(cu[:], cu[:], dd[:], ALU.add), [cu, dd], [cu])
                rr_, k_ = cu[:, 0:1024], cu[:, 1024:2048]
                V("act", lambda e: e.activation(th[:], cu[:, 2048:2144], AF.Tanh), [cu], [th])
                V("pe", lambda e: e.transpose(ptT[0][:], th[:], ident_f[:]), [th, ident_f], [ptT[0]])
                V("act", lambda e: e.copy(thT[:], ptT[0][:]), [ptT[0]], [thT])
                V("pe", lambda e: e.transpose(ptT[1][:], cu[:, 2144:2240], ident_f[:]), [cu, ident_f], [ptT[1]])
                V("dve", lambda e: e.tensor_copy(alT[:], ptT[1][:]), [ptT[1]], [alT])
                for n2 in range(2):
                    V("pe", lambda e: e.matmul(pz[0][:, n2, :], thT[:], ww2[:, n2 * 512:(n2 + 1) * 512], start=True, stop=True), [thT, ww2], [pz[0]])
                    V("pe", lambda e: e.matmul(pz[1][:, n2, :], alT[:], wa2[:, n2 * 512:(n2 + 1) * 512], start=True, stop=True), [alT, wa2], [pz[1]])
                V("dve", lambda e: e.tensor_tensor(zs[:], pz[0][:].rearrange("p a b -> p (a b)"), w0b[:], ALU.add), [pz[0], w0b], [zs])
                V("act", lambda e: e.activation(zs[:], zs[:], AF.Sigmoid), [zs], [zs])
                V("act", lambda e: e.activation(o5[:, 0, :], zs[:], AF.Exp, scale=-0.606531), [zs], [o5])
                V("dve", lambda e: e.tensor_tensor(aa[:], pz[1][:].rearrange("p a b -> p (a b)"), a0b[:], ALU.add), [pz[1], a0b], [aa])
                V("act", lambda e: e.activation(aa[:], aa[:], AF.Sigmoid), [aa], [aa])
                V("dve", lambda e: e.tensor_tensor(kk[:], k_, kkb[:], ALU.mult), [cu, kkb], [kk])
                V("act", lambda e: e.activation(sq[:], kk[:], AF.Square), [kk], [sq])
                V("dve", lambda e: e.tensor_reduce(ssq[:], sq[:].rearrange("p (h k) -> p h k", k=64), AX.X, ALU.add), [sq], [ssq])
                V("act", lambda e: e.activation(ssq[:], ssq[:], AF.Sqrt, bias=1e-12, scale=1.0), [ssq], [ssq])
                V("dve", lambda e: e.reciprocal(ssq[:], ssq[:]), [ssq], [ssq])
                V("dve", lambda e: e.tensor_tensor(o5[:, 1, :].rearrange("p (h k) -> p h k", k=64), kk[:].rearrange("p (h k) -> p h k", k=64),
                                                   ssq[:, :].unsqueeze(2).to_broadcast([128, 16, 64]), ALU.mult), [kk, ssq], [o5])
                V("dve", lambda e: e.scalar_tensor_tensor(o5[:, 2, :], o5[:, 1, :], -1.0, aa[:], op0=ALU.mult, op1=ALU.mult), [o5, aa], [o5])
                V("pool", lambda e: e.tensor_tensor(tt[:], aa[:], kab[:], ALU.mult), [aa, kab], [tt])
                V("pool", lambda e: e.tensor_tensor(tt[:], tt[:], omka[:], ALU.add), [tt, omka], [tt])
                V("dve", lambda e: e.tensor_tensor(o5[:, 3, :], k_, tt[:], ALU.mult), [cu, tt], [o5])
                V("pool", lambda e: e.tensor_copy(o5[:, 4, :], rr_), [cu], [o5])
                V("pool", lambda e: e.tensor_tensor(tt[:], rr_, rkb[:], ALU.mult), [cu, rkb], [tt])
                V("dve", lambda e: e.tensor_tensor(tt[:], tt[:], o5[:, 3, :], ALU.mult), [tt, o5], [tt])
                V("dve", lambda e: e.tensor_reduce(bn[:], tt[:].rearrange("p (h k) -> p h k", k=64), AX.X, ALU.add), [tt], [bn])
                P.dma("sp", [(rwtm[i * 128:(i + 1) * 128, :, :], o5[:])], reads=[o5], writes=[rwtm])
                P.dma("sp", [(rwbon[i * 128:(i + 1) * 128, :], bn[:])], reads=[bn], writes=[rwbon])

    def phase_rws(l):
        with P.scope():
            sel = P.sb([128, 64, 128], F32, "wsel")
            blk = P.sb([128, 128], F32, "wblk")
            blkm = P.sb([128, 128], F32, "wblkm")
            i2 = P.sb([128, 64], F32, "wi2")
            P.dma("sp", [(sel[:], I["c_sel"][:, :, :])], reads=[I["c_sel"]], writes=[sel])
            P.dma("sp", [(blk[:], I["c_blk"][:, :])], reads=[I["c_blk"]], writes=[blk])
            P.dma("sp", [(i2[:], I["c_i2"][:, :])], reads=[I["c_i2"]], writes=[i2])
            V("dve", lambda e: e.tensor_scalar(blkm[:], blk[:], 1.0 / 64.0, None, op0=ALU.mult), [blk], [blkm])

            def hv(name, off=0):
                t = P.sb([128, 8], F32, "w" + name)
                src = I[name]
                P.dma("sp", [(t[hf * 64:(hf + 1) * 64, :], src.t[l, off + hf * 512:off + (hf + 1) * 512].rearrange("(h v) -> v h", v=64)) for hf in range(2)],
                      reads=[src], writes=[t], allow_slow_non_contiguous=True)
                return t
            lng, lnb = hv("rwkv_lnx_g"), hv("rwkv_lnx_b")
            muv, mug = hv("rwkv_mu", 2048), hv("rwkv_mu", 3264)
            St = P.sb([128, 8, 64], F32, "wS")
            V("pool", lambda e: e.memset(St[:], 0.0), [], [St])
            X = [P.sb([128, 5, 512], F32, "wX") for _ in range(2)]
            vch = [P.sb([128, 8, 65], F32, "wvch") for _ in range(2)]
            gch = [P.sb([128, 8, 65], F32, "wgch") for _ in range(2)]
            s2 = [P.sb([128, 8], F32, "ws2") for _ in range(2)]
            vl = P.sb([128, 8, 64], F32, "wvl")
            gl = P.sb([128, 8, 64], F32, "wgl")
            bcs = [[P.sb([128, 512], F32, "wbc") for _ in range(5)] for _ in range(2)]
            pb = [P.ps([128, 512], F32, "wpb") for _ in range(5)]
            pq = [P.ps([128, 512], F32, "wpq") for _ in range(2)]
            Y = P.sb([128, 8, 64], F32, "wY")
            tA = P.sb([128, 8, 64], F32, "wtA")
            tB = [P.sb([128, 8, 64], F32, "wtB") for _ in range(2)]
            sa = P.sb([128, 8], F32, "wsa")
            mean = P.sb([128, 512], F32, "wmean")
            yc = P.sb([128, 8, 64], F32, "wyc")
            sq = P.sb([128, 512], F32, "wsq")
            rstd = P.sb([128, 512], F32, "wrstd")
            rh = [P.sb([128, 64], F32, "wrh") for _ in range(2)]
            yb = [P.sb([128, 8, 64], BF16, "wyb") for _ in range(2)]
            rv, rg = FM_ROWS["rw_v"], FM_ROWS["rw_gate"]

            def b3(t2):
                return t2[:, :].unsqueeze(2).to_broadcast([128, 8, 64])

            def loadc(c):
                t0 = c * 64
                P.dma("sp", [(X[c % 2][hf * 64:(hf + 1) * 64, :, :], rwtm.t[t0:t0 + 64, :, hf * 512:(hf + 1) * 512]) for hf in range(2)], reads=[rwtm], writes=[X[c % 2]])
                P.dma("sp", [(vch[c % 2][hf * 64:(hf + 1) * 64, :, :], fm.t[rv + hf * 512:rv + (hf + 1) * 512, PAD + t0 - 1:PAD + t0 + 64].rearrange("(h v) t -> v h t", v=64)) for hf in range(2)],
                      reads=[fm], writes=[vch[c % 2]])
                P.dma("sp", [(gch[c % 2][hf * 64:(hf + 1) * 64, :, :], fm.t[rg + hf * 512:rg + (hf + 1) * 512, PAD + t0 - 1:PAD + t0 + 64].rearrange("(h v) t -> v h t", v=64)) for hf in range(2)],
                      reads=[fm], writes=[gch[c % 2]])
                P.dma("sp", [(s2[c % 2][hf * 64:(hf + 1) * 64, :], rwbon[t0:t0 + 64, hf * 8:(hf + 1) * 8]) for hf in range(2)], reads=[rwbon], writes=[s2[c % 2]])
            loadc(0)
            step = 0
            for c in range(NCH):
                if c + 1 < NCH:
                    loadc(c + 1)
                t0 = c * 64
                Xc, vc, gc, s2c = X[c % 2], vch[c % 2], gch[c % 2], s2[c % 2]
                for (src, dst, mu_) in ((vc, vl, muv), (gc, gl, mug)):
                    V("pool", lambda e: e.tensor_tensor(dst[:], src[:, :, 0:64], src[:, :, 1:65], ALU.subtract), [src], [dst])
                    V("pool", lambda e: e.tensor_tensor(dst[:], dst[:], b3(mu_), ALU.mult), [dst, mu_], [dst])
                    V("pool", lambda e: e.tensor_tensor(dst[:], dst[:], src[:, :, 1:65], ALU.add), [dst, src], [dst])
                for tl in range(64):
                    bset = bcs[step % 2]
                    tBs = tB[step % 2]
                    step += 1
                    for p in range(5):
                        V("pe", lambda e: e.matmul(pb[p][:], sel[:, tl, :], Xc[:, p, :], start=True, stop=True), [sel, Xc], [pb[p]])
                        V("act", lambda e: e.copy(bset[p][:], pb[p][:]), [pb[p]], [bset[p]])
                    wB, kkB, kkanB, k2B, rB = [b[:].rearrange("p (h k) -> p h k", k=64) for b in bset]
                    V("pool", lambda e: e.tensor_tensor(tBs[:], k2B, vl[:, :, tl:tl + 1].to_broadcast([128, 8, 64]), ALU.mult), [bset[3], vl], [tBs])
                    V("dve", lambda e: e.tensor_tensor(tA[:], St[:], kkB, ALU.mult), [St, bset[1]], [tA])
                    V("dve", lambda e: e.tensor_reduce(sa[:], tA[:], AX.X, ALU.add), [tA], [sa])
                    V("dve", lambda e: e.tensor_tensor(St[:], St[:], wB, ALU.mult), [St, bset[0]], [St])
                    V("dve", lambda e: e.tensor_tensor(tA[:], kkanB, b3(sa), ALU.mult), [bset[2], sa], [tA])
                    V("dve", lambda e: e.tensor_tensor(St[:], St[:], tA[:], ALU.add), [St, tA], [St])
                    V("dve", lambda e: e.tensor_tensor(St[:], St[:], tBs[:], ALU.add), [St, tBs], [St])
                    V("dve", lambda e: e.tensor_tensor(tA[:], St[:], rB, ALU.mult), [St, bset[4]], [tA])
                    V("dve", lambda e: e.tensor_reduce(Y[:, :, tl], tA[:], AX.X, ALU.add), [tA], [Y])
                Yf = Y[:].rearrange("p h t -> p (h t)")
                V("pe", lambda e: e.matmul(pq[0][:], blkm[:], Yf, start=True, stop=True), [blkm, Y], [pq[0]])
                V("dve", lambda e: e.tensor_tensor(yc[:].rearrange("p h t -> p (h t)"), Yf, pq[0][:], ALU.subtract), [Y, pq[0]], [yc])
                V("act", lambda e: e.activation(sq[:], yc[:].rearrange("p h t -> p (h t)"), AF.Square), [yc], [sq])
                V("pe", lambda e: e.matmul(pq[1][:], blkm[:], sq[:], start=True, stop=True), [blkm, sq], [pq[1]])
                V("act", lambda e: e.activation(rstd[:], pq[1][:], AF.Sqrt, bias=64e-5, scale=1.0), [pq[1]], [rstd])
                V("dve", lambda e: e.reciprocal(rstd[:], rstd[:]), [rstd], [rstd])
                V("dve", lambda e: e.tensor_tensor(yc[:].rearrange("p h t -> p (h t)"), yc[:].rearrange("p h t -> p (h t)"), rstd[:], ALU.mult), [yc, rstd], [yc])
                V("dve", lambda e: e.tensor_tensor(yc[:], yc[:], b3(lng), ALU.mult), [yc, lng], [yc])
                V("dve", lambda e: e.tensor_tensor(yc[:], yc[:], b3(lnb), ALU.add), [yc, lnb], [yc])
                for h8 in range(8):
                    rhh = rh[h8 % 2]
                    V("pool", lambda e: e.tensor_scalar(rhh[:], i2[:], s2c[:, h8:h8 + 1], None, op0=ALU.mult), [i2, s2c], [rhh])
                    V("pe", lambda e: e.matmul(pq[0][:, h8 * 64:(h8 + 1) * 64], blk[:], rhh[:], start=True, stop=True), [blk, rhh], [pq[0]])
                V("dve", lambda e: e.tensor_tensor(tA[:].rearrange("p h t -> p (h t)"), pq[0][:], vl[:].rearrange("p h t -> p (h t)"), ALU.mult), [pq[0], vl], [tA])
                V("dve", lambda e: e.tensor_tensor(yc[:], yc[:], tA[:], ALU.add), [yc, tA], [yc])
                V("act", lambda e: e.activation(gl[:], gl[:], AF.Silu), [gl], [gl])
                ybc = yb[c % 2]
                V("dve", lambda e: e.tensor_tensor(ybc[:], yc[:], gl[:], ALU.mult), [yc, gl], [ybc])
                P.dma("sp", [(ysT.t[2048 + hf * 512:2048 + (hf + 1) * 512, t0:t0 + 64].rearrange("(h v) t -> v h t", v=64), ybc[hf * 64:(hf + 1) * 64, :, :]) for hf in range(2)],
                      reads=[ybc], writes=[ysT])


    def phase_rwp2(l):
        with P.scope():
            c0 = TM_COLS["rw_r"]
            NCc = 3264
            mub = P.sb([128, NCc], F32, "rmub")
            mu = I["rwkv_mu"]
            P.dma("sp", [(mub[:], mu[l:l + 1, 0:NCc].to_broadcast([128, NCc]))], reads=[mu], writes=[mub])

            def bc(name, n=1024):
                t = P.sb([128, n], F32, "rb" + name)
                src = I[name]
                if name == "rwkv_r_k":
                    ap = src.t[l:l + 1, :, :].rearrange("a h k -> a (h k)").to_broadcast([128, n])
                else:
                    ap = src[l:l + 1, :].to_broadcast([128, n])
                P.dma("sp", [(t[:], ap)], reads=[src], writes=[t])
                return t
            kkb, kab, w0b, a0b, rkb = bc("rwkv_k_k"), bc("rwkv_k_a"), bc("rwkv_w0"), bc("rwkv_a0"), bc("rwkv_r_k")
            omka = P.sb([128, 1024], F32, "romka")
            V("dve", lambda e: e.tensor_scalar(omka[:], kab[:], -1.0, 1.0, op0=ALU.mult, op1=ALU.add), [kab], [omka])
            ww2 = P.sb([96, 1024], F32, "rww2")
            wa2 = P.sb([96, 1024], F32, "rwa2")
            lmat = P.sb([128, 128], F32, "rlmat")
            umat = P.sb([128, 128], F32, "rumat")
            ind = P.sb([128, 4], F32, "rind")
            P.dma("sp", [(ww2[:], I["rwkv_w_w2"].t[l, :, :])], reads=[I["rwkv_w_w2"]], writes=[ww2])
            P.dma("sp", [(wa2[:], I["rwkv_w_a2"].t[l, :, :])], reads=[I["rwkv_w_a2"]], writes=[wa2])
            P.dma("sp", [(lmat[:], I["c_lmat32"][:, :])], reads=[I["c_lmat32"]], writes=[lmat])
            P.dma("sp", [(umat[:], I["c_umat32"][:, :])], reads=[I["c_umat32"]], writes=[umat])
            P.dma("sp", [(ind[:], I["c_ind32"][:, :])], reads=[I["c_ind32"]], writes=[ind])
            cur = [P.sb([128, NCc], F32, "rcur") for _ in range(2)]
            prv = P.sb([128, NCc], F32, "rprv")
            dd = P.sb([128, NCc], F32, "rdd")
            th = P.sb([128, 96], F32, "rth")
            thT = P.sb([96, 128], F32, "rthT")
            alT = P.sb([96, 128], F32, "ralT")
            pz = [P.ps([128, 2, 512], F32, "rpz") for _ in range(2)]
            ptr = [P.ps([64, 4, 128], F32, "rptr") for _ in range(2)]
            ptT2 = P.ps([96, 2, 128], F32, "rptT")
            pdec = P.ps([64, 16, 4], F32, "rpdec")
            zs = P.sb([128, 1024], F32, "rzs")
            aa = P.sb([128, 1024], F32, "raa")
            kk = P.sb([128, 1024], F32, "rkk")
            kkn = P.sb([128, 1024], F32, "rkkn")
            kkan = P.sb([128, 1024], F32, "rkkan")
            k2 = P.sb([128, 1024], F32, "rk2")
            ssq = P.sb([128, 16], F32, "rssq")
            tt = P.sb([128, 1024], F32, "rtt")
            E1 = P.sb([128, 1024], F32, "rE1")
            Q = P.sb([128, 4, 1024], F32, "rQ")
            o3 = [P.sb([128, 3, 1024], F32, "ro3") for _ in range(2)]
            bon = [P.sb([128, 16], F32, "rbon") for _ in range(2)]
            XTs = P.sb([64, 16, 4, 128], F32, "rXTs")
            decs = P.sb([64, 16, 4], F32, "rdecs")

            def loadt(i):
                P.dma("sp", [(cur[i % 2][:], tm[i * 128:(i + 1) * 128, c0:c0 + NCc])], reads=[tm], writes=[cur[i % 2]])
            loadt(0)
            for i in range(NT):
                if i + 1 < NT:
                    loadt(i + 1)
                cu, o3i, bn = cur[i % 2], o3[i % 2], bon[i % 2]
                if i == 0:
                    V("pool", lambda e: e.memset(prv[0:1, :], 0.0), [], [prv])
                    P.dma("sp", [(prv[1:128, :], tm[0:127, c0:c0 + NCc])], reads=[tm], writes=[prv])
                else:
                    P.dma("sp", [(prv[:], tm[i * 128 - 1:i * 128 + 127, c0:c0 + NCc])], reads=[tm], writes=[prv])
                V("dve", lambda e: e.tensor_tensor(dd[:], prv[:], cu[:], ALU.subtract), [prv, cu], [dd])
                V("pool", lambda e: e.tensor_tensor(dd[:], dd[:], mub[:], ALU.mult), [dd, mub], [dd])
                V("dve", lambda e: e.tensor_tensor(cu[:], cu[:], dd[:], ALU.add), [cu, dd], [cu])
                rr_, k_, v_ = cu[:, 0:1024], cu[:, 1024:2048], cu[:, 2048:3072]
                V("act", lambda e: e.activation(th[:], cu[:, 3072:3168], AF.Tanh), [cu], [th])
                V("pe", lambda e: e.transpose(ptT2[:, 0, :], th[:], ident_f[:]), [th, ident_f], [ptT2])
                V("pe", lambda e: e.transpose(ptT2[:, 1, :], cu[:, 3168:3264], ident_f[:]), [cu, ident_f], [ptT2])
                V("act", lambda e: e.copy(thT[:], ptT2[:, 0, :]), [ptT2], [thT])
                V("dve", lambda e: e.tensor_copy(alT[:], ptT2[:, 1, :]), [ptT2], [alT])
                for n2 in range(2):
                    V("pe", lambda e: e.matmul(pz[0][:, n2, :], thT[:], ww2[:, n2 * 512:(n2 + 1) * 512], start=True, stop=True), [thT, ww2], [pz[0]])
                    V("pe", lambda e: e.matmul(pz[1][:, n2, :], alT[:], wa2[:, n2 * 512:(n2 + 1) * 512], start=True, stop=True), [alT, wa2], [pz[1]])
                V("dve", lambda e: e.tensor_tensor(zs[:], pz[0][:].rearrange("p a b -> p (a b)"), w0b[:], ALU.add), [pz[0], w0b], [zs])
                V("act", lambda e: e.activation(zs[:], zs[:], AF.Sigmoid), [zs], [zs])
                V("dve", lambda e: e.tensor_scalar(zs[:], zs[:], -0.606531, None, op0=ALU.mult), [zs], [zs])
                V("dve", lambda e: e.tensor_tensor(aa[:], pz[1][:].rearrange("p a b -> p (a b)"), a0b[:], ALU.add), [pz[1], a0b], [aa])
                V("act", lambda e: e.activation(aa[:], aa[:], AF.Sigmoid), [aa], [aa])
                V("dve", lambda e: e.tensor_tensor(kk[:], k_, kkb[:], ALU.mult), [cu, kkb], [kk])
                V("act", lambda e: e.activation(tt[:], kk[:], AF.Square), [kk], [tt])
                V("dve", lambda e: e.tensor_reduce(ssq[:], tt[:].rearrange("p (h k) -> p h k", k=64), AX.X, ALU.add), [tt], [ssq])
                V("act", lambda e: e.activation(ssq[:], ssq[:], AF.Sqrt, bias=1e-12, scale=1.0), [ssq], [ssq])
                V("dve", lambda e: e.reciprocal(ssq[:], ssq[:]), [ssq], [ssq])
                V("dve", lambda e: e.tensor_tensor(kkn[:].rearrange("p (h k) -> p h k", k=64), kk[:].rearrange("p (h k) -> p h k", k=64),
                                                   ssq[:, :].unsqueeze(2).to_broadcast([128, 16, 64]), ALU.mult), [kk, ssq], [kkn])
                V("dve", lambda e: e.scalar_tensor_tensor(kkan[:], kkn[:], -1.0, aa[:], op0=ALU.mult, op1=ALU.mult), [kkn, aa], [kkan])
                V("pool", lambda e: e.tensor_tensor(tt[:], aa[:], kab[:], ALU.mult), [aa, kab], [tt])
                V("pool", lambda e: e.tensor_tensor(tt[:], tt[:], omka[:], ALU.add), [tt, omka], [tt])
                V("dve", lambda e: e.tensor_tensor(k2[:], k_, tt[:], ALU.mult), [cu, tt], [k2])
                V("pool", lambda e: e.tensor_tensor(tt[:], rr_, rkb[:], ALU.mult), [cu, rkb], [tt])
                V("dve", lambda e: e.tensor_tensor(tt[:], tt[:], k2[:], ALU.mult), [tt, k2], [tt])
                V("dve", lambda e: e.tensor_reduce(bn[:], tt[:].rearrange("p (h k) -> p h k", k=64), AX.X, ALU.add), [tt], [bn])
                for n2 in range(2):
                    V("pe", lambda e: e.matmul(pz[0][:, n2, :], lmat[:], zs[:, n2 * 512:(n2 + 1) * 512], start=True, stop=True), [lmat, zs], [pz[0]])
                    V("pe", lambda e: e.matmul(pz[1][:, n2, :], umat[:], zs[:, n2 * 512:(n2 + 1) * 512], start=True, stop=True), [umat, zs], [pz[1]])
                cwf = pz[0][:].rearrange("p a b -> p (a b)")
                gf = pz[1][:].rearrange("p a b -> p (a b)")
                for n2 in range(2):
                    V("act", lambda e: e.activation(E1[:, n2 * 512:(n2 + 1) * 512], pz[0][:, n2, :], AF.Exp), [pz[0]], [E1])
                V("dve", lambda e: e.tensor_tensor(Q[:, 3, :], rr_, E1[:], ALU.mult), [cu, E1], [Q])
                for n2 in range(2):
                    V("act", lambda e: e.activation(E1[:, n2 * 512:(n2 + 1) * 512], pz[0][:, n2, :], AF.Exp, scale=-1.0), [pz[0]], [E1])
                V("dve", lambda e: e.tensor_tensor(Q[:, 0, :], kkan[:], E1[:], ALU.mult), [kkan, E1], [Q])
                V("pool", lambda e: e.tensor_tensor(Q[:, 1, :], k2[:], E1[:], ALU.mult), [k2, E1], [Q])
                V("dve", lambda e: e.tensor_tensor(tt[:], cwf, zs[:], ALU.subtract), [pz[0], zs], [tt])
                V("act", lambda e: e.activation(E1[:], tt[:], AF.Exp), [tt], [E1])
                V("dve", lambda e: e.tensor_tensor(Q[:, 2, :], kkn[:], E1[:], ALU.mult), [kkn, E1], [Q])
                for n2 in range(2):
                    V("act", lambda e: e.activation(E1[:, n2 * 512:(n2 + 1) * 512], pz[1][:, n2, :], AF.Exp), [pz[1]], [E1])
                V("dve", lambda e: e.tensor_tensor(o3i[:, 0, :], kkan[:], E1[:], ALU.mult), [kkan, E1], [o3i])
                V("pool", lambda e: e.tensor_tensor(o3i[:, 1, :], k2[:], E1[:], ALU.mult), [k2, E1], [o3i])
                V("pool", lambda e: e.tensor_copy(o3i[:, 2, :], v_), [cu], [o3i])
                for h in range(16):
                    V("pe", lambda e: e.matmul(pdec[:, h, :], zs[:, h * 64:(h + 1) * 64], ind[:], start=True, stop=True), [zs, ind], [pdec])
                V("act", lambda e: e.activation(decs[:], pdec[:], AF.Exp), [pdec], [decs])
                P.dma("sp", [(rwdec[:, :, i * 4:(i + 1) * 4], decs[:])], reads=[decs], writes=[rwdec])
                for h in range(16):
                    pt = ptr[h % 2]
                    for q in range(4):
                        V("pe", lambda e: e.transpose(pt[:, q, :], Q[:, q, h * 64:(h + 1) * 64], ident_f[:]), [Q, ident_f], [pt])
                    evac(h, XTs[:, h, :, :], pt[:], [pt], [XTs])
                P.dma("sp", [(rwxt.t[:, :, i, c4 * 128:(c4 + 1) * 128].rearrange("k h (q t) -> k h q t", t=32), XTs[:, :, :, c4 * 32:(c4 + 1) * 32]) for c4 in range(4)],
                      reads=[XTs], writes=[rwxt])
                P.dma("sp", [(rwtm2[i * 128:(i + 1) * 128, :, :], o3i[:])], reads=[o3i], writes=[rwtm2])
                P.dma("sp", [(rwbon[i * 128:(i + 1) * 128, :], bn[:])], reads=[bn], writes=[rwbon])

    def phase_rws2(l):
        with P.scope():
            mtp = P.sb([64, 64], F32, "wmtp")
            mn = P.sb([32, 32], F32, "wmn")
            ones64 = P.sb([64, 64], F32, "wones")
            onesm = P.sb([64, 64], F32, "wonesm")
            P.dma("sp", [(mtp[:], I["c_m32tp"][:, :])], reads=[I["c_m32tp"]], writes=[mtp])
            P.dma("sp", [(mn[:], I["c_m32n"][:, :])], reads=[I["c_m32n"]], writes=[mn])
            V("pool", lambda e: e.memset(ones64[:], 1.0), [], [ones64])
            V("pool", lambda e: e.memset(onesm[:], 1.0 / 64.0), [], [onesm])

            def hv(name, off=0):
                t = P.sb([64, 16], F32, "w" + name)
                src = I[name]
                P.dma("sp", [(t[:], src.t[l, off:off + 1024].rearrange("(h v) -> v h", v=64))], reads=[src], writes=[t], allow_slow_non_contiguous=True)
                return t
            lng, lnb = hv("rwkv_lnx_g"), hv("rwkv_lnx_b")
            muv, mug = hv("rwkv_mu", 2048), hv("rwkv_mu", 3264)
            decall = P.sb([64, 16, NC32], F32, "wdec")
            P.dma("sp", [(decall[:], rwdec[:, :, :])], reads=[rwdec], writes=[decall])
            H = P.sb([64, 16, 64], F32, "wH")
            V("pool", lambda e: e.memset(H[:], 0.0), [], [H])
            XT = [P.sb([64, 16, 4, 4, 32], F32, "wXT") for _ in range(2)]
            BK = [P.sb([64, 16, 64], F32, "wBK") for _ in range(2)]
            UV = [P.sb([64, 16, 64], F32, "wUV") for _ in range(2)]
            MT = P.sb([64, 16, 64], F32, "wMT")
            Pm = [P.sb([32, 16, 32], F32, "wPm") for _ in range(2)]
            PTm = [P.sb([32, 16, 32], F32, "wPTm") for _ in range(2)]
            Zs = P.sb([32, 16, 64], F32, "wZs")
            pTP = P.ps([64, 16, 64], F32, "wpTP")
            pZ = P.ps([64, 16, 64], F32, "wpZ")
            pN = P.ps([64, 16, 32], F32, "wpN")
            pP2 = P.ps([32, 16, 32], F32, "wpP2")
            pP2T = P.ps([32, 16, 32], F32, "wpP2T")
            Yt = P.sb([64, 16, 64], F32, "wYt")
            vch = [P.sb([64, 16, 65], F32, "wvch") for _ in range(2)]
            gch = [P.sb([64, 16, 65], F32, "wgch") for _ in range(2)]
            s2 = [P.sb([64, 16], F32, "ws2") for _ in range(2)]
            vl = P.sb([64, 16, 64], F32, "wvl")
            gl = P.sb([64, 16, 64], F32, "wgl")
            yc = P.sb([64, 16, 64], F32, "wyc")
            sq = P.sb([64, 16, 64], F32, "wsq")
            rstd = P.sb([64, 16, 64], F32, "wrstd")
            tA = P.sb([64, 16, 64], F32, "wtA")
            rh = [P.sb([64, 64], F32, "wrh") for _ in range(2)]
            yb = [P.sb([64, 16, 64], BF16, "wyb") for _ in range(2)]
            rv, rg = FM_ROWS["rw_v"], FM_ROWS["rw_gate"]

            def b3(t2):
                return t2[:, :].unsqueeze(2).to_broadcast([64, 16, 64])

            def fl(b, np_=64):
                return b[0:np_, :, :].rearrange("p h t -> p (h t)")

            def loadtile(i):
                P.dma("sp", [(XT[i % 2][:].rearrange("k h c q t -> k h (c q t)"), rwxt.t[:, :, i, :])], reads=[rwxt], writes=[XT[i % 2]])

            def loadchunk(c):
                t0 = c * 32
                P.dma("sp", [(BK[c % 2][0:32, :, :].rearrange("p h k -> p (h k)"), rwtm2.t[t0:t0 + 32, 0, :]),
                             (BK[c % 2][32:64, :, :].rearrange("p h k -> p (h k)"), rwtm2.t[t0:t0 + 32, 1, :])], reads=[rwtm2], writes=[BK[c % 2]])
                P.dma("sp", [(UV[c % 2][32:64, :, :].rearrange("p h k -> p (h k)"), rwtm2.t[t0:t0 + 32, 2, :])], reads=[rwtm2], writes=[UV[c % 2]])

            def loadepi(g):
                t0 = g * 64
                P.dma("sp", [(vch[g % 2][:], fm.t[rv:rv + 1024, PAD + t0 - 1:PAD + t0 + 64].rearrange("(h v) t -> v h t", v=64))], reads=[fm], writes=[vch[g % 2]])
                P.dma("sp", [(gch[g % 2][:], fm.t[rg:rg + 1024, PAD + t0 - 1:PAD + t0 + 64].rearrange("(h v) t -> v h t", v=64))], reads=[fm], writes=[gch[g % 2]])
                P.dma("sp", [(s2[g % 2][:], rwbon[t0:t0 + 64, :])], reads=[rwbon], writes=[s2[g % 2]])
            loadtile(0)
            loadchunk(0)
            loadepi(0)
            ke = 0
            for i in range(NT):
                if i + 1 < NT:
                    loadtile(i + 1)
                X = XT[i % 2]
                for cc in range(4):
                    c = i * 4 + cc
                    if c + 1 < NC32:
                        loadchunk(c + 1)
                    BKc, UVc = BK[c % 2], UV[c % 2]
                    for h in range(16):
                        V("pe", lambda e: e.matmul(pTP[:, h, :], X[:, h, cc, 0:2, :].rearrange("k q t -> k (q t)"), X[:, h, cc, 2:4, :].rearrange("k q t -> k (q t)"),
                                                   start=True, stop=True), [X], [pTP])
                    for h in range(16):
                        V("pe", lambda e: e.matmul(pN[0:32, h, :], X[:, h, cc, 2, :], X[:, h, cc, 0, :], start=True, stop=True), [X], [pN])
                    V("dve", lambda e: e.tensor_tensor(MT[:], pTP[:], mtp[:, :].unsqueeze(1).to_broadcast([64, 16, 64]), ALU.mult), [pTP, mtp], [MT])
                    V("dve", lambda e: e.tensor_tensor(Pm[0][:], pN[0:32, :, :], mn[:, :].unsqueeze(1).to_broadcast([32, 16, 32]), ALU.mult), [pN, mn], [Pm[0]])
                    for h in range(16):
                        V("pe", lambda e: e.matmul(pZ[0:32, h, :], X[:, h, cc, 2, :], H[:, h, :], start=True, stop=False), [X, H], [pZ])
                        V("pe", lambda e: e.matmul(pZ[0:32, h, :], MT[32:64, h, 0:32], UVc[32:64, h, :], start=False, stop=True), [MT, UVc], [pZ])
                    for h2 in range(2):
                        V("act", lambda e: e.copy(Zs[:, h2 * 8:(h2 + 1) * 8, :], pZ[0:32, h2 * 8:(h2 + 1) * 8, :]), [pZ], [Zs])
                    for lv in range(5):
                        if lv == 0:
                            PTb, PTv = MT, (lambda h: MT[0:32, h, 0:32])
                        else:
                            PTb, PTv = PTm[lv % 2], (lambda h, b=PTm[lv % 2]: b[:, h, :])
                        Pb = Pm[lv % 2]
                        for h in range(16):
                            V("pe", lambda e: e.matmul(pZ[0:32, h, :], PTv(h), Zs[:, h, :], start=True, stop=True), [PTb, Zs], [pZ])
                        if lv < 4:
                            for h in range(16):
                                V("pe", lambda e: e.matmul(pP2[:, h, :], PTv(h), Pb[:, h, :], start=True, stop=True), [PTb, Pb], [pP2])
                                V("pe", lambda e: e.matmul(pP2T[:, h, :], Pb[:, h, :], PTv(h), start=True, stop=True), [PTb, Pb], [pP2T])
                            V("dve", lambda e: e.tensor_tensor(Zs[:], Zs[:], pZ[0:32, :, :], ALU.add), [Zs, pZ], [Zs])
                            V("act", lambda e: e.copy(Pm[(lv + 1) % 2][:], pP2[:]), [pP2], [Pm[(lv + 1) % 2]])
                            V("dve", lambda e: e.tensor_copy(PTm[(lv + 1) % 2][:], pP2T[:]), [pP2T], [PTm[(lv + 1) % 2]])
                        else:
                            V("dve", lambda e: e.tensor_tensor(UVc[0:32, :, :], Zs[:], pZ[0:32, :, :], ALU.add), [Zs, pZ], [UVc])
                    for h in range(16):
                        V("pe", lambda e: e.matmul(pN[:, h, :], H[:, h, :], X[:, h, cc, 3, :], start=True, stop=False), [H, X], [pN])
                        V("pe", lambda e: e.matmul(pN[:, h, :], UVc[:, h, :], MT[:, h, 32:64], start=False, stop=True), [UVc, MT], [pN])
                    V("act", lambda e: e.copy(Yt[:, :, (cc % 2) * 32:(cc % 2) * 32 + 32], pN[:]), [pN], [Yt])
                    for h in range(16):
                        V("pe", lambda e: e.matmul(pTP[:, h, :], BKc[:, h, :], UVc[:, h, :], start=True, stop=True), [BKc, UVc], [pTP])
                    V("dve", lambda e: e.tensor_tensor(H[:], H[:], decall[:, :, c:c + 1].to_broadcast([64, 16, 64]), ALU.mult), [H, decall], [H])
                    V("dve", lambda e: e.tensor_tensor(H[:], H[:], pTP[:], ALU.add), [H, pTP], [H])
                    if cc % 2 == 1:
                        g = c // 2
                        t0 = g * 64
                        if g + 1 < NCH:
                            loadepi(g + 1)
                        vc, gc, s2c = vch[g % 2], gch[g % 2], s2[g % 2]
                        for (src, dst, mu_) in ((vc, vl, muv), (gc, gl, mug)):
                            V("pool", lambda e: e.tensor_tensor(dst[:], src[:, :, 0:64], src[:, :, 1:65], ALU.subtract), [src], [dst])
                            V("pool", lambda e: e.tensor_tensor(dst[:], dst[:], b3(mu_), ALU.mult), [dst, mu_], [dst])
                            V("pool", lambda e: e.tensor_tensor(dst[:], dst[:], src[:, :, 1:65], ALU.add), [dst, src], [dst])
                        for n2 in range(2):
                            V("pe", lambda e: e.matmul(fl(pTP)[:, n2 * 512:(n2 + 1) * 512], onesm[:], fl(Yt)[:, n2 * 512:(n2 + 1) * 512], start=True, stop=True), [onesm, Yt], [pTP])
                        V("dve", lambda e: e.tensor_tensor(yc[:], Yt[:], pTP[:], ALU.subtract), [Yt, pTP], [yc])
                        V("act", lambda e: e.activation(sq[:], yc[:], AF.Square), [yc], [sq])
                        for n2 in range(2):
                            V("pe", lambda e: e.matmul(fl(pZ)[:, n2 * 512:(n2 + 1) * 512], onesm[:], fl(sq)[:, n2 * 512:(n2 + 1) * 512], start=True, stop=True), [onesm, sq], [pZ])
                        for h2 in range(2):
                            V("act", lambda e: e.activation(rstd[:, h2 * 8:(h2 + 1) * 8, :], pZ[:, h2 * 8:(h2 + 1) * 8, :], AF.Sqrt, bias=64e-5, scale=1.0), [pZ], [rstd])
                        V("dve", lambda e: e.reciprocal(rstd[:], rstd[:]), [rstd], [rstd])
                        V("dve", lambda e: e.tensor_tensor(yc[:], yc[:], rstd[:], ALU.mult), [yc, rstd], [yc])
                        V("dve", lambda e: e.tensor_tensor(yc[:], yc[:], b3(lng), ALU.mult), [yc, lng], [yc])
                        V("dve", lambda e: e.tensor_tensor(yc[:], yc[:], b3(lnb), ALU.add), [yc, lnb], [yc])
                        for h in range(16):
                            rhh = rh[h % 2]
                            V("pool", lambda e: e.tensor_scalar(rhh[:], ident_f[0:64, 0:64], s2c[:, h:h + 1], None, op0=ALU.mult), [ident_f, s2c], [rhh])
                            V("pe", lambda e: e.matmul(pTP[:, h, :], ones64[:], rhh[:], start=True, stop=True), [ones64, rhh], [pTP])
                        V("dve", lambda e: e.tensor_tensor(tA[:], pTP[:], vl[:], ALU.mult), [pTP, vl], [tA])
                        V("dve", lambda e: e.tensor_tensor(yc[:], yc[:], tA[:], ALU.add), [yc, tA], [yc])
                        V("act", lambda e: e.activation(gl[:], gl[:], AF.Silu), [gl], [gl])
                        ybc = yb[g % 2]
                        V("dve", lambda e: e.tensor_tensor(ybc[:], yc[:], gl[:], ALU.mult), [yc, gl], [ybc])
                        P.dma("sp", [(ysT.t[2048:3072, t0:t0 + 64].rearrange("(h v) t -> v h t", v=64), ybc[:])], reads=[ybc], writes=[ysT])


    def phase_rws3(l):
        with P.scope():
            HG = 8
            mtp = P.sb([64, 64], F32, "wmtp")
            mn = P.sb([32, 32], F32, "wmn")
            ones64 = P.sb([64, 64], F32, "wones")
            onesm = P.sb([64, 64], F32, "wonesm")
            P.dma("sp", [(mtp[:], I["c_m32tp"][:, :])], reads=[I["c_m32tp"]], writes=[mtp])
            P.dma("sp", [(mn[:], I["c_m32n"][:, :])], reads=[I["c_m32n"]], writes=[mn])
            V("pool", lambda e: e.memset(ones64[:], 1.0), [], [ones64])
            V("pool", lambda e: e.memset(onesm[:], 1.0 / 64.0), [], [onesm])

            def hv(name, off=0):
                t = P.sb([64, 16], F32, "w" + name)
                src = I[name]
                P.dma("sp", [(t[:], src.t[l, off:off + 1024].rearrange("(h v) -> v h", v=64))], reads=[src], writes=[t], allow_slow_non_contiguous=True)
                return t
            lng, lnb = hv("rwkv_lnx_g"), hv("rwkv_lnx_b")
            muv, mug = hv("rwkv_mu", 2048), hv("rwkv_mu", 3264)
            decall = P.sb([64, 16, NC32], F32, "wdec")
            P.dma("sp", [(decall[:], rwdec[:, :, :])], reads=[rwdec], writes=[decall])
            XT = [P.sb([64, 16, 4, 4, 32], F32, "wXT") for _ in range(2)]
            vch = [P.sb([64, 16, 65], F32, "wvch") for _ in range(2)]
            gch = [P.sb([64, 16, 65], F32, "wgch") for _ in range(2)]
            s2 = [P.sb([64, 16], F32, "ws2") for _ in range(2)]
            vl = P.sb([64, 16, 64], F32, "wvl")
            gl = P.sb([64, 16, 64], F32, "wgl")
            rh = [P.sb([64, 64], F32, "wrh") for _ in range(4)]
            H, MT, PP, Zs, BK, UV, Yt, yc, sq, rstd, tA, yb = [], [], [], [], [], [], [], [], [], [], [], []
            pTP, pZ, pN, pPP = [], [], [], []
            for g in range(2):
                H.append(P.sb([64, HG, 64], F32, "wH"))
                V("pool", lambda e: e.memset(H[g][:], 0.0), [], [H[g]])
                MT.append(P.sb([64, HG, 64], F32, "wMT"))
                PP.append([P.sb([32, 2, HG, 32], F32, "wPP") for _ in range(2)])
                Zs.append(P.sb([32, HG, 64], F32, "wZs"))
                BK.append([P.sb([64, HG, 64], F32, "wBK") for _ in range(2)])
                UV.append([P.sb([64, HG, 64], F32, "wUV") for _ in range(2)])
                Yt.append(P.sb([64, HG, 64], F32, "wYt"))
                yc.append(P.sb([64, HG, 64], F32, "wyc"))
                sq.append(P.sb([64, HG, 64], F32, "wsq"))
                rstd.append(P.sb([64, HG, 64], F32, "wrstd"))
                tA.append(P.sb([64, HG, 64], F32, "wtA"))
                yb.append([P.sb([64, HG, 64], BF16, "wyb") for _ in range(2)])
                pTP.append(P.ps([64, HG, 64], F32, "wpTP"))
                pZ.append(P.ps([64, HG, 64], F32, "wpZ"))
                pN.append(P.ps([64, HG, 32], F32, "wpN"))
                pPP.append(P.ps([32, 2, HG, 32], F32, "wpPP"))
            rv, rg = FM_ROWS["rw_v"], FM_ROWS["rw_gate"]

            def b3(t2, g):
                return t2[:, g * HG:(g + 1) * HG].unsqueeze(2).to_broadcast([64, HG, 64])

            def fl(b):
                return b[:, :, :].rearrange("p h t -> p (h t)")

            def loadtile(i):
                P.dma("sp", [(XT[i % 2][:].rearrange("k h c q t -> k h (c q t)"), rwxt.t[:, :, i, :])], reads=[rwxt], writes=[XT[i % 2]])

            def loadchunk(c):
                t0 = c * 32
                for g in range(2):
                    cs = slice(g * 512, (g + 1) * 512)
                    P.dma("sp", [(BK[g][c % 2][0:32, :, :].rearrange("p h k -> p (h k)"), rwtm2.t[t0:t0 + 32, 0, cs]),
                                 (BK[g][c % 2][32:64, :, :].rearrange("p h k -> p (h k)"), rwtm2.t[t0:t0 + 32, 1, cs])], reads=[rwtm2], writes=[BK[g][c % 2]])
                    P.dma("sp", [(UV[g][c % 2][32:64, :, :].rearrange("p h k -> p (h k)"), rwtm2.t[t0:t0 + 32, 2, cs])], reads=[rwtm2], writes=[UV[g][c % 2]])

            def loadepi(gi):
                t0 = gi * 64
                P.dma("sp", [(vch[gi % 2][:], fm.t[rv:rv + 1024, PAD + t0 - 1:PAD + t0 + 64].rearrange("(h v) t -> v h t", v=64))], reads=[fm], writes=[vch[gi % 2]])
                P.dma("sp", [(gch[gi % 2][:], fm.t[rg:rg + 1024, PAD + t0 - 1:PAD + t0 + 64].rearrange("(h v) t -> v h t", v=64))], reads=[fm], writes=[gch[gi % 2]])
                P.dma("sp", [(s2[gi % 2][:], rwbon[t0:t0 + 64, :])], reads=[rwbon], writes=[s2[gi % 2]])
            loadtile(0)
            loadchunk(0)
            loadepi(0)
            GR = (0, 1)
            for i in range(NT):
                if i + 1 < NT:
                    loadtile(i + 1)
                X = XT[i % 2]
                for cc in range(4):
                    c = i * 4 + cc
                    if c + 1 < NC32:
                        loadchunk(c + 1)
                    for g in GR:
                        for hh in range(HG):
                            h = g * HG + hh
                            V("pe", lambda e: e.matmul(pTP[g][:, hh, :], X[:, h, cc, 0:2, :].rearrange("k q t -> k (q t)"), X[:, h, cc, 2:4, :].rearrange("k q t -> k (q t)"),
                                                       start=True, stop=True), [X], [pTP[g]])
                        for hh in range(HG):
                            h = g * HG + hh
                            V("pe", lambda e: e.matmul(pN[g][0:32, hh, :], X[:, h, cc, 2, :], X[:, h, cc, 0, :], start=True, stop=True), [X], [pN[g]])
                    for g in GR:
                        V("dve", lambda e: e.tensor_tensor(MT[g][:], pTP[g][:], mtp[:, :].unsqueeze(1).to_broadcast([64, HG, 64]), ALU.mult), [pTP[g], mtp], [MT[g]])
                        V("dve", lambda e: e.tensor_tensor(PP[g][0][:, 0, :, :], pN[g][0:32, :, :], mn[:, :].unsqueeze(1).to_broadcast([32, HG, 32]), ALU.mult), [pN[g], mn], [PP[g][0]])
                    for g in GR:
                        UVc = UV[g][c % 2]
                        for hh in range(HG):
                            h = g * HG + hh
                            V("pe", lambda e: e.matmul(pZ[g][0:32, hh, :], X[:, h, cc, 2, :], H[g][:, hh, :], start=True, stop=False), [X, H[g]], [pZ[g]])
                            V("pe", lambda e: e.matmul(pZ[g][0:32, hh, :], MT[g][32:64, hh, 0:32], UVc[32:64, hh, :], start=False, stop=True), [MT[g], UVc], [pZ[g]])
                    for g in GR:
                        V("act", lambda e: e.copy(Zs[g][:], pZ[g][0:32, :, :]), [pZ[g]], [Zs[g]])
                    for lv in range(5):
                        for g in GR:
                            UVc = UV[g][c % 2]
                            if lv == 0:
                                PTb, PTv = MT[g], (lambda hh, b=MT[g]: b[0:32, hh, 0:32])
                            else:
                                PTb, PTv = PP[g][lv % 2], (lambda hh, b=PP[g][lv % 2]: b[:, 1, hh, :])
                            Pb = PP[g][lv % 2]
                            for hh in range(HG):
                                V("pe", lambda e: e.matmul(pZ[g][0:32, hh, :], PTv(hh), Zs[g][:, hh, :], start=True, stop=True), [PTb, Zs[g]], [pZ[g]])
                            if lv < 4:
                                for hh in range(HG):
                                    V("pe", lambda e: e.matmul(pPP[g][:, 0, hh, :], PTv(hh), Pb[:, 0, hh, :], start=True, stop=True), [PTb, Pb], [pPP[g]])
                                    V("pe", lambda e: e.matmul(pPP[g][:, 1, hh, :], Pb[:, 0, hh, :], PTv(hh), start=True, stop=True), [PTb, Pb], [pPP[g]])
                        for g in GR:
                            UVc = UV[g][c % 2]
                            if lv < 4:
                                V("dve", lambda e: e.tensor_tensor(Zs[g][:], Zs[g][:], pZ[g][0:32, :, :], ALU.add), [Zs[g], pZ[g]], [Zs[g]])
                                V("act", lambda e: e.copy(PP[g][(lv + 1) % 2][:], pPP[g][:]), [pPP[g]], [PP[g][(lv + 1) % 2]])
                            else:
                                V("dve", lambda e: e.tensor_tensor(UVc[0:32, :, :], Zs[g][:], pZ[g][0:32, :, :], ALU.add), [Zs[g], pZ[g]], [UVc])
                    for g in GR:
                        UVc = UV[g][c % 2]
                        for hh in range(HG):
                            h = g * HG + hh
                            V("pe", lambda e: e.matmul(pN[g][:, hh, :], H[g][:, hh, :], X[:, h, cc, 3, :], start=True, stop=False), [H[g], X], [pN[g]])
                            V("pe", lambda e: e.matmul(pN[g][:, hh, :], UVc[:, hh, :], MT[g][:, hh, 32:64], start=False, stop=True), [UVc, MT[g]], [pN[g]])
                    for g in GR:
                        V("act", lambda e: e.copy(Yt[g][:, :, (cc % 2) * 32:(cc % 2) * 32 + 32], pN[g][:]), [pN[g]], [Yt[g]])
                    for g in GR:
                        UVc, BKc = UV[g][c % 2], BK[g][c % 2]
                        for hh in range(HG):
                            V("pe", lambda e: e.matmul(pTP[g][:, hh, :], BKc[:, hh, :], UVc[:, hh, :], start=True, stop=True), [BKc, UVc], [pTP[g]])
                    for g in GR:
                        V("dve", lambda e: e.tensor_tensor(H[g][:], H[g][:], decall[:, g * HG:(g + 1) * HG, c:c + 1].to_broadcast([64, HG, 64]), ALU.mult), [H[g], decall], [H[g]])
                        V("dve", lambda e: e.tensor_tensor(H[g][:], H[g][:], pTP[g][:], ALU.add), [H[g], pTP[g]], [H[g]])
                    if cc % 2 == 1:
                        gi = c // 2
                        t0 = gi * 64
                        if gi + 1 < NCH:
                            loadepi(gi + 1)
                        vc, gc, s2c = vch[gi % 2], gch[gi % 2], s2[gi % 2]
                        for (src, dst, mu_) in ((vc, vl, muv), (gc, gl, mug)):
                            V("pool", lambda e: e.tensor_tensor(dst[:], src[:, :, 0:64], src[:, :, 1:65], ALU.subtract), [src], [dst])
                            V("pool", lambda e: e.tensor_tensor(dst[:], dst[:], mu_[:, :].unsqueeze(2).to_broadcast([64, 16, 64]), ALU.mult), [dst, mu_], [dst])
                            V("pool", lambda e: e.tensor_tensor(dst[:], dst[:], src[:, :, 1:65], ALU.add), [dst, src], [dst])
                        V("act", lambda e: e.activation(gl[:], gl[:], AF.Silu), [gl], [gl])
                        for g in GR:
                            V("pe", lambda e: e.matmul(fl(pTP[g]), onesm[:], fl(Yt[g]), start=True, stop=True), [onesm, Yt[g]], [pTP[g]])
                        for g in GR:
                            V("dve", lambda e: e.tensor_tensor(yc[g][:], Yt[g][:], pTP[g][:], ALU.subtract), [Yt[g], pTP[g]], [yc[g]])
                            V("act", lambda e: e.activation(sq[g][:], yc[g][:], AF.Square), [yc[g]], [sq[g]])
                        for g in GR:
                            V("pe", lambda e: e.matmul(fl(pZ[g]), onesm[:], fl(sq[g]), start=True, stop=True), [onesm, sq[g]], [pZ[g]])
                        k4 = 0
                        for g in GR:
                            for hh in range(HG):
                                h = g * HG + hh
                                rhh = rh[k4 % 4]
                                k4 += 1
                                V("pool", lambda e: e.tensor_scalar(rhh[:], ident_f[0:64, 0:64], s2c[:, h:h + 1], None, op0=ALU.mult), [ident_f, s2c], [rhh])
                                V("pe", lambda e: e.matmul(pTP[g][:, hh, :], ones64[:], rhh[:], start=True, stop=True), [ones64, rhh], [pTP[g]])
                        for g in GR:
                            hs = slice(g * HG, (g + 1) * HG)
                            V("act", lambda e: e.activation(rstd[g][:], pZ[g][:], AF.Sqrt, bias=64e-5, scale=1.0), [pZ[g]], [rstd[g]])
                            V("dve", lambda e: e.reciprocal(rstd[g][:], rstd[g][:]), [rstd[g]], [rstd[g]])
                            V("dve", lambda e: e.tensor_tensor(yc[g][:], yc[g][:], rstd[g][:], ALU.mult), [yc[g], rstd[g]], [yc[g]])
                            V("dve", lambda e: e.tensor_tensor(yc[g][:], yc[g][:], b3(lng, g), ALU.mult), [yc[g], lng], [yc[g]])
                            V("dve", lambda e: e.tensor_tensor(yc[g][:], yc[g][:], b3(lnb, g), ALU.add), [yc[g], lnb], [yc[g]])
                            V("dve", lambda e: e.tensor_tensor(tA[g][:], pTP[g][:], vl[:, hs, :], ALU.mult), [pTP[g], vl], [tA[g]])
                            V("dve", lambda e: e.tensor_tensor(yc[g][:], yc[g][:], tA[g][:], ALU.add), [yc[g], tA[g]], [yc[g]])
                            ybc = yb[g][gi % 2]
                            V("dve", lambda e: e.tensor_tensor(ybc[:], yc[g][:], gl[:, hs, :], ALU.mult), [yc[g], gl], [ybc])
                            P.dma("sp", [(ysT.t[2048 + g * 512:2048 + (g + 1) * 512, t0:t0 + 64].rearrange("(h v) t -> v h t", v=64), ybc[:])], reads=[ybc], writes=[ysT])

    PH = {"tables": phase_tables, "proj": phase_proj, "rope": phase_rope, "gla": phase_gla, "sgu": phase_sgu,
          "m1": phase_m1, "m2": phase_m2, "dsa1": phase_dsa1, "dsa2": phase_dsa2, "rwp": phase_rwp, "rws": phase_rws, "rwp2": phase_rwp2, "rws2": phase_rws2, "rws3": phase_rws3}
    plan = dbg.get("plan")
    if plan is None:
        plan = [("tables",)]
        for l in range(L):
            plan += [("proj", l, l), ("rope", l), ("gla", l), ("dsa1", l), ("dsa2", l), ("rwp2", l), ("rws3", l), ("sgu", l), ("m1", l), ("m2", l, l, l + 1)]
    for st in plan:
        nm = st[0]
        if nm == "tables":
            phase_tables()
        elif nm == "proj":
            phase_proj(st[1], xcur[st[2]])
        elif nm == "m2":
            phase_m2(st[1], xcur[st[2]], xcur[st[3]])
        else:
            PH[nm](st[1])
    P.barrier()
    es.close()
    global LAST_P
    LAST_P = P
    return nc


def make_inputs(inputs, b):
    m = {"x": np.ascontiguousarray(inputs["x"][b]), "pos": np.ascontiguousarray(inputs["positions"][b:b + 1]).astype(np.int32)}
    for k, v in inputs.items():
        if k in ("x", "positions"):
            continue
        m[k] = np.ascontiguousarray(v)
    m.update(host_consts())
    return m


def kernel(**inputs):
    nc = build_nc()
    in_maps = [make_inputs(inputs, c % 4) for c in range(8)]
    res = run_bass_kernel_spmd(nc, in_maps, core_ids=list(range(8)))
    return np.stack([res.results[c]["y"] for c in range(4)], axis=0).astype(np.float32)
```
